# Optimizing a Trainium2 kernel written in Bass

```python
import jax
import jax.numpy as jnp
from jax import lax
import numpy as np

D_MODEL = 1024
BATCH = 4
SEQ = 4096
DEPTH = 2

GRID_W = 64
CTX_LEN = 256
N_EVEN = (DEPTH + 1) // 2
N_ODD = DEPTH // 2
EPS = 1e-6
ROPE_BASE = 10000.0
Q_BLOCK = 128
F32 = jnp.float32

MLA_HEADS = 8
MLA_Q_RANK = 256
MLA_KV_RANK = 128
MLA_NOPE = 64
MLA_ROPE = 32
MLA_V = 64
WIN_HEADS = 8
WIN_KV_HEADS = 2
WIN_HEAD_DIM = 64
WINDOW = 128
WIN_BLOCK = 128
GLA_HEADS = 4
GLA_DK = 64
GLA_DV = 128
GLA_GATE_RANK = 16
GLA_TAU = 16.0
GLA_CHUNK = 64
SG_GROUPS = 4
SG_CHUNK = 128
SG_WIDTH = 512
MOE_GROUPS = 4
MOE_PER_GROUP = 8
MOE_EXPERTS = MOE_GROUPS * MOE_PER_GROUP
MOE_TOPK = 2
MOE_HIDDEN = 512

WIN_Q = WIN_HEADS * WIN_HEAD_DIM
WIN_KV = WIN_KV_HEADS * WIN_HEAD_DIM
EVEN_IN = MLA_Q_RANK + MLA_KV_RANK + MLA_ROPE + WIN_Q + 2 * WIN_KV
EVEN_MIX = MLA_HEADS * MLA_V + WIN_Q
GLA_K = GLA_HEADS * GLA_DK
GLA_V = GLA_HEADS * GLA_DV
ODD_IN = 2 * GLA_K + GLA_V + 2 * GLA_GATE_RANK + GLA_V + 2 * SG_WIDTH
ODD_MIX = GLA_V + SG_WIDTH

kernel_name = "hybrid_mla_swa_gla_gmlp_hmoe_dit"


def rmsnorm(x, g):
    xf = x.astype(F32)
    y = xf * lax.rsqrt(jnp.mean(xf * xf, axis=-1, keepdims=True) + EPS)
    return (y * g.astype(F32)).astype(x.dtype)


def layernorm(x, g, b):
    xf = x.astype(F32)
    mu = jnp.mean(xf, axis=-1, keepdims=True)
    xc = xf - mu
    var = jnp.mean(xc * xc, axis=-1, keepdims=True)
    return (xc * lax.rsqrt(var + EPS) * g.astype(F32) + b.astype(F32)).astype(x.dtype)


def split_cols(z, sizes):
    cuts, acc = [], 0
    for s in sizes[:-1]:
        acc += s
        cuts.append(acc)
    return jnp.split(z, cuts, axis=-1)


def axial_rope(rows, dim):
    row = jnp.repeat(jnp.arange(rows, dtype=F32), GRID_W)
    col = jnp.tile(jnp.arange(GRID_W, dtype=F32), rows)
    half = dim // 2
    inv = jnp.power(ROPE_BASE, -jnp.arange(0, half, 2, dtype=F32) / half)
    ar = row[:, None] * inv[None, :]
    ac = col[:, None] * inv[None, :]
    ang = jnp.concatenate([ar, ar, ac, ac], axis=-1)
    return jnp.cos(ang), jnp.sin(ang)


def rotate_half(t):
    t1, t2 = jnp.split(t, 2, axis=-1)
    return jnp.concatenate([-t2, t1], axis=-1)


def apply_axial_rope(t, rope):
    cos, sin = rope
    half = t.shape[-1] // 2
    rot = jnp.concatenate([rotate_half(t[..., :half]), rotate_half(t[..., half:])], axis=-1)
    return (t * cos[None, :, None, :] + rot * sin[None, :, None, :]).astype(t.dtype)


def adaln(cond, w, b):
    return jnp.split(jax.nn.silu(cond) @ w + b, 6, axis=-1)


def modulate(h, shift, scale):
    return h * (1 + scale) + shift


def mla_heads(cq, ckv, kr, q_norm_g, w_uq, kv_norm_g, w_ukv, rope):
    B, n, _ = cq.shape
    q = (rmsnorm(cq, q_norm_g) @ w_uq).reshape(B, n, MLA_HEADS, MLA_NOPE + MLA_ROPE)
    kv = (rmsnorm(ckv, kv_norm_g) @ w_ukv).reshape(B, n, MLA_HEADS, MLA_NOPE + MLA_V)
    q_nope, q_rope = q[..., :MLA_NOPE], q[..., MLA_NOPE:]
    k_nope, v = kv[..., :MLA_NOPE], kv[..., MLA_NOPE:]
    k_rope = kr[:, :, None, :]
    if rope is not None:
        q_rope = apply_axial_rope(q_rope, rope)
        k_rope = apply_axial_rope(k_rope, rope)
    k_rope = jnp.broadcast_to(k_rope, (B, n, MLA_HEADS, MLA_ROPE))
    q = jnp.concatenate([q_nope, q_rope], axis=-1)
    k = jnp.concatenate([k_nope, k_rope], axis=-1)
    return q, k, v


def dense_attention(q, k, v):
    B, n, H, d = q.shape
    nb = n // Q_BLOCK
    scale = d ** -0.5
    qb = jnp.moveaxis(q.reshape(B, nb, Q_BLOCK, H, d), 1, 0)

    def one_block(qblk):
        s = jnp.einsum("bqhd,bkhd->bhqk", qblk, k).astype(F32) * scale
        p = jax.nn.softmax(s, axis=-1).astype(v.dtype)
        return jnp.einsum("bhqk,bkhd->bqhd", p, v)

    o = lax.map(one_block, qb)
    return jnp.moveaxis(o, 0, 1).reshape(B, n, H, v.shape[-1])


def window_gqa_latent(q, k, v, kc, vc, sink):
    B, S, H, d = q.shape
    W = WIN_BLOCK
    nb = S // W
    rep = H // WIN_KV_HEADS
    Lc = kc.shape[1]
    qb = q.reshape(B, nb, W, WIN_KV_HEADS, rep, d)

    def band(t):
        tb = t.reshape(B, nb, W, WIN_KV_HEADS, d)
        tp = jnp.pad(tb, ((0, 0), (1, 1), (0, 0), (0, 0), (0, 0)))
        return jnp.concatenate([tp[:, :-2], tp[:, 1:-1], tp[:, 2:]], axis=2)

    kw, vw = band(k), band(v)
    qpos = jnp.arange(nb)[:, None] * W + jnp.arange(W)[None, :]
    kpos = jnp.arange(nb)[:, None] * W + jnp.arange(-W, 2 * W)[None, :]
    mask = ((jnp.abs(qpos[:, :, None] - kpos[:, None, :]) <= WINDOW)
            & (kpos[:, None, :] >= 0) & (kpos[:, None, :] < S))
    scale = d ** -0.5
    s_win = jnp.einsum("bnqgrd,bnkgd->bngrqk", qb, kw).astype(F32) * scale
    s_win = jnp.where(mask[None, :, None, None], s_win, -jnp.inf)
    s_ctx = jnp.einsum("bnqgrd,bkgd->bngrqk", qb, kc).astype(F32) * scale
    s_sink = jnp.broadcast_to(sink.astype(F32).reshape(1, 1, WIN_KV_HEADS, rep, 1, 1),
                              s_win.shape[:-1] + (1,))
    p = jax.nn.softmax(jnp.concatenate([s_win, s_ctx, s_sink], axis=-1), axis=-1).astype(v.dtype)
    o = (jnp.einsum("bngrqk,bnkgd->bnqgrd", p[..., :3 * W], vw)
         + jnp.einsum("bngrqk,bkgd->bnqgrd", p[..., 3 * W:3 * W + Lc], vc))
    return o.reshape(B, S, H, d)


def gqa_context(qc, kc, vc, sink):
    B, L, H, d = qc.shape
    rep = H // WIN_KV_HEADS
    qg = qc.reshape(B, L, WIN_KV_HEADS, rep, d)
    s = jnp.einsum("bqgrd,bkgd->bgrqk", qg, kc).astype(F32) * (d ** -0.5)
    s_sink = jnp.broadcast_to(sink.astype(F32).reshape(1, WIN_KV_HEADS, rep, 1, 1), s.shape[:-1] + (1,))
    p = jax.nn.softmax(jnp.concatenate([s, s_sink], axis=-1), axis=-1)[..., :L].astype(vc.dtype)
    return jnp.einsum("bgrqk,bkgd->bqgrd", p, vc).reshape(B, L, H, d)


def even_mixer(hl, hc, w_in, q_norm_g, w_uq, kv_norm_g, w_ukv, sink, w_out, rope_mla, rope_win, with_ctx):
    def heads(h, r_mla, r_win):
        B, n, _ = h.shape
        cq, ckv, kr, qw, kw, vw = split_cols(
            h @ w_in, (MLA_Q_RANK, MLA_KV_RANK, MLA_ROPE, WIN_Q, WIN_KV, WIN_KV))
        qa, ka, va = mla_heads(cq, ckv, kr, q_norm_g, w_uq, kv_norm_g, w_ukv, r_mla)
        qb = qw.reshape(B, n, WIN_HEADS, WIN_HEAD_DIM)
        kb = kw.reshape(B, n, WIN_KV_HEADS, WIN_HEAD_DIM)
        vb = vw.reshape(B, n, WIN_KV_HEADS, WIN_HEAD_DIM)
        if r_win is not None:
            qb = apply_axial_rope(qb, r_win)
            kb = apply_axial_rope(kb, r_win)
        return qa, ka, va, qb, kb, vb

    qa_l, ka_l, va_l, qb_l, kb_l, vb_l = heads(hl, rope_mla, rope_win)
    qa_c, ka_c, va_c, qb_c, kb_c, vb_c = heads(hc, None, None)
    B, n, _ = hl.shape
    oa_l = dense_attention(qa_l, jnp.concatenate([ka_l, ka_c], axis=1), jnp.concatenate([va_l, va_c], axis=1))
    ob_l = window_gqa_latent(qb_l, kb_l, vb_l, kb_c, vb_c, sink)
    out_l = jnp.concatenate([oa_l.reshape(B, n, -1), ob_l.reshape(B, n, -1)], axis=-1) @ w_out
    if not with_ctx:
        return out_l, None
    L = hc.shape[1]
    oa_c = dense_attention(qa_c, ka_c, va_c)
    ob_c = gqa_context(qb_c, kb_c, vb_c, sink)
    out_c = jnp.concatenate([oa_c.reshape(B, L, -1), ob_c.reshape(B, L, -1)], axis=-1) @ w_out
    return out_l, out_c


def gla_chunked(q, k, v, log_a, s0):
    B, n, H, dk = q.shape
    C = GLA_CHUNK
    nc = n // C

    def to_chunks(t):
        return jnp.moveaxis(t.astype(F32).reshape(B, nc, C, *t.shape[2:]), 1, 0)

    tri = jnp.tril(jnp.ones((C, C), dtype=bool))

    def step(S, inp):
        qc, kc, vc, gc = inp
        b = jnp.cumsum(gc, axis=1)
        inter = jnp.einsum("bthk,bhkv->bthv", qc * jnp.exp(b), S)
        decay = jnp.exp(jnp.where(tri[None, :, :, None, None], b[:, :, None] - b[:, None, :], -jnp.inf))
        A = jnp.einsum("bthk,btshk,bshk->bhts", qc, decay, kc)
        intra = jnp.einsum("bhts,bshv->bthv", A, vc)
        b_last = b[:, -1]
        k_dec = kc * jnp.exp(b_last[:, None] - b)
        S_new = S * jnp.exp(b_last)[..., None] + jnp.einsum("bshk,bshv->bhkv", k_dec, vc)
        return S_new, inter + intra

    S_fin, o = lax.scan(step, s0.astype(F32), (to_chunks(q), to_chunks(k), to_chunks(v), to_chunks(log_a)))
    return jnp.moveaxis(o, 0, 1).reshape(B, n, H, v.shape[-1]), S_fin


def gla_bidirectional(lat, ctx_, with_ctx):
    ql, kl, vl, fl, bl = lat
    qc, kc, vc, fc, bc = ctx_
    B = ql.shape[0]
    s0 = jnp.zeros((B, GLA_HEADS, GLA_DK, GLA_DV), F32)

    def flip(t):
        return jnp.flip(t, axis=1)

    oc_f, sc_f = gla_chunked(qc, kc, vc, fc, s0)
    oc_b, sc_b = gla_chunked(flip(qc), flip(kc), flip(vc), flip(bc), s0)
    ol_f, _ = gla_chunked(ql, kl, vl, fl, sc_f)
    ol_b, _ = gla_chunked(flip(ql), flip(kl), flip(vl), flip(bl), sc_b)
    ol = ol_f + flip(ol_b)
    oc = oc_f + flip(oc_b) if with_ctx else None
    return ol, oc


def head_rmsnorm(o, g):
    B, n = o.shape[:2]
    o = o * lax.rsqrt(jnp.mean(o * o, axis=-1, keepdims=True) + EPS)
    return o.reshape(B, n, -1) * g.astype(F32)


def spatial_gating(u, vg, ln_g, ln_b, w_s, b_s):
    u = jax.nn.gelu(u)
    vg = layernorm(jax.nn.gelu(vg), ln_g, ln_b)
    B, n, _ = vg.shape
    nck = n // SG_CHUNK
    vb = vg.reshape(B, nck, SG_CHUNK, SG_GROUPS, SG_WIDTH // SG_GROUPS)
    s = jnp.einsum("gts,bnsgc->bntgc", w_s, vb) + b_s.T[None, None, :, :, None]
    return u * s.reshape(B, n, SG_WIDTH)


def odd_mixer(hl, hc, w_in, w_g2, b_g, gla_g, ln_g, ln_b, w_s, b_s, w_out, with_ctx):
    def project(h):
        B, n, _ = h.shape
        q, k, v, g, r, u, vg = split_cols(
            h @ w_in, (GLA_K, GLA_K, GLA_V, 2 * GLA_GATE_RANK, GLA_V, SG_WIDTH, SG_WIDTH))
        q = q.reshape(B, n, GLA_HEADS, GLA_DK) * (GLA_DK ** -0.5)
        k = k.reshape(B, n, GLA_HEADS, GLA_DK)
        v = v.reshape(B, n, GLA_HEADS, GLA_DV)
        z = jnp.einsum("bnzr,zrk->bnzk", g.reshape(B, n, 2, GLA_GATE_RANK).astype(F32),
                       w_g2.astype(F32)) + b_g.astype(F32)
        log_a = jax.nn.log_sigmoid(z) / GLA_TAU
        la_f = log_a[:, :, 0].reshape(B, n, GLA_HEADS, GLA_DK)
        la_b = log_a[:, :, 1].reshape(B, n, GLA_HEADS, GLA_DK)
        return (q, k, v, la_f, la_b), r, u, vg

    gl, rl, ul, vl = project(hl)
    gc, rc, uc, vc = project(hc)
    ol, oc = gla_bidirectional(gl, gc, with_ctx)
    cl = (head_rmsnorm(ol, gla_g) * jax.nn.silu(rl.astype(F32))).astype(hl.dtype)
    dl = spatial_gating(ul, vl, ln_g, ln_b, w_s, b_s)
    out_l = jnp.concatenate([cl, dl], axis=-1) @ w_out
    if not with_ctx:
        return out_l, None
    cc = (head_rmsnorm(oc, gla_g) * jax.nn.silu(rc.astype(F32))).astype(hc.dtype)
    dc = spatial_gating(uc, vc, ln_g, ln_b, w_s, b_s)
    out_c = jnp.concatenate([cc, dc], axis=-1) @ w_out
    return out_l, out_c


def hier_moe(h, w_rg, w_re, w_gate, w_up, w_down):
    shp = h.shape
    x = h.reshape(-1, shp[-1])
    N = x.shape[0]
    g_logits = (x @ w_rg).astype(F32)
    g_prob = jax.nn.softmax(g_logits, axis=-1)
    g_sel = jnp.argmax(g_logits, axis=-1)
    g_oh = jax.nn.one_hot(g_sel, MOE_GROUPS, dtype=F32)
    e_logits = (x @ w_re).astype(F32).reshape(N, MOE_GROUPS, MOE_PER_GROUP)
    e_in = jnp.einsum("ngp,ng->np", e_logits, g_oh)
    top_v, top_i = lax.top_k(e_in, MOE_TOPK)
    w_top = jax.nn.softmax(top_v, axis=-1) * jnp.max(g_prob, axis=-1, keepdims=True)
    local = jnp.sum(jax.nn.one_hot(top_i, MOE_PER_GROUP, dtype=F32) * w_top[..., None], axis=1)
    combine = (g_oh[:, :, None] * local[:, None, :]).astype(x.dtype)
    y = jnp.zeros_like(x)
    for g in range(MOE_GROUPS):
        sl = slice(g * MOE_PER_GROUP, (g + 1) * MOE_PER_GROUP)
        hg = (jax.nn.silu(jnp.einsum("nd,edf->nef", x, w_gate[sl]))
              * jnp.einsum("nd,edf->nef", x, w_up[sl]))
        y = y + jnp.einsum("nef,efd->nd", hg * combine[:, g, :, None], w_down[sl])
    return y.reshape(shp)


def setup_inputs(seed: int = 0) -> dict:
    key = jax.random.key(seed)
    ks = iter(jax.random.split(key, 32))

    def nrm(shape, scale):
        return jax.random.normal(next(ks), shape, F32) * scale

    def gain(shape):
        return 1.0 + 0.02 * jax.random.normal(next(ks), shape, F32)

    D = D_MODEL
    return {
        "x": nrm((BATCH, SEQ, D), 1.0),
        "c": nrm((BATCH, D), 1.0),
        "ctx": nrm((BATCH, CTX_LEN, D), 1.0),
        "c_ctx": nrm((D,), 1.0),
        "ada_w": nrm((DEPTH, D, 6 * D), 0.5 * D ** -0.5),
        "ada_b": nrm((DEPTH, 6 * D), 0.02),
        "norm_mix_g": gain((DEPTH, D)),
        "norm_ffn_g": gain((DEPTH, D)),
        "even_w_in": nrm((N_EVEN, D, EVEN_IN), D ** -0.5),
        "mla_q_norm_g": gain((N_EVEN, MLA_Q_RANK)),
        "mla_w_uq": nrm((N_EVEN, MLA_Q_RANK, MLA_HEADS * (MLA_NOPE + MLA_ROPE)), MLA_Q_RANK ** -0.5),
        "mla_kv_norm_g": gain((N_EVEN, MLA_KV_RANK)),
        "mla_w_ukv": nrm((N_EVEN, MLA_KV_RANK, MLA_HEADS * (MLA_NOPE + MLA_V)), MLA_KV_RANK ** -0.5),
        "win_sink": nrm((N_EVEN, WIN_HEADS), 0.5),
        "even_w_out": nrm((N_EVEN, EVEN_MIX, D), EVEN_MIX ** -0.5),
        "odd_w_in": nrm((N_ODD, D, ODD_IN), D ** -0.5),
        "gla_w_g2": nrm((N_ODD, 2, GLA_GATE_RANK, GLA_K), GLA_GATE_RANK ** -0.5),
        "gla_b_g": nrm((N_ODD, 2, GLA_K), 0.1),
        "gla_norm_g": gain((N_ODD, GLA_V)),
        "sg_ln_g": gain((N_ODD, SG_WIDTH)),
        "sg_ln_b": nrm((N_ODD, SG_WIDTH), 0.02),
        "sg_w_s": nrm((N_ODD, SG_GROUPS, SG_CHUNK, SG_CHUNK), SG_CHUNK ** -0.5),
        "sg_b_s": gain((N_ODD, SG_GROUPS, SG_CHUNK)),
        "odd_w_out": nrm((N_ODD, ODD_MIX, D), ODD_MIX ** -0.5),
        "moe_w_rg": nrm((DEPTH, D, MOE_GROUPS), D ** -0.5),
        "moe_w_re": nrm((DEPTH, D, MOE_EXPERTS), D ** -0.5),
        "moe_w_gate": nrm((DEPTH, MOE_EXPERTS, D, MOE_HIDDEN), D ** -0.5),
        "moe_w_up": nrm((DEPTH, MOE_EXPERTS, D, MOE_HIDDEN), D ** -0.5),
        "moe_w_down": nrm((DEPTH, MOE_EXPERTS, MOE_HIDDEN, D), MOE_HIDDEN ** -0.5),
        "final_norm_g": gain((D,)),
    }


def reference(x, c, ctx, c_ctx, ada_w, ada_b, norm_mix_g, norm_ffn_g,
              even_w_in, mla_q_norm_g, mla_w_uq, mla_kv_norm_g, mla_w_ukv, win_sink, even_w_out,
              odd_w_in, gla_w_g2, gla_b_g, gla_norm_g, sg_ln_g, sg_ln_b, sg_w_s, sg_b_s, odd_w_out,
              moe_w_rg, moe_w_re, moe_w_gate, moe_w_up, moe_w_down, final_norm_g):
    n = x.shape[1]
    ROWS = n // GRID_W
    rope_mla = axial_rope(ROWS, MLA_ROPE)
    rope_win = axial_rope(ROWS, WIN_HEAD_DIM)
    xl, xc = x, ctx
    for layer in range(DEPTH):
        with_ctx = layer < DEPTH - 1
        sh1, sc1, gt1, sh2, sc2, gt2 = [m[:, None, :] for m in adaln(c, ada_w[layer], ada_b[layer])]
        csh1, csc1, cgt1, csh2, csc2, cgt2 = adaln(c_ctx, ada_w[layer], ada_b[layer])
        hl = modulate(rmsnorm(xl, norm_mix_g[layer]), sh1, sc1)
        hc = modulate(rmsnorm(xc, norm_mix_g[layer]), csh1, csc1)
        i = layer // 2
        if layer % 2 == 0:
            ml, mc = even_mixer(hl, hc, even_w_in[i], mla_q_norm_g[i], mla_w_uq[i], mla_kv_norm_g[i],
                                mla_w_ukv[i], win_sink[i], even_w_out[i], rope_mla, rope_win, with_ctx)
        else:
            ml, mc = odd_mixer(hl, hc, odd_w_in[i], gla_w_g2[i], gla_b_g[i], gla_norm_g[i], sg_ln_g[i],
                               sg_ln_b[i], sg_w_s[i], sg_b_s[i], odd_w_out[i], with_ctx)
        xl = xl + gt1 * ml
        xl = xl + gt2 * hier_moe(modulate(rmsnorm(xl, norm_ffn_g[layer]), sh2, sc2),
                                 moe_w_rg[layer], moe_w_re[layer], moe_w_gate[layer],
                                 moe_w_up[layer], moe_w_down[layer])
        if with_ctx:
            xc = xc + cgt1 * mc
            xc = xc + cgt2 * hier_moe(modulate(rmsnorm(xc, norm_ffn_g[layer]), csh2, csc2),
                                      moe_w_rg[layer], moe_w_re[layer], moe_w_gate[layer],
                                      moe_w_up[layer], moe_w_down[layer])
    return rmsnorm(xl, final_norm_g)
```

```python
import contextlib
import numpy as np
import concourse.bass as bass
import concourse.mybir as mybir
from concourse.bass_utils import run_bass_kernel_spmd

F32 = mybir.dt.float32
BF16 = mybir.dt.bfloat16
AF = mybir.ActivationFunctionType
ALU = mybir.AluOpType
AX = mybir.AxisListType
N_DMA_SEMS = 10


class Op:
    __slots__ = ("eng", "fn", "deps", "signal", "sem", "val", "is_dma", "idx", "kind", "sem_eng")

    def __init__(self, eng, fn, is_dma, kind):
        self.eng = eng
        self.fn = fn
        self.deps = set()
        self.signal = False
        self.sem = None
        self.val = 0
        self.is_dma = is_dma
        self.kind = kind
        self.sem_eng = None


class Rot:
    def __init__(self, name, aps):
        self.name = name
        self.aps = aps
        self.i = 0

    def next(self):
        k = self.i % len(self.aps)
        self.i += 1
        return self.aps[k], (self.name, k)


class Prog:
    ENGS = ("pe", "act", "dve", "pool", "sp")

    def __init__(self, nc):
        self.nc = nc
        self.st = contextlib.ExitStack()
        st = self.st
        self.esem = {e: st.enter_context(nc.semaphore("s_" + e)) for e in ("pe", "act", "dve", "pool", "cc")}
        self.dsem = {e: [st.enter_context(nc.semaphore("d_%s%d" % (e, i))) for i in range(N_DMA_SEMS)]
                     for e in ("sp", "act", "pool")}
        self.cnt = {e: 0 for e in self.esem}
        self.dcnt = {e: [0] * N_DMA_SEMS for e in self.dsem}
        self.drr = {e: 0 for e in self.dsem}
        self.nflush = 0
        self.total_ops = 0
        self._reset()

    def _reset(self):
        self.ops = []
        self.last_w = {}
        self.readers = {}

    def op(self, eng, fn, reads=(), writes=(), dma=False, kind=""):
        o = Op(eng, fn, dma, kind)
        o.idx = len(self.ops)
        ex = [r for r in reads if isinstance(r, tuple) and r[0] == "ps"]
        if ex and eng != "pe":
            reads = [r for r in reads if r not in ex]
            writes = list(writes) + ex
        for r in reads:
            w = self.last_w.get(r)
            if w is not None:
                o.deps.add(w)
        for wkey in writes:
            w = self.last_w.get(wkey)
            if w is not None:
                o.deps.add(w)
            for rd in self.readers.get(wkey, ()):
                o.deps.add(rd)
        for r in reads:
            self.readers.setdefault(r, []).append(o.idx)
        for wkey in writes:
            self.last_w[wkey] = o.idx
            self.readers[wkey] = []
        o.deps.discard(o.idx)
        self.ops.append(o)
        return o

    def dma(self, out, in_, reads, writes, eng="sp", **kw):
        return self.op(eng, lambda e: e.dma_start(out=out, in_=in_, **kw), reads, writes, dma=True, kind="dma")

    def mm(self, out, lhsT, rhs, start, stop, reads, writes, **kw):
        return self.op("pe", lambda e: e.matmul(out, lhsT, rhs, start=start, stop=stop, **kw),
                       reads, writes, kind="mm")

    def tr(self, out, in_, ident, reads, writes):
        return self.op("pe", lambda e: e.transpose(out, in_, ident), reads, writes, kind="mm")

    def act(self, out, in_, func, reads, writes, **kw):
        return self.op("act", lambda e: e.activation(out, in_, func, **kw), reads, writes, kind="act")

    def cc(self, fn, reads, writes):
        o = self.op("pool", fn, reads, writes, kind="cc")
        o.sem_eng = "cc"
        o.signal = True
        return o

    def v(self, fn, reads, writes, eng="dve"):
        return self.op(eng, fn, reads, writes, kind="v")

    def flush(self, final=False):
        nc = self.nc
        ops = self.ops
        self.total_ops += len(ops)
        for o in ops:
            if o.eng == "pe":
                o.deps = {d for d in o.deps if ops[d].eng != "pe"}
        for o in ops:
            for d in o.deps:
                ops[d].signal = True
        base_cnt = dict(self.cnt)
        base_dcnt = {e: list(v) for e, v in self.dcnt.items()}
        dprev = {e: [None] * N_DMA_SEMS for e in self.dsem}
        for o in ops:
            if o.is_dma:
                o.signal = True
                k = self.drr[o.eng]
                self.drr[o.eng] = (k + 1) % N_DMA_SEMS
                self.dcnt[o.eng][k] += 16
                o.sem = self.dsem[o.eng][k]
                o.val = self.dcnt[o.eng][k]
                p = dprev[o.eng][k]
                if p is not None:
                    o.deps.add(p)
                dprev[o.eng][k] = o.idx
            elif o.signal:
                se = o.sem_eng or o.eng
                self.cnt[se] += 1
                o.sem = self.esem[se]
                o.val = self.cnt[se]
        first = self.nflush == 0
        self.nflush += 1
        with nc.Block() as blk:
            getters = {"pe": blk.tensor, "act": blk.scalar, "dve": blk.vector, "pool": blk.gpsimd, "sp": blk.sync}
            for ename in self.ENGS:
                mine = [o for o in ops if o.eng == ename]

                def body(e, mine=mine, ename=ename):
                    waited = {}
                    if not first:
                        for en, sem in self.esem.items():
                            if base_cnt[en] > 0 and en != ename:
                                e.wait_ge(sem, base_cnt[en])
                                waited[sem.num] = base_cnt[en]
                        for en, sems in self.dsem.items():
                            for k, sem in enumerate(sems):
                                if base_dcnt[en][k] > 0:
                                    e.wait_ge(sem, base_dcnt[en][k])
                                    waited[sem.num] = base_dcnt[en][k]
                    for o in mine:
                        need = {}
                        for d in o.deps:
                            do = ops[d]
                            if need.get(do.sem.num, (None, 0))[1] < do.val:
                                need[do.sem.num] = (do.sem, do.val)
                        for num, (sem, val) in need.items():
                            if waited.get(num, 0) >= val:
                                continue
                            e.wait_ge(sem, val)
                            waited[num] = val
                        ins = o.fn(e)
                        if o.signal:
                            if o.kind == "cc":
                                ins.then_inc(o.sem)
                            else:
                                ins.then_inc(o.sem, 16 if o.is_dma else 1)
                    if final and ename == "sp":
                        for en, sems in self.dsem.items():
                            for k, sem in enumerate(sems):
                                if self.dcnt[en][k] > 0:
                                    e.wait_ge(sem, self.dcnt[en][k])
                        for en, sem in self.esem.items():
                            if self.cnt[en] > 0:
                                e.wait_ge(sem, self.cnt[en])

                getters[ename](body)
        self._reset()
        if final:
            self.st.close()


def bank_rot(banks, lo, hi):
    state = {"i": 0}

    def nxt():
        k = lo + state["i"] % (hi - lo)
        state["i"] += 1
        return banks[k], ("ps", k)
    return nxt


EPS = 1e-6
NTOK = 4352
NOWN = 2304
SH1, SC1, GT1, SH2, SC2, GT2 = range(6)


def own_tiles():
    return [(i, i, 0) for i in range(16)] + [(16, 32, 1), (17, 33, 1)]


def rms_rstd(P, ss_ap, ss_key, out_ap, out_key, neghalf, D, tmp_ap, tmp_key):
    P.v(lambda e: e.tensor_scalar(tmp_ap, ss_ap, 1.0 / D, EPS, ALU.mult, ALU.add), [ss_key], [tmp_key])
    P.v(lambda e: e.tensor_tensor(out_ap, tmp_ap, neghalf, ALU.pow), [tmp_key, "consts"], [out_key], eng="pool")


def load_mod_rows(P, nc, st, mods_d, layer, which, names):
    out = []
    for v in range(2):
        t = st.enter_context(nc.sbuf_tensor("%s%d" % (names, v), [128, 1024], F32))
        P.dma(t[:], mods_d[layer, v:v + 1, which * 1024:(which + 1) * 1024].partition_broadcast(128), ["mods"],
              [(names, v)])
        out.append((t, (names, v)))
    return out


def phase_ada(nc, P, D, banks):
    with contextlib.ExitStack() as st:
        sb = lambda name, shape, dt=F32: st.enter_context(nc.sbuf_tensor(name, shape, dt))
        PS = Rot("ps", banks)
        ident = sb("ada_ident", [128, 128])
        cb = sb("ada_cb", [128, 2, 1024])
        screp = sb("ada_screp", [128, 2, 8, 128])
        bias = sb("ada_bias", [96, 2, 128])
        wbuf = Rot("ada_w", [sb("ada_wbuf%d" % i, [128, 8, 512]) for i in range(4)])
        accs = sb("ada_accs", [128, 2, 96])
        outT = sb("ada_outT", [96, 2, 128])
        P.dma(ident[:], D["ident32"][:, :], [], ["ident"])
        for v in range(2):
            P.dma(cb[:, v, :], D["cvec"][v:v + 1, :].partition_broadcast(128), [], [("cb", v)])
        for l in range(2):
            for v in range(2):
                P.dma(bias[48 * v:48 * v + 48, l, :], D["ada_b"][l].rearrange("(c p) -> c p", p=128), [], [("bias", l, v)])
        for v in range(2):
            P.act(cb[:, v, :], cb[:, v, :], AF.Silu, [("cb", v)], [("cb", v)])
            for half in range(2):
                p, pk = PS.next()
                for j in range(4):
                    kc = half * 4 + j
                    P.tr(p[:, j * 128:(j + 1) * 128], cb[:, v, kc * 128:(kc + 1) * 128], ident[:],
                         [("cb", v), "ident"], [pk])
                P.v(lambda e, p=p, v=v, half=half: e.tensor_copy(
                    screp[:, v, half * 4:(half + 1) * 4, :], p[:, :].rearrange("p (a b) -> p a b", a=4)),
                    [pk], [("screp", v)])
        for l in range(2):
            wl = D["ada_w"][l].rearrange("(kc p) n -> p kc n", p=128)
            acc, acck = PS.next()
            accv = acc[:, 0:96].rearrange("p (v c) -> p v c", v=2)
            for cblk in range(12):
                wb, wk = wbuf.next()
                P.dma(wb[:], wl[:, :, cblk * 512:(cblk + 1) * 512], [], [wk])
                for j in range(4):
                    c = cblk * 4 + j
                    for kc in range(8):
                        P.mm(accv[:, :, c], wb[:, kc, j * 128:(j + 1) * 128], screp[:, :, kc, 0], kc == 0, kc == 7,
                             [("screp", 0), ("screp", 1), wk], [acck])
            P.v(lambda e, acc=acc, l=l: e.tensor_copy(accs[:, l, :], acc[:, 0:96]), [acck], [("accs", l)])
            tp, tpk = PS.next()
            P.tr(tp[0:96, 0:128], accs[:, l, :], ident[:], [("accs", l), "ident"], [tpk])
            P.v(lambda e, tp=tp, l=l: e.tensor_tensor(outT[:, l, :], tp[0:96, 0:128], bias[:, l, :], ALU.add),
                [tpk, ("bias", l, 0), ("bias", l, 1)], [("outT", l)])
            P.dma(D["mods"][l].rearrange("v (c p) -> (v c) p", p=128), outT[:, l, :], [("outT", l)], ["mods"])
        P.flush()


def phase_l0a(nc, P, D, banks):
    NT = NTOK // 128
    with contextlib.ExitStack() as st:
        sb = lambda name, shape, dt=F32: st.enter_context(nc.sbuf_tensor(name, shape, dt))
        PS = Rot("ps", banks)
        ident = sb("a_ident", [128, 128], BF16)
        consts = sb("a_consts", [128, 4])
        gbc = sb("a_gbc", [128, 1024]); qg = sb("a_qg", [128, 256]); kvg = sb("a_kvg", [128, 128])
        w_in = sb("a_w_in", [128, 8, 1184], BF16)
        w_uq = sb("a_w_uq", [128, 2, 768], BF16)
        w_ukv = sb("a_w_ukv", [128, 1024], BF16)
        xt = Rot("xt", [sb("a_xt%d" % i, [128, 1024]) for i in range(2)])
        tabs = Rot("tabs", [sb("a_tabs%d" % i, [128, 1536]) for i in range(3)])
        junk = sb("a_junk", [128, 1024])
        t1r = Rot("t1", [sb("a_t1_%d" % i, [128, 1024]) for i in range(2)])
        hbr = Rot("hb", [sb("a_hb_%d" % i, [128, 1024], BF16) for i in range(2)])
        hTr = Rot("hT", [sb("a_hT_%d" % i, [128, 8, 128], BF16) for i in range(2)])
        small = Rot("small", [sb("a_small%d" % i, [128, 8]) for i in range(3)])
        zsr = Rot("zs", [sb("a_zs%d" % i, [128, 1184]) for i in range(2)])
        cqn = sb("a_cqn", [128, 384], BF16)
        cqnT = sb("a_cqnT", [128, 3, 128], BF16)
        krr = sb("a_krr", [128, 32], BF16)
        ropet = sb("a_ropet", [128, 2, 512])
        QA = sb("a_QA", [128, 8, 96], BF16); KA = sb("a_KA", [128, 8, 96], BF16)
        QB = sb("a_QB", [128, 8, 64], BF16); KB = sb("a_KB", [128, 2, 64], BF16)
        VAs = Rot("VAs", [sb("a_VAs%d" % i, [128, 8, 65], BF16) for i in range(2)])
        VBs = Rot("VBs", [sb("a_VBs%d" % i, [128, 2, 65], BF16) for i in range(2)])
        oQA = Rot("oQA", [sb("a_oQA%d" % i, [96, 8, 128], BF16) for i in range(2)])
        oKA = Rot("oKA", [sb("a_oKA%d" % i, [96, 8, 128], BF16) for i in range(2)])
        oQB = Rot("oQB", [sb("a_oQB%d" % i, [64, 8, 128], BF16) for i in range(2)])
        oKB = Rot("oKB", [sb("a_oKB%d" % i, [64, 2, 128], BF16) for i in range(2)])
        Abc = load_mod_rows(P, nc, st, D["mods"], 0, SC1, "a_A")
        Bbc = load_mod_rows(P, nc, st, D["mods"], 0, SH1, "a_B")

        P.dma(ident[:], D["identbf"][:, :], [], ["ident"])
        P.v(lambda e: e.memset(consts[:, 0:1], -0.5), [], ["consts"])
        P.dma(gbc[:], D["norm_mix_g"][0:1, :].partition_broadcast(128), [], ["gbc"])
        P.dma(qg[:], D["mla_q_norm_g"][0:1, :].partition_broadcast(128), [], ["qg"])
        P.dma(kvg[:], D["mla_kv_norm_g"][0:1, :].partition_broadcast(128), [], ["kvg"])
        P.dma(w_in[:], D["even_w_in"][0].rearrange("(kc p) n -> p kc n", p=128), [], ["w_in"], eng="pool")
        P.dma(w_uq[:], D["mla_w_uq"][0].rearrange("(kc p) n -> p kc n", p=128), [], ["w_uq"], eng="pool")
        P.dma(w_ukv[:], D["mla_w_ukv"][0], [], ["w_ukv"], eng="pool")
        for i, r in enumerate(VAs.aps):
            P.v(lambda e, r=r: e.memset(r[:, :, 64:65], 1.0), [], [("VAs", i)])
        for i, r in enumerate(VBs.aps):
            P.v(lambda e, r=r: e.memset(r[:, :, 64:65], 1.0), [], [("VBs", i)])
        for v in range(2):
            a, ak = Abc[v]
            P.v(lambda e, a=a: e.scalar_tensor_tensor(a[:], a[:], 1.0, gbc[:], ALU.add, ALU.mult), [ak, "gbc"], [ak])
        neghalf = consts[:, 0:1]
        v3 = lambda ap, h: ap.rearrange("p (h w) -> p h w", h=h)

        def rope(src4, dst4, cos4, ssin4, nh, blk, rkeys, wkeys):
            W = 4 * blk
            tmpa = ropet[:, 0, 0:nh * W].rearrange("p (h w) -> p h w", h=nh)
            tmpb = ropet[:, 1, 0:nh * W].rearrange("p (h w) -> p h w", h=nh)
            P.v(lambda e: e.tensor_tensor(tmpa, src4, cos4, ALU.mult), rkeys, ["ropeA"])
            v5 = lambda a: a.rearrange("p h (q w b) -> p h q w b", q=2, w=2)
            for w in range(2):
                P.v(lambda e, w=w: e.tensor_tensor(
                    v5(tmpb)[:, :, :, w, :], v5(src4)[:, :, :, 1 - w, :], v5(ssin4)[:, :, :, w, :], ALU.mult),
                    rkeys, ["ropeB%d" % w])
            P.v(lambda e: e.tensor_tensor(dst4, tmpa, tmpb, ALU.add), ["ropeA", "ropeB0", "ropeB1"], wkeys)

        def stage_a(t):
            rs = slice(t * 128, (t + 1) * 128)
            var = 1 if t >= 32 else 0
            need_q = (t < 16) or (t >= 32)
            A, Ak = Abc[var]; B, Bk = Bbc[var]
            x_ap, x_key = xt.next()
            P.dma(x_ap[:], D["xtok"][rs, :], [], [x_key])
            tb_ap, tb_key = tabs.next()
            P.dma(tb_ap[:, 0:256], D["cA"][rs, :], [], [(tb_key, 0)])
            P.dma(tb_ap[:, 256:512], D["sA"][rs, :], [], [(tb_key, 1)])
            P.dma(tb_ap[:, 512:1024], D["cB"][rs, :], [], [(tb_key, 2)])
            P.dma(tb_ap[:, 1024:1536], D["sB"][rs, :], [], [(tb_key, 3)])
            tkeys = [(tb_key, i) for i in range(4)]
            cA = tb_ap[:, 0:256]; sA = tb_ap[:, 256:512]; cB = tb_ap[:, 512:1024]; sB = tb_ap[:, 1024:1536]
            sm, sm_key = small.next()
            P.act(junk[:], x_ap[:], AF.Square, [x_key], ["junk", (sm_key, 0)], accum_out=sm[:, 0:1])
            rms_rstd(P, sm[:, 0:1], (sm_key, 0), sm[:, 2:3], (sm_key, 2), neghalf, 1024, sm[:, 1:2], (sm_key, 1))
            t1, t1k = t1r.next(); hb, hbk = hbr.next(); hT, hTk = hTr.next()
            P.v(lambda e, x_ap=x_ap, sm=sm, A=A, t1=t1: e.scalar_tensor_tensor(
                t1[:], x_ap[:], sm[:, 2:3], A[:], ALU.mult, ALU.mult), [x_key, (sm_key, 2), Ak], [t1k])
            P.v(lambda e, B=B, t1=t1, hb=hb: e.tensor_tensor(hb[:], t1[:], B[:], ALU.add), [t1k, Bk], [hbk])
            bk, bkey = PS.next()
            bkb = bk[:].bitcast(BF16)
            for kc in range(8):
                P.tr(bkb[:, kc * 128:(kc + 1) * 128], hb[:, kc * 128:(kc + 1) * 128], ident[:], [hbk, "ident"], [bkey])
            P.act(hT[:].rearrange("p a b -> p (a b)"), bkb, AF.Copy, [bkey], [hTk])
            blocks = [(0, 416), (416, 928), (928, 1184)]
            zb = []
            for bi, (c0, c1) in enumerate(blocks):
                if bi == 1 and not need_q:
                    zb.append((None, None))
                    continue
                bk, bkey = PS.next()
                for kc in range(8):
                    P.mm(bk[:, 0:c1 - c0], hT[:, kc, :], w_in[:, kc, c0:c1], kc == 0, kc == 7, [hTk, "w_in"], [bkey])
                zb.append((bk, bkey))
            zs, zsk = zsr.next()
            for bi, (c0, c1) in enumerate(blocks):
                if zb[bi][0] is None:
                    continue
                P.act(zs[:, c0:c1], zb[bi][0][:, 0:c1 - c0], AF.Copy, [zb[bi][1]], [(zsk, bi)])
            return dict(t=t, rs=rs, need_q=need_q, zs=zs, zsk=zsk, tb_ap=tb_ap, tkeys=tkeys, sm=sm, sm_key=sm_key)

        def stage_b(c):
            t, rs, need_q, zs, zsk, tb_ap, tkeys, sm, sm_key = (c[k] for k in ("t", "rs", "need_q", "zs", "zsk", "tb_ap", "tkeys", "sm", "sm_key"))
            cA = tb_ap[:, 0:256]; sA = tb_ap[:, 256:512]; cB = tb_ap[:, 512:1024]; sB = tb_ap[:, 1024:1536]
            z0, z0k = zs[:, 0:416], (zsk, 0)
            z1, z1k = zs[:, 416:928], (zsk, 1)
            z2, z2k = zs[:, 928:1184], (zsk, 2)
            if need_q:
                P.act(junk[:, 0:256], z0[:, 0:256], AF.Square, [z0k], ["junk", (sm_key, 3)], accum_out=sm[:, 3:4])
                rms_rstd(P, sm[:, 3:4], (sm_key, 3), sm[:, 4:5], (sm_key, 4), neghalf, 256, sm[:, 1:2], (sm_key, 1))
                P.v(lambda e, z0=z0, sm=sm: e.scalar_tensor_tensor(
                    cqn[:, 0:256], z0[:, 0:256], sm[:, 4:5], qg[:], ALU.mult, ALU.mult),
                    [z0k, (sm_key, 4), "qg"], ["cqn"])
            P.act(junk[:, 0:128], z0[:, 256:384], AF.Square, [z0k], ["junk", (sm_key, 5)], accum_out=sm[:, 5:6])
            rms_rstd(P, sm[:, 5:6], (sm_key, 5), sm[:, 6:7], (sm_key, 6), neghalf, 128, sm[:, 1:2], (sm_key, 1))
            P.v(lambda e, z0=z0, sm=sm: e.scalar_tensor_tensor(
                cqn[:, 256:384], z0[:, 256:384], sm[:, 6:7], kvg[:], ALU.mult, ALU.mult),
                [z0k, (sm_key, 6), "kvg"], ["cqn"])
            bk, bkey = PS.next()
            bkb = bk[:].bitcast(BF16)
            for j in (range(3) if need_q else [2]):
                P.tr(bkb[:, j * 128:(j + 1) * 128], cqn[:, j * 128:(j + 1) * 128], ident[:], ["cqn", "ident"], [bkey])
            P.act(cqnT[:].rearrange("p a b -> p (a b)"), bkb[:, 0:384], AF.Copy, [bkey], ["cqnT"])
            rope(v3(z0[:, 384:416], 1), v3(krr[:, :], 1), v3(cA[:, 0:32], 1), v3(sA[:, 0:32], 1), 1, 8,
                 tkeys + [z0k], ["krr"])
            if need_q:
                for hh in range(2):
                    bk, bkey = PS.next()
                    for kc in range(2):
                        P.mm(bk[:, 0:384], cqnT[:, kc, :], w_uq[:, kc, hh * 384:(hh + 1) * 384], kc == 0, kc == 1,
                             ["cqnT", "w_uq"], [bkey])
                    q4 = bk[:, 0:384].rearrange("p (h w) -> p h w", h=4)
                    P.act(QA[:, hh * 4:(hh + 1) * 4, 0:64], q4[:, :, 0:64], AF.Copy, [bkey], [("QA", hh, 0)])
                    rope(q4[:, :, 64:96], QA[:, hh * 4:(hh + 1) * 4, 64:96],
                         v3(cA[:, hh * 128:(hh + 1) * 128], 4), v3(sA[:, hh * 128:(hh + 1) * 128], 4), 4, 8,
                         tkeys + [bkey], [("QA", hh, 1)])
            va, va_key = VAs.next()
            for hh in range(2):
                bk, bkey = PS.next()
                P.mm(bk[:, :], cqnT[:, 2, :], w_ukv[:, hh * 512:(hh + 1) * 512], True, True, ["cqnT", "w_ukv"], [bkey])
                k4 = bk[:, :].rearrange("p (h w) -> p h w", h=4)
                P.act(KA[:, hh * 4:(hh + 1) * 4, 0:64], k4[:, :, 0:64], AF.Copy, [bkey], [("KA", hh, 0)])
                P.v(lambda e, va=va, k4=k4, hh=hh: e.tensor_copy(va[:, hh * 4:(hh + 1) * 4, 0:64], k4[:, :, 64:128]),
                    [bkey], [(va_key, hh)])
            for h in range(8):
                P.v(lambda e, h=h: e.tensor_copy(KA[:, h, 64:96], krr[:, :]), ["krr"], [("KA", h, 1)], eng="pool")
            P.dma(D["VA"][rs, :], va[:].rearrange("p h w -> p (h w)"), [(va_key, 0), (va_key, 1)], [("VA", t)])
            if need_q:
                q8 = z1[:, :].rearrange("p (h w) -> p h w", h=8)
                rope(q8, QB[:, :, :], v3(cB[:, :], 8), v3(sB[:, :], 8), 8, 16, tkeys + [z1k], ["QB"])
            k2 = z2[:, 0:128].rearrange("p (h w) -> p h w", h=2)
            rope(k2, KB[:, :, :], v3(cB[:, 0:128], 2), v3(sB[:, 0:128], 2), 2, 16, tkeys + [z2k], ["KB"])
            vb, vb_key = VBs.next()
            P.act(vb[:, :, 0:64], z2[:, 128:256].rearrange("p (h w) -> p h w", h=2), AF.Copy, [z2k], [vb_key])
            P.dma(D["VB"][rs, :], vb[:].rearrange("p h w -> p (h w)"), [vb_key], [("VB", t)])
            QAk = [("QA", hh, j) for hh in range(2) for j in range(2)]
            KAk = [("KA", hh, 0) for hh in range(2)] + [("KA", h, 1) for h in range(8)]
            jobs = [(KA, KAk, 8, 96, oKA, "KAT"), (KB, ["KB"], 2, 64, oKB, "KBT")]
            if need_q:
                jobs += [(QA, QAk, 8, 96, oQA, "QAT"), (QB, ["QB"], 8, 64, oQB, "QBT")]
            for (src, skeys, nh, Dh, pool, dname) in jobs:
                bk, bkey = PS.next()
                bkb = bk[:].bitcast(BF16)
                for h in range(nh):
                    P.tr(bkb[0:Dh, h * 128:(h + 1) * 128], src[:, h, :], ident[:], skeys + ["ident"], [bkey])
                ob, okey = pool.next()
                P.act(ob[:].rearrange("p a b -> p (a b)"), bkb[0:Dh, 0:nh * 128], AF.Copy, [bkey], [okey])
                P.dma(D[dname][:, :, rs], ob[:], [okey], [(dname, t)])

        pend = None
        for t in range(NT):
            cur = stage_a(t)
            if pend is not None:
                stage_b(pend)
            pend = cur
        stage_b(pend)
        P.flush()


def phase_l0b(nc, P, D, banks):
    with contextlib.ExitStack() as st:
        sb = lambda name, shape, dt=F32: st.enter_context(nc.sbuf_tensor(name, shape, dt))
        nS, nO, nR = bank_rot(banks, 0, 4), bank_rot(banks, 4, 6), bank_rot(banks, 6, 8)
        nOP = bank_rot(banks, 0, 4)
        KBT = sb("b_KBT", [64, 2, NTOK], BF16)
        VB = sb("b_VB", [128, 34, 130], BF16)
        VA = sb("b_VA", [128, 34, 520], BF16)
        w_out = sb("b_wout", [64, 16, 1024], BF16)
        sel = sb("b_sel", [65, 64])
        es = sb("b_es", [64, 8])
        masks = sb("b_masks", [128, 2, 512], BF16)
        G = load_mod_rows(P, nc, st, D["mods"], 0, GT1, "b_G")
        KAh = Rot("KAh", [sb("b_KAh%d" % i, [96, NTOK], BF16) for i in range(2)])
        QAg = Rot("QAg", [sb("b_QAg%d" % i, [96, 8, 512], BF16) for i in range(2)])
        QBg = Rot("QBg", [sb("b_QBg%d" % i, [64, 8, 512], BF16) for i in range(2)])
        LA = 2
        PT = Rot("PT", [sb("b_PT%d" % i, [128, 512], BF16) for i in range(LA + 2)])
        Osb = Rot("Osb", [sb("b_Osb%d" % i, [65, 512]) for i in range(2)])
        rec = Rot("rec", [sb("b_rec%d" % i, [64, 512]) for i in range(2)])
        mixT = sb("b_mixT", [64, 16, 512], BF16)
        xt = Rot("bxt", [sb("b_xt%d" % i, [128, 1024]) for i in range(2)])
        ot = Rot("bot", [sb("b_ot%d" % i, [128, 1024]) for i in range(2)])

        P.dma(KBT[:], D["KBT"][:, :, :], [], ["KBT"])
        P.dma(VB[:], D["VB"].rearrange("(c p) w -> p c w", p=128), [], ["VB"])
        P.dma(VA[:], D["VA"].rearrange("(c p) w -> p c w", p=128), [], ["VA"])
        P.dma(w_out[:], D["even_w_out"][0].rearrange("(j p) n -> p j n", p=64), [], ["w_out"], eng="pool")
        P.v(lambda e: e.memset(sel[:], 0.0), [], ["sel"])
        P.v(lambda e: e.memset(sel[64:65, :], 1.0), ["sel"], ["sel"])
        P.dma(es[:], D["win_sink"][0:1, :].partition_broadcast(64), [], ["es"])
        P.act(es[:], es[:], AF.Exp, ["es"], ["es"])
        P.dma(masks[:], D["wmask"].rearrange("a p n -> p a n"), [], ["masks"])

        groups = [(g * 512, 512, g * 512, 0) for g in range(4)] + [(4096, 256, 2048, 1)]
        SC_A = 96.0 ** -0.5
        SC_B = 64.0 ** -0.5
        for (tok0, N, row0, var) in groups:
            qa, qak = QAg.next()
            P.dma(qa[:, :, 0:N], D["QAT"][:, :, tok0:tok0 + N], [], [qak])
            qb, qbk = QBg.next()
            P.dma(qb[:, :, 0:N], D["QBT"][:, :, tok0:tok0 + N], [], [qbk])
            chunks = list(range(34)) if var == 0 else [32, 33]
            def mla_norm(h, O, Ok, N=N):
                osb, osk = Osb.next()
                P.v(lambda e: e.tensor_copy(osb[0:65, 0:N], O[0:65, 0:N]), [Ok], [osk])
                R, Rk = nR()
                P.mm(R[0:64, 0:N], sel[0:65, 0:64], osb[0:65, 0:N], True, True, ["sel", osk], [Rk])
                rc, rck = rec.next()
                P.v(lambda e: e.reciprocal(rc[0:64, 0:N], R[0:64, 0:N]), [Rk], [rck])
                P.v(lambda e: e.tensor_tensor(mixT[0:64, h, 0:N], osb[0:64, 0:N], rc[0:64, 0:N], ALU.mult),
                    [osk, rck], [("mixT", h)])

            defer = None
            for h in range(8):
                ka, kak = KAh.next()
                P.dma(ka[:], D["KAT"][:, h, :], [], [kak])
                O, Ok = nO()
                pend = []
                for ci, kc in enumerate(chunks + [None] * LA):
                    if kc is not None:
                        S, Sk = nS()
                        P.mm(S[:, 0:N], ka[0:96, kc * 128:(kc + 1) * 128], qa[0:96, h, 0:N], True, True, [kak, qak], [Sk])
                        pt, ptk = PT.next()
                        P.act(pt[:, 0:N], S[:, 0:N], AF.Exp, [Sk], [ptk], scale=SC_A)
                        pend.append((ci, kc, pt, ptk))
                    if defer is not None and (ci == LA or kc is None):
                        mla_norm(*defer)
                        defer = None
                    if len(pend) > LA or (kc is None and pend):
                        pci, pkc, ppt, pptk = pend.pop(0)
                        P.mm(O[0:65, 0:N], VA[:, pkc, h * 65:(h + 1) * 65], ppt[:, 0:N], pci == 0, pci == len(chunks) - 1,
                             ["VA", pptk], [Ok])
                defer = (h, O, Ok)
            mla_norm(*defer)
            nb = N // 128

            def win_norm(g, b, O, Ok):
                osb, osk = Osb.next()
                P.v(lambda e: e.tensor_copy(osb[0:65, :], O[0:65, :]), [Ok], [osk])
                R, Rk = nR()
                P.mm(R[0:64, :], sel[0:65, 0:64], osb[0:65, :], True, True, ["sel", osk], [Rk])
                rc, rck = rec.next()
                for j in range(4):
                    P.v(lambda e, j=j: e.tensor_scalar(
                        rc[0:64, j * 128:(j + 1) * 128], R[0:64, j * 128:(j + 1) * 128],
                        es[0:64, g * 4 + j:g * 4 + j + 1], None, ALU.add), [Rk, "es"], [rck])
                P.v(lambda e: e.reciprocal(rc[0:64, :], rc[0:64, :]), [rck], [rck])
                P.v(lambda e: e.tensor_tensor(
                    mixT[0:64, 8 + g * 4:8 + g * 4 + 4, b * 128:(b + 1) * 128],
                    osb[0:64, :].rearrange("p (h q) -> p h q", h=4),
                    rc[0:64, :].rearrange("p (h q) -> p h q", h=4), ALU.mult),
                    [osk, rck], [("mixT", 8 + g * 4 + j) for j in range(4)])

            deferw = None
            for b in range(nb):
                tt = tok0 // 128 + b
                if var == 0:
                    cl = []
                    if tt > 0:
                        cl.append((tt - 1, 0))
                    cl.append((tt, None))
                    cl.append((tt + 1, 1))
                    cl += [(32, None), (33, None)]
                else:
                    cl = [(32, None), (33, None)]
                for g in range(2):
                    O, Ok = nO()
                    Q4 = qb[0:64, g * 4:(g + 1) * 4, b * 128:(b + 1) * 128]
                    pend = []
                    for ci, item in enumerate(cl + [None] * LA):
                        if item is not None:
                            kc, mk_ = item
                            S, Sk = nS()
                            P.mm(S[:, :].rearrange("p (h q) -> p h q", h=4), KBT[0:64, g, kc * 128:(kc + 1) * 128], Q4,
                                 True, True, ["KBT", qbk], [Sk])
                            pt, ptk = PT.next()
                            P.act(pt[:, :], S[:, :], AF.Exp, [Sk], [ptk], scale=SC_B)
                            if mk_ is not None:
                                P.v(lambda e, pt=pt, mk_=mk_: e.tensor_tensor(pt[:, :], pt[:, :], masks[:, mk_, :], ALU.mult),
                                    [ptk, "masks"], [ptk], eng="pool")
                            pend.append((ci, kc, pt, ptk))
                        if deferw is not None and (ci == min(LA, len(cl) - 1)):
                            win_norm(*deferw)
                            deferw = None
                        if len(pend) > LA or (item is None and pend):
                            pci, pkc, ppt, pptk = pend.pop(0)
                            P.mm(O[0:65, :], VB[:, pkc, g * 65:(g + 1) * 65], ppt[:, :], pci == 0, pci == len(cl) - 1,
                                 ["VB", pptk], [Ok])
                    deferw = (g, b, O, Ok)
            if deferw is not None:
                win_norm(*deferw)
                deferw = None
            mkeys = [("mixT", j) for j in range(16)]
            for b in range(nb):
                x_ap, xk = xt.next()
                P.dma(x_ap[:], D["xtok"][tok0 + b * 128:tok0 + (b + 1) * 128, :], [], [xk])
                o_ap, ok_ = ot.next()
                for dh in range(2):
                    Ob, Obk = nOP()
                    for j in range(16):
                        P.mm(Ob[:, :], mixT[0:64, j, b * 128:(b + 1) * 128], w_out[0:64, j, dh * 512:(dh + 1) * 512],
                             j == 0, j == 15, mkeys + ["w_out"], [Obk])
                    P.v(lambda e, o_ap=o_ap, Ob=Ob, dh=dh, var=var: e.tensor_tensor(
                        o_ap[:, dh * 512:(dh + 1) * 512], Ob[:, :], G[var][0][:, dh * 512:(dh + 1) * 512], ALU.mult),
                        [Obk, G[var][1]], [ok_])
                P.v(lambda e, o_ap=o_ap, x_ap=x_ap: e.tensor_tensor(o_ap[:], o_ap[:], x_ap[:], ALU.add),
                    [ok_, xk], [ok_], eng="pool")
                r0 = row0 + b * 128
                P.dma(D["x1"][r0:r0 + 128, :], o_ap[:], [ok_], [("x1", r0)])
        P.flush()


def phase_moe(nc, P, D, banks, layer, xin, xout, tiles, tag, final_norm=False):
    NTl = len(tiles)
    T = NTl * 128
    with contextlib.ExitStack() as st0:
        sb0 = lambda name, shape, dt=F32: st0.enter_context(nc.sbuf_tensor(tag + name, shape, dt))
        h2T = sb0("h2T", [128, 8, T], BF16)
        comb = sb0("comb", [128, NTl, 32])
        consts = sb0("consts", [128, 4])
        P.v(lambda e: e.memset(consts[:, 0:1], -0.5), [], ["consts"])
        neghalf = consts[:, 0:1]
        with contextlib.ExitStack() as st:
            sb = lambda name, shape, dt=F32: st.enter_context(nc.sbuf_tensor(tag + name, shape, dt))
            nA, nB = bank_rot(banks, 0, 4), bank_rot(banks, 4, 8)
            ident = sb("ident32", [128, 128])
            gbc = sb("gbc", [128, 1024])
            w_r = sb("w_r", [128, 8, 36])
            Abc = load_mod_rows(P, nc, st, D["mods"], layer, SC2, tag + "A")
            Bbc = load_mod_rows(P, nc, st, D["mods"], layer, SH2, tag + "B")
            xt = Rot("mxt", [sb("xt%d" % i, [128, 1024]) for i in range(2)])
            junk = sb("junk", [128, 1024])
            t1 = sb("t1", [128, 1024])
            h2 = sb("h2", [128, 1024])
            h2T32 = sb("h2T32", [128, 8, 128])
            small = Rot("msmall", [sb("small%d" % i, [128, 16]) for i in range(2)])
            rt = Rot("mrt", [sb("rt%d" % i, [128, 96]) for i in range(2)])
            P.dma(ident[:], D["ident32"][:, :], [], ["ident"])
            P.dma(gbc[:], D["norm_ffn_g"][layer:layer + 1, :].partition_broadcast(128), [], ["gbc"])
            P.dma(w_r[:, :, 0:4], D["moe_w_rg"][layer].rearrange("(kc p) n -> p kc n", p=128), [], [("w_r", 0)])
            P.dma(w_r[:, :, 4:36], D["moe_w_re"][layer].rearrange("(kc p) n -> p kc n", p=128), [], [("w_r", 1)])
            for v in range(2):
                a, ak = Abc[v]
                P.v(lambda e, a=a: e.scalar_tensor_tensor(a[:], a[:], 1.0, gbc[:], ALU.add, ALU.mult), [ak, "gbc"], [ak])
            for ti, (r, var) in enumerate(tiles):
                A, Ak = Abc[var]; B, Bk = Bbc[var]
                x_ap, xk = xt.next()
                P.dma(x_ap[:], D[xin][r * 128:(r + 1) * 128, :], [], [xk])
                sm, smk = small.next()
                P.act(junk[:], x_ap[:], AF.Square, [xk], ["junk", (smk, 0)], accum_out=sm[:, 0:1])
                rms_rstd(P, sm[:, 0:1], (smk, 0), sm[:, 2:3], (smk, 2), neghalf, 1024, sm[:, 1:2], (smk, 1))
                P.v(lambda e, x_ap=x_ap, sm=sm, A=A: e.scalar_tensor_tensor(
                    t1[:], x_ap[:], sm[:, 2:3], A[:], ALU.mult, ALU.mult), [xk, (smk, 2), Ak], ["t1"])
                P.v(lambda e, B=B: e.tensor_tensor(h2[:], t1[:], B[:], ALU.add), ["t1", Bk], ["h2"])
                for half in range(2):
                    bk, bkey = nA()
                    for j in range(4):
                        kc = half * 4 + j
                        P.tr(bk[:, j * 128:(j + 1) * 128], h2[:, kc * 128:(kc + 1) * 128], ident[:], ["h2", "ident"], [bkey])
                    P.act(h2T[:, half * 4:(half + 1) * 4, ti * 128:(ti + 1) * 128],
                          bk[:, :].rearrange("p (a b) -> p a b", a=4), AF.Copy, [bkey], [("h2T", ti, half)])
                    P.v(lambda e, bk=bk, half=half: e.tensor_copy(
                        h2T32[:, half * 4:(half + 1) * 4, :], bk[:, :].rearrange("p (a b) -> p a b", a=4)),
                        [bkey], [("h2T32", half)])
                lg, lgk = nB()
                for kc in range(8):
                    P.mm(lg[:, 0:36], h2T32[:, kc, :], w_r[:, kc, :], kc == 0, kc == 7,
                         [("h2T32", kc // 4), ("w_r", 0), ("w_r", 1)], [lgk])
                R, Rk = rt.next()
                P.v(lambda e, R=R, lg=lg: e.tensor_copy(R[:, 0:36], lg[:, 0:36]), [lgk], [(Rk, "lg")])
                s_ = lambda c: sm[:, c:c + 1]
                P.v(lambda e, R=R, sm=sm: e.reduce_max(sm[:, 3:4], R[:, 0:4], AX.X), [(Rk, "lg")], [(smk, 3)])
                P.v(lambda e, R=R, sm=sm: e.tensor_scalar(R[:, 36:40], R[:, 0:4], sm[:, 3:4], None, ALU.is_equal),
                    [(Rk, "lg"), (smk, 3)], [(Rk, "goh")])
                P.v(lambda e, sm=sm: e.tensor_scalar(sm[:, 4:5], sm[:, 3:4], -1.0, None, ALU.mult), [(smk, 3)], [(smk, 4)])
                P.act(R[:, 80:84], R[:, 0:4], AF.Exp, [(Rk, "lg"), (smk, 4)], [(Rk, "gexp"), (smk, 5)],
                      bias=sm[:, 4:5], accum_out=sm[:, 5:6])
                P.v(lambda e, sm=sm: e.reciprocal(sm[:, 6:7], sm[:, 5:6]), [(smk, 5)], [(smk, 6)])
                P.v(lambda e, R=R: e.tensor_scalar(R[:, 40:48], R[:, 4:12], R[:, 36:37], None, ALU.mult),
                    [(Rk, "lg"), (Rk, "goh")], [(Rk, "ein")])
                for g in range(1, 4):
                    P.v(lambda e, R=R, g=g: e.scalar_tensor_tensor(
                        R[:, 40:48], R[:, 4 + 8 * g:12 + 8 * g], R[:, 36 + g:37 + g], R[:, 40:48], ALU.mult, ALU.add),
                        [(Rk, "lg"), (Rk, "goh"), (Rk, "ein")], [(Rk, "ein")])
                P.v(lambda e, R=R, sm=sm: e.reduce_max(sm[:, 7:8], R[:, 40:48], AX.X), [(Rk, "ein")], [(smk, 7)])
                P.v(lambda e, R=R, sm=sm: e.tensor_scalar(R[:, 48:56], R[:, 40:48], sm[:, 7:8], None, ALU.is_equal),
                    [(Rk, "ein"), (smk, 7)], [(Rk, "oh1")])
                P.v(lambda e, R=R: e.scalar_tensor_tensor(R[:, 56:64], R[:, 48:56], -1e30, R[:, 40:48], ALU.mult, ALU.add),
                    [(Rk, "oh1"), (Rk, "ein")], [(Rk, "e2")])
                P.v(lambda e, R=R, sm=sm: e.reduce_max(sm[:, 8:9], R[:, 56:64], AX.X), [(Rk, "e2")], [(smk, 8)])
                P.v(lambda e, R=R, sm=sm: e.tensor_scalar(R[:, 64:72], R[:, 56:64], sm[:, 8:9], None, ALU.is_equal),
                    [(Rk, "e2"), (smk, 8)], [(Rk, "oh2")])
                P.v(lambda e, sm=sm: e.tensor_tensor(sm[:, 9:10], sm[:, 8:9], sm[:, 7:8], ALU.subtract),
                    [(smk, 7), (smk, 8)], [(smk, 9)])
                P.act(sm[:, 10:11], sm[:, 9:10], AF.Exp, [(smk, 9)], [(smk, 10)])
                P.v(lambda e, sm=sm: e.tensor_scalar(sm[:, 11:12], sm[:, 10:11], 1.0, None, ALU.add), [(smk, 10)], [(smk, 11)])
                P.v(lambda e, sm=sm: e.reciprocal(sm[:, 11:12], sm[:, 11:12]), [(smk, 11)], [(smk, 11)])
                P.v(lambda e, sm=sm: e.tensor_tensor(sm[:, 12:13], sm[:, 11:12], sm[:, 6:7], ALU.mult),
                    [(smk, 11), (smk, 6)], [(smk, 12)])
                P.v(lambda e, sm=sm: e.tensor_tensor(sm[:, 13:14], sm[:, 12:13], sm[:, 10:11], ALU.mult),
                    [(smk, 12), (smk, 10)], [(smk, 13)])
                P.v(lambda e, R=R, sm=sm: e.tensor_scalar(R[:, 72:80], R[:, 48:56], sm[:, 12:13], None, ALU.mult),
                    [(Rk, "oh1"), (smk, 12)], [(Rk, "loc")])
                P.v(lambda e, R=R, sm=sm: e.scalar_tensor_tensor(
                    R[:, 72:80], R[:, 64:72], sm[:, 13:14], R[:, 72:80], ALU.mult, ALU.add),
                    [(Rk, "oh2"), (smk, 13), (Rk, "loc")], [(Rk, "loc")])
                for g in range(4):
                    P.v(lambda e, R=R, g=g, ti=ti: e.tensor_scalar(
                        comb[:, ti, g * 8:(g + 1) * 8], R[:, 72:80], R[:, 36 + g:37 + g], None, ALU.mult),
                        [(Rk, "loc"), (Rk, "goh")], [("comb", ti)])
            P.flush()
        with contextlib.ExitStack() as st:
            sb = lambda name, shape, dt=F32: st.enter_context(nc.sbuf_tensor(tag + name, shape, dt))
            nGU, nY = bank_rot(banks, 0, 4), bank_rot(banks, 4, 8)
            yacc = sb("yacc", [128, NTl, 1024])
            wg = Rot("wg", [sb("wg%d" % i, [128, 8, 512], BF16) for i in range(2)])
            wu = Rot("wu", [sb("wu%d" % i, [128, 8, 512], BF16) for i in range(2)])
            wd = Rot("wd", [sb("wd%d" % i, [128, 4, 1024], BF16) for i in range(2)])
            hg = Rot("hg", [sb("hg%d" % i, [128, 4, 512], BF16) for i in range(2)])
            sg = Rot("sg", [sb("sg%d" % i, [128, 512]) for i in range(2)])
            G = load_mod_rows(P, nc, st, D["mods"], layer, GT2, tag + "G")
            xt = Rot("mxt2", [sb("xt2_%d" % i, [128, 1024]) for i in range(2)])
            groups = []
            t0 = 0
            while t0 < NTl:
                n = min(4, NTl - t0)
                groups.append((t0, n))
                t0 += n
            def emit_gu(ex, t0, n, g_ap, gk, u_ap, uk):
                N = n * 128
                ts = slice(t0 * 128, t0 * 128 + N)
                hgt, hgk = hg.next()
                for fc in range(4):
                    Gp, Gk = nGU(); Up, Uk = nGU()
                    for kc in range(8):
                        P.mm(Gp[:, 0:N], g_ap[:, kc, fc * 128:(fc + 1) * 128], h2T[:, kc, ts], kc == 0, kc == 7,
                             [gk, "h2T"], [Gk])
                    for kc in range(8):
                        P.mm(Up[:, 0:N], u_ap[:, kc, fc * 128:(fc + 1) * 128], h2T[:, kc, ts], kc == 0, kc == 7,
                             [uk, "h2T"], [Uk])
                    s_ap, sk = sg.next()
                    P.act(s_ap[:, 0:N], Gp[:, 0:N], AF.Silu, [Gk], [sk])
                    P.v(lambda e, hgt=hgt, Up=Up, s_ap=s_ap, fc=fc, N=N: e.tensor_tensor(
                        hgt[:, fc, 0:N], Up[:, 0:N], s_ap[:, 0:N], ALU.mult), [Uk, sk], [(hgk, fc)])
                return hgt, hgk

            def emit_down(ex, t0, n, hgt, hgk, d_ap, dk):
                for b in range(n):
                    ti = t0 + b
                    for dh in range(2):
                        Yp, Yk = nY()
                        for fc in range(4):
                            P.mm(Yp[:, :], hgt[:, fc, b * 128:(b + 1) * 128], d_ap[:, fc, dh * 512:(dh + 1) * 512],
                                 fc == 0, fc == 3, [(hgk, f) for f in range(4)] + [dk], [Yk])
                        ysl = yacc[:, ti, dh * 512:(dh + 1) * 512]
                        if ex == 0:
                            P.v(lambda e, ysl=ysl, Yp=Yp, ti=ti, ex=ex: e.tensor_scalar(
                                ysl, Yp[:, :], comb[:, ti, ex:ex + 1], None, ALU.mult), [Yk], [("yacc", ti, dh)])
                        else:
                            P.v(lambda e, ysl=ysl, Yp=Yp, ti=ti, ex=ex: e.scalar_tensor_tensor(
                                ysl, Yp[:, :], comb[:, ti, ex:ex + 1], ysl, ALU.mult, ALU.add),
                                [Yk, ("yacc", ti, dh)], [("yacc", ti, dh)])

            pend = None
            for ex in range(32):
                g_ap, gk = wg.next(); u_ap, uk = wu.next(); d_ap, dk = wd.next()
                P.dma(g_ap[:], D["moe_w_gate%d" % layer][ex].rearrange("(kc p) f -> p kc f", p=128), [], [gk], eng="pool")
                P.dma(u_ap[:], D["moe_w_up%d" % layer][ex].rearrange("(kc p) f -> p kc f", p=128), [], [uk], eng="pool")
                P.dma(d_ap[:], D["moe_w_down%d" % layer][ex].rearrange("(kc p) f -> p kc f", p=128), [], [dk], eng="pool")
                for (t0, n) in groups:
                    hgt, hgk = emit_gu(ex, t0, n, g_ap, gk, u_ap, uk)
                    if pend is not None:
                        emit_down(*pend)
                    pend = (ex, t0, n, hgt, hgk, d_ap, dk)
            emit_down(*pend)
            if final_norm:
                fg = sb("fg", [128, 1024])
                P.dma(fg[:], D["final_norm_g"][0:1, :].partition_broadcast(128), [], ["fg"])
                junk = sb("junk3", [128, 1024])
                small = Rot("fsmall", [sb("fsmall%d" % i, [128, 4]) for i in range(2)])
            for ti, (r, var) in enumerate(tiles):
                x_ap, xk = xt.next()
                P.dma(x_ap[:], D[xin][r * 128:(r + 1) * 128, :], [], [xk])
                ysl = yacc[:, ti, :]
                yk = [("yacc", ti, 0), ("yacc", ti, 1)]
                P.v(lambda e, ysl=ysl, var=var: e.tensor_tensor(ysl, ysl, G[var][0][:], ALU.mult), yk + [G[var][1]], yk)
                P.v(lambda e, ysl=ysl, x_ap=x_ap: e.tensor_tensor(ysl, ysl, x_ap[:], ALU.add), yk + [xk], yk, eng="pool")
                if final_norm:
                    sm, smk = small.next()
                    P.act(junk[:], ysl, AF.Square, yk, ["junk3", (smk, 0)], accum_out=sm[:, 0:1])
                    rms_rstd(P, sm[:, 0:1], (smk, 0), sm[:, 2:3], (smk, 2), neghalf, 1024, sm[:, 1:2], (smk, 1))
                    P.v(lambda e, ysl=ysl, sm=sm: e.scalar_tensor_tensor(
                        ysl, ysl, sm[:, 2:3], fg[:], ALU.mult, ALU.mult), yk + [(smk, 2), "fg"], yk)
                P.dma(D[xout][r * 128:(r + 1) * 128, :], ysl, yk, [(xout, r)])
            P.flush()


GELU_C = 0.7978845608028654


def l1_tiles():
    return [(i, 0) for i in range(16)] + [(16, 1), (17, 1)]


def phase_l1a(nc, P, D, banks):
    with contextlib.ExitStack() as st:
        sb = lambda name, shape, dt=F32: st.enter_context(nc.sbuf_tensor("c_" + name, shape, dt))
        nP = bank_rot(banks, 0, 8)
        ident = sb("ident", [128, 128], BF16)
        ident32 = sb("ident32", [128, 128])
        consts = sb("consts", [128, 4])
        gbc = sb("gbc", [128, 1024])
        w_in = sb("w_in", [128, 8, 2592], BF16)
        Abc = load_mod_rows(P, nc, st, D["mods"], 1, SC1, "c_A")
        Bbc = load_mod_rows(P, nc, st, D["mods"], 1, SH1, "c_B")
        W2 = sb("W2", [32, 512])
        bg = sb("bg", [128, 512])
        gm = sb("gm", [128, 3, 128])
        wsT = sb("wsT", [128, 4, 128], BF16)
        bsT = sb("bsT", [128, 4])
        lng = sb("lng", [128, 512]); lnb = sb("lnb", [128, 512])
        xt = Rot("cxt", [sb("xt%d" % i, [128, 1024]) for i in range(2)])
        junk = sb("junk", [128, 1024])
        t1r = Rot("t1", [sb("t1_%d" % i, [128, 1024]) for i in range(2)])
        hbr = Rot("hb", [sb("hb_%d" % i, [128, 1024], BF16) for i in range(2)])
        hTr = Rot("hT", [sb("hT_%d" % i, [128, 8, 128], BF16) for i in range(2)])
        small = Rot("csmall", [sb("small%d" % i, [128, 16]) for i in range(3)])
        qkr = Rot("qk", [sb("qk%d" % i, [128, 512]) for i in range(2)])
        g32r = Rot("g32", [sb("g32_%d" % i, [128, 32]) for i in range(2)])
        uvr = Rot("uv", [sb("uv%d" % i, [128, 2, 512]) for i in range(2)])
        g32T = sb("g32T", [32, 128])
        zs = sb("zs", [128, 512]); la = sb("la", [128, 512])
        bsb = sb("bsb", [128, 2, 256])
        ex = sb("ex", [128, 256]); tmp = sb("tmp", [128, 256])
        gl = sb("gl", [128, 6, 256], BF16)
        glT = Rot("glT", [sb("glT%d" % i, [128, 8, 128], BF16) for i in range(2)])
        dec = Rot("dec", [sb("dec%d" % i, [128, 4]) for i in range(2)])
        vb = Rot("cvb", [sb("vb%d" % i, [128, 512], BF16) for i in range(2)])
        rsb = Rot("rsb", [sb("rsb%d" % i, [128, 512], BF16) for i in range(2)])
        ge = sb("ge", [128, 2, 512])
        gt_ = sb("gt_", [128, 512]); gt2_ = sb("gt2_", [128, 512])
        vgn = sb("vgn", [128, 512], BF16)
        dlb = Rot("dlb", [sb("dlb%d" % i, [128, 512], BF16) for i in range(2)])

        P.dma(ident[:], D["identbf"][:, :], [], ["ident"])
        P.dma(ident32[:], D["ident32"][:, :], [], ["ident32"])
        P.v(lambda e: e.memset(consts[:, 0:1], -0.5), [], ["consts"])
        neghalf = consts[:, 0:1]
        P.dma(gbc[:], D["norm_mix_g"][1:2, :].partition_broadcast(128), [], ["gbc"])
        for c in range(3):
            lo, hi = c * 864, (c + 1) * 864
            P.dma(w_in[:, :, lo:hi], D["odd_w_in"][0].rearrange("(kc p) n -> p kc n", p=128)[:, :, lo:hi], [],
                  [("w_in", c)], eng="pool")
        wkeys = [("w_in", c) for c in range(3)]
        P.v(lambda e: e.memset(W2[:], 0.0), [], ["W2"])
        P.dma(W2[0:16, 0:256], D["gla_w_g2"][0, 0], ["W2"], ["W2"])
        P.dma(W2[16:32, 256:512], D["gla_w_g2"][0, 1], ["W2"], ["W2"])
        P.dma(bg[:], D["gla_b_g"][0:1].rearrange("o a n -> o (a n)").partition_broadcast(128), [], ["bg"])
        P.dma(gm[:], D["gmask"][0:3].rearrange("a p n -> p a n"), [], ["gm"])
        P.dma(wsT[:], D["sg_w_sT"].rearrange("g s t -> s g t"), [], ["wsT"], eng="pool")
        P.dma(bsT[:], D["sg_b_sT"][:, :], [], ["bsT"])
        P.dma(lng[:], D["sg_ln_g"][0:1, :].partition_broadcast(128), [], ["lng"])
        P.dma(lnb[:], D["sg_ln_b"][0:1, :].partition_broadcast(128), [], ["lnb"])
        for v in range(2):
            a, ak = Abc[v]
            P.v(lambda e, a=a: e.scalar_tensor_tensor(a[:], a[:], 1.0, gbc[:], ALU.add, ALU.mult), [ak, "gbc"], [ak])

        def gelu(src, src_key, dst, dst_key):
            P.v(lambda e: e.tensor_tensor(gt2_[:], src, src, ALU.mult), [src_key], ["gt2_"])
            P.v(lambda e: e.tensor_scalar(gt2_[:], gt2_[:], 0.044715, 1.0, ALU.mult, ALU.add), ["gt2_"], ["gt2_"])
            P.v(lambda e: e.tensor_tensor(gt2_[:], gt2_[:], src, ALU.mult), ["gt2_", src_key], ["gt2_"], eng="pool")
            P.act(gt2_[:], gt2_[:], AF.Sigmoid, ["gt2_"], ["gt2_"], scale=2.0 * GELU_C)
            P.v(lambda e: e.tensor_tensor(dst, src, gt2_[:], ALU.mult), [src_key, "gt2_"], [dst_key])

        def stage_a(r, var):
            lat = var == 0
            A, Ak = Abc[var]; B, Bk = Bbc[var]
            rs = slice(r * 128, (r + 1) * 128)
            x_ap, xk = xt.next()
            P.dma(x_ap[:], D["x2"][rs, :], [], [xk])
            sm, smk = small.next()
            P.act(junk[:], x_ap[:], AF.Square, [xk], ["junk", (smk, 0)], accum_out=sm[:, 0:1])
            rms_rstd(P, sm[:, 0:1], (smk, 0), sm[:, 2:3], (smk, 2), neghalf, 1024, sm[:, 1:2], (smk, 1))
            t1, t1k = t1r.next(); hb, hbk = hbr.next(); hT, hTk = hTr.next()
            P.v(lambda e, x_ap=x_ap, sm=sm, A=A, t1=t1: e.scalar_tensor_tensor(
                t1[:], x_ap[:], sm[:, 2:3], A[:], ALU.mult, ALU.mult), [xk, (smk, 2), Ak], [t1k])
            P.v(lambda e, B=B, t1=t1, hb=hb: e.tensor_tensor(hb[:], t1[:], B[:], ALU.add), [t1k, Bk], [hbk])
            bk, bkey = nP()
            bkb = bk[:].bitcast(BF16)
            for kc in range(8):
                P.tr(bkb[:, kc * 128:(kc + 1) * 128], hb[:, kc * 128:(kc + 1) * 128], ident[:], [hbk, "ident"], [bkey])
            P.act(hT[:].rearrange("p a b -> p (a b)"), bkb, AF.Copy, [bkey], [hTk])

            def proj(c0, c1):
                bk, bkey = nP()
                for kc in range(8):
                    P.mm(bk[:, 0:c1 - c0], hT[:, kc, :], w_in[:, kc, c0:c1], kc == 0, kc == 7, [hTk] + wkeys, [bkey])
                return bk, bkey
            zqk, zqkk = proj(0, 512)
            qk, qkk = qkr.next()
            P.act(qk[:], zqk[:, :], AF.Copy, [zqkk], [qkk])
            zv, zvk = proj(512, 1024)
            v_ap, vk = vb.next()
            P.act(v_ap[:], zv[:, :], AF.Copy, [zvk], [vk])
            P.dma(D["g_v"][rs, :], v_ap[:], [vk], [("g_v", r)])
            zg, zgk = proj(1024, 1056)
            g32, g32k = g32r.next()
            P.v(lambda e, zg=zg, g32=g32: e.tensor_copy(g32[:], zg[:, 0:32]), [zgk], [g32k])
            uv, uvk = uvr.next()
            if lat:
                zr, zrk = proj(1056, 1568)
                r_ap, rk = rsb.next()
                P.act(r_ap[:], zr[:, :], AF.Silu, [zrk], [rk])
                P.dma(D["rsilu"][rs, :], r_ap[:], [rk], [("rsilu", r)])
                zu, zuk = proj(1568, 2080)
                P.act(uv[:, 0, :], zu[:, :], AF.Copy, [zuk], [(uvk, 0)])
                zvg, zvgk = proj(2080, 2592)
                P.v(lambda e, uv=uv, zvg=zvg: e.tensor_copy(uv[:, 1, :], zvg[:, :]), [zvgk], [(uvk, 1)])
            return dict(r=r, lat=lat, rs=rs, sm=sm, smk=smk, qk=qk, qkk=qkk, g32=g32, g32k=g32k, uv=uv, uvk=uvk)

        def stage_b(c):
            r, lat, rs, sm, smk, qk, qkk, g32, g32k, uv, uvk = (c[k] for k in (
                "r", "lat", "rs", "sm", "smk", "qk", "qkk", "g32", "g32k", "uv", "uvk"))
            bk, bkey = nP()
            P.tr(bk[0:32, 0:128], g32[:, :], ident32[:], [g32k, "ident32"], [bkey])
            P.v(lambda e, bk=bk: e.tensor_copy(g32T[:], bk[0:32, 0:128]), [bkey], ["g32T"])
            zz, zzk = nP()
            P.mm(zz[:, :], g32T[0:32, :], W2[0:32, :], True, True, ["g32T", "W2"], [zzk])
            P.v(lambda e, zz=zz: e.tensor_tensor(zs[:], zz[:, :], bg[:], ALU.add), [zzk, "bg"], ["zs"])
            P.act(zs[:], zs[:], AF.Exp, ["zs"], ["zs"], scale=-1.0)
            P.act(zs[:], zs[:], AF.Ln, ["zs"], ["zs"], bias=1.0)
            P.v(lambda e: e.tensor_scalar(la[:], zs[:], -1.0 / 16.0, None, ALU.mult), ["zs"], ["la"])
            cA, cAk = nP()
            P.mm(cA[:, 0:256], gm[:, 0, :], la[:, 0:256], True, True, ["gm", "la"], [cAk])
            P.mm(cA[:, 256:512], gm[:, 1, :], la[:, 256:512], True, True, ["gm", "la"], [cAk])
            cL, cLk = nP()
            P.mm(cL[:, :], gm[:, 2, :], la[:, :], True, True, ["gm", "la"], [cLk])
            P.act(bsb[:].rearrange("p a b -> p (a b)"), cA[:, :], AF.Copy, [cAk], ["bsb"])
            cT, cTk = nP()
            for j in range(4):
                P.mm(cT[:, j:j + 1], la[:, j * 128:(j + 1) * 128], gm[:, 2, 0:1], True, True, ["gm", "la"], [cTk])
            d_ap, dk = dec.next()
            P.act(d_ap[:], cT[:, 0:4], AF.Exp, [cTk], [dk])
            P.dma(D["g_dec"][:, r, :], d_ap[:], [dk], [("g_dec", r)])
            for p in range(2):
                if p == 1 and not lat:
                    continue
                bp = bsb[:, p, :]
                if lat:
                    P.act(ex[:], bp, AF.Exp, ["bsb"], ["ex"])
                    P.v(lambda e, p=p: e.scalar_tensor_tensor(gl[:, 2 * p, :], qk[:, 0:256], 0.125, ex[:], ALU.mult, ALU.mult),
                        [qkk, "ex"], [("gl", 2 * p)])
                P.act(ex[:], bp, AF.Exp, ["bsb"], ["ex"], scale=-1.0)
                P.v(lambda e, p=p: e.tensor_tensor(gl[:, 2 * p + 1, :], qk[:, 256:512], ex[:], ALU.mult),
                    [qkk, "ex"], [("gl", 2 * p + 1)])
                P.v(lambda e, p=p, bp=bp, cL=cL: e.tensor_tensor(tmp[:], cL[:, p * 256:(p + 1) * 256], bp, ALU.subtract),
                    [cLk, "bsb"], ["tmp"])
                P.act(ex[:], tmp[:], AF.Exp, ["tmp"], ["ex"])
                P.v(lambda e, p=p: e.tensor_tensor(gl[:, 4 + p, :], qk[:, 256:512], ex[:], ALU.mult),
                    [qkk, "ex"], [("gl", 4 + p)])
                P.dma(D["g_kd"][p, rs, :], gl[:, 4 + p, :], [("gl", 4 + p)], [("g_kd", p, r)])
            bk, bkey = nP()
            bkb = bk[:].bitcast(BF16)
            arrs = [0, 1, 2, 3] if lat else [1]
            for a_ in arrs:
                for j in range(2):
                    P.tr(bkb[:, (a_ * 2 + j) * 128:(a_ * 2 + j + 1) * 128], gl[:, a_, j * 128:(j + 1) * 128], ident[:],
                         [("gl", a_), "ident"], [bkey])
            gT, gTk = glT.next()
            if lat:
                P.act(gT[:].rearrange("p a b -> p (a b)"), bkb, AF.Copy, [bkey], [gTk])
                P.dma(D["g_T"][:, r, :, :], gT[:], [gTk], [("g_T", r)])
            else:
                P.act(gT[:, 2:4, :].rearrange("p a b -> p (a b)"), bkb[:, 256:512], AF.Copy, [bkey], [gTk])
                P.dma(D["g_T"][:, r, 2:4, :], gT[:, 2:4, :], [gTk], [("g_T", r)])
            if not lat:
                return
            gelu(uv[:, 0, :], (uvk, 0), ge[:, 0, :], ("ge", 0))
            gelu(uv[:, 1, :], (uvk, 1), ge[:, 1, :], ("ge", 1))
            P.v(lambda e, sm=sm: e.reduce_sum(sm[:, 8:9], ge[:, 1, :], AX.X), [("ge", 1)], [(smk, 8)])
            P.act(junk[:, 0:512], ge[:, 1, :], AF.Square, [("ge", 1)], ["junk", (smk, 9)], accum_out=sm[:, 9:10])
            P.v(lambda e, sm=sm: e.tensor_scalar(sm[:, 10:11], sm[:, 8:9], 1.0 / 512, None, ALU.mult), [(smk, 8)], [(smk, 10)])
            P.v(lambda e, sm=sm: e.tensor_tensor(sm[:, 11:12], sm[:, 10:11], sm[:, 10:11], ALU.mult), [(smk, 10)], [(smk, 11)])
            P.v(lambda e, sm=sm: e.scalar_tensor_tensor(sm[:, 12:13], sm[:, 9:10], 1.0 / 512, sm[:, 11:12], ALU.mult, ALU.subtract),
                [(smk, 9), (smk, 11)], [(smk, 12)])
            P.v(lambda e, sm=sm: e.tensor_scalar(sm[:, 12:13], sm[:, 12:13], EPS, None, ALU.add), [(smk, 12)], [(smk, 12)])
            P.v(lambda e, sm=sm: e.tensor_tensor(sm[:, 13:14], sm[:, 12:13], neghalf, ALU.pow), [(smk, 12), "consts"],
                [(smk, 13)], eng="pool")
            P.v(lambda e, sm=sm: e.tensor_scalar(gt_[:], ge[:, 1, :], sm[:, 10:11], sm[:, 13:14], ALU.subtract, ALU.mult),
                [("ge", 1), (smk, 10), (smk, 13)], ["gt_"])
            P.v(lambda e: e.tensor_tensor(gt_[:], gt_[:], lng[:], ALU.mult), ["gt_", "lng"], ["gt_"])
            P.v(lambda e: e.tensor_tensor(vgn[:], gt_[:], lnb[:], ALU.add), ["gt_", "lnb"], ["vgn"])
            sp_, spk = nP()
            for gi in range(4):
                P.mm(sp_[:, gi * 128:(gi + 1) * 128], wsT[:, gi, :], vgn[:, gi * 128:(gi + 1) * 128], True, True,
                     ["wsT", "vgn"], [spk])
            dl_ap, dlk = dlb.next()
            for gi in range(4):
                P.v(lambda e, gi=gi, sp_=sp_, dl_ap=dl_ap: e.scalar_tensor_tensor(
                    dl_ap[:, gi * 128:(gi + 1) * 128], sp_[:, gi * 128:(gi + 1) * 128], bsT[:, gi:gi + 1],
                    ge[:, 0, gi * 128:(gi + 1) * 128], ALU.add, ALU.mult), [spk, "bsT", ("ge", 0)], [dlk])
            P.dma(D["dl"][rs, :], dl_ap[:], [dlk], [("dl", r)])

        pend = None
        for (r, var) in l1_tiles():
            cur = stage_a(r, var)
            if pend is not None:
                stage_b(pend)
            pend = cur
        stage_b(pend)
        P.flush()


def _gla_pass(nc, P, D, banks, st, sb, pidx, order, S, Sb, on_out):
    nAT, nO, nU = bank_rot(banks, 0, 2), bank_rot(banks, 2, 4), bank_rot(banks, 4, 6)
    gT = Rot("gTl", [sb("gTl%d" % i, [128, 4, 128], BF16) for i in range(2)])
    kd = Rot("kdl", [sb("kdl%d" % i, [128, 256], BF16) for i in range(2)])
    vv = Rot("vl", [sb("vl%d" % i, [128, 512], BF16) for i in range(2)])
    dc = Rot("dcl", [sb("dcl%d" % i, [128, 2]) for i in range(2)])
    ATm = Rot("ATm", [sb("ATm%d" % i, [128, 128], BF16) for i in range(2)])
    mask = sb("gmask_sb", [128, 128])
    P.dma(mask[:], D["gmask"][pidx], [], ["gmask"])
    for r in order:
        lat = r < 16
        rs = slice(r * 128, (r + 1) * 128)
        g_ap, gk = gT.next()
        if lat:
            P.dma(g_ap[:], D["g_T"][:, r, 4 * pidx:4 * pidx + 4, :], [], [gk])
        else:
            P.dma(g_ap[:, 2:4, :], D["g_T"][:, r, 4 * pidx + 2:4 * pidx + 4, :], [], [gk])
        k_ap, kk = kd.next()
        P.dma(k_ap[:], D["g_kd"][pidx, rs, :], [], [kk])
        v_ap, vk = vv.next()
        P.dma(v_ap[:], D["g_v"][rs, :], [], [vk])
        d_ap, dk = dc.next()
        P.dma(d_ap[:], D["g_dec"][:, r, 2 * pidx:2 * pidx + 2], [], [dk])
        if lat:
            O, Ok = nO()
            for h in range(4):
                j, po = h // 2, (h % 2) * 64
                AT, ATk = nAT()
                P.mm(AT[:, 0:128], g_ap[po:po + 64, 2 + j, :], g_ap[po:po + 64, j, :], True, True, [gk], [ATk])
                am, amk = ATm.next()
                P.v(lambda e, am=am, AT=AT: e.tensor_tensor(am[:], AT[:, 0:128], mask[:], ALU.mult), [ATk, "gmask"], [amk])
                P.mm(O[:, h * 128:(h + 1) * 128], am[:], v_ap[:, h * 128:(h + 1) * 128], True, False, [amk, vk], [Ok])
                P.mm(O[:, h * 128:(h + 1) * 128], g_ap[po:po + 64, j, :], Sb[po:po + 64, j, :], False, True,
                     [gk, ("Sb", j, h % 2)], [Ok])
            on_out(r, O, Ok)
        for j in range(2):
            for hh in range(2):
                po = hh * 64
                U, Uk = nU()
                P.mm(U[:, 0:128], k_ap[:, j * 128:(j + 1) * 128], v_ap[:, (2 * j + hh) * 128:(2 * j + hh + 1) * 128],
                     True, True, [kk, vk], [Uk])
                P.v(lambda e, U=U, j=j, po=po, d_ap=d_ap: e.scalar_tensor_tensor(
                    S[po:po + 64, j, :], S[po:po + 64, j, :], d_ap[po:po + 64, j:j + 1], U[po:po + 64, 0:128],
                    ALU.mult, ALU.add), [Uk, dk, ("S", j, hh)], [("S", j, hh)])
                P.act(Sb[po:po + 64, j, :], S[po:po + 64, j, :], AF.Copy, [("S", j, hh)], [("Sb", j, hh)])


def phase_l1b_a(nc, P, D, banks):
    with contextlib.ExitStack() as st:
        sb = lambda name, shape, dt=F32: st.enter_context(nc.sbuf_tensor("d_" + name, shape, dt))
        S = sb("S", [128, 2, 128]); Sb = sb("Sb", [128, 2, 128], BF16)
        oa = Rot("oa", [sb("oa%d" % i, [128, 512]) for i in range(2)])
        P.v(lambda e: e.memset(S[:], 0.0), [], [("S", j, hh) for j in range(2) for hh in range(2)])
        P.v(lambda e: e.memset(Sb[:], 0.0), [], [("Sb", j, hh) for j in range(2) for hh in range(2)])

        def on_out(r, O, Ok):
            o_ap, ok_ = oa.next()
            P.act(o_ap[:], O[:, :], AF.Copy, [Ok], [ok_])
            P.dma(D["OA"][r * 128:(r + 1) * 128, :], o_ap[:], [ok_], [("OA", r)])
        _gla_pass(nc, P, D, banks, st, sb, 0, [16, 17] + list(range(16)), S, Sb, on_out)
        P.dma(D["cc_in"].rearrange("(a p) n -> p a n", p=128), S[:], [("S", j, hh) for j in range(2) for hh in range(2)],
              ["cc_in"])
        P.cc(lambda e: e.collective_compute("AllGather", ALU.bypass, replica_groups=[[0, 1], [2, 3], [4, 5], [6, 7]],
                                            ins=[D["cc_in"].opt()], outs=[D["cc_out"].opt()]), ["cc_in"], ["cc_out"])
        P.flush()


def phase_l1b_b(nc, P, D, banks):
    with contextlib.ExitStack() as st:
        sb = lambda name, shape, dt=F32: st.enter_context(nc.sbuf_tensor("e_" + name, shape, dt))
        nT, nW = bank_rot(banks, 6, 7), bank_rot(banks, 6, 8)
        S = sb("S", [128, 2, 128]); Sb = sb("Sb", [128, 2, 128], BF16)
        skeys = [("S", j, hh) for j in range(2) for hh in range(2)]
        both = sb("both", [128, 4, 128])
        sel = sb("sel", [128, 2])
        P.dma(both[:], D["cc_out"].rearrange("(a p) n -> p a n", p=128), [], ["both"])
        P.dma(sel[:], D["sel"][:, :], [], ["sel"])
        P.v(lambda e: e.tensor_scalar(S[:].rearrange("p a b -> p (a b)"), both[:, 0:2, :].rearrange("p a b -> p (a b)"),
                                      sel[:, 0:1], None, ALU.mult), ["both", "sel"], skeys)
        P.v(lambda e: e.scalar_tensor_tensor(S[:].rearrange("p a b -> p (a b)"),
                                             both[:, 2:4, :].rearrange("p a b -> p (a b)"), sel[:, 1:2],
                                             S[:].rearrange("p a b -> p (a b)"), ALU.mult, ALU.add),
            ["both", "sel"] + skeys, skeys)
        P.act(Sb[:], S[:], AF.Copy, skeys, [("Sb", j, hh) for j in range(2) for hh in range(2)])
        ident = sb("ident", [128, 128], BF16)
        P.dma(ident[:], D["identbf"][:, :], [], ["ident"])
        consts = sb("consts", [128, 4])
        P.v(lambda e: e.memset(consts[:], -0.5), [], ["consts"])
        gng = sb("gng", [128, 512])
        P.dma(gng[:], D["gla_norm_g"][0:1, :].partition_broadcast(128), [], ["gng"])
        w_out = sb("w_out", [128, 8, 1024], BF16)
        P.dma(w_out[:], D["odd_w_out"][0].rearrange("(kc p) n -> p kc n", p=128), [], ["w_out"], eng="pool")
        G = load_mod_rows(P, nc, st, D["mods"], 1, GT1, "e_G")
        oa = Rot("eoa", [sb("oa%d" % i, [128, 512]) for i in range(2)])
        rsl = Rot("ersl", [sb("rsl%d" % i, [128, 512], BF16) for i in range(2)])
        gr = sb("gr", [128, 512])
        junk = sb("junk", [128, 128])
        small = Rot("esmall", [sb("small%d" % i, [128, 8]) for i in range(2)])
        mix = Rot("emix", [sb("mix%d" % i, [128, 1024], BF16) for i in range(2)])
        mixT = sb("mixT", [128, 8, 128], BF16)
        xt = Rot("ext", [sb("xt%d" % i, [128, 1024]) for i in range(2)])
        ot = Rot("eot", [sb("ot%d" % i, [128, 1024]) for i in range(2)])

        def on_out(r, O, Ok):
            rs = slice(r * 128, (r + 1) * 128)
            o_ap, ok_ = oa.next()
            P.dma(o_ap[:], D["OA"][rs, :], [], [ok_])
            P.v(lambda e, o_ap=o_ap, O=O: e.tensor_tensor(o_ap[:], O[:, :], o_ap[:], ALU.add), [Ok, ok_], [ok_])
            r_ap, rk = rsl.next()
            P.dma(r_ap[:], D["rsilu"][rs, :], [], [rk])
            m_ap, mk = mix.next()
            P.dma(m_ap[:, 512:1024], D["dl"][rs, :], [], [(mk, 1)])
            sm, smk = small.next()
            for h in range(4):
                P.act(junk[:], o_ap[:, h * 128:(h + 1) * 128], AF.Square, [ok_], ["junk", (smk, h)], accum_out=sm[:, h:h + 1])
            hk = [(smk, h) for h in range(4)]
            P.v(lambda e, sm=sm: e.tensor_scalar(sm[:, 0:4], sm[:, 0:4], 1.0 / 128, EPS, ALU.mult, ALU.add), hk, hk)
            P.v(lambda e, sm=sm: e.tensor_tensor(sm[:, 4:8], sm[:, 0:4], consts[:, 0:4], ALU.pow), hk + ["consts"],
                [(smk, 4)], eng="pool")
            P.v(lambda e, r_ap=r_ap: e.tensor_tensor(gr[:], gng[:], r_ap[:], ALU.mult), ["gng", rk], ["gr"])
            for h in range(4):
                P.v(lambda e, h=h, o_ap=o_ap, sm=sm, m_ap=m_ap: e.scalar_tensor_tensor(
                    m_ap[:, h * 128:(h + 1) * 128], o_ap[:, h * 128:(h + 1) * 128], sm[:, 4 + h:5 + h],
                    gr[:, h * 128:(h + 1) * 128], ALU.mult, ALU.mult), [ok_, (smk, 4), "gr"], [(mk, 0)])
            bk, bkey = nT()
            bkb = bk[:].bitcast(BF16)
            for kc in range(8):
                P.tr(bkb[:, kc * 128:(kc + 1) * 128], m_ap[:, kc * 128:(kc + 1) * 128], ident[:],
                     [(mk, 0), (mk, 1), "ident"], [bkey])
            P.act(mixT[:].rearrange("p a b -> p (a b)"), bkb, AF.Copy, [bkey], ["mixT"])
            x_ap, xk = xt.next()
            P.dma(x_ap[:], D["x2"][rs, :], [], [xk])
            t_ap, tk = ot.next()
            for dh in range(2):
                W, Wk = nW()
                for kc in range(8):
                    P.mm(W[:, :], mixT[:, kc, :], w_out[:, kc, dh * 512:(dh + 1) * 512], kc == 0, kc == 7,
                         ["mixT", "w_out"], [Wk])
                P.v(lambda e, t_ap=t_ap, W=W, dh=dh: e.tensor_tensor(
                    t_ap[:, dh * 512:(dh + 1) * 512], W[:, :], G[0][0][:, dh * 512:(dh + 1) * 512], ALU.mult),
                    [Wk, G[0][1]], [tk])
            P.v(lambda e, t_ap=t_ap, x_ap=x_ap: e.tensor_tensor(t_ap[:], t_ap[:], x_ap[:], ALU.add), [tk, xk], [tk],
                eng="pool")
            P.dma(D["x3"][rs, :], t_ap[:], [tk], [("x3", r)])
        _gla_pass(nc, P, D, banks, st, sb, 1, list(range(15, -1, -1)), S, Sb, on_out)
        P.flush()


I32 = mybir.dt.int32
STILE = 256
NB = STILE // 128


def n_stiles(T):
    return (2 * T + 32 * (STILE - 1) + STILE - 1) // STILE


def phase_moe_sparse(nc, P, D, banks, layer, xin, xout, tiles, tag, final_norm=False):
    NTl = len(tiles)
    T = NTl * 128
    NST = n_stiles(T)
    NSLOT = NST * STILE
    Xs, Ys = D["Xs"], D["Ys"]
    wgv = D["moe_w_gate%d" % layer].rearrange("e (p a kc) f -> (e p a) (kc f)", p=128, a=2)
    wuv = D["moe_w_up%d" % layer].rearrange("e (p a kc) f -> (e p a) (kc f)", p=128, a=2)
    wdv = D["moe_w_down%d" % layer].rearrange("e f d -> (e f) d")
    with contextlib.ExitStack() as st0:
        sb0 = lambda name, shape, dt=F32: st0.enter_context(nc.sbuf_tensor(tag + name, shape, dt))
        consts = sb0("consts", [128, 4])
        P.v(lambda e: e.memset(consts[:, 0:1], -0.5), [], ["consts"])
        neghalf = consts[:, 0:1]
        idxA = sb0("idxA", [128, NTl], I32); idxB = sb0("idxB", [128, NTl], I32)
        wAB = sb0("wAB", [128, 2, NTl])
        widx = sb0("widx", [128, NST, 6], I32)
        with contextlib.ExitStack() as st:
            sb = lambda name, shape, dt=F32: st.enter_context(nc.sbuf_tensor(tag + name, shape, dt))
            nA, nB = bank_rot(banks, 0, 4), bank_rot(banks, 4, 7)
            cntb, cntk = banks[7], ("ps", 7)
            ident = sb("ident32", [128, 128])
            gbc = sb("gbc", [128, 1024])
            w_r = sb("w_r", [128, 8, 36])
            gm = sb("gm", [128, 2, 128])
            eidrow = sb("eidrow", [128, 32])
            pc2 = sb("pc2", [128, 6])
            Abc = load_mod_rows(P, nc, st, D["mods"], layer, SC2, tag + "A")
            Bbc = load_mod_rows(P, nc, st, D["mods"], layer, SH2, tag + "B")
            xt = Rot("mxt", [sb("xt%d" % i, [128, 1024]) for i in range(2)])
            junk = sb("junk", [128, 1024])
            t1r = Rot("t1", [sb("t1_%d" % i, [128, 1024]) for i in range(2)])
            h2r = Rot("h2", [sb("h2_%d" % i, [128, 1024]) for i in range(2)])
            h2b = sb("h2b", [128, NTl, 1024], BF16)
            h2Tr = Rot("h2T32", [sb("h2T32_%d" % i, [128, 8, 128]) for i in range(2)])
            small = Rot("msmall", [sb("small%d" % i, [128, 16]) for i in range(2)])
            rt = Rot("mrt", [sb("rt%d" % i, [128, 96]) for i in range(2)])
            selA = sb("selA", [128, NTl, 32]); selB = sb("selB", [128, NTl, 32]); selm = sb("selm", [128, NTl, 32])
            LG = sb("LG", [128, NTl, 36])
            rs_ = sb("rs_", [128, 8, NTl])
            goh = sb("goh", [128, NTl, 4]); gex = sb("gex", [128, NTl, 4])
            etmp = sb("etmp", [128, NTl, 4, 8])
            ein = sb("ein", [128, NTl, 8]); oh1 = sb("oh1", [128, NTl, 8]); e2 = sb("e2", [128, NTl, 8]); oh2 = sb("oh2", [128, NTl, 8])
            zt = sb("zt", [128, 8, 1024], BF16)
            P.v(lambda e: e.memset(zt[:], 0.0), [], ["zt"], eng="pool")
            zkeys = []
            for z0 in range(0, NSLOT, 1024):
                nrow = min(1024, NSLOT - z0)
                P.dma(Xs[z0:z0 + nrow, :].rearrange("(a p) c -> p a c", p=128), zt[:, 0:nrow // 128, :], ["zt"], [("Xsz", z0)])
                zkeys.append(("Xsz", z0))
            P.dma(ident[:], D["ident32"][:, :], [], ["ident"])
            P.dma(gbc[:], D["norm_ffn_g"][layer:layer + 1, :].partition_broadcast(128), [], ["gbc"])
            P.dma(w_r[:, :, 0:4], D["moe_w_rg"][layer].rearrange("(kc p) n -> p kc n", p=128), [], [("w_r", 0)])
            P.dma(w_r[:, :, 4:36], D["moe_w_re"][layer].rearrange("(kc p) n -> p kc n", p=128), [], [("w_r", 1)])
            P.dma(gm[:], D["gmask"][3:5].rearrange("a p n -> p a n"), [], ["gm"])
            P.dma(eidrow[:], D["eidrow"][:, :], [], ["eidrow"])
            P.dma(pc2[:], D["pc2"][:, :], [], ["pc2"])
            for v in range(2):
                a, ak = Abc[v]
                P.v(lambda e, a=a: e.scalar_tensor_tensor(a[:], a[:], 1.0, gbc[:], ALU.add, ALU.mult), [ak, "gbc"], [ak])
            for ti, (r, var) in enumerate(tiles):
                A, Ak = Abc[var]; B, Bk = Bbc[var]
                x_ap, xk = xt.next()
                P.dma(x_ap[:], D[xin][r * 128:(r + 1) * 128, :], [], [xk])
                sm, smk = small.next()
                P.act(junk[:], x_ap[:], AF.Square, [xk], ["junk", (smk, 0)], accum_out=sm[:, 0:1])
                rms_rstd(P, sm[:, 0:1], (smk, 0), sm[:, 2:3], (smk, 2), neghalf, 1024, sm[:, 1:2], (smk, 1))
                t1, t1k = t1r.next(); h2, h2k = h2r.next(); h2T32, hTk = h2Tr.next()
                P.v(lambda e, x_ap=x_ap, sm=sm, A=A, t1=t1: e.scalar_tensor_tensor(
                    t1[:], x_ap[:], sm[:, 2:3], A[:], ALU.mult, ALU.mult), [xk, (smk, 2), Ak], [t1k])
                P.v(lambda e, B=B, t1=t1, h2=h2: e.tensor_tensor(h2[:], t1[:], B[:], ALU.add), [t1k, Bk], [h2k])
                P.act(h2b[:, ti, :], h2[:], AF.Copy, [h2k], [("h2b", ti)])
                for half in range(2):
                    bk, bkey = nA()
                    for j in range(4):
                        kc = half * 4 + j
                        P.tr(bk[:, j * 128:(j + 1) * 128], h2[:, kc * 128:(kc + 1) * 128], ident[:], [h2k, "ident"], [bkey])
                    P.v(lambda e, bk=bk, half=half, h2T32=h2T32: e.tensor_copy(
                        h2T32[:, half * 4:(half + 1) * 4, :], bk[:, :].rearrange("p (a b) -> p a b", a=4)),
                        [bkey], [(hTk, half)])
                lg, lgk = nB()
                for kc in range(8):
                    P.mm(lg[:, 0:36], h2T32[:, kc, :], w_r[:, kc, :], kc == 0, kc == 7,
                         [(hTk, kc // 4), ("w_r", 0), ("w_r", 1)], [lgk])
                P.v(lambda e, lg=lg, ti=ti: e.tensor_copy(LG[:, ti, :], lg[:, 0:36]), [lgk], [("LG", ti)])
            lgk_all = [("LG", ti) for ti in range(NTl)]
            NT = NTl
            G = LG[:, :, 0:4]
            E4 = LG[:, :, 4:36].rearrange("p t (g e) -> p t g e", g=4)
            bc = lambda ap2, n: ap2.unsqueeze(2).to_broadcast([128, NT, n])
            P.v(lambda e: e.tensor_reduce(rs_[:, 0, :], G, AX.X, ALU.max), lgk_all, ["gmax"])
            P.v(lambda e: e.tensor_tensor(goh[:], G, bc(rs_[:, 0, :], 4), ALU.is_equal), lgk_all + ["gmax"], ["goh"])
            P.v(lambda e: e.tensor_tensor(gex[:], G, bc(rs_[:, 0, :], 4), ALU.subtract), lgk_all + ["gmax"], ["gex"])
            P.act(gex[:], gex[:], AF.Exp, ["gex"], ["gex"])
            P.v(lambda e: e.tensor_reduce(rs_[:, 1, :], gex[:], AX.X, ALU.add), ["gex"], ["gsum"])
            P.v(lambda e: e.reciprocal(rs_[:, 2, :], rs_[:, 1, :]), ["gsum"], ["pmax"])
            P.v(lambda e: e.tensor_tensor(etmp[:], E4, goh[:].unsqueeze(3).to_broadcast([128, NT, 4, 8]), ALU.mult),
                lgk_all + ["goh"], ["etmp"])
            P.v(lambda e: e.tensor_tensor(ein[:], etmp[:, :, 0, :], etmp[:, :, 1, :], ALU.add), ["etmp"], ["ein"])
            P.v(lambda e: e.tensor_tensor(ein[:], ein[:], etmp[:, :, 2, :], ALU.add), ["etmp", "ein"], ["ein"])
            P.v(lambda e: e.tensor_tensor(ein[:], ein[:], etmp[:, :, 3, :], ALU.add), ["etmp", "ein"], ["ein"])
            P.v(lambda e: e.tensor_reduce(rs_[:, 3, :], ein[:], AX.X, ALU.max), ["ein"], ["m1"])
            P.v(lambda e: e.tensor_tensor(oh1[:], ein[:], bc(rs_[:, 3, :], 8), ALU.is_equal), ["ein", "m1"], ["oh1"])
            P.v(lambda e: e.scalar_tensor_tensor(e2[:], oh1[:], -1e30, ein[:], ALU.mult, ALU.add), ["oh1", "ein"], ["e2"])
            P.v(lambda e: e.tensor_reduce(rs_[:, 4, :], e2[:], AX.X, ALU.max), ["e2"], ["m2"])
            P.v(lambda e: e.tensor_tensor(oh2[:], e2[:], bc(rs_[:, 4, :], 8), ALU.is_equal), ["e2", "m2"], ["oh2"])
            P.v(lambda e: e.tensor_tensor(rs_[:, 5, :], rs_[:, 4, :], rs_[:, 3, :], ALU.subtract), ["m1", "m2"], ["dd"])
            P.act(rs_[:, 6, :], rs_[:, 5, :], AF.Exp, ["dd"], ["ed"])
            P.v(lambda e: e.tensor_scalar(rs_[:, 7, :], rs_[:, 6, :], 1.0, None, ALU.add), ["ed"], ["w1"])
            P.v(lambda e: e.reciprocal(rs_[:, 7, :], rs_[:, 7, :]), ["w1"], ["w1"])
            P.v(lambda e: e.tensor_tensor(wAB[:, 0, :], rs_[:, 7, :], rs_[:, 2, :], ALU.mult), ["w1", "pmax"], ["wA"])
            P.v(lambda e: e.tensor_tensor(wAB[:, 1, :], wAB[:, 0, :], rs_[:, 6, :], ALU.mult), ["wA", "ed"], ["wB"])
            s4 = lambda ap3: ap3[:].rearrange("p t (g e) -> p t g e", g=4)
            P.v(lambda e: e.tensor_tensor(s4(selA), oh1[:].unsqueeze(2).to_broadcast([128, NT, 4, 8]),
                                          goh[:].unsqueeze(3).to_broadcast([128, NT, 4, 8]), ALU.mult), ["oh1", "goh"], ["selA"])
            P.v(lambda e: e.tensor_tensor(s4(selB), oh2[:].unsqueeze(2).to_broadcast([128, NT, 4, 8]),
                                          goh[:].unsqueeze(3).to_broadcast([128, NT, 4, 8]), ALU.mult), ["oh2", "goh"], ["selB"])
            P.v(lambda e: e.tensor_tensor(selm[:], selA[:], selB[:], ALU.add), ["selA", "selB"], ["selm"])
            for ti in range(NTl):
                P.mm(cntb[:, 0:32], gm[:, 1, :], selm[:, ti, :], ti == 0, ti == NTl - 1, ["gm", "selm"], [cntk])
            seg = sb("seg", [128, 8, 32])
            segT = sb("segT", [32, 128])
            ecol = sb("ecol", [128, NST])
            sti = sb("sti", [128, 2, NST, 32])
            stc = sb("stc", [128, 64])
            P.dma(stc[:], D["stile_c"][:, :], [], ["stc"])
            widxf = sb("widxf", [128, NST, 6])
            slotf = sb("slotf", [128, 2, NTl])
            P.v(lambda e: e.tensor_copy(seg[:, 0, :], cntb[:, 0:32]), [cntk], ["cnt"])
            P.v(lambda e: e.tensor_scalar(seg[:, 1, :], seg[:, 0, :], 0.0, None, ALU.is_gt), ["cnt"], ["nst"])
            for k in range(1, (T + STILE - 1) // STILE + 1):
                P.v(lambda e, k=k: e.scalar_tensor_tensor(seg[:, 1, :], seg[:, 0, :], float(STILE * k), seg[:, 1, :],
                                                          ALU.is_gt, ALU.add), ["cnt", "nst"], ["nst"])
            P.v(lambda e: e.tensor_scalar(seg[:, 2, :], seg[:, 1, :], float(STILE), None, ALU.mult), ["nst"], ["pc"])
            bk, bkey = nA()
            P.tr(bk[0:32, 0:128], seg[:, 2, :], ident[:], ["pc", "ident"], [bkey])
            P.v(lambda e, bk=bk: e.tensor_copy(segT[:], bk[0:32, 0:128]), [bkey], ["segT"])
            bk2, bkey2 = nA()
            P.mm(bk2[:, 0:32], segT[0:32, :], gm[0:32, 0, 0:32], True, True, ["segT", "gm"], [bkey2])
            P.v(lambda e, bk2=bk2: e.tensor_copy(seg[:, 3, :], bk2[:, 0:32]), [bkey2], ["start"])
            P.v(lambda e: e.tensor_tensor(seg[:, 4, :], seg[:, 3, :], seg[:, 2, :], ALU.add), ["start", "pc"], ["end"])
            P.v(lambda e: e.tensor_copy(seg[:, 5, :], seg[:, 3, :]), ["start"], ["base"])
            bci = lambda ap2: ap2.unsqueeze(1).to_broadcast([128, NST, 32])
            cI = stc[:, 0:NST].unsqueeze(2).to_broadcast([128, NST, 32])
            P.v(lambda e: e.tensor_tensor(sti[:, 0, :, :], bci(seg[:, 3, :]), cI, ALU.is_le), ["start", "stc"], ["sti0"])
            P.v(lambda e: e.tensor_tensor(sti[:, 1, :, :], bci(seg[:, 4, :]), cI, ALU.is_gt), ["end", "stc"], ["sti1"])
            P.v(lambda e: e.tensor_tensor(sti[:, 0, :, :], sti[:, 0, :, :], sti[:, 1, :, :], ALU.mult), ["sti0", "sti1"], ["sti0"])
            P.v(lambda e: e.tensor_tensor(sti[:, 0, :, :], sti[:, 0, :, :], bci(eidrow[:]), ALU.mult), ["sti0", "eidrow"], ["sti0"])
            P.v(lambda e: e.tensor_reduce(ecol[:, :], sti[:, 0, :, :], AX.X, ALU.add), ["sti0"], ["ecol"])
            ek = ["ecol"]
            for a_ in range(2):
                P.v(lambda e, a_=a_: e.tensor_scalar(widxf[:, :, a_], ecol[:, :], 256.0, pc2[:, a_:a_ + 1], ALU.mult, ALU.add),
                    ek + ["pc2"], [("widxf", a_)])
            for fc in range(4):
                P.v(lambda e, fc=fc: e.tensor_scalar(widxf[:, :, 2 + fc], ecol[:, :], 512.0, pc2[:, 2 + fc:3 + fc], ALU.mult, ALU.add),
                    ek + ["pc2"], [("widxf", 2 + fc)])
            P.v(lambda e: e.tensor_copy(widx[:].rearrange("p a b -> p (a b)"), widxf[:].rearrange("p a b -> p (a b)")),
                [("widxf", j) for j in range(6)], ["widx"])
            for ti in range(NTl):
                wi, wik = nB()
                P.mm(wi[:, 0:32], gm[:, 0, :], selm[:, ti, :], True, True, ["gm", "selm"], [wik])
                P.mm(wi[:, 32:64], gm[:, 1, :], selm[:, ti, :], True, True, ["gm", "selm"], [wik])
                P.v(lambda e, wi=wi: e.tensor_tensor(seg[:, 6, :], wi[:, 0:32], seg[:, 5, :], ALU.add), [wik, "base"], ["segtmp"])
                P.v(lambda e, ti=ti: e.scalar_tensor_tensor(seg[:, 7, :], seg[:, 6, :], 1.0, selA[:, ti, :], ALU.mult, ALU.mult,
                                                            accum_out=slotf[:, 0, ti:ti + 1]), ["segtmp", "selA"],
                    ["segtmp2", ("slotA", ti)])
                P.v(lambda e, ti=ti: e.scalar_tensor_tensor(seg[:, 7, :], seg[:, 6, :], 1.0, selB[:, ti, :], ALU.mult, ALU.mult,
                                                            accum_out=slotf[:, 1, ti:ti + 1]), ["segtmp", "selB"],
                    ["segtmp2", ("slotB", ti)])
                P.v(lambda e, wi=wi: e.tensor_tensor(seg[:, 5, :], wi[:, 32:64], seg[:, 5, :], ALU.add), [wik, "base"], ["base"])
            P.v(lambda e: e.tensor_copy(idxA[:], slotf[:, 0, :]), [("slotA", ti) for ti in range(NTl)], ["idxA"])
            P.v(lambda e: e.tensor_copy(idxB[:], slotf[:, 1, :]), [("slotB", ti) for ti in range(NTl)], ["idxB"])
            for ti in range(NTl):
                for (ix, ixk) in ((idxA, "idxA"), (idxB, "idxB")):
                    P.op("pool", lambda e, ix=ix, ti=ti: e.indirect_dma_start(
                        out=Xs[0:NSLOT, :], out_offset=bass.IndirectOffsetOnAxis(ap=ix[:, ti:ti + 1], axis=0),
                        in_=h2b[:, ti, :], in_offset=None, bounds_check=None),
                        [("h2b", ti), ixk] + zkeys, [("Xs", ti, ixk)], dma=True, kind="dma")
            P.flush()
        with contextlib.ExitStack() as st:
            sb = lambda name, shape, dt=F32: st.enter_context(nc.sbuf_tensor(tag + name, shape, dt))
            nT, nGU, nY = bank_rot(banks, 0, 2), bank_rot(banks, 2, 6), bank_rot(banks, 6, 8)
            ident = sb("identb", [128, 128], BF16)
            P.dma(ident[:], D["identbf"][:, :], [], ["ident"])
            wg = Rot("wg", [sb("wg%d" % i, [128, 8, 512], BF16) for i in range(2)])
            wu = Rot("wu", [sb("wu%d" % i, [128, 8, 512], BF16) for i in range(2)])
            wd = Rot("wd", [sb("wd%d" % i, [128, 4, 1024], BF16) for i in range(2)])
            xr = Rot("xr", [sb("xr%d" % i, [128, 1024], BF16) for i in range(4)])
            XT = Rot("XT", [sb("XT%d" % i, [128, 8, STILE], BF16) for i in range(2)])
            hg = Rot("hg", [sb("hg%d" % i, [128, 4, STILE], BF16) for i in range(2)])
            sg = Rot("sg", [sb("sg%d" % i, [128, STILE]) for i in range(2)])
            yb = Rot("yb", [sb("yb%d" % i, [128, 1024]) for i in range(3)])

            def gather(dst2d, src, col, i, key):
                P.op("pool", lambda e: e.indirect_dma_start(
                    out=dst2d, out_offset=None, in_=src,
                    in_offset=bass.IndirectOffsetOnAxis(ap=widx[:, i, col:col + 1], axis=0),
                    bounds_check=None), [], [key], dma=True, kind="dma")

            def emit_gu(i):
                g_ap, gk = wg.next(); u_ap, uk = wu.next(); d_ap, dk = wd.next()
                for a_ in range(2):
                    gather(g_ap[:, 4 * a_:4 * a_ + 4, :].rearrange("p a f -> p (a f)"), wgv, a_, i, (gk, a_))
                    gather(u_ap[:, 4 * a_:4 * a_ + 4, :].rearrange("p a f -> p (a f)"), wuv, a_, i, (uk, a_))
                for fc in range(4):
                    gather(d_ap[:, fc, :], wdv, 2 + fc, i, (dk, fc))
                xT, xTk = XT.next()
                for b in range(NB):
                    x_ap, xk = xr.next()
                    P.dma(x_ap[:], Xs[i * STILE + b * 128:i * STILE + (b + 1) * 128, :], [], [xk])
                    bk, bkey = nT()
                    bkb = bk[:].bitcast(BF16)
                    xv = x_ap[:].rearrange("p (m kc) -> p kc m", kc=8)
                    for kc in range(8):
                        P.tr(bkb[:, kc * 128:(kc + 1) * 128], xv[:, kc, :], ident[:], [xk, "ident"], [bkey])
                    P.act(xT[:, :, b * 128:(b + 1) * 128], bkb.rearrange("p (a b) -> p a b", a=8), AF.Copy, [bkey], [(xTk, b)])
                xkeys = [(xTk, b) for b in range(NB)]
                hgt, hgk = hg.next()
                for fc in range(4):
                    Gp, Gk = nGU(); Up, Uk = nGU()
                    for kc in range(8):
                        P.mm(Gp[:, 0:STILE], g_ap[:, kc, fc * 128:(fc + 1) * 128], xT[:, kc, :], kc == 0, kc == 7,
                             [(gk, kc // 4)] + xkeys, [Gk])
                    for kc in range(8):
                        P.mm(Up[:, 0:STILE], u_ap[:, kc, fc * 128:(fc + 1) * 128], xT[:, kc, :], kc == 0, kc == 7,
                             [(uk, kc // 4)] + xkeys, [Uk])
                    s_ap, sk = sg.next()
                    P.act(s_ap[:, :], Gp[:, 0:STILE], AF.Silu, [Gk], [sk])
                    P.v(lambda e, hgt=hgt, Up=Up, s_ap=s_ap, fc=fc: e.tensor_tensor(
                        hgt[:, fc, :], Up[:, 0:STILE], s_ap[:, :], ALU.mult), [Uk, sk], [(hgk, fc)])
                return (i, hgt, hgk, d_ap, dk)

            def emit_down(i, hgt, hgk, d_ap, dk):
                for b in range(NB):
                    y_ap, yk = yb.next()
                    for dh in range(2):
                        Yp, Yk = nY()
                        for fc in range(4):
                            P.mm(Yp[:, :], hgt[:, fc, b * 128:(b + 1) * 128], d_ap[:, fc, dh * 512:(dh + 1) * 512],
                                 fc == 0, fc == 3, [(hgk, f) for f in range(4)] + [(dk, fc)], [Yk])
                        if dh == 0:
                            P.act(y_ap[:, 0:512], Yp[:, :], AF.Copy, [Yk], [(yk, 0)])
                        else:
                            P.v(lambda e, y_ap=y_ap, Yp=Yp: e.tensor_copy(y_ap[:, 512:1024], Yp[:, :]), [Yk], [(yk, 1)])
                    r0 = i * STILE + b * 128
                    P.dma(Ys[r0:r0 + 128, :], y_ap[:], [(yk, 0), (yk, 1)], [("Ys", r0)])

            pend = None
            for i in range(NST):
                cur = emit_gu(i)
                if pend is not None:
                    emit_down(*pend)
                pend = cur
            emit_down(*pend)
            P.flush()
        with contextlib.ExitStack() as st:
            sb = lambda name, shape, dt=F32: st.enter_context(nc.sbuf_tensor(tag + name, shape, dt))
            G = load_mod_rows(P, nc, st, D["mods"], layer, GT2, tag + "G")
            xt = Rot("mxt2", [sb("xt2_%d" % i, [128, 1024]) for i in range(4)])
            ya = Rot("ya", [sb("ya%d" % i, [128, 1024]) for i in range(4)])
            ybb = Rot("ybb", [sb("ybb%d" % i, [128, 1024]) for i in range(4)])
            if final_norm:
                fg = sb("fg", [128, 1024])
                P.dma(fg[:], D["final_norm_g"][0:1, :].partition_broadcast(128), [], ["fg"])
                junk = sb("junk3", [128, 1024])
                small = Rot("fsmall", [sb("fsmall%d" % i, [128, 4]) for i in range(2)])
            for ti, (r, var) in enumerate(tiles):
                x_ap, xk = xt.next()
                P.dma(x_ap[:], D[xin][r * 128:(r + 1) * 128, :], [], [xk])
                a_ap, ak = ya.next(); b_ap, bk_ = ybb.next()
                for (dst, dkey, ix) in ((a_ap, ak, idxA), (b_ap, bk_, idxB)):
                    P.op("pool", lambda e, dst=dst, ix=ix, ti=ti: e.indirect_dma_start(
                        out=dst[:, :], out_offset=None, in_=Ys[0:NSLOT, :],
                        in_offset=bass.IndirectOffsetOnAxis(ap=ix[:, ti:ti + 1], axis=0),
                        bounds_check=None), [], [dkey], dma=True, kind="dma")
                P.v(lambda e, a_ap=a_ap, ti=ti: e.tensor_scalar(a_ap[:], a_ap[:], wAB[:, 0, ti:ti + 1], None, ALU.mult), [ak], [ak])
                P.v(lambda e, a_ap=a_ap, b_ap=b_ap, ti=ti: e.scalar_tensor_tensor(
                    a_ap[:], b_ap[:], wAB[:, 1, ti:ti + 1], a_ap[:], ALU.mult, ALU.add), [ak, bk_], [ak])
                P.v(lambda e, a_ap=a_ap, var=var: e.tensor_tensor(a_ap[:], a_ap[:], G[var][0][:], ALU.mult), [ak, G[var][1]], [ak])
                P.v(lambda e, a_ap=a_ap, x_ap=x_ap: e.tensor_tensor(a_ap[:], a_ap[:], x_ap[:], ALU.add), [ak, xk], [ak])
                if final_norm:
                    sm, smk = small.next()
                    P.act(junk[:], a_ap[:], AF.Square, [ak], ["junk3", (smk, 0)], accum_out=sm[:, 0:1])
                    rms_rstd(P, sm[:, 0:1], (smk, 0), sm[:, 2:3], (smk, 2), neghalf, 1024, sm[:, 1:2], (smk, 1))
                    P.v(lambda e, a_ap=a_ap, sm=sm: e.scalar_tensor_tensor(
                        a_ap[:], a_ap[:], sm[:, 2:3], fg[:], ALU.mult, ALU.mult), [ak, (smk, 2), "fg"], [ak])
                P.dma(D[xout][r * 128:(r + 1) * 128, :], a_ap[:], [ak], [(xout, r)])
            P.flush()

import numpy as np
import ml_dtypes

BF = ml_dtypes.bfloat16
GRID_W = 64

W_SMALL = {
    "ada_w": [2, 1024, 6144], "ada_b": [2, 6144], "norm_mix_g": [2, 1024], "norm_ffn_g": [2, 1024],
    "even_w_in": [1, 1024, 1184], "mla_q_norm_g": [1, 256], "mla_w_uq": [1, 256, 768], "mla_kv_norm_g": [1, 128],
    "mla_w_ukv": [1, 128, 1024], "win_sink": [1, 8], "even_w_out": [1, 1024, 1024],
    "odd_w_in": [1, 1024, 2592], "gla_w_g2": [1, 2, 16, 256], "gla_b_g": [1, 2, 256], "gla_norm_g": [1, 512],
    "sg_ln_g": [1, 512], "sg_ln_b": [1, 512], "odd_w_out": [1, 1024, 1024],
    "moe_w_rg": [2, 1024, 4], "moe_w_re": [2, 1024, 32], "final_norm_g": [1, 1024],
}
W_MOE = {"moe_w_gate": [32, 1024, 512], "moe_w_up": [32, 1024, 512], "moe_w_down": [32, 512, 1024]}
CONSTS = {"wmask": ([2, 128, 512], BF16), "ident32": ([128, 128], F32), "identbf": ([128, 128], BF16),
          "gmask": ([5, 128, 128], F32), "eidrow": ([128, 32], F32), "pc2": ([128, 6], F32), "stile_c": ([128, 64], F32)}
PERCORE = {"xtok": [NTOK, 1024], "cvec": [2, 1024], "cA": [NTOK, 256], "sA": [NTOK, 256], "cB": [NTOK, 512],
           "sB": [NTOK, 512], "sg_w_sT": [4, 128, 128], "sg_b_sT": [128, 4], "sel": [128, 2]}
SCRATCH = {"mods": ([2, 2, 6144], F32), "QAT": ([96, 8, NTOK], BF16), "KAT": ([96, 8, NTOK], BF16),
           "VA": ([NTOK, 520], BF16), "QBT": ([64, 8, NTOK], BF16), "KBT": ([64, 2, NTOK], BF16),
           "VB": ([NTOK, 130], BF16), "x1": ([NOWN, 1024], F32), "x2": ([NOWN, 1024], F32),
           "g_T": ([128, 18, 8, 128], BF16), "g_kd": ([2, NOWN, 256], BF16), "g_v": ([NOWN, 512], BF16),
           "g_dec": ([128, 18, 4], F32), "rsilu": ([2048, 512], BF16), "dl": ([2048, 512], BF16),
           "OA": ([2048, 512], F32), "cc_in": ([256, 128], F32), "cc_out": ([512, 128], F32),
           "x3": ([2048, 1024], F32), "out": ([2048, 1024], F32),
           "Xs": ([n_stiles(NOWN) * STILE, 1024], BF16), "Ys": ([n_stiles(NOWN) * STILE, 1024], F32)}
HANDOFF = ["mods", "x2", "g_T", "g_kd", "g_v", "g_dec", "rsilu", "dl", "OA"]


def rope_tables(pos, dim, nheads):
    pos = np.asarray(pos)
    half = dim // 2
    inv = np.power(np.float32(10000.0), -np.arange(0, half, 2, dtype=np.float32) / np.float32(half)).astype(np.float32)
    row = (pos // GRID_W).astype(np.float32)
    col = (pos % GRID_W).astype(np.float32)
    ar = row[:, None] * inv[None, :]
    ac = col[:, None] * inv[None, :]
    ang = np.concatenate([ar, ar, ac, ac], axis=-1).astype(np.float32)
    cos = np.cos(ang).astype(np.float32)
    sin = np.sin(ang).astype(np.float32)
    blk = dim // 4
    sign = np.concatenate([-np.ones(blk), np.ones(blk), -np.ones(blk), np.ones(blk)]).astype(np.float32)
    ssin = sin * sign[None, :]
    no = pos < 0
    cos[no] = 1.0
    ssin[no] = 0.0
    return np.tile(cos, (1, nheads)), np.tile(ssin, (1, nheads))


_CONST = {}


def const_inputs():
    if not _CONST:
        j = np.arange(128)[:, None]
        i = np.arange(128)[None, :]
        m0 = np.tile((j >= i).astype(np.float32), (1, 4))
        m1 = np.tile((j <= i).astype(np.float32), (1, 4))
        _CONST["wmask"] = np.stack([m0, m1]).astype(BF)
        _CONST["ident32"] = np.eye(128, dtype=np.float32)
        _CONST["identbf"] = np.eye(128, dtype=np.float32).astype(BF)
        one = np.ones((128, 128), bool)
        _CONST["gmask"] = np.stack([(j <= i), (j >= i), one, (j < i), one]).astype(np.float32)
        _CONST["eidrow"] = np.tile(np.arange(32, dtype=np.float32)[None, :], (128, 1))
        p = np.arange(128, dtype=np.float32)
        _CONST["stile_c"] = np.tile((np.arange(64, dtype=np.float32) * STILE)[None, :], (128, 1))
        _CONST["pc2"] = np.stack([2 * p, 2 * p + 1, p, 128 + p, 256 + p, 384 + p], 1).astype(np.float32)
    return _CONST


def local_order(hf):
    own = np.arange(hf * 2048, (hf + 1) * 2048)
    oth = np.arange((1 - hf) * 2048, (2 - hf) * 2048)
    cidx = np.arange(256)
    if hf == 1:
        own, oth, cidx = own[::-1], oth[::-1], cidx[::-1]
    return own, oth, cidx


def weights_for(core, inp, layers=(0, 1)):
    hf = core % 2
    m = {}
    for k, shp in W_SMALL.items():
        m[k] = np.ascontiguousarray(np.asarray(inp[k]).reshape(shp))
    for l in layers:
        for k in W_MOE:
            m["%s%d" % (k, l)] = np.asarray(inp[k][l])
    ws = np.asarray(inp["sg_w_s"][0])
    bs = np.asarray(inp["sg_b_s"][0])
    if hf == 1:
        m["gla_w_g2"] = np.ascontiguousarray(m["gla_w_g2"][:, ::-1])
        m["gla_b_g"] = np.ascontiguousarray(m["gla_b_g"][:, ::-1])
        w = m["odd_w_in"].copy()
        w[:, :, 1024:1040] = m["odd_w_in"][:, :, 1040:1056]
        w[:, :, 1040:1056] = m["odd_w_in"][:, :, 1024:1040]
        m["odd_w_in"] = w
        ws = ws[:, ::-1, ::-1]
        bs = bs[:, ::-1]
    m["sg_w_sT"] = np.ascontiguousarray(ws.transpose(0, 2, 1))
    m["sg_b_sT"] = np.ascontiguousarray(bs.T)
    return m


def core_inputs(core, inp):
    b, hf = core // 2, core % 2
    own, oth, cidx = local_order(hf)
    pos = np.concatenate([own, oth, -np.ones(256, dtype=np.int64)])
    xtok = np.concatenate([inp["x"][b][own], inp["x"][b][oth], inp["ctx"][b][cidx]], 0)
    cA, sA = rope_tables(pos, 32, 8)
    cB, sB = rope_tables(pos, 64, 8)
    m = {"xtok": np.ascontiguousarray(xtok), "cvec": np.stack([inp["c"][b], inp["c_ctx"]]).astype(np.float32),
         "cA": cA, "sA": sA, "cB": cB, "sB": sB}
    sel = np.zeros((128, 2), np.float32)
    sel[:, 1 - hf] = 1.0
    m["sel"] = sel
    m.update(const_inputs())
    return m


def declare(nc, ext_in, ext_out, moe_layers=(0, 1)):
    D = {}
    dr = lambda n, s, dt=F32, k="ExternalInput": nc.dram_tensor(n, s, dt, kind=k).ap()
    for k, shp in PERCORE.items():
        D[k] = dr(k, shp)
    for k, (shp, dt) in CONSTS.items():
        D[k] = dr(k, shp, dt)
    for k, shp in W_SMALL.items():
        D[k] = dr(k, shp)
    for l in moe_layers:
        for k, shp in W_MOE.items():
            D["%s%d" % (k, l)] = dr("%s%d" % (k, l), shp)
    for k, (shp, dt) in SCRATCH.items():
        kind = "ExternalOutput" if k in ext_out else ("ExternalInput" if k in ext_in else "Internal")
        D[k] = dr(k, shp, dt, kind)
    return D


MOE0_TILES = [(i, 0) for i in range(16)] + [(16, 1), (17, 1)]
MOE1_TILES = [(i, 0) for i in range(16)]


def build_fused(extra_out=()):
    nc = bass.Bass("TRN2", target_bir_lowering=False)
    D = declare(nc, (), ["out"] + list(extra_out), moe_layers=(0, 1))
    banks = [nc.alloc_psum_tensor("bank%d" % i, [128, 512], F32) for i in range(8)]
    P = Prog(nc)
    phase_ada(nc, P, D, banks)
    phase_l0a(nc, P, D, banks)
    phase_l0b(nc, P, D, banks)
    phase_moe_sparse(nc, P, D, banks, 0, "x1", "x2", MOE0_TILES, "m0_")
    phase_l1a(nc, P, D, banks)
    phase_l1b_a(nc, P, D, banks)
    phase_l1b_b(nc, P, D, banks)
    phase_moe_sparse(nc, P, D, banks, 1, "x3", "out", MOE1_TILES, "m1_", final_norm=True)
    P.flush(final=True)
    return nc


def kernel(**inputs):
    inp = {k: np.asarray(v) for k, v in inputs.items()}
    n = 8
    nc = build_fused()
    in_maps = []
    for c in range(n):
        m = core_inputs(c, inp)
        m.update(weights_for(c, inp, layers=(0, 1)))
        in_maps.append(m)
    res = run_bass_kernel_spmd(nc, in_maps, core_ids=list(range(n))).results
    out = np.zeros((4, 4096, 1024), np.float32)
    for c in range(n):
        b, hf = c // 2, c % 2
        own = local_order(hf)[0]
        out[b][own] = np.asarray(res[c]["out"], dtype=np.float32)
    return out
```

```python
import contextlib
import numpy as np
import concourse.bass as bass
import concourse.mybir as mybir
from concourse.bass_utils import run_bass_kernel_spmd

F32 = mybir.dt.float32
BF16 = mybir.dt.bfloat16
AF = mybir.ActivationFunctionType
ALU = mybir.AluOpType
AX = mybir.AxisListType
N_DMA_SEMS = 10


class Op:
    __slots__ = ("eng", "fn", "deps", "signal", "sem", "val", "is_dma", "idx", "kind", "sem_eng")

    def __init__(self, eng, fn, is_dma, kind):
        self.eng = eng
        self.fn = fn
        self.deps = set()
        self.signal = False
        self.sem = None
        self.val = 0
        self.is_dma = is_dma
        self.kind = kind
        self.sem_eng = None


class Rot:
    def __init__(self, name, aps):
        self.name = name
        self.aps = aps
        self.i = 0

    def next(self):
        k = self.i % len(self.aps)
        self.i += 1
        return self.aps[k], (self.name, k)


class Prog:
    ENGS = ("pe", "act", "dve", "pool", "sp")

    def __init__(self, nc):
        self.nc = nc
        self.st = contextlib.ExitStack()
        st = self.st
        self.esem = {e: st.enter_context(nc.semaphore("s_" + e)) for e in ("pe", "act", "dve", "pool", "cc")}
        self.dsem = {e: [st.enter_context(nc.semaphore("d_%s%d" % (e, i))) for i in range(N_DMA_SEMS)]
                     for e in ("sp", "act", "pool")}
        self.cnt = {e: 0 for e in self.esem}
        self.dcnt = {e: [0] * N_DMA_SEMS for e in self.dsem}
        self.drr = {e: 0 for e in self.dsem}
        self.nflush = 0
        self.total_ops = 0
        self._reset()

    def _reset(self):
        self.ops = []
        self.last_w = {}
        self.readers = {}

    def op(self, eng, fn, reads=(), writes=(), dma=False, kind=""):
        o = Op(eng, fn, dma, kind)
        o.idx = len(self.ops)
        ex = [r for r in reads if isinstance(r, tuple) and r[0] == "ps"]
        if ex and eng != "pe":
            reads = [r for r in reads if r not in ex]
            writes = list(writes) + ex
        for r in reads:
            w = self.last_w.get(r)
            if w is not None:
                o.deps.add(w)
        for wkey in writes:
            w = self.last_w.get(wkey)
            if w is not None:
                o.deps.add(w)
            for rd in self.readers.get(wkey, ()):
                o.deps.add(rd)
        for r in reads:
            self.readers.setdefault(r, []).append(o.idx)
        for wkey in writes:
            self.last_w[wkey] = o.idx
            self.readers[wkey] = []
        o.deps.discard(o.idx)
        self.ops.append(o)
        return o

    def dma(self, out, in_, reads, writes, eng="sp", **kw):
        return self.op(eng, lambda e: e.dma_start(out=out, in_=in_, **kw), reads, writes, dma=True, kind="dma")

    def mm(self, out, lhsT, rhs, start, stop, reads, writes, **kw):
        return self.op("pe", lambda e: e.matmul(out, lhsT, rhs, start=start, stop=stop, **kw),
                       reads, writes, kind="mm")

    def tr(self, out, in_, ident, reads, writes):
        return self.op("pe", lambda e: e.transpose(out, in_, ident), reads, writes, kind="mm")

    def act(self, out, in_, func, reads, writes, **kw):
        return self.op("act", lambda e: e.activation(out, in_, func, **kw), reads, writes, kind="act")

    def cc(self, fn, reads, writes):
        o = self.op("pool", fn, reads, writes, kind="cc")
        o.sem_eng = "cc"
        o.signal = True
        return o

    def v(self, fn, reads, writes, eng="dve"):
        return self.op(eng, fn, reads, writes, kind="v")

    def flush(self, final=False):
        nc = self.nc
        ops = self.ops
        self.total_ops += len(ops)
        for o in ops:
            if o.eng == "pe":
                o.deps = {d for d in o.deps if ops[d].eng != "pe"}
        for o in ops:
            for d in o.deps:
                ops[d].signal = True
        base_cnt = dict(self.cnt)
        base_dcnt = {e: list(v) for e, v in self.dcnt.items()}
        dprev = {e: [None] * N_DMA_SEMS for e in self.dsem}
        for o in ops:
            if o.is_dma:
                o.signal = True
                k = self.drr[o.eng]
                self.drr[o.eng] = (k + 1) % N_DMA_SEMS
                self.dcnt[o.eng][k] += 16
                o.sem = self.dsem[o.eng][k]
                o.val = self.dcnt[o.eng][k]
                p = dprev[o.eng][k]
                if p is not None:
                    o.deps.add(p)
                dprev[o.eng][k] = o.idx
            elif o.signal:
                se = o.sem_eng or o.eng
                self.cnt[se] += 1
                o.sem = self.esem[se]
                o.val = self.cnt[se]
        first = self.nflush == 0
        self.nflush += 1
        with nc.Block() as blk:
            getters = {"pe": blk.tensor, "act": blk.scalar, "dve": blk.vector, "pool": blk.gpsimd, "sp": blk.sync}
            for ename in self.ENGS:
                mine = [o for o in ops if o.eng == ename]

                def body(e, mine=mine, ename=ename):
                    waited = {}
                    if not first:
                        for en, sem in self.esem.items():
                            if base_cnt[en] > 0 and en != ename:
                                e.wait_ge(sem, base_cnt[en])
                                waited[sem.num] = base_cnt[en]
                        for en, sems in self.dsem.items():
                            for k, sem in enumerate(sems):
                                if base_dcnt[en][k] > 0:
                                    e.wait_ge(sem, base_dcnt[en][k])
                                    waited[sem.num] = base_dcnt[en][k]
                    for o in mine:
                        need = {}
                        for d in o.deps:
                            do = ops[d]
                            if need.get(do.sem.num, (None, 0))[1] < do.val:
                                need[do.sem.num] = (do.sem, do.val)
                        for num, (sem, val) in need.items():
                            if waited.get(num, 0) >= val:
                                continue
                            e.wait_ge(sem, val)
                            waited[num] = val
                        ins = o.fn(e)
                        if o.signal:
                            if o.kind == "cc":
                                ins.then_inc(o.sem)
                            else:
                                ins.then_inc(o.sem, 16 if o.is_dma else 1)
                    if final and ename == "sp":
                        for en, sems in self.dsem.items():
                            for k, sem in enumerate(sems):
                                if self.dcnt[en][k] > 0:
                                    e.wait_ge(sem, self.dcnt[en][k])
                        for en, sem in self.esem.items():
                            if self.cnt[en] > 0:
                                e.wait_ge(sem, self.cnt[en])

                getters[ename](body)
        self._reset()
        if final:
            self.st.close()


def bank_rot(banks, lo, hi):
    state = {"i": 0}

    def nxt():
        k = lo + state["i"] % (hi - lo)
        state["i"] += 1
        return banks[k], ("ps", k)
    return nxt


EPS = 1e-6
NTOK = 4352
NOWN = 2304
SH1, SC1, GT1, SH2, SC2, GT2 = range(6)


def own_tiles():
    return [(i, i, 0) for i in range(16)] + [(16, 32, 1), (17, 33, 1)]


def rms_rstd(P, ss_ap, ss_key, out_ap, out_key, neghalf, D, tmp_ap, tmp_key):
    P.v(lambda e: e.tensor_scalar(tmp_ap, ss_ap, 1.0 / D, EPS, ALU.mult, ALU.add), [ss_key], [tmp_key])
    P.v(lambda e: e.tensor_tensor(out_ap, tmp_ap, neghalf, ALU.pow), [tmp_key, "consts"], [out_key], eng="pool")


def load_mod_rows(P, nc, st, mods_d, layer, which, names):
    out = []
    for v in range(2):
        t = st.enter_context(nc.sbuf_tensor("%s%d" % (names, v), [128, 1024], F32))
        P.dma(t[:], mods_d[layer, v:v + 1, which * 1024:(which + 1) * 1024].partition_broadcast(128), ["mods"],
              [(names, v)])
        out.append((t, (names, v)))
    return out


def phase_ada(nc, P, D, banks):
    with contextlib.ExitStack() as st:
        sb = lambda name, shape, dt=F32: st.enter_context(nc.sbuf_tensor(name, shape, dt))
        PS = Rot("ps", banks)
        ident = sb("ada_ident", [128, 128])
        cb = sb("ada_cb", [128, 2, 1024])
        screp = sb("ada_screp", [128, 2, 8, 128])
        bias = sb("ada_bias", [96, 2, 128])
        wbuf = Rot("ada_w", [sb("ada_wbuf%d" % i, [128, 8, 512]) for i in range(4)])
        accs = sb("ada_accs", [128, 2, 96])
        outT = sb("ada_outT", [96, 2, 128])
        P.dma(ident[:], D["ident32"][:, :], [], ["ident"])
        for v in range(2):
            P.dma(cb[:, v, :], D["cvec"][v:v + 1, :].partition_broadcast(128), [], [("cb", v)])
        for l in range(2):
            for v in range(2):
                P.dma(bias[48 * v:48 * v + 48, l, :], D["ada_b"][l].rearrange("(c p) -> c p", p=128), [], [("bias", l, v)])
        for v in range(2):
            P.act(cb[:, v, :], cb[:, v, :], AF.Silu, [("cb", v)], [("cb", v)])
            for half in range(2):
                p, pk = PS.next()
                for j in range(4):
                    kc = half * 4 + j
                    P.tr(p[:, j * 128:(j + 1) * 128], cb[:, v, kc * 128:(kc + 1) * 128], ident[:],
                         [("cb", v), "ident"], [pk])
                P.v(lambda e, p=p, v=v, half=half: e.tensor_copy(
                    screp[:, v, half * 4:(half + 1) * 4, :], p[:, :].rearrange("p (a b) -> p a b", a=4)),
                    [pk], [("screp", v)])
        for l in range(2):
            wl = D["ada_w"][l].rearrange("(kc p) n -> p kc n", p=128)
            acc, acck = PS.next()
            accv = acc[:, 0:96].rearrange("p (v c) -> p v c", v=2)
            for cblk in range(12):
                wb, wk = wbuf.next()
                P.dma(wb[:], wl[:, :, cblk * 512:(cblk + 1) * 512], [], [wk])
                for j in range(4):
                    c = cblk * 4 + j
                    for kc in range(8):
                        P.mm(accv[:, :, c], wb[:, kc, j * 128:(j + 1) * 128], screp[:, :, kc, 0], kc == 0, kc == 7,
                             [("screp", 0), ("screp", 1), wk], [acck])
            P.v(lambda e, acc=acc, l=l: e.tensor_copy(accs[:, l, :], acc[:, 0:96]), [acck], [("accs", l)])
            tp, tpk = PS.next()
            P.tr(tp[0:96, 0:128], accs[:, l, :], ident[:], [("accs", l), "ident"], [tpk])
            P.v(lambda e, tp=tp, l=l: e.tensor_tensor(outT[:, l, :], tp[0:96, 0:128], bias[:, l, :], ALU.add),
                [tpk, ("bias", l, 0), ("bias", l, 1)], [("outT", l)])
            P.dma(D["mods"][l].rearrange("v (c p) -> (v c) p", p=128), outT[:, l, :], [("outT", l)], ["mods"])
        P.flush()


def phase_l0a(nc, P, D, banks):
    NT = NTOK // 128
    with contextlib.ExitStack() as st:
        sb = lambda name, shape, dt=F32: st.enter_context(nc.sbuf_tensor(name, shape, dt))
        PS = Rot("ps", banks)
        ident = sb("a_ident", [128, 128], BF16)
        consts = sb("a_consts", [128, 4])
        gbc = sb("a_gbc", [128, 1024]); qg = sb("a_qg", [128, 256]); kvg = sb("a_kvg", [128, 128])
        w_in = sb("a_w_in", [128, 8, 1184], BF16)
        w_uq = sb("a_w_uq", [128, 2, 768], BF16)
        w_ukv = sb("a_w_ukv", [128, 1024], BF16)
        xt = Rot("xt", [sb("a_xt%d" % i, [128, 1024]) for i in range(2)])
        tabs = Rot("tabs", [sb("a_tabs%d" % i, [128, 1536]) for i in range(3)])
        junk = sb("a_junk", [128, 1024])
        t1r = Rot("t1", [sb("a_t1_%d" % i, [128, 1024]) for i in range(2)])
        hbr = Rot("hb", [sb("a_hb_%d" % i, [128, 1024], BF16) for i in range(2)])
        hTr = Rot("hT", [sb("a_hT_%d" % i, [128, 8, 128], BF16) for i in range(2)])
        small = Rot("small", [sb("a_small%d" % i, [128, 8]) for i in range(3)])
        zsr = Rot("zs", [sb("a_zs%d" % i, [128, 1184]) for i in range(2)])
        cqn = sb("a_cqn", [128, 384], BF16)
        cqnT = sb("a_cqnT", [128, 3, 128], BF16)
        krr = sb("a_krr", [128, 32], BF16)
        ropet = sb("a_ropet", [128, 2, 512])
        QA = sb("a_QA", [128, 8, 96], BF16); KA = sb("a_KA", [128, 8, 96], BF16)
        QB = sb("a_QB", [128, 8, 64], BF16); KB = sb("a_KB", [128, 2, 64], BF16)
        VAs = Rot("VAs", [sb("a_VAs%d" % i, [128, 8, 65], BF16) for i in range(2)])
        VBs = Rot("VBs", [sb("a_VBs%d" % i, [128, 2, 65], BF16) for i in range(2)])
        oQA = Rot("oQA", [sb("a_oQA%d" % i, [96, 8, 128], BF16) for i in range(2)])
        oKA = Rot("oKA", [sb("a_oKA%d" % i, [96, 8, 128], BF16) for i in range(2)])
        oQB = Rot("oQB", [sb("a_oQB%d" % i, [64, 8, 128], BF16) for i in range(2)])
        oKB = Rot("oKB", [sb("a_oKB%d" % i, [64, 2, 128], BF16) for i in range(2)])
        Abc = load_mod_rows(P, nc, st, D["mods"], 0, SC1, "a_A")
        Bbc = load_mod_rows(P, nc, st, D["mods"], 0, SH1, "a_B")

        P.dma(ident[:], D["identbf"][:, :], [], ["ident"])
        P.v(lambda e: e.memset(consts[:, 0:1], -0.5), [], ["consts"])
        P.dma(gbc[:], D["norm_mix_g"][0:1, :].partition_broadcast(128), [], ["gbc"])
        P.dma(qg[:], D["mla_q_norm_g"][0:1, :].partition_broadcast(128), [], ["qg"])
        P.dma(kvg[:], D["mla_kv_norm_g"][0:1, :].partition_broadcast(128), [], ["kvg"])
        P.dma(w_in[:], D["even_w_in"][0].rearrange("(kc p) n -> p kc n", p=128), [], ["w_in"], eng="pool")
        P.dma(w_uq[:], D["mla_w_uq"][0].rearrange("(kc p) n -> p kc n", p=128), [], ["w_uq"], eng="pool")
        P.dma(w_ukv[:], D["mla_w_ukv"][0], [], ["w_ukv"], eng="pool")
        for i, r in enumerate(VAs.aps):
            P.v(lambda e, r=r: e.memset(r[:, :, 64:65], 1.0), [], [("VAs", i)])
        for i, r in enumerate(VBs.aps):
            P.v(lambda e, r=r: e.memset(r[:, :, 64:65], 1.0), [], [("VBs", i)])
        for v in range(2):
            a, ak = Abc[v]
            P.v(lambda e, a=a: e.scalar_tensor_tensor(a[:], a[:], 1.0, gbc[:], ALU.add, ALU.mult), [ak, "gbc"], [ak])
        neghalf = consts[:, 0:1]
        v3 = lambda ap, h: ap.rearrange("p (h w) -> p h w", h=h)

        def rope(src4, dst4, cos4, ssin4, nh, blk, rkeys, wkeys):
            W = 4 * blk
            tmpa = ropet[:, 0, 0:nh * W].rearrange("p (h w) -> p h w", h=nh)
            tmpb = ropet[:, 1, 0:nh * W].rearrange("p (h w) -> p h w", h=nh)
            P.v(lambda e: e.tensor_tensor(tmpa, src4, cos4, ALU.mult), rkeys, ["ropeA"])
            v5 = lambda a: a.rearrange("p h (q w b) -> p h q w b", q=2, w=2)
            for w in range(2):
                P.v(lambda e, w=w: e.tensor_tensor(
                    v5(tmpb)[:, :, :, w, :], v5(src4)[:, :, :, 1 - w, :], v5(ssin4)[:, :, :, w, :], ALU.mult),
                    rkeys, ["ropeB%d" % w])
            P.v(lambda e: e.tensor_tensor(dst4, tmpa, tmpb, ALU.add), ["ropeA", "ropeB0", "ropeB1"], wkeys)

        def stage_a(t):
            rs = slice(t * 128, (t + 1) * 128)
            var = 1 if t >= 32 else 0
            need_q = (t < 16) or (t >= 32)
            A, Ak = Abc[var]; B, Bk = Bbc[var]
            x_ap, x_key = xt.next()
            P.dma(x_ap[:], D["xtok"][rs, :], [], [x_key])
            tb_ap, tb_key = tabs.next()
            P.dma(tb_ap[:, 0:256], D["cA"][rs, :], [], [(tb_key, 0)])
            P.dma(tb_ap[:, 256:512], D["sA"][rs, :], [], [(tb_key, 1)])
            P.dma(tb_ap[:, 512:1024], D["cB"][rs, :], [], [(tb_key, 2)])
            P.dma(tb_ap[:, 1024:1536], D["sB"][rs, :], [], [(tb_key, 3)])
            tkeys = [(tb_key, i) for i in range(4)]
            cA = tb_ap[:, 0:256]; sA = tb_ap[:, 256:512]; cB = tb_ap[:, 512:1024]; sB = tb_ap[:, 1024:1536]
            sm, sm_key = small.next()
            P.act(junk[:], x_ap[:], AF.Square, [x_key], ["junk", (sm_key, 0)], accum_out=sm[:, 0:1])
            rms_rstd(P, sm[:, 0:1], (sm_key, 0), sm[:, 2:3], (sm_key, 2), neghalf, 1024, sm[:, 1:2], (sm_key, 1))
            t1, t1k = t1r.next(); hb, hbk = hbr.next(); hT, hTk = hTr.next()
            P.v(lambda e, x_ap=x_ap, sm=sm, A=A, t1=t1: e.scalar_tensor_tensor(
                t1[:], x_ap[:], sm[:, 2:3], A[:], ALU.mult, ALU.mult), [x_key, (sm_key, 2), Ak], [t1k])
            P.v(lambda e, B=B, t1=t1, hb=hb: e.tensor_tensor(hb[:], t1[:], B[:], ALU.add), [t1k, Bk], [hbk])
            bk, bkey = PS.next()
            bkb = bk[:].bitcast(BF16)
            for kc in range(8):
                P.tr(bkb[:, kc * 128:(kc + 1) * 128], hb[:, kc * 128:(kc + 1) * 128], ident[:], [hbk, "ident"], [bkey])
            P.act(hT[:].rearrange("p a b -> p (a b)"), bkb, AF.Copy, [bkey], [hTk])
            blocks = [(0, 416), (416, 928), (928, 1184)]
            zb = []
            for bi, (c0, c1) in enumerate(blocks):
                if bi == 1 and not need_q:
                    zb.append((None, None))
                    continue
                bk, bkey = PS.next()
                for kc in range(8):
                    P.mm(bk[:, 0:c1 - c0], hT[:, kc, :], w_in[:, kc, c0:c1], kc == 0, kc == 7, [hTk, "w_in"], [bkey])
                zb.append((bk, bkey))
            zs, zsk = zsr.next()
            for bi, (c0, c1) in enumerate(blocks):
                if zb[bi][0] is None:
                    continue
                P.act(zs[:, c0:c1], zb[bi][0][:, 0:c1 - c0], AF.Copy, [zb[bi][1]], [(zsk, bi)])
            return dict(t=t, rs=rs, need_q=need_q, zs=zs, zsk=zsk, tb_ap=tb_ap, tkeys=tkeys, sm=sm, sm_key=sm_key)

        def stage_b(c):
            t, rs, need_q, zs, zsk, tb_ap, tkeys, sm, sm_key = (c[k] for k in ("t", "rs", "need_q", "zs", "zsk", "tb_ap", "tkeys", "sm", "sm_key"))
            cA = tb_ap[:, 0:256]; sA = tb_ap[:, 256:512]; cB = tb_ap[:, 512:1024]; sB = tb_ap[:, 1024:1536]
            z0, z0k = zs[:, 0:416], (zsk, 0)
            z1, z1k = zs[:, 416:928], (zsk, 1)
            z2, z2k = zs[:, 928:1184], (zsk, 2)
            if need_q:
                P.act(junk[:, 0:256], z0[:, 0:256], AF.Square, [z0k], ["junk", (sm_key, 3)], accum_out=sm[:, 3:4])
                rms_rstd(P, sm[:, 3:4], (sm_key, 3), sm[:, 4:5], (sm_key, 4), neghalf, 256, sm[:, 1:2], (sm_key, 1))
                P.v(lambda e, z0=z0, sm=sm: e.scalar_tensor_tensor(
                    cqn[:, 0:256], z0[:, 0:256], sm[:, 4:5], qg[:], ALU.mult, ALU.mult),
                    [z0k, (sm_key, 4), "qg"], ["cqn"])
            P.act(junk[:, 0:128], z0[:, 256:384], AF.Square, [z0k], ["junk", (sm_key, 5)], accum_out=sm[:, 5:6])
            rms_rstd(P, sm[:, 5:6], (sm_key, 5), sm[:, 6:7], (sm_key, 6), neghalf, 128, sm[:, 1:2], (sm_key, 1))
            P.v(lambda e, z0=z0, sm=sm: e.scalar_tensor_tensor(
                cqn[:, 256:384], z0[:, 256:384], sm[:, 6:7], kvg[:], ALU.mult, ALU.mult),
                [z0k, (sm_key, 6), "kvg"], ["cqn"])
            bk, bkey = PS.next()
            bkb = bk[:].bitcast(BF16)
            for j in (range(3) if need_q else [2]):
                P.tr(bkb[:, j * 128:(j + 1) * 128], cqn[:, j * 128:(j + 1) * 128], ident[:], ["cqn", "ident"], [bkey])
            P.act(cqnT[:].rearrange("p a b -> p (a b)"), bkb[:, 0:384], AF.Copy, [bkey], ["cqnT"])
            rope(v3(z0[:, 384:416], 1), v3(krr[:, :], 1), v3(cA[:, 0:32], 1), v3(sA[:, 0:32], 1), 1, 8,
                 tkeys + [z0k], ["krr"])
            if need_q:
                for hh in range(2):
                    bk, bkey = PS.next()
                    for kc in range(2):
                        P.mm(bk[:, 0:384], cqnT[:, kc, :], w_uq[:, kc, hh * 384:(hh + 1) * 384], kc == 0, kc == 1,
                             ["cqnT", "w_uq"], [bkey])
                    q4 = bk[:, 0:384].rearrange("p (h w) -> p h w", h=4)
                    P.act(QA[:, hh * 4:(hh + 1) * 4, 0:64], q4[:, :, 0:64], AF.Copy, [bkey], [("QA", hh, 0)])
                    rope(q4[:, :, 64:96], QA[:, hh * 4:(hh + 1) * 4, 64:96],
                         v3(cA[:, hh * 128:(hh + 1) * 128], 4), v3(sA[:, hh * 128:(hh + 1) * 128], 4), 4, 8,
                         tkeys + [bkey], [("QA", hh, 1)])
            va, va_key = VAs.next()
            for hh in range(2):
                bk, bkey = PS.next()
                P.mm(bk[:, :], cqnT[:, 2, :], w_ukv[:, hh * 512:(hh + 1) * 512], True, True, ["cqnT", "w_ukv"], [bkey])
                k4 = bk[:, :].rearrange("p (h w) -> p h w", h=4)
                P.act(KA[:, hh * 4:(hh + 1) * 4, 0:64], k4[:, :, 0:64], AF.Copy, [bkey], [("KA", hh, 0)])
                P.v(lambda e, va=va, k4=k4, hh=hh: e.tensor_copy(va[:, hh * 4:(hh + 1) * 4, 0:64], k4[:, :, 64:128]),
                    [bkey], [(va_key, hh)])
            for h in range(8):
                P.v(lambda e, h=h: e.tensor_copy(KA[:, h, 64:96], krr[:, :]), ["krr"], [("KA", h, 1)], eng="pool")
            P.dma(D["VA"][rs, :], va[:].rearrange("p h w -> p (h w)"), [(va_key, 0), (va_key, 1)], [("VA", t)])
            if need_q:
                q8 = z1[:, :].rearrange("p (h w) -> p h w", h=8)
                rope(q8, QB[:, :, :], v3(cB[:, :], 8), v3(sB[:, :], 8), 8, 16, tkeys + [z1k], ["QB"])
            k2 = z2[:, 0:128].rearrange("p (h w) -> p h w", h=2)
            rope(k2, KB[:, :, :], v3(cB[:, 0:128], 2), v3(sB[:, 0:128], 2), 2, 16, tkeys + [z2k], ["KB"])
            vb, vb_key = VBs.next()
            P.act(vb[:, :, 0:64], z2[:, 128:256].rearrange("p (h w) -> p h w", h=2), AF.Copy, [z2k], [vb_key])
            P.dma(D["VB"][rs, :], vb[:].rearrange("p h w -> p (h w)"), [vb_key], [("VB", t)])
            QAk = [("QA", hh, j) for hh in range(2) for j in range(2)]
            KAk = [("KA", hh, 0) for hh in range(2)] + [("KA", h, 1) for h in range(8)]
            jobs = [(KA, KAk, 8, 96, oKA, "KAT"), (KB, ["KB"], 2, 64, oKB, "KBT")]
            if need_q:
                jobs += [(QA, QAk, 8, 96, oQA, "QAT"), (QB, ["QB"], 8, 64, oQB, "QBT")]
            for (src, skeys, nh, Dh, pool, dname) in jobs:
                bk, bkey = PS.next()
                bkb = bk[:].bitcast(BF16)
                for h in range(nh):
                    P.tr(bkb[0:Dh, h * 128:(h + 1) * 128], src[:, h, :], ident[:], skeys + ["ident"], [bkey])
                ob, okey = pool.next()
                P.act(ob[:].rearrange("p a b -> p (a b)"), bkb[0:Dh, 0:nh * 128], AF.Copy, [bkey], [okey])
                P.dma(D[dname][:, :, rs], ob[:], [okey], [(dname, t)])

        pend = None
        for t in range(NT):
            cur = stage_a(t)
            if pend is not None:
                stage_b(pend)
            pend = cur
        stage_b(pend)
        P.flush()


def phase_l0b(nc, P, D, banks):
    with contextlib.ExitStack() as st:
        sb = lambda name, shape, dt=F32: st.enter_context(nc.sbuf_tensor(name, shape, dt))
        nS, nO, nR = bank_rot(banks, 0, 4), bank_rot(banks, 4, 6), bank_rot(banks, 6, 8)
        nOP = bank_rot(banks, 0, 4)
        KBT = sb("b_KBT", [64, 2, NTOK], BF16)
        VB = sb("b_VB", [128, 34, 130], BF16)
        VA = sb("b_VA", [128, 34, 520], BF16)
        w_out = sb("b_wout", [64, 16, 1024], BF16)
        sel = sb("b_sel", [65, 64])
        es = sb("b_es", [64, 8])
        masks = sb("b_masks", [128, 2, 512], BF16)
        G = load_mod_rows(P, nc, st, D["mods"], 0, GT1, "b_G")
        KAh = Rot("KAh", [sb("b_KAh%d" % i, [96, NTOK], BF16) for i in range(2)])
        QAg = Rot("QAg", [sb("b_QAg%d" % i, [96, 8, 512], BF16) for i in range(2)])
        QBg = Rot("QBg", [sb("b_QBg%d" % i, [64, 8, 512], BF16) for i in range(2)])
        LA = 2
        PT = Rot("PT", [sb("b_PT%d" % i, [128, 512], BF16) for i in range(LA + 2)])
        Osb = Rot("Osb", [sb("b_Osb%d" % i, [65, 512]) for i in range(2)])
        rec = Rot("rec", [sb("b_rec%d" % i, [64, 512]) for i in range(2)])
        mixT = sb("b_mixT", [64, 16, 512], BF16)
        xt = Rot("bxt", [sb("b_xt%d" % i, [128, 1024]) for i in range(2)])
        ot = Rot("bot", [sb("b_ot%d" % i, [128, 1024]) for i in range(2)])

        P.dma(KBT[:], D["KBT"][:, :, :], [], ["KBT"])
        P.dma(VB[:], D["VB"].rearrange("(c p) w -> p c w", p=128), [], ["VB"])
        P.dma(VA[:], D["VA"].rearrange("(c p) w -> p c w", p=128), [], ["VA"])
        P.dma(w_out[:], D["even_w_out"][0].rearrange("(j p) n -> p j n", p=64), [], ["w_out"], eng="pool")
        P.v(lambda e: e.memset(sel[:], 0.0), [], ["sel"])
        P.v(lambda e: e.memset(sel[64:65, :], 1.0), ["sel"], ["sel"])
        P.dma(es[:], D["win_sink"][0:1, :].partition_broadcast(64), [], ["es"])
        P.act(es[:], es[:], AF.Exp, ["es"], ["es"])
        P.dma(masks[:], D["wmask"].rearrange("a p n -> p a n"), [], ["masks"])

        groups = [(g * 512, 512, g * 512, 0) for g in range(4)] + [(4096, 256, 2048, 1)]
        SC_A = 96.0 ** -0.5
        SC_B = 64.0 ** -0.5
        for (tok0, N, row0, var) in groups:
            qa, qak = QAg.next()
            P.dma(qa[:, :, 0:N], D["QAT"][:, :, tok0:tok0 + N], [], [qak])
            qb, qbk = QBg.next()
            P.dma(qb[:, :, 0:N], D["QBT"][:, :, tok0:tok0 + N], [], [qbk])
            chunks = list(range(34)) if var == 0 else [32, 33]
            def mla_norm(h, O, Ok, N=N):
                osb, osk = Osb.next()
                P.v(lambda e: e.tensor_copy(osb[0:65, 0:N], O[0:65, 0:N]), [Ok], [osk])
                R, Rk = nR()
                P.mm(R[0:64, 0:N], sel[0:65, 0:64], osb[0:65, 0:N], True, True, ["sel", osk], [Rk])
                rc, rck = rec.next()
                P.v(lambda e: e.reciprocal(rc[0:64, 0:N], R[0:64, 0:N]), [Rk], [rck])
                P.v(lambda e: e.tensor_tensor(mixT[0:64, h, 0:N], osb[0:64, 0:N], rc[0:64, 0:N], ALU.mult),
                    [osk, rck], [("mixT", h)])

            defer = None
            for h in range(8):
                ka, kak = KAh.next()
                P.dma(ka[:], D["KAT"][:, h, :], [], [kak])
                O, Ok = nO()
                pend = []
                for ci, kc in enumerate(chunks + [None] * LA):
                    if kc is not None:
                        S, Sk = nS()
                        P.mm(S[:, 0:N], ka[0:96, kc * 128:(kc + 1) * 128], qa[0:96, h, 0:N], True, True, [kak, qak], [Sk])
                        pt, ptk = PT.next()
                        P.act(pt[:, 0:N], S[:, 0:N], AF.Exp, [Sk], [ptk], scale=SC_A)
                        pend.append((ci, kc, pt, ptk))
                    if defer is not None and (ci == LA or kc is None):
                        mla_norm(*defer)
                        defer = None
                    if len(pend) > LA or (kc is None and pend):
                        pci, pkc, ppt, pptk = pend.pop(0)
                        P.mm(O[0:65, 0:N], VA[:, pkc, h * 65:(h + 1) * 65], ppt[:, 0:N], pci == 0, pci == len(chunks) - 1,
                             ["VA", pptk], [Ok])
                defer = (h, O, Ok)
            mla_norm(*defer)
            nb = N // 128

            def win_norm(g, b, O, Ok):
                osb, osk = Osb.next()
                P.v(lambda e: e.tensor_copy(osb[0:65, :], O[0:65, :]), [Ok], [osk])
                R, Rk = nR()
                P.mm(R[0:64, :], sel[0:65, 0:64], osb[0:65, :], True, True, ["sel", osk], [Rk])
                rc, rck = rec.next()
                for j in range(4):
                    P.v(lambda e, j=j: e.tensor_scalar(
                        rc[0:64, j * 128:(j + 1) * 128], R[0:64, j * 128:(j + 1) * 128],
                        es[0:64, g * 4 + j:g * 4 + j + 1], None, ALU.add), [Rk, "es"], [rck])
                P.v(lambda e: e.reciprocal(rc[0:64, :], rc[0:64, :]), [rck], [rck])
                P.v(lambda e: e.tensor_tensor(
                    mixT[0:64, 8 + g * 4:8 + g * 4 + 4, b * 128:(b + 1) * 128],
                    osb[0:64, :].rearrange("p (h q) -> p h q", h=4),
                    rc[0:64, :].rearrange("p (h q) -> p h q", h=4), ALU.mult),
                    [osk, rck], [("mixT", 8 + g * 4 + j) for j in range(4)])

            deferw = None
            for b in range(nb):
                tt = tok0 // 128 + b
                if var == 0:
                    cl = []
                    if tt > 0:
                        cl.append((tt - 1, 0))
                    cl.append((tt, None))
                    cl.append((tt + 1, 1))
                    cl += [(32, None), (33, None)]
                else:
                    cl = [(32, None), (33, None)]
                for g in range(2):
                    O, Ok = nO()
                    Q4 = qb[0:64, g * 4:(g + 1) * 4, b * 128:(b + 1) * 128]
                    pend = []
                    for ci, item in enumerate(cl + [None] * LA):
                        if item is not None:
                            kc, mk_ = item
                            S, Sk = nS()
                            P.mm(S[:, :].rearrange("p (h q) -> p h q", h=4), KBT[0:64, g, kc * 128:(kc + 1) * 128], Q4,
                                 True, True, ["KBT", qbk], [Sk])
                            pt, ptk = PT.next()
                            P.act(pt[:, :], S[:, :], AF.Exp, [Sk], [ptk], scale=SC_B)
                            if mk_ is not None:
                                P.v(lambda e, pt=pt, mk_=mk_: e.tensor_tensor(pt[:, :], pt[:, :], masks[:, mk_, :], ALU.mult),
                                    [ptk, "masks"], [ptk], eng="pool")
                            pend.append((ci, kc, pt, ptk))
                        if deferw is not None and (ci == min(LA, len(cl) - 1)):
                            win_norm(*deferw)
                            deferw = None
                        if len(pend) > LA or (item is None and pend):
                            pci, pkc, ppt, pptk = pend.pop(0)
                            P.mm(O[0:65, :], VB[:, pkc, g * 65:(g + 1) * 65], ppt[:, :], pci == 0, pci == len(cl) - 1,
                                 ["VB", pptk], [Ok])
                    deferw = (g, b, O, Ok)
            if deferw is not None:
                win_norm(*deferw)
                deferw = None
            mkeys = [("mixT", j) for j in range(16)]
            for b in range(nb):
                x_ap, xk = xt.next()
                P.dma(x_ap[:], D["xtok"][tok0 + b * 128:tok0 + (b + 1) * 128, :], [], [xk])
                o_ap, ok_ = ot.next()
                for dh in range(2):
                    Ob, Obk = nOP()
                    for j in range(16):
                        P.mm(Ob[:, :], mixT[0:64, j, b * 128:(b + 1) * 128], w_out[0:64, j, dh * 512:(dh + 1) * 512],
                             j == 0, j == 15, mkeys + ["w_out"], [Obk])
                    P.v(lambda e, o_ap=o_ap, Ob=Ob, dh=dh, var=var: e.tensor_tensor(
                        o_ap[:, dh * 512:(dh + 1) * 512], Ob[:, :], G[var][0][:, dh * 512:(dh + 1) * 512], ALU.mult),
                        [Obk, G[var][1]], [ok_])
                P.v(lambda e, o_ap=o_ap, x_ap=x_ap: e.tensor_tensor(o_ap[:], o_ap[:], x_ap[:], ALU.add),
                    [ok_, xk], [ok_], eng="pool")
                r0 = row0 + b * 128
                P.dma(D["x1"][r0:r0 + 128, :], o_ap[:], [ok_], [("x1", r0)])
        P.flush()


def phase_moe(nc, P, D, banks, layer, xin, xout, tiles, tag, final_norm=False):
    NTl = len(tiles)
    T = NTl * 128
    with contextlib.ExitStack() as st0:
        sb0 = lambda name, shape, dt=F32: st0.enter_context(nc.sbuf_tensor(tag + name, shape, dt))
        h2T = sb0("h2T", [128, 8, T], BF16)
        comb = sb0("comb", [128, NTl, 32])
        consts = sb0("consts", [128, 4])
        P.v(lambda e: e.memset(consts[:, 0:1], -0.5), [], ["consts"])
        neghalf = consts[:, 0:1]
        with contextlib.ExitStack() as st:
            sb = lambda name, shape, dt=F32: st.enter_context(nc.sbuf_tensor(tag + name, shape, dt))
            nA, nB = bank_rot(banks, 0, 4), bank_rot(banks, 4, 8)
            ident = sb("ident32", [128, 128])
            gbc = sb("gbc", [128, 1024])
            w_r = sb("w_r", [128, 8, 36])
            Abc = load_mod_rows(P, nc, st, D["mods"], layer, SC2, tag + "A")
            Bbc = load_mod_rows(P, nc, st, D["mods"], layer, SH2, tag + "B")
            xt = Rot("mxt", [sb("xt%d" % i, [128, 1024]) for i in range(2)])
            junk = sb("junk", [128, 1024])
            t1 = sb("t1", [128, 1024])
            h2 = sb("h2", [128, 1024])
            h2T32 = sb("h2T32", [128, 8, 128])
            small = Rot("msmall", [sb("small%d" % i, [128, 16]) for i in range(2)])
            rt = Rot("mrt", [sb("rt%d" % i, [128, 96]) for i in range(2)])
            P.dma(ident[:], D["ident32"][:, :], [], ["ident"])
            P.dma(gbc[:], D["norm_ffn_g"][layer:layer + 1, :].partition_broadcast(128), [], ["gbc"])
            P.dma(w_r[:, :, 0:4], D["moe_w_rg"][layer].rearrange("(kc p) n -> p kc n", p=128), [], [("w_r", 0)])
            P.dma(w_r[:, :, 4:36], D["moe_w_re"][layer].rearrange("(kc p) n -> p kc n", p=128), [], [("w_r", 1)])
            for v in range(2):
                a, ak = Abc[v]
                P.v(lambda e, a=a: e.scalar_tensor_tensor(a[:], a[:], 1.0, gbc[:], ALU.add, ALU.mult), [ak, "gbc"], [ak])
            for ti, (r, var) in enumerate(tiles):
                A, Ak = Abc[var]; B, Bk = Bbc[var]
                x_ap, xk = xt.next()
                P.dma(x_ap[:], D[xin][r * 128:(r + 1) * 128, :], [], [xk])
                sm, smk = small.next()
                P.act(junk[:], x_ap[:], AF.Square, [xk], ["junk", (smk, 0)], accum_out=sm[:, 0:1])
                rms_rstd(P, sm[:, 0:1], (smk, 0), sm[:, 2:3], (smk, 2), neghalf, 1024, sm[:, 1:2], (smk, 1))
                P.v(lambda e, x_ap=x_ap, sm=sm, A=A: e.scalar_tensor_tensor(
                    t1[:], x_ap[:], sm[:, 2:3], A[:], ALU.mult, ALU.mult), [xk, (smk, 2), Ak], ["t1"])
                P.v(lambda e, B=B: e.tensor_tensor(h2[:], t1[:], B[:], ALU.add), ["t1", Bk], ["h2"])
                for half in range(2):
                    bk, bkey = nA()
                    for j in range(4):
                        kc = half * 4 + j
                        P.tr(bk[:, j * 128:(j + 1) * 128], h2[:, kc * 128:(kc + 1) * 128], ident[:], ["h2", "ident"], [bkey])
                    P.act(h2T[:, half * 4:(half + 1) * 4, ti * 128:(ti + 1) * 128],
                          bk[:, :].rearrange("p (a b) -> p a b", a=4), AF.Copy, [bkey], [("h2T", ti, half)])
                    P.v(lambda e, bk=bk, half=half: e.tensor_copy(
                        h2T32[:, half * 4:(half + 1) * 4, :], bk[:, :].rearrange("p (a b) -> p a b", a=4)),
                        [bkey], [("h2T32", half)])
                lg, lgk = nB()
                for kc in range(8):
                    P.mm(lg[:, 0:36], h2T32[:, kc, :], w_r[:, kc, :], kc == 0, kc == 7,
                         [("h2T32", kc // 4), ("w_r", 0), ("w_r", 1)], [lgk])
                R, Rk = rt.next()
                P.v(lambda e, R=R, lg=lg: e.tensor_copy(R[:, 0:36], lg[:, 0:36]), [lgk], [(Rk, "lg")])
                s_ = lambda c: sm[:, c:c + 1]
                P.v(lambda e, R=R, sm=sm: e.reduce_max(sm[:, 3:4], R[:, 0:4], AX.X), [(Rk, "lg")], [(smk, 3)])
                P.v(lambda e, R=R, sm=sm: e.tensor_scalar(R[:, 36:40], R[:, 0:4], sm[:, 3:4], None, ALU.is_equal),
                    [(Rk, "lg"), (smk, 3)], [(Rk, "goh")])
                P.v(lambda e, sm=sm: e.tensor_scalar(sm[:, 4:5], sm[:, 3:4], -1.0, None, ALU.mult), [(smk, 3)], [(smk, 4)])
                P.act(R[:, 80:84], R[:, 0:4], AF.Exp, [(Rk, "lg"), (smk, 4)], [(Rk, "gexp"), (smk, 5)],
                      bias=sm[:, 4:5], accum_out=sm[:, 5:6])
                P.v(lambda e, sm=sm: e.reciprocal(sm[:, 6:7], sm[:, 5:6]), [(smk, 5)], [(smk, 6)])
                P.v(lambda e, R=R: e.tensor_scalar(R[:, 40:48], R[:, 4:12], R[:, 36:37], None, ALU.mult),
                    [(Rk, "lg"), (Rk, "goh")], [(Rk, "ein")])
                for g in range(1, 4):
                    P.v(lambda e, R=R, g=g: e.scalar_tensor_tensor(
                        R[:, 40:48], R[:, 4 + 8 * g:12 + 8 * g], R[:, 36 + g:37 + g], R[:, 40:48], ALU.mult, ALU.add),
                        [(Rk, "lg"), (Rk, "goh"), (Rk, "ein")], [(Rk, "ein")])
                P.v(lambda e, R=R, sm=sm: e.reduce_max(sm[:, 7:8], R[:, 40:48], AX.X), [(Rk, "ein")], [(smk, 7)])
                P.v(lambda e, R=R, sm=sm: e.tensor_scalar(R[:, 48:56], R[:, 40:48], sm[:, 7:8], None, ALU.is_equal),
                    [(Rk, "ein"), (smk, 7)], [(Rk, "oh1")])
                P.v(lambda e, R=R: e.scalar_tensor_tensor(R[:, 56:64], R[:, 48:56], -1e30, R[:, 40:48], ALU.mult, ALU.add),
                    [(Rk, "oh1"), (Rk, "ein")], [(Rk, "e2")])
                P.v(lambda e, R=R, sm=sm: e.reduce_max(sm[:, 8:9], R[:, 56:64], AX.X), [(Rk, "e2")], [(smk, 8)])
                P.v(lambda e, R=R, sm=sm: e.tensor_scalar(R[:, 64:72], R[:, 56:64], sm[:, 8:9], None, ALU.is_equal),
                    [(Rk, "e2"), (smk, 8)], [(Rk, "oh2")])
                P.v(lambda e, sm=sm: e.tensor_tensor(sm[:, 9:10], sm[:, 8:9], sm[:, 7:8], ALU.subtract),
                    [(smk, 7), (smk, 8)], [(smk, 9)])
                P.act(sm[:, 10:11], sm[:, 9:10], AF.Exp, [(smk, 9)], [(smk, 10)])
                P.v(lambda e, sm=sm: e.tensor_scalar(sm[:, 11:12], sm[:, 10:11], 1.0, None, ALU.add), [(smk, 10)], [(smk, 11)])
                P.v(lambda e, sm=sm: e.reciprocal(sm[:, 11:12], sm[:, 11:12]), [(smk, 11)], [(smk, 11)])
                P.v(lambda e, sm=sm: e.tensor_tensor(sm[:, 12:13], sm[:, 11:12], sm[:, 6:7], ALU.mult),
                    [(smk, 11), (smk, 6)], [(smk, 12)])
                P.v(lambda e, sm=sm: e.tensor_tensor(sm[:, 13:14], sm[:, 12:13], sm[:, 10:11], ALU.mult),
                    [(smk, 12), (smk, 10)], [(smk, 13)])
                P.v(lambda e, R=R, sm=sm: e.tensor_scalar(R[:, 72:80], R[:, 48:56], sm[:, 12:13], None, ALU.mult),
                    [(Rk, "oh1"), (smk, 12)], [(Rk, "loc")])
                P.v(lambda e, R=R, sm=sm: e.scalar_tensor_tensor(
                    R[:, 72:80], R[:, 64:72], sm[:, 13:14], R[:, 72:80], ALU.mult, ALU.add),
                    [(Rk, "oh2"), (smk, 13), (Rk, "loc")], [(Rk, "loc")])
                for g in range(4):
                    P.v(lambda e, R=R, g=g, ti=ti: e.tensor_scalar(
                        comb[:, ti, g * 8:(g + 1) * 8], R[:, 72:80], R[:, 36 + g:37 + g], None, ALU.mult),
                        [(Rk, "loc"), (Rk, "goh")], [("comb", ti)])
            P.flush()
        with contextlib.ExitStack() as st:
            sb = lambda name, shape, dt=F32: st.enter_context(nc.sbuf_tensor(tag + name, shape, dt))
            nGU, nY = bank_rot(banks, 0, 4), bank_rot(banks, 4, 8)
            yacc = sb("yacc", [128, NTl, 1024])
            wg = Rot("wg", [sb("wg%d" % i, [128, 8, 512], BF16) for i in range(2)])
            wu = Rot("wu", [sb("wu%d" % i, [128, 8, 512], BF16) for i in range(2)])
            wd = Rot("wd", [sb("wd%d" % i, [128, 4, 1024], BF16) for i in range(2)])
            hg = Rot("hg", [sb("hg%d" % i, [128, 4, 512], BF16) for i in range(2)])
            sg = Rot("sg", [sb("sg%d" % i, [128, 512]) for i in range(2)])
            G = load_mod_rows(P, nc, st, D["mods"], layer, GT2, tag + "G")
            xt = Rot("mxt2", [sb("xt2_%d" % i, [128, 1024]) for i in range(2)])
            groups = []
            t0 = 0
            while t0 < NTl:
                n = min(4, NTl - t0)
                groups.append((t0, n))
                t0 += n
            def emit_gu(ex, t0, n, g_ap, gk, u_ap, uk):
                N = n * 128
                ts = slice(t0 * 128, t0 * 128 + N)
                hgt, hgk = hg.next()
                for fc in range(4):
                    Gp, Gk = nGU(); Up, Uk = nGU()
                    for kc in range(8):
                        P.mm(Gp[:, 0:N], g_ap[:, kc, fc * 128:(fc + 1) * 128], h2T[:, kc, ts], kc == 0, kc == 7,
                             [gk, "h2T"], [Gk])
                    for kc in range(8):
                        P.mm(Up[:, 0:N], u_ap[:, kc, fc * 128:(fc + 1) * 128], h2T[:, kc, ts], kc == 0, kc == 7,
                             [uk, "h2T"], [Uk])
                    s_ap, sk = sg.next()
                    P.act(s_ap[:, 0:N], Gp[:, 0:N], AF.Silu, [Gk], [sk])
                    P.v(lambda e, hgt=hgt, Up=Up, s_ap=s_ap, fc=fc, N=N: e.tensor_tensor(
                        hgt[:, fc, 0:N], Up[:, 0:N], s_ap[:, 0:N], ALU.mult), [Uk, sk], [(hgk, fc)])
                return hgt, hgk

            def emit_down(ex, t0, n, hgt, hgk, d_ap, dk):
                for b in range(n):
                    ti = t0 + b
                    for dh in range(2):
                        Yp, Yk = nY()
                        for fc in range(4):
                            P.mm(Yp[:, :], hgt[:, fc, b * 128:(b + 1) * 128], d_ap[:, fc, dh * 512:(dh + 1) * 512],
                                 fc == 0, fc == 3, [(hgk, f) for f in range(4)] + [dk], [Yk])
                        ysl = yacc[:, ti, dh * 512:(dh + 1) * 512]
                        if ex == 0:
                            P.v(lambda e, ysl=ysl, Yp=Yp, ti=ti, ex=ex: e.tensor_scalar(
                                ysl, Yp[:, :], comb[:, ti, ex:ex + 1], None, ALU.mult), [Yk], [("yacc", ti, dh)])
                        else:
                            P.v(lambda e, ysl=ysl, Yp=Yp, ti=ti, ex=ex: e.scalar_tensor_tensor(
                                ysl, Yp[:, :], comb[:, ti, ex:ex + 1], ysl, ALU.mult, ALU.add),
                                [Yk, ("yacc", ti, dh)], [("yacc", ti, dh)])

            pend = None
            for ex in range(32):
                g_ap, gk = wg.next(); u_ap, uk = wu.next(); d_ap, dk = wd.next()
                P.dma(g_ap[:], D["moe_w_gate%d" % layer][ex].rearrange("(kc p) f -> p kc f", p=128), [], [gk], eng="pool")
                P.dma(u_ap[:], D["moe_w_up%d" % layer][ex].rearrange("(kc p) f -> p kc f", p=128), [], [uk], eng="pool")
                P.dma(d_ap[:], D["moe_w_down%d" % layer][ex].rearrange("(kc p) f -> p kc f", p=128), [], [dk], eng="pool")
                for (t0, n) in groups:
                    hgt, hgk = emit_gu(ex, t0, n, g_ap, gk, u_ap, uk)
                    if pend is not None:
                        emit_down(*pend)
                    pend = (ex, t0, n, hgt, hgk, d_ap, dk)
            emit_down(*pend)
            if final_norm:
                fg = sb("fg", [128, 1024])
                P.dma(fg[:], D["final_norm_g"][0:1, :].partition_broadcast(128), [], ["fg"])
                junk = sb("junk3", [128, 1024])
                small = Rot("fsmall", [sb("fsmall%d" % i, [128, 4]) for i in range(2)])
            for ti, (r, var) in enumerate(tiles):
                x_ap, xk = xt.next()
                P.dma(x_ap[:], D[xin][r * 128:(r + 1) * 128, :], [], [xk])
                ysl = yacc[:, ti, :]
                yk = [("yacc", ti, 0), ("yacc", ti, 1)]
                P.v(lambda e, ysl=ysl, var=var: e.tensor_tensor(ysl, ysl, G[var][0][:], ALU.mult), yk + [G[var][1]], yk)
                P.v(lambda e, ysl=ysl, x_ap=x_ap: e.tensor_tensor(ysl, ysl, x_ap[:], ALU.add), yk + [xk], yk, eng="pool")
                if final_norm:
                    sm, smk = small.next()
                    P.act(junk[:], ysl, AF.Square, yk, ["junk3", (smk, 0)], accum_out=sm[:, 0:1])
                    rms_rstd(P, sm[:, 0:1], (smk, 0), sm[:, 2:3], (smk, 2), neghalf, 1024, sm[:, 1:2], (smk, 1))
                    P.v(lambda e, ysl=ysl, sm=sm: e.scalar_tensor_tensor(
                        ysl, ysl, sm[:, 2:3], fg[:], ALU.mult, ALU.mult), yk + [(smk, 2), "fg"], yk)
                P.dma(D[xout][r * 128:(r + 1) * 128, :], ysl, yk, [(xout, r)])
            P.flush()


GELU_C = 0.7978845608028654


def l1_tiles():
    return [(i, 0) for i in range(16)] + [(16, 1), (17, 1)]


def phase_l1a(nc, P, D, banks):
    with contextlib.ExitStack() as st:
        sb = lambda name, shape, dt=F32: st.enter_context(nc.sbuf_tensor("c_" + name, shape, dt))
        nP = bank_rot(banks, 0, 8)
        ident = sb("ident", [128, 128], BF16)
        ident32 = sb("ident32", [128, 128])
        consts = sb("consts", [128, 4])
        gbc = sb("gbc", [128, 1024])
        w_in = sb("w_in", [128, 8, 2592], BF16)
        Abc = load_mod_rows(P, nc, st, D["mods"], 1, SC1, "c_A")
        Bbc = load_mod_rows(P, nc, st, D["mods"], 1, SH1, "c_B")
        W2 = sb("W2", [32, 512])
        bg = sb("bg", [128, 512])
        gm = sb("gm", [128, 3, 128])
        wsT = sb("wsT", [128, 4, 128], BF16)
        bsT = sb("bsT", [128, 4])
        lng = sb("lng", [128, 512]); lnb = sb("lnb", [128, 512])
        xt = Rot("cxt", [sb("xt%d" % i, [128, 1024]) for i in range(2)])
        junk = sb("junk", [128, 1024])
        t1r = Rot("t1", [sb("t1_%d" % i, [128, 1024]) for i in range(2)])
        hbr = Rot("hb", [sb("hb_%d" % i, [128, 1024], BF16) for i in range(2)])
        hTr = Rot("hT", [sb("hT_%d" % i, [128, 8, 128], BF16) for i in range(2)])
        small = Rot("csmall", [sb("small%d" % i, [128, 16]) for i in range(3)])
        qkr = Rot("qk", [sb("qk%d" % i, [128, 512]) for i in range(2)])
        g32r = Rot("g32", [sb("g32_%d" % i, [128, 32]) for i in range(2)])
        uvr = Rot("uv", [sb("uv%d" % i, [128, 2, 512]) for i in range(2)])
        g32T = sb("g32T", [32, 128])
        zs = sb("zs", [128, 512]); la = sb("la", [128, 512])
        bsb = sb("bsb", [128, 2, 256])
        ex = sb("ex", [128, 256]); tmp = sb("tmp", [128, 256])
        gl = sb("gl", [128, 6, 256], BF16)
        glT = Rot("glT", [sb("glT%d" % i, [128, 8, 128], BF16) for i in range(2)])
        dec = Rot("dec", [sb("dec%d" % i, [128, 4]) for i in range(2)])
        vb = Rot("cvb", [sb("vb%d" % i, [128, 512], BF16) for i in range(2)])
        rsb = Rot("rsb", [sb("rsb%d" % i, [128, 512], BF16) for i in range(2)])
        ge = sb("ge", [128, 2, 512])
        gt_ = sb("gt_", [128, 512]); gt2_ = sb("gt2_", [128, 512])
        vgn = sb("vgn", [128, 512], BF16)
        dlb = Rot("dlb", [sb("dlb%d" % i, [128, 512], BF16) for i in range(2)])

        P.dma(ident[:], D["identbf"][:, :], [], ["ident"])
        P.dma(ident32[:], D["ident32"][:, :], [], ["ident32"])
        P.v(lambda e: e.memset(consts[:, 0:1], -0.5), [], ["consts"])
        neghalf = consts[:, 0:1]
        P.dma(gbc[:], D["norm_mix_g"][1:2, :].partition_broadcast(128), [], ["gbc"])
        for c in range(3):
            lo, hi = c * 864, (c + 1) * 864
            P.dma(w_in[:, :, lo:hi], D["odd_w_in"][0].rearrange("(kc p) n -> p kc n", p=128)[:, :, lo:hi], [],
                  [("w_in", c)], eng="pool")
        wkeys = [("w_in", c) for c in range(3)]
        P.v(lambda e: e.memset(W2[:], 0.0), [], ["W2"])
        P.dma(W2[0:16, 0:256], D["gla_w_g2"][0, 0], ["W2"], ["W2"])
        P.dma(W2[16:32, 256:512], D["gla_w_g2"][0, 1], ["W2"], ["W2"])
        P.dma(bg[:], D["gla_b_g"][0:1].rearrange("o a n -> o (a n)").partition_broadcast(128), [], ["bg"])
        P.dma(gm[:], D["gmask"][0:3].rearrange("a p n -> p a n"), [], ["gm"])
        P.dma(wsT[:], D["sg_w_sT"].rearrange("g s t -> s g t"), [], ["wsT"], eng="pool")
        P.dma(bsT[:], D["sg_b_sT"][:, :], [], ["bsT"])
        P.dma(lng[:], D["sg_ln_g"][0:1, :].partition_broadcast(128), [], ["lng"])
        P.dma(lnb[:], D["sg_ln_b"][0:1, :].partition_broadcast(128), [], ["lnb"])
        for v in range(2):
            a, ak = Abc[v]
            P.v(lambda e, a=a: e.scalar_tensor_tensor(a[:], a[:], 1.0, gbc[:], ALU.add, ALU.mult), [ak, "gbc"], [ak])

        def gelu(src, src_key, dst, dst_key):
            P.v(lambda e: e.tensor_tensor(gt2_[:], src, src, ALU.mult), [src_key], ["gt2_"])
            P.v(lambda e: e.tensor_scalar(gt2_[:], gt2_[:], 0.044715, 1.0, ALU.mult, ALU.add), ["gt2_"], ["gt2_"])
            P.v(lambda e: e.tensor_tensor(gt2_[:], gt2_[:], src, ALU.mult), ["gt2_", src_key], ["gt2_"], eng="pool")
            P.act(gt2_[:], gt2_[:], AF.Sigmoid, ["gt2_"], ["gt2_"], scale=2.0 * GELU_C)
            P.v(lambda e: e.tensor_tensor(dst, src, gt2_[:], ALU.mult), [src_key, "gt2_"], [dst_key])

        def stage_a(r, var):
            lat = var == 0
            A, Ak = Abc[var]; B, Bk = Bbc[var]
            rs = slice(r * 128, (r + 1) * 128)
            x_ap, xk = xt.next()
            P.dma(x_ap[:], D["x2"][rs, :], [], [xk])
            sm, smk = small.next()
            P.act(junk[:], x_ap[:], AF.Square, [xk], ["junk", (smk, 0)], accum_out=sm[:, 0:1])
            rms_rstd(P, sm[:, 0:1], (smk, 0), sm[:, 2:3], (smk, 2), neghalf, 1024, sm[:, 1:2], (smk, 1))
            t1, t1k = t1r.next(); hb, hbk = hbr.next(); hT, hTk = hTr.next()
            P.v(lambda e, x_ap=x_ap, sm=sm, A=A, t1=t1: e.scalar_tensor_tensor(
                t1[:], x_ap[:], sm[:, 2:3], A[:], ALU.mult, ALU.mult), [xk, (smk, 2), Ak], [t1k])
            P.v(lambda e, B=B, t1=t1, hb=hb: e.tensor_tensor(hb[:], t1[:], B[:], ALU.add), [t1k, Bk], [hbk])
            bk, bkey = nP()
            bkb = bk[:].bitcast(BF16)
            for kc in range(8):
                P.tr(bkb[:, kc * 128:(kc + 1) * 128], hb[:, kc * 128:(kc + 1) * 128], ident[:], [hbk, "ident"], [bkey])
            P.act(hT[:].rearrange("p a b -> p (a b)"), bkb, AF.Copy, [bkey], [hTk])

            def proj(c0, c1):
                bk, bkey = nP()
                for kc in range(8):
                    P.mm(bk[:, 0:c1 - c0], hT[:, kc, :], w_in[:, kc, c0:c1], kc == 0, kc == 7, [hTk] + wkeys, [bkey])
                return bk, bkey
            zqk, zqkk = proj(0, 512)
            qk, qkk = qkr.next()
            P.act(qk[:], zqk[:, :], AF.Copy, [zqkk], [qkk])
            zv, zvk = proj(512, 1024)
            v_ap, vk = vb.next()
            P.act(v_ap[:], zv[:, :], AF.Copy, [zvk], [vk])
            P.dma(D["g_v"][rs, :], v_ap[:], [vk], [("g_v", r)])
            zg, zgk = proj(1024, 1056)
            g32, g32k = g32r.next()
            P.v(lambda e, zg=zg, g32=g32: e.tensor_copy(g32[:], zg[:, 0:32]), [zgk], [g32k])
            uv, uvk = uvr.next()
            if lat:
                zr, zrk = proj(1056, 1568)
                r_ap, rk = rsb.next()
                P.act(r_ap[:], zr[:, :], AF.Silu, [zrk], [rk])
                P.dma(D["rsilu"][rs, :], r_ap[:], [rk], [("rsilu", r)])
                zu, zuk = proj(1568, 2080)
                P.act(uv[:, 0, :], zu[:, :], AF.Copy, [zuk], [(uvk, 0)])
                zvg, zvgk = proj(2080, 2592)
                P.v(lambda e, uv=uv, zvg=zvg: e.tensor_copy(uv[:, 1, :], zvg[:, :]), [zvgk], [(uvk, 1)])
            return dict(r=r, lat=lat, rs=rs, sm=sm, smk=smk, qk=qk, qkk=qkk, g32=g32, g32k=g32k, uv=uv, uvk=uvk)

        def stage_b(c):
            r, lat, rs, sm, smk, qk, qkk, g32, g32k, uv, uvk = (c[k] for k in (
                "r", "lat", "rs", "sm", "smk", "qk", "qkk", "g32", "g32k", "uv", "uvk"))
            bk, bkey = nP()
            P.tr(bk[0:32, 0:128], g32[:, :], ident32[:], [g32k, "ident32"], [bkey])
            P.v(lambda e, bk=bk: e.tensor_copy(g32T[:], bk[0:32, 0:128]), [bkey], ["g32T"])
            zz, zzk = nP()
            P.mm(zz[:, :], g32T[0:32, :], W2[0:32, :], True, True, ["g32T", "W2"], [zzk])
            P.v(lambda e, zz=zz: e.tensor_tensor(zs[:], zz[:, :], bg[:], ALU.add), [zzk, "bg"], ["zs"])
            P.act(zs[:], zs[:], AF.Exp, ["zs"], ["zs"], scale=-1.0)
            P.act(zs[:], zs[:], AF.Ln, ["zs"], ["zs"], bias=1.0)
            P.v(lambda e: e.tensor_scalar(la[:], zs[:], -1.0 / 16.0, None, ALU.mult), ["zs"], ["la"])
            cA, cAk = nP()
            P.mm(cA[:, 0:256], gm[:, 0, :], la[:, 0:256], True, True, ["gm", "la"], [cAk])
            P.mm(cA[:, 256:512], gm[:, 1, :], la[:, 256:512], True, True, ["gm", "la"], [cAk])
            cL, cLk = nP()
            P.mm(cL[:, :], gm[:, 2, :], la[:, :], True, True, ["gm", "la"], [cLk])
            P.act(bsb[:].rearrange("p a b -> p (a b)"), cA[:, :], AF.Copy, [cAk], ["bsb"])
            cT, cTk = nP()
            for j in range(4):
                P.mm(cT[:, j:j + 1], la[:, j * 128:(j + 1) * 128], gm[:, 2, 0:1], True, True, ["gm", "la"], [cTk])
            d_ap, dk = dec.next()
            P.act(d_ap[:], cT[:, 0:4], AF.Exp, [cTk], [dk])
            P.dma(D["g_dec"][:, r, :], d_ap[:], [dk], [("g_dec", r)])
            for p in range(2):
                if p == 1 and not lat:
                    continue
                bp = bsb[:, p, :]
                if lat:
                    P.act(ex[:], bp, AF.Exp, ["bsb"], ["ex"])
                    P.v(lambda e, p=p: e.scalar_tensor_tensor(gl[:, 2 * p, :], qk[:, 0:256], 0.125, ex[:], ALU.mult, ALU.mult),
                        [qkk, "ex"], [("gl", 2 * p)])
                P.act(ex[:], bp, AF.Exp, ["bsb"], ["ex"], scale=-1.0)
                P.v(lambda e, p=p: e.tensor_tensor(gl[:, 2 * p + 1, :], qk[:, 256:512], ex[:], ALU.mult),
                    [qkk, "ex"], [("gl", 2 * p + 1)])
                P.v(lambda e, p=p, bp=bp, cL=cL: e.tensor_tensor(tmp[:], cL[:, p * 256:(p + 1) * 256], bp, ALU.subtract),
                    [cLk, "bsb"], ["tmp"])
                P.act(ex[:], tmp[:], AF.Exp, ["tmp"], ["ex"])
                P.v(lambda e, p=p: e.tensor_tensor(gl[:, 4 + p, :], qk[:, 256:512], ex[:], ALU.mult),
                    [qkk, "ex"], [("gl", 4 + p)])
                P.dma(D["g_kd"][p, rs, :], gl[:, 4 + p, :], [("gl", 4 + p)], [("g_kd", p, r)])
            bk, bkey = nP()
            bkb = bk[:].bitcast(BF16)
            arrs = [0, 1, 2, 3] if lat else [1]
            for a_ in arrs:
                for j in range(2):
                    P.tr(bkb[:, (a_ * 2 + j) * 128:(a_ * 2 + j + 1) * 128], gl[:, a_, j * 128:(j + 1) * 128], ident[:],
                         [("gl", a_), "ident"], [bkey])
            gT, gTk = glT.next()
            if lat:
                P.act(gT[:].rearrange("p a b -> p (a b)"), bkb, AF.Copy, [bkey], [gTk])
                P.dma(D["g_T"][:, r, :, :], gT[:], [gTk], [("g_T", r)])
            else:
                P.act(gT[:, 2:4, :].rearrange("p a b -> p (a b)"), bkb[:, 256:512], AF.Copy, [bkey], [gTk])
                P.dma(D["g_T"][:, r, 2:4, :], gT[:, 2:4, :], [gTk], [("g_T", r)])
            if not lat:
                return
            gelu(uv[:, 0, :], (uvk, 0), ge[:, 0, :], ("ge", 0))
            gelu(uv[:, 1, :], (uvk, 1), ge[:, 1, :], ("ge", 1))
            P.v(lambda e, sm=sm: e.reduce_sum(sm[:, 8:9], ge[:, 1, :], AX.X), [("ge", 1)], [(smk, 8)])
            P.act(junk[:, 0:512], ge[:, 1, :], AF.Square, [("ge", 1)], ["junk", (smk, 9)], accum_out=sm[:, 9:10])
            P.v(lambda e, sm=sm: e.tensor_scalar(sm[:, 10:11], sm[:, 8:9], 1.0 / 512, None, ALU.mult), [(smk, 8)], [(smk, 10)])
            P.v(lambda e, sm=sm: e.tensor_tensor(sm[:, 11:12], sm[:, 10:11], sm[:, 10:11], ALU.mult), [(smk, 10)], [(smk, 11)])
            P.v(lambda e, sm=sm: e.scalar_tensor_tensor(sm[:, 12:13], sm[:, 9:10], 1.0 / 512, sm[:, 11:12], ALU.mult, ALU.subtract),
                [(smk, 9), (smk, 11)], [(smk, 12)])
            P.v(lambda e, sm=sm: e.tensor_scalar(sm[:, 12:13], sm[:, 12:13], EPS, None, ALU.add), [(smk, 12)], [(smk, 12)])
            P.v(lambda e, sm=sm: e.tensor_tensor(sm[:, 13:14], sm[:, 12:13], neghalf, ALU.pow), [(smk, 12), "consts"],
                [(smk, 13)], eng="pool")
            P.v(lambda e, sm=sm: e.tensor_scalar(gt_[:], ge[:, 1, :], sm[:, 10:11], sm[:, 13:14], ALU.subtract, ALU.mult),
                [("ge", 1), (smk, 10), (smk, 13)], ["gt_"])
            P.v(lambda e: e.tensor_tensor(gt_[:], gt_[:], lng[:], ALU.mult), ["gt_", "lng"], ["gt_"])
            P.v(lambda e: e.tensor_tensor(vgn[:], gt_[:], lnb[:], ALU.add), ["gt_", "lnb"], ["vgn"])
            sp_, spk = nP()
            for gi in range(4):
                P.mm(sp_[:, gi * 128:(gi + 1) * 128], wsT[:, gi, :], vgn[:, gi * 128:(gi + 1) * 128], True, True,
                     ["wsT", "vgn"], [spk])
            dl_ap, dlk = dlb.next()
            for gi in range(4):
                P.v(lambda e, gi=gi, sp_=sp_, dl_ap=dl_ap: e.scalar_tensor_tensor(
                    dl_ap[:, gi * 128:(gi + 1) * 128], sp_[:, gi * 128:(gi + 1) * 128], bsT[:, gi:gi + 1],
                    ge[:, 0, gi * 128:(gi + 1) * 128], ALU.add, ALU.mult), [spk, "bsT", ("ge", 0)], [dlk])
            P.dma(D["dl"][rs, :], dl_ap[:], [dlk], [("dl", r)])

        pend = None
        for (r, var) in l1_tiles():
            cur = stage_a(r, var)
            if pend is not None:
                stage_b(pend)
            pend = cur
        stage_b(pend)
        P.flush()


def _gla_pass(nc, P, D, banks, st, sb, pidx, order, S, Sb, on_out):
    nAT, nO, nU = bank_rot(banks, 0, 2), bank_rot(banks, 2, 4), bank_rot(banks, 4, 6)
    gT = Rot("gTl", [sb("gTl%d" % i, [128, 4, 128], BF16) for i in range(2)])
    kd = Rot("kdl", [sb("kdl%d" % i, [128, 256], BF16) for i in range(2)])
    vv = Rot("vl", [sb("vl%d" % i, [128, 512], BF16) for i in range(2)])
    dc = Rot("dcl", [sb("dcl%d" % i, [128, 2]) for i in range(2)])
    ATm = Rot("ATm", [sb("ATm%d" % i, [128, 128], BF16) for i in range(2)])
    mask = sb("gmask_sb", [128, 128])
    P.dma(mask[:], D["gmask"][pidx], [], ["gmask"])
    for r in order:
        lat = r < 16
        rs = slice(r * 128, (r + 1) * 128)
        g_ap, gk = gT.next()
        if lat:
            P.dma(g_ap[:], D["g_T"][:, r, 4 * pidx:4 * pidx + 4, :], [], [gk])
        else:
            P.dma(g_ap[:, 2:4, :], D["g_T"][:, r, 4 * pidx + 2:4 * pidx + 4, :], [], [gk])
        k_ap, kk = kd.next()
        P.dma(k_ap[:], D["g_kd"][pidx, rs, :], [], [kk])
        v_ap, vk = vv.next()
        P.dma(v_ap[:], D["g_v"][rs, :], [], [vk])
        d_ap, dk = dc.next()
        P.dma(d_ap[:], D["g_dec"][:, r, 2 * pidx:2 * pidx + 2], [], [dk])
        if lat:
            O, Ok = nO()
            for h in range(4):
                j, po = h // 2, (h % 2) * 64
                AT, ATk = nAT()
                P.mm(AT[:, 0:128], g_ap[po:po + 64, 2 + j, :], g_ap[po:po + 64, j, :], True, True, [gk], [ATk])
                am, amk = ATm.next()
                P.v(lambda e, am=am, AT=AT: e.tensor_tensor(am[:], AT[:, 0:128], mask[:], ALU.mult), [ATk, "gmask"], [amk])
                P.mm(O[:, h * 128:(h + 1) * 128], am[:], v_ap[:, h * 128:(h + 1) * 128], True, False, [amk, vk], [Ok])
                P.mm(O[:, h * 128:(h + 1) * 128], g_ap[po:po + 64, j, :], Sb[po:po + 64, j, :], False, True,
                     [gk, ("Sb", j, h % 2)], [Ok])
            on_out(r, O, Ok)
        for j in range(2):
            for hh in range(2):
                po = hh * 64
                U, Uk = nU()
                P.mm(U[:, 0:128], k_ap[:, j * 128:(j + 1) * 128], v_ap[:, (2 * j + hh) * 128:(2 * j + hh + 1) * 128],
                     True, True, [kk, vk], [Uk])
                P.v(lambda e, U=U, j=j, po=po, d_ap=d_ap: e.scalar_tensor_tensor(
                    S[po:po + 64, j, :], S[po:po + 64, j, :], d_ap[po:po + 64, j:j + 1], U[po:po + 64, 0:128],
                    ALU.mult, ALU.add), [Uk, dk, ("S", j, hh)], [("S", j, hh)])
                P.act(Sb[po:po + 64, j, :], S[po:po + 64, j, :], AF.Copy, [("S", j, hh)], [("Sb", j, hh)])


def phase_l1b_a(nc, P, D, banks):
    with contextlib.ExitStack() as st:
        sb = lambda name, shape, dt=F32: st.enter_context(nc.sbuf_tensor("d_" + name, shape, dt))
        S = sb("S", [128, 2, 128]); Sb = sb("Sb", [128, 2, 128], BF16)
        oa = Rot("oa", [sb("oa%d" % i, [128, 512]) for i in range(2)])
        P.v(lambda e: e.memset(S[:], 0.0), [], [("S", j, hh) for j in range(2) for hh in range(2)])
        P.v(lambda e: e.memset(Sb[:], 0.0), [], [("Sb", j, hh) for j in range(2) for hh in range(2)])

        def on_out(r, O, Ok):
            o_ap, ok_ = oa.next()
            P.act(o_ap[:], O[:, :], AF.Copy, [Ok], [ok_])
            P.dma(D["OA"][r * 128:(r + 1) * 128, :], o_ap[:], [ok_], [("OA", r)])
        _gla_pass(nc, P, D, banks, st, sb, 0, [16, 17] + list(range(16)), S, Sb, on_out)
        P.dma(D["cc_in"].rearrange("(a p) n -> p a n", p=128), S[:], [("S", j, hh) for j in range(2) for hh in range(2)],
              ["cc_in"])
        P.cc(lambda e: e.collective_compute("AllGather", ALU.bypass, replica_groups=[[0, 1], [2, 3], [4, 5], [6, 7]],
                                            ins=[D["cc_in"].opt()], outs=[D["cc_out"].opt()]), ["cc_in"], ["cc_out"])
        P.flush()


def phase_l1b_b(nc, P, D, banks):
    with contextlib.ExitStack() as st:
        sb = lambda name, shape, dt=F32: st.enter_context(nc.sbuf_tensor("e_" + name, shape, dt))
        nT, nW = bank_rot(banks, 0, 2), bank_rot(banks, 2, 8)
        S = sb("S", [128, 2, 128]); Sb = sb("Sb", [128, 2, 128], BF16)
        skeys = [("S", j, hh) for j in range(2) for hh in range(2)]
        both = sb("both", [128, 4, 128])
        sel = sb("sel", [128, 2])
        P.dma(both[:], D["cc_out"].rearrange("(a p) n -> p a n", p=128), [], ["both"])
        P.dma(sel[:], D["sel"][:, :], [], ["sel"])
        P.v(lambda e: e.tensor_scalar(S[:].rearrange("p a b -> p (a b)"), both[:, 0:2, :].rearrange("p a b -> p (a b)"),
                                      sel[:, 0:1], None, ALU.mult), ["both", "sel"], skeys)
        P.v(lambda e: e.scalar_tensor_tensor(S[:].rearrange("p a b -> p (a b)"),
                                             both[:, 2:4, :].rearrange("p a b -> p (a b)"), sel[:, 1:2],
                                             S[:].rearrange("p a b -> p (a b)"), ALU.mult, ALU.add),
            ["both", "sel"] + skeys, skeys)
        P.act(Sb[:], S[:], AF.Copy, skeys, [("Sb", j, hh) for j in range(2) for hh in range(2)])
        ident = sb("ident", [128, 128], BF16)
        P.dma(ident[:], D["identbf"][:, :], [], ["ident"])
        consts = sb("consts", [128, 4])
        P.v(lambda e: e.memset(consts[:], -0.5), [], ["consts"])
        gng = sb("gng", [128, 512])
        P.dma(gng[:], D["gla_norm_g"][0:1, :].partition_broadcast(128), [], ["gng"])
        w_out = sb("w_out", [128, 8, 1024], BF16)
        P.dma(w_out[:], D["odd_w_out"][0].rearrange("(kc p) n -> p kc n", p=128), [], ["w_out"], eng="pool")
        G = load_mod_rows(P, nc, st, D["mods"], 1, GT1, "e_G")
        oa = Rot("eoa", [sb("oa%d" % i, [128, 512]) for i in range(2)])
        rsl = Rot("ersl", [sb("rsl%d" % i, [128, 512], BF16) for i in range(2)])
        gr = sb("gr", [128, 512])
        junk = sb("junk", [128, 128])
        small = Rot("esmall", [sb("small%d" % i, [128, 8]) for i in range(2)])
        mix_all = sb("mix_all", [128, 16, 1024], BF16)
        mixTr = Rot("emixT", [sb("mixT%d" % i, [128, 8, 128], BF16) for i in range(2)])
        xt = Rot("ext", [sb("xt%d" % i, [128, 1024]) for i in range(2)])
        ot = Rot("eot", [sb("ot%d" % i, [128, 1024]) for i in range(2)])

        def on_out(r, O, Ok):
            rs = slice(r * 128, (r + 1) * 128)
            o_ap, ok_ = oa.next()
            P.dma(o_ap[:], D["OA"][rs, :], [], [ok_])
            P.v(lambda e, o_ap=o_ap, O=O: e.tensor_tensor(o_ap[:], O[:, :], o_ap[:], ALU.add), [Ok, ok_], [ok_])
            r_ap, rk = rsl.next()
            P.dma(r_ap[:], D["rsilu"][rs, :], [], [rk])
            m_ap, mk = mix_all[:, r, :], ("mix", r)
            P.dma(m_ap[:, 512:1024], D["dl"][rs, :], [], [(mk, 1)])
            sm, smk = small.next()
            for h in range(4):
                P.act(junk[:], o_ap[:, h * 128:(h + 1) * 128], AF.Square, [ok_], ["junk", (smk, h)], accum_out=sm[:, h:h + 1])
            hk = [(smk, h) for h in range(4)]
            P.v(lambda e, sm=sm: e.tensor_scalar(sm[:, 0:4], sm[:, 0:4], 1.0 / 128, EPS, ALU.mult, ALU.add), hk, hk)
            P.v(lambda e, sm=sm: e.tensor_tensor(sm[:, 4:8], sm[:, 0:4], consts[:, 0:4], ALU.pow), hk + ["consts"],
                [(smk, 4)], eng="pool")
            P.v(lambda e, r_ap=r_ap: e.tensor_tensor(gr[:], gng[:], r_ap[:], ALU.mult), ["gng", rk], ["gr"])
            for h in range(4):
                P.v(lambda e, h=h, o_ap=o_ap, sm=sm, m_ap=m_ap: e.scalar_tensor_tensor(
                    m_ap[:, h * 128:(h + 1) * 128], o_ap[:, h * 128:(h + 1) * 128], sm[:, 4 + h:5 + h],
                    gr[:, h * 128:(h + 1) * 128], ALU.mult, ALU.mult), [ok_, (smk, 4), "gr"], [(mk, 0)])

        def out_proj(r):
            rs = slice(r * 128, (r + 1) * 128)
            m_ap, mk = mix_all[:, r, :], ("mix", r)
            bk, bkey = nT()
            bkb = bk[:].bitcast(BF16)
            for kc in range(8):
                P.tr(bkb[:, kc * 128:(kc + 1) * 128], m_ap[:, kc * 128:(kc + 1) * 128], ident[:],
                     [(mk, 0), (mk, 1), "ident"], [bkey])
            mT, mTk = mixTr.next()
            P.act(mT[:].rearrange("p a b -> p (a b)"), bkb, AF.Copy, [bkey], [mTk])
            x_ap, xk = xt.next()
            P.dma(x_ap[:], D["x2"][rs, :], [], [xk])
            t_ap, tk = ot.next()
            for dh in range(2):
                W, Wk = nW()
                for kc in range(8):
                    P.mm(W[:, :], mT[:, kc, :], w_out[:, kc, dh * 512:(dh + 1) * 512], kc == 0, kc == 7,
                         [mTk, "w_out"], [Wk])
                P.v(lambda e, t_ap=t_ap, W=W, dh=dh: e.tensor_tensor(
                    t_ap[:, dh * 512:(dh + 1) * 512], W[:, :], G[0][0][:, dh * 512:(dh + 1) * 512], ALU.mult),
                    [Wk, G[0][1]], [tk])
            P.v(lambda e, t_ap=t_ap, x_ap=x_ap: e.tensor_tensor(t_ap[:], t_ap[:], x_ap[:], ALU.add), [tk, xk], [tk],
                eng="pool")
            P.dma(D["x3"][rs, :], t_ap[:], [tk], [("x3", r)])
        _gla_pass(nc, P, D, banks, st, sb, 1, list(range(15, -1, -1)), S, Sb, on_out)
        for r in range(15, -1, -1):
            out_proj(r)
        P.flush()


I32 = mybir.dt.int32
STILE = 256
NB = STILE // 128


def n_stiles(T):
    return (2 * T + 32 * (STILE - 1) + STILE - 1) // STILE


def phase_moe_sparse(nc, P, D, banks, layer, xin, xout, tiles, tag, final_norm=False):
    NTl = len(tiles)
    T = NTl * 128
    NST = n_stiles(T)
    NSLOT = NST * STILE
    Xs, Ys = D["Xs"], D["Ys"]
    wgv = D["moe_w_gate%d" % layer].rearrange("e (p a kc) f -> (e p a) (kc f)", p=128, a=2)
    wuv = D["moe_w_up%d" % layer].rearrange("e (p a kc) f -> (e p a) (kc f)", p=128, a=2)
    wdv = D["moe_w_down%d" % layer].rearrange("e f d -> (e f) d")
    with contextlib.ExitStack() as st0:
        sb0 = lambda name, shape, dt=F32: st0.enter_context(nc.sbuf_tensor(tag + name, shape, dt))
        consts = sb0("consts", [128, 4])
        P.v(lambda e: e.memset(consts[:, 0:1], -0.5), [], ["consts"])
        neghalf = consts[:, 0:1]
        idxA = sb0("idxA", [128, NTl], I32); idxB = sb0("idxB", [128, NTl], I32)
        wAB = sb0("wAB", [128, 2, NTl])
        widx = sb0("widx", [128, NST, 6], I32)
        with contextlib.ExitStack() as st:
            sb = lambda name, shape, dt=F32: st.enter_context(nc.sbuf_tensor(tag + name, shape, dt))
            nA, nB = bank_rot(banks, 0, 4), bank_rot(banks, 4, 7)
            cntb, cntk = banks[7], ("ps", 7)
            ident = sb("ident32", [128, 128])
            gbc = sb("gbc", [128, 1024])
            w_r = sb("w_r", [128, 8, 36])
            gm = sb("gm", [128, 2, 128])
            eidrow = sb("eidrow", [128, 32])
            pc2 = sb("pc2", [128, 6])
            Abc = load_mod_rows(P, nc, st, D["mods"], layer, SC2, tag + "A")
            Bbc = load_mod_rows(P, nc, st, D["mods"], layer, SH2, tag + "B")
            xt = Rot("mxt", [sb("xt%d" % i, [128, 1024]) for i in range(2)])
            junk = sb("junk", [128, 1024])
            t1r = Rot("t1", [sb("t1_%d" % i, [128, 1024]) for i in range(2)])
            h2r = Rot("h2", [sb("h2_%d" % i, [128, 1024]) for i in range(2)])
            h2b = sb("h2b", [128, NTl, 1024], BF16)
            h2Tr = Rot("h2T32", [sb("h2T32_%d" % i, [128, 8, 128]) for i in range(2)])
            small = Rot("msmall", [sb("small%d" % i, [128, 16]) for i in range(2)])
            rt = Rot("mrt", [sb("rt%d" % i, [128, 96]) for i in range(2)])
            selA = sb("selA", [128, NTl, 32]); selB = sb("selB", [128, NTl, 32]); selm = sb("selm", [128, NTl, 32])
            LG = sb("LG", [128, NTl, 36])
            rs_ = sb("rs_", [128, 8, NTl])
            goh = sb("goh", [128, NTl, 4]); gex = sb("gex", [128, NTl, 4])
            etmp = sb("etmp", [128, NTl, 4, 8])
            ein = sb("ein", [128, NTl, 8]); oh1 = sb("oh1", [128, NTl, 8]); e2 = sb("e2", [128, NTl, 8]); oh2 = sb("oh2", [128, NTl, 8])
            zt = sb("zt", [128, 8, 1024], BF16)
            P.v(lambda e: e.memset(zt[:], 0.0), [], ["zt"], eng="pool")
            zkeys = []
            for z0 in range(0, NSLOT, 1024):
                nrow = min(1024, NSLOT - z0)
                P.dma(Xs[z0:z0 + nrow, :].rearrange("(a p) c -> p a c", p=128), zt[:, 0:nrow // 128, :], ["zt"], [("Xsz", z0)])
                zkeys.append(("Xsz", z0))
            P.dma(ident[:], D["ident32"][:, :], [], ["ident"])
            P.dma(gbc[:], D["norm_ffn_g"][layer:layer + 1, :].partition_broadcast(128), [], ["gbc"])
            P.dma(w_r[:, :, 0:4], D["moe_w_rg"][layer].rearrange("(kc p) n -> p kc n", p=128), [], [("w_r", 0)])
            P.dma(w_r[:, :, 4:36], D["moe_w_re"][layer].rearrange("(kc p) n -> p kc n", p=128), [], [("w_r", 1)])
            P.dma(gm[:], D["gmask"][3:5].rearrange("a p n -> p a n"), [], ["gm"])
            P.dma(eidrow[:], D["eidrow"][:, :], [], ["eidrow"])
            P.dma(pc2[:], D["pc2"][:, :], [], ["pc2"])
            for v in range(2):
                a, ak = Abc[v]
                P.v(lambda e, a=a: e.scalar_tensor_tensor(a[:], a[:], 1.0, gbc[:], ALU.add, ALU.mult), [ak, "gbc"], [ak])
            for ti, (r, var) in enumerate(tiles):
                A, Ak = Abc[var]; B, Bk = Bbc[var]
                x_ap, xk = xt.next()
                P.dma(x_ap[:], D[xin][r * 128:(r + 1) * 128, :], [], [xk])
                sm, smk = small.next()
                P.act(junk[:], x_ap[:], AF.Square, [xk], ["junk", (smk, 0)], accum_out=sm[:, 0:1])
                rms_rstd(P, sm[:, 0:1], (smk, 0), sm[:, 2:3], (smk, 2), neghalf, 1024, sm[:, 1:2], (smk, 1))
                t1, t1k = t1r.next(); h2, h2k = h2r.next(); h2T32, hTk = h2Tr.next()
                P.v(lambda e, x_ap=x_ap, sm=sm, A=A, t1=t1: e.scalar_tensor_tensor(
                    t1[:], x_ap[:], sm[:, 2:3], A[:], ALU.mult, ALU.mult), [xk, (smk, 2), Ak], [t1k])
                P.v(lambda e, B=B, t1=t1, h2=h2: e.tensor_tensor(h2[:], t1[:], B[:], ALU.add), [t1k, Bk], [h2k])
                P.act(h2b[:, ti, :], h2[:], AF.Copy, [h2k], [("h2b", ti)])
                for half in range(2):
                    bk, bkey = nA()
                    for j in range(4):
                        kc = half * 4 + j
                        P.tr(bk[:, j * 128:(j + 1) * 128], h2[:, kc * 128:(kc + 1) * 128], ident[:], [h2k, "ident"], [bkey])
                    P.v(lambda e, bk=bk, half=half, h2T32=h2T32: e.tensor_copy(
                        h2T32[:, half * 4:(half + 1) * 4, :], bk[:, :].rearrange("p (a b) -> p a b", a=4)),
                        [bkey], [(hTk, half)])
                lg, lgk = nB()
                for kc in range(8):
                    P.mm(lg[:, 0:36], h2T32[:, kc, :], w_r[:, kc, :], kc == 0, kc == 7,
                         [(hTk, kc // 4), ("w_r", 0), ("w_r", 1)], [lgk])
                P.v(lambda e, lg=lg, ti=ti: e.tensor_copy(LG[:, ti, :], lg[:, 0:36]), [lgk], [("LG", ti)])
            lgk_all = [("LG", ti) for ti in range(NTl)]
            NT = NTl
            G = LG[:, :, 0:4]
            E4 = LG[:, :, 4:36].rearrange("p t (g e) -> p t g e", g=4)
            bc = lambda ap2, n: ap2.unsqueeze(2).to_broadcast([128, NT, n])
            P.v(lambda e: e.tensor_reduce(rs_[:, 0, :], G, AX.X, ALU.max), lgk_all, ["gmax"])
            P.v(lambda e: e.tensor_tensor(goh[:], G, bc(rs_[:, 0, :], 4), ALU.is_equal), lgk_all + ["gmax"], ["goh"])
            P.v(lambda e: e.tensor_tensor(gex[:], G, bc(rs_[:, 0, :], 4), ALU.subtract), lgk_all + ["gmax"], ["gex"])
            P.act(gex[:], gex[:], AF.Exp, ["gex"], ["gex"])
            P.v(lambda e: e.tensor_reduce(rs_[:, 1, :], gex[:], AX.X, ALU.add), ["gex"], ["gsum"])
            P.v(lambda e: e.reciprocal(rs_[:, 2, :], rs_[:, 1, :]), ["gsum"], ["pmax"])
            P.v(lambda e: e.tensor_tensor(etmp[:], E4, goh[:].unsqueeze(3).to_broadcast([128, NT, 4, 8]), ALU.mult),
                lgk_all + ["goh"], ["etmp"])
            P.v(lambda e: e.tensor_tensor(ein[:], etmp[:, :, 0, :], etmp[:, :, 1, :], ALU.add), ["etmp"], ["ein"])
            P.v(lambda e: e.tensor_tensor(ein[:], ein[:], etmp[:, :, 2, :], ALU.add), ["etmp", "ein"], ["ein"])
            P.v(lambda e: e.tensor_tensor(ein[:], ein[:], etmp[:, :, 3, :], ALU.add), ["etmp", "ein"], ["ein"])
            P.v(lambda e: e.tensor_reduce(rs_[:, 3, :], ein[:], AX.X, ALU.max), ["ein"], ["m1"])
            P.v(lambda e: e.tensor_tensor(oh1[:], ein[:], bc(rs_[:, 3, :], 8), ALU.is_equal), ["ein", "m1"], ["oh1"])
            P.v(lambda e: e.scalar_tensor_tensor(e2[:], oh1[:], -1e30, ein[:], ALU.mult, ALU.add), ["oh1", "ein"], ["e2"])
            P.v(lambda e: e.tensor_reduce(rs_[:, 4, :], e2[:], AX.X, ALU.max), ["e2"], ["m2"])
            P.v(lambda e: e.tensor_tensor(oh2[:], e2[:], bc(rs_[:, 4, :], 8), ALU.is_equal), ["e2", "m2"], ["oh2"])
            P.v(lambda e: e.tensor_tensor(rs_[:, 5, :], rs_[:, 4, :], rs_[:, 3, :], ALU.subtract), ["m1", "m2"], ["dd"])
            P.act(rs_[:, 6, :], rs_[:, 5, :], AF.Exp, ["dd"], ["ed"])
            P.v(lambda e: e.tensor_scalar(rs_[:, 7, :], rs_[:, 6, :], 1.0, None, ALU.add), ["ed"], ["w1"])
            P.v(lambda e: e.reciprocal(rs_[:, 7, :], rs_[:, 7, :]), ["w1"], ["w1"])
            P.v(lambda e: e.tensor_tensor(wAB[:, 0, :], rs_[:, 7, :], rs_[:, 2, :], ALU.mult), ["w1", "pmax"], ["wA"])
            P.v(lambda e: e.tensor_tensor(wAB[:, 1, :], wAB[:, 0, :], rs_[:, 6, :], ALU.mult), ["wA", "ed"], ["wB"])
            s4 = lambda ap3: ap3[:].rearrange("p t (g e) -> p t g e", g=4)
            P.v(lambda e: e.tensor_tensor(s4(selA), oh1[:].unsqueeze(2).to_broadcast([128, NT, 4, 8]),
                                          goh[:].unsqueeze(3).to_broadcast([128, NT, 4, 8]), ALU.mult), ["oh1", "goh"], ["selA"])
            P.v(lambda e: e.tensor_tensor(s4(selB), oh2[:].unsqueeze(2).to_broadcast([128, NT, 4, 8]),
                                          goh[:].unsqueeze(3).to_broadcast([128, NT, 4, 8]), ALU.mult), ["oh2", "goh"], ["selB"])
            P.v(lambda e: e.tensor_tensor(selm[:], selA[:], selB[:], ALU.add), ["selA", "selB"], ["selm"])
            for ti in range(NTl):
                P.mm(cntb[:, 0:32], gm[:, 1, :], selm[:, ti, :], ti == 0, ti == NTl - 1, ["gm", "selm"], [cntk])
            seg = sb("seg", [128, 8, 32])
            segT = sb("segT", [32, 128])
            ecol = sb("ecol", [128, NST])
            sti = sb("sti", [128, 2, NST, 32])
            stc = sb("stc", [128, 64])
            P.dma(stc[:], D["stile_c"][:, :], [], ["stc"])
            widxf = sb("widxf", [128, NST, 6])
            slotf = sb("slotf", [128, 2, NTl])
            P.v(lambda e: e.tensor_copy(seg[:, 0, :], cntb[:, 0:32]), [cntk], ["cnt"])
            P.v(lambda e: e.tensor_scalar(seg[:, 1, :], seg[:, 0, :], 0.0, None, ALU.is_gt), ["cnt"], ["nst"])
            for k in range(1, (T + STILE - 1) // STILE + 1):
                P.v(lambda e, k=k: e.scalar_tensor_tensor(seg[:, 1, :], seg[:, 0, :], float(STILE * k), seg[:, 1, :],
                                                          ALU.is_gt, ALU.add), ["cnt", "nst"], ["nst"])
            P.v(lambda e: e.tensor_scalar(seg[:, 2, :], seg[:, 1, :], float(STILE), None, ALU.mult), ["nst"], ["pc"])
            bk, bkey = nA()
            P.tr(bk[0:32, 0:128], seg[:, 2, :], ident[:], ["pc", "ident"], [bkey])
            P.v(lambda e, bk=bk: e.tensor_copy(segT[:], bk[0:32, 0:128]), [bkey], ["segT"])
            bk2, bkey2 = nA()
            P.mm(bk2[:, 0:32], segT[0:32, :], gm[0:32, 0, 0:32], True, True, ["segT", "gm"], [bkey2])
            P.v(lambda e, bk2=bk2: e.tensor_copy(seg[:, 3, :], bk2[:, 0:32]), [bkey2], ["start"])
            P.v(lambda e: e.tensor_tensor(seg[:, 4, :], seg[:, 3, :], seg[:, 2, :], ALU.add), ["start", "pc"], ["end"])
            P.v(lambda e: e.tensor_copy(seg[:, 5, :], seg[:, 3, :]), ["start"], ["base"])
            bci = lambda ap2: ap2.unsqueeze(1).to_broadcast([128, NST, 32])
            cI = stc[:, 0:NST].unsqueeze(2).to_broadcast([128, NST, 32])
            P.v(lambda e: e.tensor_tensor(sti[:, 0, :, :], bci(seg[:, 3, :]), cI, ALU.is_le), ["start", "stc"], ["sti0"])
            P.v(lambda e: e.tensor_tensor(sti[:, 1, :, :], bci(seg[:, 4, :]), cI, ALU.is_gt), ["end", "stc"], ["sti1"])
            P.v(lambda e: e.tensor_tensor(sti[:, 0, :, :], sti[:, 0, :, :], sti[:, 1, :, :], ALU.mult), ["sti0", "sti1"], ["sti0"])
            P.v(lambda e: e.tensor_tensor(sti[:, 0, :, :], sti[:, 0, :, :], bci(eidrow[:]), ALU.mult), ["sti0", "eidrow"], ["sti0"])
            P.v(lambda e: e.tensor_reduce(ecol[:, :], sti[:, 0, :, :], AX.X, ALU.add), ["sti0"], ["ecol"])
            ek = ["ecol"]
            for a_ in range(2):
                P.v(lambda e, a_=a_: e.tensor_scalar(widxf[:, :, a_], ecol[:, :], 256.0, pc2[:, a_:a_ + 1], ALU.mult, ALU.add),
                    ek + ["pc2"], [("widxf", a_)])
            for fc in range(4):
                P.v(lambda e, fc=fc: e.tensor_scalar(widxf[:, :, 2 + fc], ecol[:, :], 512.0, pc2[:, 2 + fc:3 + fc], ALU.mult, ALU.add),
                    ek + ["pc2"], [("widxf", 2 + fc)])
            P.v(lambda e: e.tensor_copy(widx[:].rearrange("p a b -> p (a b)"), widxf[:].rearrange("p a b -> p (a b)")),
                [("widxf", j) for j in range(6)], ["widx"])
            for ti in range(NTl):
                wi, wik = nB()
                P.mm(wi[:, 0:32], gm[:, 0, :], selm[:, ti, :], True, True, ["gm", "selm"], [wik])
                P.mm(wi[:, 32:64], gm[:, 1, :], selm[:, ti, :], True, True, ["gm", "selm"], [wik])
                P.v(lambda e, wi=wi: e.tensor_tensor(seg[:, 6, :], wi[:, 0:32], seg[:, 5, :], ALU.add), [wik, "base"], ["segtmp"])
                P.v(lambda e, ti=ti: e.scalar_tensor_tensor(seg[:, 7, :], seg[:, 6, :], 1.0, selA[:, ti, :], ALU.mult, ALU.mult,
                                                            accum_out=slotf[:, 0, ti:ti + 1]), ["segtmp", "selA"],
                    ["segtmp2", ("slotA", ti)])
                P.v(lambda e, ti=ti: e.scalar_tensor_tensor(seg[:, 7, :], seg[:, 6, :], 1.0, selB[:, ti, :], ALU.mult, ALU.mult,
                                                            accum_out=slotf[:, 1, ti:ti + 1]), ["segtmp", "selB"],
                    ["segtmp2", ("slotB", ti)])
                P.v(lambda e, wi=wi: e.tensor_tensor(seg[:, 5, :], wi[:, 32:64], seg[:, 5, :], ALU.add), [wik, "base"], ["base"])
            P.v(lambda e: e.tensor_copy(idxA[:], slotf[:, 0, :]), [("slotA", ti) for ti in range(NTl)], ["idxA"])
            P.v(lambda e: e.tensor_copy(idxB[:], slotf[:, 1, :]), [("slotB", ti) for ti in range(NTl)], ["idxB"])
            for ti in range(NTl):
                for (ix, ixk) in ((idxA, "idxA"), (idxB, "idxB")):
                    P.op("pool", lambda e, ix=ix, ti=ti: e.indirect_dma_start(
                        out=Xs[0:NSLOT, :], out_offset=bass.IndirectOffsetOnAxis(ap=ix[:, ti:ti + 1], axis=0),
                        in_=h2b[:, ti, :], in_offset=None, bounds_check=None),
                        [("h2b", ti), ixk] + zkeys, [("Xs", ti, ixk)], dma=True, kind="dma")
            P.flush()
        with contextlib.ExitStack() as st:
            sb = lambda name, shape, dt=F32: st.enter_context(nc.sbuf_tensor(tag + name, shape, dt))
            nT, nGU, nY = bank_rot(banks, 0, 2), bank_rot(banks, 2, 6), bank_rot(banks, 6, 8)
            ident = sb("identb", [128, 128], BF16)
            P.dma(ident[:], D["identbf"][:, :], [], ["ident"])
            wg = Rot("wg", [sb("wg%d" % i, [128, 8, 512], BF16) for i in range(2)])
            wu = Rot("wu", [sb("wu%d" % i, [128, 8, 512], BF16) for i in range(2)])
            wd = Rot("wd", [sb("wd%d" % i, [128, 4, 1024], BF16) for i in range(2)])
            xr = Rot("xr", [sb("xr%d" % i, [128, 1024], BF16) for i in range(4)])
            XT = Rot("XT", [sb("XT%d" % i, [128, 8, STILE], BF16) for i in range(2)])
            hg = Rot("hg", [sb("hg%d" % i, [128, 4, STILE], BF16) for i in range(2)])
            sg = Rot("sg", [sb("sg%d" % i, [128, STILE]) for i in range(2)])
            yb = Rot("yb", [sb("yb%d" % i, [128, 1024]) for i in range(3)])

            def gather(dst2d, src, col, i, key):
                P.op("pool", lambda e: e.indirect_dma_start(
                    out=dst2d, out_offset=None, in_=src,
                    in_offset=bass.IndirectOffsetOnAxis(ap=widx[:, i, col:col + 1], axis=0),
                    bounds_check=None), [], [key], dma=True, kind="dma")

            def emit_gu(i):
                g_ap, gk = wg.next(); u_ap, uk = wu.next(); d_ap, dk = wd.next()
                for a_ in range(2):
                    gather(g_ap[:, 4 * a_:4 * a_ + 4, :].rearrange("p a f -> p (a f)"), wgv, a_, i, (gk, a_))
                    gather(u_ap[:, 4 * a_:4 * a_ + 4, :].rearrange("p a f -> p (a f)"), wuv, a_, i, (uk, a_))
                for fc in range(4):
                    gather(d_ap[:, fc, :], wdv, 2 + fc, i, (dk, fc))
                xT, xTk = XT.next()
                for b in range(NB):
                    x_ap, xk = xr.next()
                    P.dma(x_ap[:], Xs[i * STILE + b * 128:i * STILE + (b + 1) * 128, :], [], [xk])
                    bk, bkey = nT()
                    bkb = bk[:].bitcast(BF16)
                    xv = x_ap[:].rearrange("p (m kc) -> p kc m", kc=8)
                    for kc in range(8):
                        P.tr(bkb[:, kc * 128:(kc + 1) * 128], xv[:, kc, :], ident[:], [xk, "ident"], [bkey])
                    P.act(xT[:, :, b * 128:(b + 1) * 128], bkb.rearrange("p (a b) -> p a b", a=8), AF.Copy, [bkey], [(xTk, b)])
                xkeys = [(xTk, b) for b in range(NB)]
                hgt, hgk = hg.next()
                for fc in range(4):
                    Gp, Gk = nGU(); Up, Uk = nGU()
                    for kc in range(8):
                        P.mm(Gp[:, 0:STILE], g_ap[:, kc, fc * 128:(fc + 1) * 128], xT[:, kc, :], kc == 0, kc == 7,
                             [(gk, kc // 4)] + xkeys, [Gk])
                    for kc in range(8):
                        P.mm(Up[:, 0:STILE], u_ap[:, kc, fc * 128:(fc + 1) * 128], xT[:, kc, :], kc == 0, kc == 7,
                             [(uk, kc // 4)] + xkeys, [Uk])
                    s_ap, sk = sg.next()
                    P.act(s_ap[:, :], Gp[:, 0:STILE], AF.Silu, [Gk], [sk])
                    P.v(lambda e, hgt=hgt, Up=Up, s_ap=s_ap, fc=fc: e.tensor_tensor(
                        hgt[:, fc, :], Up[:, 0:STILE], s_ap[:, :], ALU.mult), [Uk, sk], [(hgk, fc)])
                return (i, hgt, hgk, d_ap, dk)

            def emit_down(i, hgt, hgk, d_ap, dk):
                for b in range(NB):
                    y_ap, yk = yb.next()
                    for dh in range(2):
                        Yp, Yk = nY()
                        for fc in range(4):
                            P.mm(Yp[:, :], hgt[:, fc, b * 128:(b + 1) * 128], d_ap[:, fc, dh * 512:(dh + 1) * 512],
                                 fc == 0, fc == 3, [(hgk, f) for f in range(4)] + [(dk, fc)], [Yk])
                        if dh == 0:
                            P.act(y_ap[:, 0:512], Yp[:, :], AF.Copy, [Yk], [(yk, 0)])
                        else:
                            P.v(lambda e, y_ap=y_ap, Yp=Yp: e.tensor_copy(y_ap[:, 512:1024], Yp[:, :]), [Yk], [(yk, 1)])
                    r0 = i * STILE + b * 128
                    P.dma(Ys[r0:r0 + 128, :], y_ap[:], [(yk, 0), (yk, 1)], [("Ys", r0)])

            pend = None
            for i in range(NST):
                cur = emit_gu(i)
                if pend is not None:
                    emit_down(*pend)
                pend = cur
            emit_down(*pend)
            P.flush()
        with contextlib.ExitStack() as st:
            sb = lambda name, shape, dt=F32: st.enter_context(nc.sbuf_tensor(tag + name, shape, dt))
            G = load_mod_rows(P, nc, st, D["mods"], layer, GT2, tag + "G")
            xt = Rot("mxt2", [sb("xt2_%d" % i, [128, 1024]) for i in range(4)])
            ya = Rot("ya", [sb("ya%d" % i, [128, 1024]) for i in range(4)])
            ybb = Rot("ybb", [sb("ybb%d" % i, [128, 1024]) for i in range(4)])
            if final_norm:
                fg = sb("fg", [128, 1024])
                P.dma(fg[:], D["final_norm_g"][0:1, :].partition_broadcast(128), [], ["fg"])
                junk = sb("junk3", [128, 1024])
                small = Rot("fsmall", [sb("fsmall%d" % i, [128, 4]) for i in range(2)])
            for ti, (r, var) in enumerate(tiles):
                x_ap, xk = xt.next()
                P.dma(x_ap[:], D[xin][r * 128:(r + 1) * 128, :], [], [xk])
                a_ap, ak = ya.next(); b_ap, bk_ = ybb.next()
                for (dst, dkey, ix) in ((a_ap, ak, idxA), (b_ap, bk_, idxB)):
                    P.op("pool", lambda e, dst=dst, ix=ix, ti=ti: e.indirect_dma_start(
                        out=dst[:, :], out_offset=None, in_=Ys[0:NSLOT, :],
                        in_offset=bass.IndirectOffsetOnAxis(ap=ix[:, ti:ti + 1], axis=0),
                        bounds_check=None), [], [dkey], dma=True, kind="dma")
                P.v(lambda e, a_ap=a_ap, ti=ti: e.tensor_scalar(a_ap[:], a_ap[:], wAB[:, 0, ti:ti + 1], None, ALU.mult), [ak], [ak])
                P.v(lambda e, a_ap=a_ap, b_ap=b_ap, ti=ti: e.scalar_tensor_tensor(
                    a_ap[:], b_ap[:], wAB[:, 1, ti:ti + 1], a_ap[:], ALU.mult, ALU.add), [ak, bk_], [ak])
                P.v(lambda e, a_ap=a_ap, var=var: e.tensor_tensor(a_ap[:], a_ap[:], G[var][0][:], ALU.mult), [ak, G[var][1]], [ak])
                P.v(lambda e, a_ap=a_ap, x_ap=x_ap: e.tensor_tensor(a_ap[:], a_ap[:], x_ap[:], ALU.add), [ak, xk], [ak])
                if final_norm:
                    sm, smk = small.next()
                    P.act(junk[:], a_ap[:], AF.Square, [ak], ["junk3", (smk, 0)], accum_out=sm[:, 0:1])
                    rms_rstd(P, sm[:, 0:1], (smk, 0), sm[:, 2:3], (smk, 2), neghalf, 1024, sm[:, 1:2], (smk, 1))
                    P.v(lambda e, a_ap=a_ap, sm=sm: e.scalar_tensor_tensor(
                        a_ap[:], a_ap[:], sm[:, 2:3], fg[:], ALU.mult, ALU.mult), [ak, (smk, 2), "fg"], [ak])
                P.dma(D[xout][r * 128:(r + 1) * 128, :], a_ap[:], [ak], [(xout, r)])
            P.flush()

import numpy as np
import ml_dtypes

BF = ml_dtypes.bfloat16
GRID_W = 64

W_SMALL = {
    "ada_w": [2, 1024, 6144], "ada_b": [2, 6144], "norm_mix_g": [2, 1024], "norm_ffn_g": [2, 1024],
    "even_w_in": [1, 1024, 1184], "mla_q_norm_g": [1, 256], "mla_w_uq": [1, 256, 768], "mla_kv_norm_g": [1, 128],
    "mla_w_ukv": [1, 128, 1024], "win_sink": [1, 8], "even_w_out": [1, 1024, 1024],
    "odd_w_in": [1, 1024, 2592], "gla_w_g2": [1, 2, 16, 256], "gla_b_g": [1, 2, 256], "gla_norm_g": [1, 512],
    "sg_ln_g": [1, 512], "sg_ln_b": [1, 512], "odd_w_out": [1, 1024, 1024],
    "moe_w_rg": [2, 1024, 4], "moe_w_re": [2, 1024, 32], "final_norm_g": [1, 1024],
}
W_MOE = {"moe_w_gate": [32, 1024, 512], "moe_w_up": [32, 1024, 512], "moe_w_down": [32, 512, 1024]}
CONSTS = {"wmask": ([2, 128, 512], BF16), "ident32": ([128, 128], F32), "identbf": ([128, 128], BF16),
          "gmask": ([5, 128, 128], F32), "eidrow": ([128, 32], F32), "pc2": ([128, 6], F32), "stile_c": ([128, 64], F32)}
PERCORE = {"xtok": [NTOK, 1024], "cvec": [2, 1024], "cA": [NTOK, 256], "sA": [NTOK, 256], "cB": [NTOK, 512],
           "sB": [NTOK, 512], "sg_w_sT": [4, 128, 128], "sg_b_sT": [128, 4], "sel": [128, 2]}
SCRATCH = {"mods": ([2, 2, 6144], F32), "QAT": ([96, 8, NTOK], BF16), "KAT": ([96, 8, NTOK], BF16),
           "VA": ([NTOK, 520], BF16), "QBT": ([64, 8, NTOK], BF16), "KBT": ([64, 2, NTOK], BF16),
           "VB": ([NTOK, 130], BF16), "x1": ([NOWN, 1024], F32), "x2": ([NOWN, 1024], F32),
           "g_T": ([128, 18, 8, 128], BF16), "g_kd": ([2, NOWN, 256], BF16), "g_v": ([NOWN, 512], BF16),
           "g_dec": ([128, 18, 4], F32), "rsilu": ([2048, 512], BF16), "dl": ([2048, 512], BF16),
           "OA": ([2048, 512], F32), "cc_in": ([256, 128], F32), "cc_out": ([512, 128], F32),
           "x3": ([2048, 1024], F32), "out": ([2048, 1024], F32),
           "Xs": ([n_stiles(NOWN) * STILE, 1024], BF16), "Ys": ([n_stiles(NOWN) * STILE, 1024], F32)}
HANDOFF = ["mods", "x2", "g_T", "g_kd", "g_v", "g_dec", "rsilu", "dl", "OA"]


def rope_tables(pos, dim, nheads):
    pos = np.asarray(pos)
    half = dim // 2
    inv = np.power(np.float32(10000.0), -np.arange(0, half, 2, dtype=np.float32) / np.float32(half)).astype(np.float32)
    row = (pos // GRID_W).astype(np.float32)
    col = (pos % GRID_W).astype(np.float32)
    ar = row[:, None] * inv[None, :]
    ac = col[:, None] * inv[None, :]
    ang = np.concatenate([ar, ar, ac, ac], axis=-1).astype(np.float32)
    cos = np.cos(ang).astype(np.float32)
    sin = np.sin(ang).astype(np.float32)
    blk = dim // 4
    sign = np.concatenate([-np.ones(blk), np.ones(blk), -np.ones(blk), np.ones(blk)]).astype(np.float32)
    ssin = sin * sign[None, :]
    no = pos < 0
    cos[no] = 1.0
    ssin[no] = 0.0
    return np.tile(cos, (1, nheads)), np.tile(ssin, (1, nheads))


_CONST = {}


def const_inputs():
    if not _CONST:
        j = np.arange(128)[:, None]
        i = np.arange(128)[None, :]
        m0 = np.tile((j >= i).astype(np.float32), (1, 4))
        m1 = np.tile((j <= i).astype(np.float32), (1, 4))
        _CONST["wmask"] = np.stack([m0, m1]).astype(BF)
        _CONST["ident32"] = np.eye(128, dtype=np.float32)
        _CONST["identbf"] = np.eye(128, dtype=np.float32).astype(BF)
        one = np.ones((128, 128), bool)
        _CONST["gmask"] = np.stack([(j <= i), (j >= i), one, (j < i), one]).astype(np.float32)
        _CONST["eidrow"] = np.tile(np.arange(32, dtype=np.float32)[None, :], (128, 1))
        p = np.arange(128, dtype=np.float32)
        _CONST["stile_c"] = np.tile((np.arange(64, dtype=np.float32) * STILE)[None, :], (128, 1))
        _CONST["pc2"] = np.stack([2 * p, 2 * p + 1, p, 128 + p, 256 + p, 384 + p], 1).astype(np.float32)
    return _CONST


def local_order(hf):
    own = np.arange(hf * 2048, (hf + 1) * 2048)
    oth = np.arange((1 - hf) * 2048, (2 - hf) * 2048)
    cidx = np.arange(256)
    if hf == 1:
        own, oth, cidx = own[::-1], oth[::-1], cidx[::-1]
    return own, oth, cidx


def weights_for(core, inp, layers=(0, 1)):
    hf = core % 2
    m = {}
    for k, shp in W_SMALL.items():
        m[k] = np.ascontiguousarray(np.asarray(inp[k]).reshape(shp))
    for l in layers:
        for k in W_MOE:
            m["%s%d" % (k, l)] = np.asarray(inp[k][l])
    ws = np.asarray(inp["sg_w_s"][0])
    bs = np.asarray(inp["sg_b_s"][0])
    if hf == 1:
        m["gla_w_g2"] = np.ascontiguousarray(m["gla_w_g2"][:, ::-1])
        m["gla_b_g"] = np.ascontiguousarray(m["gla_b_g"][:, ::-1])
        w = m["odd_w_in"].copy()
        w[:, :, 1024:1040] = m["odd_w_in"][:, :, 1040:1056]
        w[:, :, 1040:1056] = m["odd_w_in"][:, :, 1024:1040]
        m["odd_w_in"] = w
        ws = ws[:, ::-1, ::-1]
        bs = bs[:, ::-1]
    m["sg_w_sT"] = np.ascontiguousarray(ws.transpose(0, 2, 1))
    m["sg_b_sT"] = np.ascontiguousarray(bs.T)
    return m


def core_inputs(core, inp):
    b, hf = core // 2, core % 2
    own, oth, cidx = local_order(hf)
    pos = np.concatenate([own, oth, -np.ones(256, dtype=np.int64)])
    xtok = np.concatenate([inp["x"][b][own], inp["x"][b][oth], inp["ctx"][b][cidx]], 0)
    cA, sA = rope_tables(pos, 32, 8)
    cB, sB = rope_tables(pos, 64, 8)
    m = {"xtok": np.ascontiguousarray(xtok), "cvec": np.stack([inp["c"][b], inp["c_ctx"]]).astype(np.float32),
         "cA": cA, "sA": sA, "cB": cB, "sB": sB}
    sel = np.zeros((128, 2), np.float32)
    sel[:, 1 - hf] = 1.0
    m["sel"] = sel
    m.update(const_inputs())
    return m


def declare(nc, ext_in, ext_out, moe_layers=(0, 1)):
    D = {}
    dr = lambda n, s, dt=F32, k="ExternalInput": nc.dram_tensor(n, s, dt, kind=k).ap()
    for k, shp in PERCORE.items():
        D[k] = dr(k, shp)
    for k, (shp, dt) in CONSTS.items():
        D[k] = dr(k, shp, dt)
    for k, shp in W_SMALL.items():
        D[k] = dr(k, shp)
    for l in moe_layers:
        for k, shp in W_MOE.items():
            D["%s%d" % (k, l)] = dr("%s%d" % (k, l), shp)
    for k, (shp, dt) in SCRATCH.items():
        kind = "ExternalOutput" if k in ext_out else ("ExternalInput" if k in ext_in else "Internal")
        D[k] = dr(k, shp, dt, kind)
    return D


MOE0_TILES = [(i, 0) for i in range(16)] + [(16, 1), (17, 1)]
MOE1_TILES = [(i, 0) for i in range(16)]


def build_fused(extra_out=()):
    nc = bass.Bass("TRN2", target_bir_lowering=False)
    D = declare(nc, (), ["out"] + list(extra_out), moe_layers=(0, 1))
    banks = [nc.alloc_psum_tensor("bank%d" % i, [128, 512], F32) for i in range(8)]
    P = Prog(nc)
    phase_ada(nc, P, D, banks)
    phase_l0a(nc, P, D, banks)
    phase_l0b(nc, P, D, banks)
    phase_moe_sparse(nc, P, D, banks, 0, "x1", "x2", MOE0_TILES, "m0_")
    phase_l1a(nc, P, D, banks)
    phase_l1b_a(nc, P, D, banks)
    phase_l1b_b(nc, P, D, banks)
    phase_moe_sparse(nc, P, D, banks, 1, "x3", "out", MOE1_TILES, "m1_", final_norm=True)
    P.flush(final=True)
    return nc


def kernel(**inputs):
    inp = {k: np.asarray(v) for k, v in inputs.items()}
    n = 8
    nc = build_fused()
    in_maps = []
    for c in range(n):
        m = core_inputs(c, inp)
        m.update(weights_for(c, inp, layers=(0, 1)))
        in_maps.append(m)
    res = run_bass_kernel_spmd(nc, in_maps, core_ids=list(range(n))).results
    out = np.zeros((4, 4096, 1024), np.float32)
    for c in range(n):
        b, hf = c // 2, c % 2
        own = local_order(hf)[0]
        out[b][own] = np.asarray(res[c]["out"], dtype=np.float32)
    return out
```

```python
import contextlib
import numpy as np
import concourse.bass as bass
import concourse.mybir as mybir
from concourse.bass_utils import run_bass_kernel_spmd

F32 = mybir.dt.float32
BF16 = mybir.dt.bfloat16
AF = mybir.ActivationFunctionType
ALU = mybir.AluOpType
AX = mybir.AxisListType
N_DMA_SEMS = 10


class Op:
    __slots__ = ("eng", "fn", "deps", "signal", "sem", "val", "is_dma", "idx", "kind", "sem_eng")

    def __init__(self, eng, fn, is_dma, kind):
        self.eng = eng
        self.fn = fn
        self.deps = set()
        self.signal = False
        self.sem = None
        self.val = 0
        self.is_dma = is_dma
        self.kind = kind
        self.sem_eng = None


class Rot:
    def __init__(self, name, aps):
        self.name = name
        self.aps = aps
        self.i = 0

    def next(self):
        k = self.i % len(self.aps)
        self.i += 1
        return self.aps[k], (self.name, k)


class Prog:
    ENGS = ("pe", "act", "dve", "pool", "sp")

    def __init__(self, nc):
        self.nc = nc
        self.st = contextlib.ExitStack()
        st = self.st
        self.esem = {e: st.enter_context(nc.semaphore("s_" + e)) for e in ("pe", "act", "dve", "pool", "cc")}
        self.dsem = {e: [st.enter_context(nc.semaphore("d_%s%d" % (e, i))) for i in range(N_DMA_SEMS)]
                     for e in ("sp", "act", "pool")}
        self.cnt = {e: 0 for e in self.esem}
        self.dcnt = {e: [0] * N_DMA_SEMS for e in self.dsem}
        self.drr = {e: 0 for e in self.dsem}
        self.nflush = 0
        self.total_ops = 0
        self._reset()

    def _reset(self):
        self.ops = []
        self.last_w = {}
        self.readers = {}

    def op(self, eng, fn, reads=(), writes=(), dma=False, kind=""):
        o = Op(eng, fn, dma, kind)
        o.idx = len(self.ops)
        ex = [r for r in reads if isinstance(r, tuple) and r[0] == "ps"]
        if ex and eng != "pe":
            reads = [r for r in reads if r not in ex]
            writes = list(writes) + ex
        for r in reads:
            w = self.last_w.get(r)
            if w is not None:
                o.deps.add(w)
        for wkey in writes:
            w = self.last_w.get(wkey)
            if w is not None:
                o.deps.add(w)
            for rd in self.readers.get(wkey, ()):
                o.deps.add(rd)
        for r in reads:
            self.readers.setdefault(r, []).append(o.idx)
        for wkey in writes:
            self.last_w[wkey] = o.idx
            self.readers[wkey] = []
        o.deps.discard(o.idx)
        self.ops.append(o)
        return o

    def dma(self, out, in_, reads, writes, eng="sp", **kw):
        return self.op(eng, lambda e: e.dma_start(out=out, in_=in_, **kw), reads, writes, dma=True, kind="dma")

    def mm(self, out, lhsT, rhs, start, stop, reads, writes, **kw):
        return self.op("pe", lambda e: e.matmul(out, lhsT, rhs, start=start, stop=stop, **kw),
                       reads, writes, kind="mm")

    def tr(self, out, in_, ident, reads, writes):
        return self.op("pe", lambda e: e.transpose(out, in_, ident), reads, writes, kind="mm")

    def act(self, out, in_, func, reads, writes, **kw):
        return self.op("act", lambda e: e.activation(out, in_, func, **kw), reads, writes, kind="act")

    def cc(self, fn, reads, writes):
        o = self.op("pool", fn, reads, writes, kind="cc")
        o.sem_eng = "cc"
        o.signal = True
        return o

    def v(self, fn, reads, writes, eng="dve"):
        return self.op(eng, fn, reads, writes, kind="v")

    def flush(self, final=False):
        nc = self.nc
        ops = self.ops
        self.total_ops += len(ops)
        for o in ops:
            if o.eng == "pe":
                o.deps = {d for d in o.deps if ops[d].eng != "pe"}
        for o in ops:
            for d in o.deps:
                ops[d].signal = True
        base_cnt = dict(self.cnt)
        base_dcnt = {e: list(v) for e, v in self.dcnt.items()}
        dprev = {e: [None] * N_DMA_SEMS for e in self.dsem}
        for o in ops:
            if o.is_dma:
                o.signal = True
                k = self.drr[o.eng]
                self.drr[o.eng] = (k + 1) % N_DMA_SEMS
                self.dcnt[o.eng][k] += 16
                o.sem = self.dsem[o.eng][k]
                o.val = self.dcnt[o.eng][k]
                p = dprev[o.eng][k]
                if p is not None:
                    o.deps.add(p)
                dprev[o.eng][k] = o.idx
            elif o.signal:
                se = o.sem_eng or o.eng
                self.cnt[se] += 1
                o.sem = self.esem[se]
                o.val = self.cnt[se]
        first = self.nflush == 0
        self.nflush += 1
        with nc.Block() as blk:
            getters = {"pe": blk.tensor, "act": blk.scalar, "dve": blk.vector, "pool": blk.gpsimd, "sp": blk.sync}
            for ename in self.ENGS:
                mine = [o for o in ops if o.eng == ename]

                def body(e, mine=mine, ename=ename):
                    waited = {}
                    if not first:
                        for en, sem in self.esem.items():
                            if base_cnt[en] > 0 and en != ename:
                                e.wait_ge(sem, base_cnt[en])
                                waited[sem.num] = base_cnt[en]
                        for en, sems in self.dsem.items():
                            for k, sem in enumerate(sems):
                                if base_dcnt[en][k] > 0:
                                    e.wait_ge(sem, base_dcnt[en][k])
                                    waited[sem.num] = base_dcnt[en][k]
                    for o in mine:
                        need = {}
                        for d in o.deps:
                            do = ops[d]
                            if need.get(do.sem.num, (None, 0))[1] < do.val:
                                need[do.sem.num] = (do.sem, do.val)
                        for num, (sem, val) in need.items():
                            if waited.get(num, 0) >= val:
                                continue
                            e.wait_ge(sem, val)
                            waited[num] = val
                        ins = o.fn(e)
                        if o.signal:
                            if o.kind == "cc":
                                ins.then_inc(o.sem)
                            else:
                                ins.then_inc(o.sem, 16 if o.is_dma else 1)
                    if final and ename == "sp":
                        for en, sems in self.dsem.items():
                            for k, sem in enumerate(sems):
                                if self.dcnt[en][k] > 0:
                                    e.wait_ge(sem, self.dcnt[en][k])
                        for en, sem in self.esem.items():
                            if self.cnt[en] > 0:
                                e.wait_ge(sem, self.cnt[en])

                getters[ename](body)
        self._reset()
        if final:
            self.st.close()


def bank_rot(banks, lo, hi):
    state = {"i": 0}

    def nxt():
        k = lo + state["i"] % (hi - lo)
        state["i"] += 1
        return banks[k], ("ps", k)
    return nxt


EPS = 1e-6
NTOK = 4352
NOWN = 2304
SH1, SC1, GT1, SH2, SC2, GT2 = range(6)


def own_tiles():
    return [(i, i, 0) for i in range(16)] + [(16, 32, 1), (17, 33, 1)]


def rms_rstd(P, ss_ap, ss_key, out_ap, out_key, neghalf, D, tmp_ap, tmp_key):
    P.v(lambda e: e.tensor_scalar(tmp_ap, ss_ap, 1.0 / D, EPS, ALU.mult, ALU.add), [ss_key], [tmp_key])
    P.v(lambda e: e.tensor_tensor(out_ap, tmp_ap, neghalf, ALU.pow), [tmp_key, "consts"], [out_key], eng="pool")


def load_mod_rows(P, nc, st, mods_d, layer, which, names):
    out = []
    for v in range(2):
        t = st.enter_context(nc.sbuf_tensor("%s%d" % (names, v), [128, 1024], F32))
        P.dma(t[:], mods_d[layer, v:v + 1, which * 1024:(which + 1) * 1024].partition_broadcast(128), ["mods"],
              [(names, v)])
        out.append((t, (names, v)))
    return out


def phase_ada(nc, P, D, banks):
    with contextlib.ExitStack() as st:
        sb = lambda name, shape, dt=F32: st.enter_context(nc.sbuf_tensor(name, shape, dt))
        PS = Rot("ps", banks)
        ident = sb("ada_ident", [128, 128])
        cb = sb("ada_cb", [128, 2, 1024])
        screp = sb("ada_screp", [128, 2, 8, 128])
        bias = sb("ada_bias", [96, 2, 128])
        wbuf = Rot("ada_w", [sb("ada_wbuf%d" % i, [128, 8, 512]) for i in range(4)])
        accs = sb("ada_accs", [128, 2, 96])
        outT = sb("ada_outT", [96, 2, 128])
        P.dma(ident[:], D["ident32"][:, :], [], ["ident"])
        for v in range(2):
            P.dma(cb[:, v, :], D["cvec"][v:v + 1, :].partition_broadcast(128), [], [("cb", v)])
        for l in range(2):
            for v in range(2):
                P.dma(bias[48 * v:48 * v + 48, l, :], D["ada_b"][l].rearrange("(c p) -> c p", p=128), [], [("bias", l, v)])
        for v in range(2):
            P.act(cb[:, v, :], cb[:, v, :], AF.Silu, [("cb", v)], [("cb", v)])
            for half in range(2):
                p, pk = PS.next()
                for j in range(4):
                    kc = half * 4 + j
                    P.tr(p[:, j * 128:(j + 1) * 128], cb[:, v, kc * 128:(kc + 1) * 128], ident[:],
                         [("cb", v), "ident"], [pk])
                P.v(lambda e, p=p, v=v, half=half: e.tensor_copy(
                    screp[:, v, half * 4:(half + 1) * 4, :], p[:, :].rearrange("p (a b) -> p a b", a=4)),
                    [pk], [("screp", v)])
        for l in range(2):
            wl = D["ada_w"][l].rearrange("(kc p) n -> p kc n", p=128)
            acc, acck = PS.next()
            accv = acc[:, 0:96].rearrange("p (v c) -> p v c", v=2)
            for cblk in range(12):
                wb, wk = wbuf.next()
                P.dma(wb[:], wl[:, :, cblk * 512:(cblk + 1) * 512], [], [wk])
                for j in range(4):
                    c = cblk * 4 + j
                    for kc in range(8):
                        P.mm(accv[:, :, c], wb[:, kc, j * 128:(j + 1) * 128], screp[:, :, kc, 0], kc == 0, kc == 7,
                             [("screp", 0), ("screp", 1), wk], [acck])
            P.v(lambda e, acc=acc, l=l: e.tensor_copy(accs[:, l, :], acc[:, 0:96]), [acck], [("accs", l)])
            tp, tpk = PS.next()
            P.tr(tp[0:96, 0:128], accs[:, l, :], ident[:], [("accs", l), "ident"], [tpk])
            P.v(lambda e, tp=tp, l=l: e.tensor_tensor(outT[:, l, :], tp[0:96, 0:128], bias[:, l, :], ALU.add),
                [tpk, ("bias", l, 0), ("bias", l, 1)], [("outT", l)])
            P.dma(D["mods"][l].rearrange("v (c p) -> (v c) p", p=128), outT[:, l, :], [("outT", l)], ["mods"])
        P.flush()


def phase_l0a(nc, P, D, banks):
    NT = NTOK // 128
    with contextlib.ExitStack() as st:
        sb = lambda name, shape, dt=F32: st.enter_context(nc.sbuf_tensor(name, shape, dt))
        PS = Rot("ps", banks)
        ident = sb("a_ident", [128, 128], BF16)
        consts = sb("a_consts", [128, 4])
        gbc = sb("a_gbc", [128, 1024]); qg = sb("a_qg", [128, 256]); kvg = sb("a_kvg", [128, 128])
        w_in = sb("a_w_in", [128, 8, 1184], BF16)
        w_uq = sb("a_w_uq", [128, 2, 768], BF16)
        w_ukv = sb("a_w_ukv", [128, 1024], BF16)
        xt = Rot("xt", [sb("a_xt%d" % i, [128, 1024]) for i in range(2)])
        tabs = Rot("tabs", [sb("a_tabs%d" % i, [128, 1536]) for i in range(3)])
        junk = sb("a_junk", [128, 1024])
        t1r = Rot("t1", [sb("a_t1_%d" % i, [128, 1024]) for i in range(2)])
        hbr = Rot("hb", [sb("a_hb_%d" % i, [128, 1024], BF16) for i in range(2)])
        hTr = Rot("hT", [sb("a_hT_%d" % i, [128, 8, 128], BF16) for i in range(2)])
        small = Rot("small", [sb("a_small%d" % i, [128, 8]) for i in range(3)])
        zsr = Rot("zs", [sb("a_zs%d" % i, [128, 1184]) for i in range(2)])
        cqn = sb("a_cqn", [128, 384], BF16)
        cqnT = sb("a_cqnT", [128, 3, 128], BF16)
        krr = sb("a_krr", [128, 32], BF16)
        ropet = sb("a_ropet", [128, 2, 512])
        QA = sb("a_QA", [128, 8, 96], BF16); KA = sb("a_KA", [128, 8, 96], BF16)
        QB = sb("a_QB", [128, 8, 64], BF16); KB = sb("a_KB", [128, 2, 64], BF16)
        VAs = Rot("VAs", [sb("a_VAs%d" % i, [128, 8, 65], BF16) for i in range(2)])
        VBs = Rot("VBs", [sb("a_VBs%d" % i, [128, 2, 65], BF16) for i in range(2)])
        oQA = Rot("oQA", [sb("a_oQA%d" % i, [96, 8, 128], BF16) for i in range(2)])
        oKA = Rot("oKA", [sb("a_oKA%d" % i, [96, 8, 128], BF16) for i in range(2)])
        oQB = Rot("oQB", [sb("a_oQB%d" % i, [64, 8, 128], BF16) for i in range(2)])
        oKB = Rot("oKB", [sb("a_oKB%d" % i, [64, 2, 128], BF16) for i in range(2)])
        Abc = load_mod_rows(P, nc, st, D["mods"], 0, SC1, "a_A")
        Bbc = load_mod_rows(P, nc, st, D["mods"], 0, SH1, "a_B")

        P.dma(ident[:], D["identbf"][:, :], [], ["ident"])
        P.v(lambda e: e.memset(consts[:, 0:1], -0.5), [], ["consts"])
        P.dma(gbc[:], D["norm_mix_g"][0:1, :].partition_broadcast(128), [], ["gbc"])
        P.dma(qg[:], D["mla_q_norm_g"][0:1, :].partition_broadcast(128), [], ["qg"])
        P.dma(kvg[:], D["mla_kv_norm_g"][0:1, :].partition_broadcast(128), [], ["kvg"])
        P.dma(w_in[:], D["even_w_in"][0].rearrange("(kc p) n -> p kc n", p=128), [], ["w_in"], eng="pool")
        P.dma(w_uq[:], D["mla_w_uq"][0].rearrange("(kc p) n -> p kc n", p=128), [], ["w_uq"], eng="pool")
        P.dma(w_ukv[:], D["mla_w_ukv"][0], [], ["w_ukv"], eng="pool")
        for i, r in enumerate(VAs.aps):
            P.v(lambda e, r=r: e.memset(r[:, :, 64:65], 1.0), [], [("VAs", i)])
        for i, r in enumerate(VBs.aps):
            P.v(lambda e, r=r: e.memset(r[:, :, 64:65], 1.0), [], [("VBs", i)])
        for v in range(2):
            a, ak = Abc[v]
            P.v(lambda e, a=a: e.scalar_tensor_tensor(a[:], a[:], 1.0, gbc[:], ALU.add, ALU.mult), [ak, "gbc"], [ak])
        neghalf = consts[:, 0:1]
        v3 = lambda ap, h: ap.rearrange("p (h w) -> p h w", h=h)

        def rope(src4, dst4, cos4, ssin4, nh, blk, rkeys, wkeys):
            W = 4 * blk
            tmpa = ropet[:, 0, 0:nh * W].rearrange("p (h w) -> p h w", h=nh)
            tmpb = ropet[:, 1, 0:nh * W].rearrange("p (h w) -> p h w", h=nh)
            P.v(lambda e: e.tensor_tensor(tmpa, src4, cos4, ALU.mult), rkeys, ["ropeA"])
            v5 = lambda a: a.rearrange("p h (q w b) -> p h q w b", q=2, w=2)
            for w in range(2):
                P.v(lambda e, w=w: e.tensor_tensor(
                    v5(tmpb)[:, :, :, w, :], v5(src4)[:, :, :, 1 - w, :], v5(ssin4)[:, :, :, w, :], ALU.mult),
                    rkeys, ["ropeB%d" % w])
            P.v(lambda e: e.tensor_tensor(dst4, tmpa, tmpb, ALU.add), ["ropeA", "ropeB0", "ropeB1"], wkeys)

        def stage_a(t):
            rs = slice(t * 128, (t + 1) * 128)
            var = 1 if t >= 32 else 0
            need_q = (t < 16) or (t >= 32)
            A, Ak = Abc[var]; B, Bk = Bbc[var]
            x_ap, x_key = xt.next()
            P.dma(x_ap[:], D["xtok"][rs, :], [], [x_key])
            tb_ap, tb_key = tabs.next()
            P.dma(tb_ap[:, 0:256], D["cA"][rs, :], [], [(tb_key, 0)])
            P.dma(tb_ap[:, 256:512], D["sA"][rs, :], [], [(tb_key, 1)])
            P.dma(tb_ap[:, 512:1024], D["cB"][rs, :], [], [(tb_key, 2)])
            P.dma(tb_ap[:, 1024:1536], D["sB"][rs, :], [], [(tb_key, 3)])
            tkeys = [(tb_key, i) for i in range(4)]
            cA = tb_ap[:, 0:256]; sA = tb_ap[:, 256:512]; cB = tb_ap[:, 512:1024]; sB = tb_ap[:, 1024:1536]
            sm, sm_key = small.next()
            P.act(junk[:], x_ap[:], AF.Square, [x_key], ["junk", (sm_key, 0)], accum_out=sm[:, 0:1])
            rms_rstd(P, sm[:, 0:1], (sm_key, 0), sm[:, 2:3], (sm_key, 2), neghalf, 1024, sm[:, 1:2], (sm_key, 1))
            t1, t1k = t1r.next(); hb, hbk = hbr.next(); hT, hTk = hTr.next()
            P.v(lambda e, x_ap=x_ap, sm=sm, A=A, t1=t1: e.scalar_tensor_tensor(
                t1[:], x_ap[:], sm[:, 2:3], A[:], ALU.mult, ALU.mult), [x_key, (sm_key, 2), Ak], [t1k])
            P.v(lambda e, B=B, t1=t1, hb=hb: e.tensor_tensor(hb[:], t1[:], B[:], ALU.add), [t1k, Bk], [hbk])
            bk, bkey = PS.next()
            bkb = bk[:].bitcast(BF16)
            for kc in range(8):
                P.tr(bkb[:, kc * 128:(kc + 1) * 128], hb[:, kc * 128:(kc + 1) * 128], ident[:], [hbk, "ident"], [bkey])
            P.act(hT[:].rearrange("p a b -> p (a b)"), bkb, AF.Copy, [bkey], [hTk])
            blocks = [(0, 416), (416, 928), (928, 1184)]
            zb = []
            for bi, (c0, c1) in enumerate(blocks):
                if bi == 1 and not need_q:
                    zb.append((None, None))
                    continue
                bk, bkey = PS.next()
                for kc in range(8):
                    P.mm(bk[:, 0:c1 - c0], hT[:, kc, :], w_in[:, kc, c0:c1], kc == 0, kc == 7, [hTk, "w_in"], [bkey])
                zb.append((bk, bkey))
            zs, zsk = zsr.next()
            for bi, (c0, c1) in enumerate(blocks):
                if zb[bi][0] is None:
                    continue
                P.act(zs[:, c0:c1], zb[bi][0][:, 0:c1 - c0], AF.Copy, [zb[bi][1]], [(zsk, bi)])
            return dict(t=t, rs=rs, need_q=need_q, zs=zs, zsk=zsk, tb_ap=tb_ap, tkeys=tkeys, sm=sm, sm_key=sm_key)

        def stage_b(c):
            t, rs, need_q, zs, zsk, tb_ap, tkeys, sm, sm_key = (c[k] for k in ("t", "rs", "need_q", "zs", "zsk", "tb_ap", "tkeys", "sm", "sm_key"))
            cA = tb_ap[:, 0:256]; sA = tb_ap[:, 256:512]; cB = tb_ap[:, 512:1024]; sB = tb_ap[:, 1024:1536]
            z0, z0k = zs[:, 0:416], (zsk, 0)
            z1, z1k = zs[:, 416:928], (zsk, 1)
            z2, z2k = zs[:, 928:1184], (zsk, 2)
            if need_q:
                P.act(junk[:, 0:256], z0[:, 0:256], AF.Square, [z0k], ["junk", (sm_key, 3)], accum_out=sm[:, 3:4])
                rms_rstd(P, sm[:, 3:4], (sm_key, 3), sm[:, 4:5], (sm_key, 4), neghalf, 256, sm[:, 1:2], (sm_key, 1))
                P.v(lambda e, z0=z0, sm=sm: e.scalar_tensor_tensor(
                    cqn[:, 0:256], z0[:, 0:256], sm[:, 4:5], qg[:], ALU.mult, ALU.mult),
                    [z0k, (sm_key, 4), "qg"], ["cqn"])
            P.act(junk[:, 0:128], z0[:, 256:384], AF.Square, [z0k], ["junk", (sm_key, 5)], accum_out=sm[:, 5:6])
            rms_rstd(P, sm[:, 5:6], (sm_key, 5), sm[:, 6:7], (sm_key, 6), neghalf, 128, sm[:, 1:2], (sm_key, 1))
            P.v(lambda e, z0=z0, sm=sm: e.scalar_tensor_tensor(
                cqn[:, 256:384], z0[:, 256:384], sm[:, 6:7], kvg[:], ALU.mult, ALU.mult),
                [z0k, (sm_key, 6), "kvg"], ["cqn"])
            bk, bkey = PS.next()
            bkb = bk[:].bitcast(BF16)
            for j in (range(3) if need_q else [2]):
                P.tr(bkb[:, j * 128:(j + 1) * 128], cqn[:, j * 128:(j + 1) * 128], ident[:], ["cqn", "ident"], [bkey])
            P.act(cqnT[:].rearrange("p a b -> p (a b)"), bkb[:, 0:384], AF.Copy, [bkey], ["cqnT"])
            rope(v3(z0[:, 384:416], 1), v3(krr[:, :], 1), v3(cA[:, 0:32], 1), v3(sA[:, 0:32], 1), 1, 8,
                 tkeys + [z0k], ["krr"])
            if need_q:
                for hh in range(2):
                    bk, bkey = PS.next()
                    for kc in range(2):
                        P.mm(bk[:, 0:384], cqnT[:, kc, :], w_uq[:, kc, hh * 384:(hh + 1) * 384], kc == 0, kc == 1,
                             ["cqnT", "w_uq"], [bkey])
                    q4 = bk[:, 0:384].rearrange("p (h w) -> p h w", h=4)
                    P.act(QA[:, hh * 4:(hh + 1) * 4, 0:64], q4[:, :, 0:64], AF.Copy, [bkey], [("QA", hh, 0)])
                    rope(q4[:, :, 64:96], QA[:, hh * 4:(hh + 1) * 4, 64:96],
                         v3(cA[:, hh * 128:(hh + 1) * 128], 4), v3(sA[:, hh * 128:(hh + 1) * 128], 4), 4, 8,
                         tkeys + [bkey], [("QA", hh, 1)])
            va, va_key = VAs.next()
            for hh in range(2):
                bk, bkey = PS.next()
                P.mm(bk[:, :], cqnT[:, 2, :], w_ukv[:, hh * 512:(hh + 1) * 512], True, True, ["cqnT", "w_ukv"], [bkey])
                k4 = bk[:, :].rearrange("p (h w) -> p h w", h=4)
                P.act(KA[:, hh * 4:(hh + 1) * 4, 0:64], k4[:, :, 0:64], AF.Copy, [bkey], [("KA", hh, 0)])
                P.v(lambda e, va=va, k4=k4, hh=hh: e.tensor_copy(va[:, hh * 4:(hh + 1) * 4, 0:64], k4[:, :, 64:128]),
                    [bkey], [(va_key, hh)])
            for h in range(8):
                P.v(lambda e, h=h: e.tensor_copy(KA[:, h, 64:96], krr[:, :]), ["krr"], [("KA", h, 1)], eng="pool")
            P.dma(D["VA"][rs, :], va[:].rearrange("p h w -> p (h w)"), [(va_key, 0), (va_key, 1)], [("VA", t)])
            if need_q:
                q8 = z1[:, :].rearrange("p (h w) -> p h w", h=8)
                rope(q8, QB[:, :, :], v3(cB[:, :], 8), v3(sB[:, :], 8), 8, 16, tkeys + [z1k], ["QB"])
            k2 = z2[:, 0:128].rearrange("p (h w) -> p h w", h=2)
            rope(k2, KB[:, :, :], v3(cB[:, 0:128], 2), v3(sB[:, 0:128], 2), 2, 16, tkeys + [z2k], ["KB"])
            vb, vb_key = VBs.next()
            P.act(vb[:, :, 0:64], z2[:, 128:256].rearrange("p (h w) -> p h w", h=2), AF.Copy, [z2k], [vb_key])
            P.dma(D["VB"][rs, :], vb[:].rearrange("p h w -> p (h w)"), [vb_key], [("VB", t)])
            QAk = [("QA", hh, j) for hh in range(2) for j in range(2)]
            KAk = [("KA", hh, 0) for hh in range(2)] + [("KA", h, 1) for h in range(8)]
            jobs = [(KA, KAk, 8, 96, oKA, "KAT"), (KB, ["KB"], 2, 64, oKB, "KBT")]
            if need_q:
                jobs += [(QA, QAk, 8, 96, oQA, "QAT"), (QB, ["QB"], 8, 64, oQB, "QBT")]
            for (src, skeys, nh, Dh, pool, dname) in jobs:
                bk, bkey = PS.next()
                bkb = bk[:].bitcast(BF16)
                for h in range(nh):
                    P.tr(bkb[0:Dh, h * 128:(h + 1) * 128], src[:, h, :], ident[:], skeys + ["ident"], [bkey])
                ob, okey = pool.next()
                P.act(ob[:].rearrange("p a b -> p (a b)"), bkb[0:Dh, 0:nh * 128], AF.Copy, [bkey], [okey])
                P.dma(D[dname][:, :, rs], ob[:], [okey], [(dname, t)])

        pend = None
        for t in range(NT):
            cur = stage_a(t)
            if pend is not None:
                stage_b(pend)
            pend = cur
        stage_b(pend)
        P.flush()


def phase_l0b(nc, P, D, banks):
    with contextlib.ExitStack() as st:
        sb = lambda name, shape, dt=F32: st.enter_context(nc.sbuf_tensor(name, shape, dt))
        nS, nO, nR = bank_rot(banks, 0, 4), bank_rot(banks, 4, 6), bank_rot(banks, 6, 8)
        nOP = bank_rot(banks, 0, 4)
        KBT = sb("b_KBT", [64, 2, NTOK], BF16)
        VB = sb("b_VB", [128, 34, 130], BF16)
        VA = sb("b_VA", [128, 34, 520], BF16)
        w_out = sb("b_wout", [128, 16, 1024], BF16)
        sel = sb("b_sel", [65, 64])
        es = sb("b_es", [64, 8])
        masks = sb("b_masks", [128, 2, 512], BF16)
        G = load_mod_rows(P, nc, st, D["mods"], 0, GT1, "b_G")
        KAh = Rot("KAh", [sb("b_KAh%d" % i, [96, NTOK], BF16) for i in range(2)])
        QAg = Rot("QAg", [sb("b_QAg%d" % i, [96, 8, 512], BF16) for i in range(2)])
        QBg = Rot("QBg", [sb("b_QBg%d" % i, [64, 8, 512], BF16) for i in range(2)])
        LA = 2
        PT = Rot("PT", [sb("b_PT%d" % i, [128, 512], BF16) for i in range(LA + 2)])
        Osb = Rot("Osb", [sb("b_Osb%d" % i, [65, 512]) for i in range(2)])
        rec = Rot("rec", [sb("b_rec%d" % i, [64, 512]) for i in range(2)])
        mixT = sb("b_mixT", [128, 16, 512], BF16)
        xt = Rot("bxt", [sb("b_xt%d" % i, [128, 1024]) for i in range(2)])
        ot = Rot("bot", [sb("b_ot%d" % i, [128, 1024]) for i in range(2)])

        P.dma(KBT[:], D["KBT"][:, :, :], [], ["KBT"])
        P.dma(VB[:], D["VB"].rearrange("(c p) w -> p c w", p=128), [], ["VB"])
        P.dma(VA[:], D["VA"].rearrange("(c p) w -> p c w", p=128), [], ["VA"])
        P.v(lambda e: e.memset(w_out[64:128, :, :], 0.0), [], ["w_out_z"], eng="pool")
        P.v(lambda e: e.memset(mixT[64:128, :, :], 0.0), [], ["mixT_z"], eng="pool")
        P.dma(w_out[0:64, :, :], D["even_w_out"][0].rearrange("(j p) n -> p j n", p=64), [], ["w_out"], eng="pool")
        P.v(lambda e: e.memset(sel[:], 0.0), [], ["sel"])
        P.v(lambda e: e.memset(sel[64:65, :], 1.0), ["sel"], ["sel"])
        P.dma(es[:], D["win_sink"][0:1, :].partition_broadcast(64), [], ["es"])
        P.act(es[:], es[:], AF.Exp, ["es"], ["es"])
        P.dma(masks[:], D["wmask"].rearrange("a p n -> p a n"), [], ["masks"])

        groups = [(g * 512, 512, g * 512, 0) for g in range(4)] + [(4096, 256, 2048, 1)]
        SC_A = 96.0 ** -0.5
        SC_B = 64.0 ** -0.5
        for (tok0, N, row0, var) in groups:
            qa, qak = QAg.next()
            P.dma(qa[:, :, 0:N], D["QAT"][:, :, tok0:tok0 + N], [], [qak])
            qb, qbk = QBg.next()
            P.dma(qb[:, :, 0:N], D["QBT"][:, :, tok0:tok0 + N], [], [qbk])
            chunks = list(range(34)) if var == 0 else [32, 33]
            def mla_norm(h, O, Ok, N=N):
                osb, osk = Osb.next()
                P.v(lambda e: e.tensor_copy(osb[0:65, 0:N], O[0:65, 0:N]), [Ok], [osk])
                R, Rk = nR()
                P.mm(R[0:64, 0:N], sel[0:65, 0:64], osb[0:65, 0:N], True, True, ["sel", osk], [Rk])
                rc, rck = rec.next()
                P.v(lambda e: e.reciprocal(rc[0:64, 0:N], R[0:64, 0:N]), [Rk], [rck])
                P.v(lambda e: e.tensor_tensor(mixT[0:64, h, 0:N], osb[0:64, 0:N], rc[0:64, 0:N], ALU.mult),
                    [osk, rck], [("mixT", h)])

            defer = None
            for h in range(8):
                ka, kak = KAh.next()
                P.dma(ka[:], D["KAT"][:, h, :], [], [kak])
                O, Ok = nO()
                pend = []
                for ci, kc in enumerate(chunks + [None] * LA):
                    if kc is not None:
                        S, Sk = nS()
                        P.mm(S[:, 0:N], ka[0:96, kc * 128:(kc + 1) * 128], qa[0:96, h, 0:N], True, True, [kak, qak], [Sk])
                        pt, ptk = PT.next()
                        P.act(pt[:, 0:N], S[:, 0:N], AF.Exp, [Sk], [ptk], scale=SC_A)
                        pend.append((ci, kc, pt, ptk))
                    if defer is not None and (ci == LA or kc is None):
                        mla_norm(*defer)
                        defer = None
                    if len(pend) > LA or (kc is None and pend):
                        pci, pkc, ppt, pptk = pend.pop(0)
                        P.mm(O[0:65, 0:N], VA[:, pkc, h * 65:(h + 1) * 65], ppt[:, 0:N], pci == 0, pci == len(chunks) - 1,
                             ["VA", pptk], [Ok])
                defer = (h, O, Ok)
            mla_norm(*defer)
            nb = N // 128

            def win_norm(g, b, O, Ok):
                osb, osk = Osb.next()
                P.v(lambda e: e.tensor_copy(osb[0:65, :], O[0:65, :]), [Ok], [osk])
                R, Rk = nR()
                P.mm(R[0:64, :], sel[0:65, 0:64], osb[0:65, :], True, True, ["sel", osk], [Rk])
                rc, rck = rec.next()
                for j in range(4):
                    P.v(lambda e, j=j: e.tensor_scalar(
                        rc[0:64, j * 128:(j + 1) * 128], R[0:64, j * 128:(j + 1) * 128],
                        es[0:64, g * 4 + j:g * 4 + j + 1], None, ALU.add), [Rk, "es"], [rck])
                P.v(lambda e: e.reciprocal(rc[0:64, :], rc[0:64, :]), [rck], [rck])
                P.v(lambda e: e.tensor_tensor(
                    mixT[0:64, 8 + g * 4:8 + g * 4 + 4, b * 128:(b + 1) * 128],
                    osb[0:64, :].rearrange("p (h q) -> p h q", h=4),
                    rc[0:64, :].rearrange("p (h q) -> p h q", h=4), ALU.mult),
                    [osk, rck], [("mixT", 8 + g * 4 + j) for j in range(4)])

            deferw = None
            for b in range(nb):
                tt = tok0 // 128 + b
                if var == 0:
                    cl = []
                    if tt > 0:
                        cl.append((tt - 1, 0))
                    cl.append((tt, None))
                    cl.append((tt + 1, 1))
                    cl += [(32, None), (33, None)]
                else:
                    cl = [(32, None), (33, None)]
                for g in range(2):
                    O, Ok = nO()
                    Q4 = qb[0:64, g * 4:(g + 1) * 4, b * 128:(b + 1) * 128]
                    pend = []
                    for ci, item in enumerate(cl + [None] * LA):
                        if item is not None:
                            kc, mk_ = item
                            S, Sk = nS()
                            P.mm(S[:, :].rearrange("p (h q) -> p h q", h=4), KBT[0:64, g, kc * 128:(kc + 1) * 128], Q4,
                                 True, True, ["KBT", qbk], [Sk])
                            pt, ptk = PT.next()
                            P.act(pt[:, :], S[:, :], AF.Exp, [Sk], [ptk], scale=SC_B)
                            if mk_ is not None:
                                P.v(lambda e, pt=pt, mk_=mk_: e.tensor_tensor(pt[:, :], pt[:, :], masks[:, mk_, :], ALU.mult),
                                    [ptk, "masks"], [ptk], eng="pool")
                            pend.append((ci, kc, pt, ptk))
                        if deferw is not None and (ci == min(LA, len(cl) - 1)):
                            win_norm(*deferw)
                            deferw = None
                        if len(pend) > LA or (item is None and pend):
                            pci, pkc, ppt, pptk = pend.pop(0)
                            P.mm(O[0:65, :], VB[:, pkc, g * 65:(g + 1) * 65], ppt[:, :], pci == 0, pci == len(cl) - 1,
                                 ["VB", pptk], [Ok])
                    deferw = (g, b, O, Ok)
            if deferw is not None:
                win_norm(*deferw)
                deferw = None
            mkeys = [("mixT", j) for j in range(16)]
            for b in range(nb):
                x_ap, xk = xt.next()
                P.dma(x_ap[:], D["xtok"][tok0 + b * 128:tok0 + (b + 1) * 128, :], [], [xk])
                o_ap, ok_ = ot.next()
                for dh in range(2):
                    Ob, Obk = nOP()
                    for j in range(16):
                        P.mm(Ob[:, :], mixT[0:128, j, b * 128:(b + 1) * 128], w_out[0:128, j, dh * 512:(dh + 1) * 512],
                             j == 0, j == 15, mkeys + ["w_out", "w_out_z", "mixT_z"], [Obk])
                    P.v(lambda e, o_ap=o_ap, Ob=Ob, dh=dh, var=var: e.tensor_tensor(
                        o_ap[:, dh * 512:(dh + 1) * 512], Ob[:, :], G[var][0][:, dh * 512:(dh + 1) * 512], ALU.mult),
                        [Obk, G[var][1]], [ok_])
                P.v(lambda e, o_ap=o_ap, x_ap=x_ap: e.tensor_tensor(o_ap[:], o_ap[:], x_ap[:], ALU.add),
                    [ok_, xk], [ok_], eng="pool")
                r0 = row0 + b * 128
                P.dma(D["x1"][r0:r0 + 128, :], o_ap[:], [ok_], [("x1", r0)])
        P.flush()


def phase_moe(nc, P, D, banks, layer, xin, xout, tiles, tag, final_norm=False):
    NTl = len(tiles)
    T = NTl * 128
    with contextlib.ExitStack() as st0:
        sb0 = lambda name, shape, dt=F32: st0.enter_context(nc.sbuf_tensor(tag + name, shape, dt))
        h2T = sb0("h2T", [128, 8, T], BF16)
        comb = sb0("comb", [128, NTl, 32])
        consts = sb0("consts", [128, 4])
        P.v(lambda e: e.memset(consts[:, 0:1], -0.5), [], ["consts"])
        neghalf = consts[:, 0:1]
        with contextlib.ExitStack() as st:
            sb = lambda name, shape, dt=F32: st.enter_context(nc.sbuf_tensor(tag + name, shape, dt))
            nA, nB = bank_rot(banks, 0, 4), bank_rot(banks, 4, 8)
            ident = sb("ident32", [128, 128])
            gbc = sb("gbc", [128, 1024])
            w_r = sb("w_r", [128, 8, 36])
            Abc = load_mod_rows(P, nc, st, D["mods"], layer, SC2, tag + "A")
            Bbc = load_mod_rows(P, nc, st, D["mods"], layer, SH2, tag + "B")
            xt = Rot("mxt", [sb("xt%d" % i, [128, 1024]) for i in range(2)])
            junk = sb("junk", [128, 1024])
            t1 = sb("t1", [128, 1024])
            h2 = sb("h2", [128, 1024])
            h2T32 = sb("h2T32", [128, 8, 128])
            small = Rot("msmall", [sb("small%d" % i, [128, 16]) for i in range(2)])
            rt = Rot("mrt", [sb("rt%d" % i, [128, 96]) for i in range(2)])
            P.dma(ident[:], D["ident32"][:, :], [], ["ident"])
            P.dma(gbc[:], D["norm_ffn_g"][layer:layer + 1, :].partition_broadcast(128), [], ["gbc"])
            P.dma(w_r[:, :, 0:4], D["moe_w_rg"][layer].rearrange("(kc p) n -> p kc n", p=128), [], [("w_r", 0)])
            P.dma(w_r[:, :, 4:36], D["moe_w_re"][layer].rearrange("(kc p) n -> p kc n", p=128), [], [("w_r", 1)])
            for v in range(2):
                a, ak = Abc[v]
                P.v(lambda e, a=a: e.scalar_tensor_tensor(a[:], a[:], 1.0, gbc[:], ALU.add, ALU.mult), [ak, "gbc"], [ak])
            for ti, (r, var) in enumerate(tiles):
                A, Ak = Abc[var]; B, Bk = Bbc[var]
                x_ap, xk = xt.next()
                P.dma(x_ap[:], D[xin][r * 128:(r + 1) * 128, :], [], [xk])
                sm, smk = small.next()
                P.act(junk[:], x_ap[:], AF.Square, [xk], ["junk", (smk, 0)], accum_out=sm[:, 0:1])
                rms_rstd(P, sm[:, 0:1], (smk, 0), sm[:, 2:3], (smk, 2), neghalf, 1024, sm[:, 1:2], (smk, 1))
                P.v(lambda e, x_ap=x_ap, sm=sm, A=A: e.scalar_tensor_tensor(
                    t1[:], x_ap[:], sm[:, 2:3], A[:], ALU.mult, ALU.mult), [xk, (smk, 2), Ak], ["t1"])
                P.v(lambda e, B=B: e.tensor_tensor(h2[:], t1[:], B[:], ALU.add), ["t1", Bk], ["h2"])
                for half in range(2):
                    bk, bkey = nA()
                    for j in range(4):
                        kc = half * 4 + j
                        P.tr(bk[:, j * 128:(j + 1) * 128], h2[:, kc * 128:(kc + 1) * 128], ident[:], ["h2", "ident"], [bkey])
                    P.act(h2T[:, half * 4:(half + 1) * 4, ti * 128:(ti + 1) * 128],
                          bk[:, :].rearrange("p (a b) -> p a b", a=4), AF.Copy, [bkey], [("h2T", ti, half)])
                    P.v(lambda e, bk=bk, half=half: e.tensor_copy(
                        h2T32[:, half * 4:(half + 1) * 4, :], bk[:, :].rearrange("p (a b) -> p a b", a=4)),
                        [bkey], [("h2T32", half)])
                lg, lgk = nB()
                for kc in range(8):
                    P.mm(lg[:, 0:36], h2T32[:, kc, :], w_r[:, kc, :], kc == 0, kc == 7,
                         [("h2T32", kc // 4), ("w_r", 0), ("w_r", 1)], [lgk])
                R, Rk = rt.next()
                P.v(lambda e, R=R, lg=lg: e.tensor_copy(R[:, 0:36], lg[:, 0:36]), [lgk], [(Rk, "lg")])
                s_ = lambda c: sm[:, c:c + 1]
                P.v(lambda e, R=R, sm=sm: e.reduce_max(sm[:, 3:4], R[:, 0:4], AX.X), [(Rk, "lg")], [(smk, 3)])
                P.v(lambda e, R=R, sm=sm: e.tensor_scalar(R[:, 36:40], R[:, 0:4], sm[:, 3:4], None, ALU.is_equal),
                    [(Rk, "lg"), (smk, 3)], [(Rk, "goh")])
                P.v(lambda e, sm=sm: e.tensor_scalar(sm[:, 4:5], sm[:, 3:4], -1.0, None, ALU.mult), [(smk, 3)], [(smk, 4)])
                P.act(R[:, 80:84], R[:, 0:4], AF.Exp, [(Rk, "lg"), (smk, 4)], [(Rk, "gexp"), (smk, 5)],
                      bias=sm[:, 4:5], accum_out=sm[:, 5:6])
                P.v(lambda e, sm=sm: e.reciprocal(sm[:, 6:7], sm[:, 5:6]), [(smk, 5)], [(smk, 6)])
                P.v(lambda e, R=R: e.tensor_scalar(R[:, 40:48], R[:, 4:12], R[:, 36:37], None, ALU.mult),
                    [(Rk, "lg"), (Rk, "goh")], [(Rk, "ein")])
                for g in range(1, 4):
                    P.v(lambda e, R=R, g=g: e.scalar_tensor_tensor(
                        R[:, 40:48], R[:, 4 + 8 * g:12 + 8 * g], R[:, 36 + g:37 + g], R[:, 40:48], ALU.mult, ALU.add),
                        [(Rk, "lg"), (Rk, "goh"), (Rk, "ein")], [(Rk, "ein")])
                P.v(lambda e, R=R, sm=sm: e.reduce_max(sm[:, 7:8], R[:, 40:48], AX.X), [(Rk, "ein")], [(smk, 7)])
                P.v(lambda e, R=R, sm=sm: e.tensor_scalar(R[:, 48:56], R[:, 40:48], sm[:, 7:8], None, ALU.is_equal),
                    [(Rk, "ein"), (smk, 7)], [(Rk, "oh1")])
                P.v(lambda e, R=R: e.scalar_tensor_tensor(R[:, 56:64], R[:, 48:56], -1e30, R[:, 40:48], ALU.mult, ALU.add),
                    [(Rk, "oh1"), (Rk, "ein")], [(Rk, "e2")])
                P.v(lambda e, R=R, sm=sm: e.reduce_max(sm[:, 8:9], R[:, 56:64], AX.X), [(Rk, "e2")], [(smk, 8)])
                P.v(lambda e, R=R, sm=sm: e.tensor_scalar(R[:, 64:72], R[:, 56:64], sm[:, 8:9], None, ALU.is_equal),
                    [(Rk, "e2"), (smk, 8)], [(Rk, "oh2")])
                P.v(lambda e, sm=sm: e.tensor_tensor(sm[:, 9:10], sm[:, 8:9], sm[:, 7:8], ALU.subtract),
                    [(smk, 7), (smk, 8)], [(smk, 9)])
                P.act(sm[:, 10:11], sm[:, 9:10], AF.Exp, [(smk, 9)], [(smk, 10)])
                P.v(lambda e, sm=sm: e.tensor_scalar(sm[:, 11:12], sm[:, 10:11], 1.0, None, ALU.add), [(smk, 10)], [(smk, 11)])
                P.v(lambda e, sm=sm: e.reciprocal(sm[:, 11:12], sm[:, 11:12]), [(smk, 11)], [(smk, 11)])
                P.v(lambda e, sm=sm: e.tensor_tensor(sm[:, 12:13], sm[:, 11:12], sm[:, 6:7], ALU.mult),
                    [(smk, 11), (smk, 6)], [(smk, 12)])
                P.v(lambda e, sm=sm: e.tensor_tensor(sm[:, 13:14], sm[:, 12:13], sm[:, 10:11], ALU.mult),
                    [(smk, 12), (smk, 10)], [(smk, 13)])
                P.v(lambda e, R=R, sm=sm: e.tensor_scalar(R[:, 72:80], R[:, 48:56], sm[:, 12:13], None, ALU.mult),
                    [(Rk, "oh1"), (smk, 12)], [(Rk, "loc")])
                P.v(lambda e, R=R, sm=sm: e.scalar_tensor_tensor(
                    R[:, 72:80], R[:, 64:72], sm[:, 13:14], R[:, 72:80], ALU.mult, ALU.add),
                    [(Rk, "oh2"), (smk, 13), (Rk, "loc")], [(Rk, "loc")])
                for g in range(4):
                    P.v(lambda e, R=R, g=g, ti=ti: e.tensor_scalar(
                        comb[:, ti, g * 8:(g + 1) * 8], R[:, 72:80], R[:, 36 + g:37 + g], None, ALU.mult),
                        [(Rk, "loc"), (Rk, "goh")], [("comb", ti)])
            P.flush()
        with contextlib.ExitStack() as st:
            sb = lambda name, shape, dt=F32: st.enter_context(nc.sbuf_tensor(tag + name, shape, dt))
            nGU, nY = bank_rot(banks, 0, 4), bank_rot(banks, 4, 8)
            yacc = sb("yacc", [128, NTl, 1024])
            wg = Rot("wg", [sb("wg%d" % i, [128, 8, 512], BF16) for i in range(2)])
            wu = Rot("wu", [sb("wu%d" % i, [128, 8, 512], BF16) for i in range(2)])
            wd = Rot("wd", [sb("wd%d" % i, [128, 4, 1024], BF16) for i in range(2)])
            hg = Rot("hg", [sb("hg%d" % i, [128, 4, 512], BF16) for i in range(2)])
            sg = Rot("sg", [sb("sg%d" % i, [128, 512]) for i in range(2)])
            G = load_mod_rows(P, nc, st, D["mods"], layer, GT2, tag + "G")
            xt = Rot("mxt2", [sb("xt2_%d" % i, [128, 1024]) for i in range(2)])
            groups = []
            t0 = 0
            while t0 < NTl:
                n = min(4, NTl - t0)
                groups.append((t0, n))
                t0 += n
            def emit_gu(ex, t0, n, g_ap, gk, u_ap, uk):
                N = n * 128
                ts = slice(t0 * 128, t0 * 128 + N)
                hgt, hgk = hg.next()
                for fc in range(4):
                    Gp, Gk = nGU(); Up, Uk = nGU()
                    for kc in range(8):
                        P.mm(Gp[:, 0:N], g_ap[:, kc, fc * 128:(fc + 1) * 128], h2T[:, kc, ts], kc == 0, kc == 7,
                             [gk, "h2T"], [Gk])
                    for kc in range(8):
                        P.mm(Up[:, 0:N], u_ap[:, kc, fc * 128:(fc + 1) * 128], h2T[:, kc, ts], kc == 0, kc == 7,
                             [uk, "h2T"], [Uk])
                    s_ap, sk = sg.next()
                    P.act(s_ap[:, 0:N], Gp[:, 0:N], AF.Silu, [Gk], [sk])
                    P.v(lambda e, hgt=hgt, Up=Up, s_ap=s_ap, fc=fc, N=N: e.tensor_tensor(
                        hgt[:, fc, 0:N], Up[:, 0:N], s_ap[:, 0:N], ALU.mult), [Uk, sk], [(hgk, fc)])
                return hgt, hgk

            def emit_down(ex, t0, n, hgt, hgk, d_ap, dk):
                for b in range(n):
                    ti = t0 + b
                    for dh in range(2):
                        Yp, Yk = nY()
                        for fc in range(4):
                            P.mm(Yp[:, :], hgt[:, fc, b * 128:(b + 1) * 128], d_ap[:, fc, dh * 512:(dh + 1) * 512],
                                 fc == 0, fc == 3, [(hgk, f) for f in range(4)] + [dk], [Yk])
                        ysl = yacc[:, ti, dh * 512:(dh + 1) * 512]
                        if ex == 0:
                            P.v(lambda e, ysl=ysl, Yp=Yp, ti=ti, ex=ex: e.tensor_scalar(
                                ysl, Yp[:, :], comb[:, ti, ex:ex + 1], None, ALU.mult), [Yk], [("yacc", ti, dh)])
                        else:
                            P.v(lambda e, ysl=ysl, Yp=Yp, ti=ti, ex=ex: e.scalar_tensor_tensor(
                                ysl, Yp[:, :], comb[:, ti, ex:ex + 1], ysl, ALU.mult, ALU.add),
                                [Yk, ("yacc", ti, dh)], [("yacc", ti, dh)])

            pend = None
            for ex in range(32):
                g_ap, gk = wg.next(); u_ap, uk = wu.next(); d_ap, dk = wd.next()
                P.dma(g_ap[:], D["moe_w_gate%d" % layer][ex].rearrange("(kc p) f -> p kc f", p=128), [], [gk], eng="pool")
                P.dma(u_ap[:], D["moe_w_up%d" % layer][ex].rearrange("(kc p) f -> p kc f", p=128), [], [uk], eng="pool")
                P.dma(d_ap[:], D["moe_w_down%d" % layer][ex].rearrange("(kc p) f -> p kc f", p=128), [], [dk], eng="pool")
                for (t0, n) in groups:
                    hgt, hgk = emit_gu(ex, t0, n, g_ap, gk, u_ap, uk)
                    if pend is not None:
                        emit_down(*pend)
                    pend = (ex, t0, n, hgt, hgk, d_ap, dk)
            emit_down(*pend)
            if final_norm:
                fg = sb("fg", [128, 1024])
                P.dma(fg[:], D["final_norm_g"][0:1, :].partition_broadcast(128), [], ["fg"])
                junk = sb("junk3", [128, 1024])
                small = Rot("fsmall", [sb("fsmall%d" % i, [128, 4]) for i in range(2)])
            for ti, (r, var) in enumerate(tiles):
                x_ap, xk = xt.next()
                P.dma(x_ap[:], D[xin][r * 128:(r + 1) * 128, :], [], [xk])
                ysl = yacc[:, ti, :]
                yk = [("yacc", ti, 0), ("yacc", ti, 1)]
                P.v(lambda e, ysl=ysl, var=var: e.tensor_tensor(ysl, ysl, G[var][0][:], ALU.mult), yk + [G[var][1]], yk)
                P.v(lambda e, ysl=ysl, x_ap=x_ap: e.tensor_tensor(ysl, ysl, x_ap[:], ALU.add), yk + [xk], yk, eng="pool")
                if final_norm:
                    sm, smk = small.next()
                    P.act(junk[:], ysl, AF.Square, yk, ["junk3", (smk, 0)], accum_out=sm[:, 0:1])
                    rms_rstd(P, sm[:, 0:1], (smk, 0), sm[:, 2:3], (smk, 2), neghalf, 1024, sm[:, 1:2], (smk, 1))
                    P.v(lambda e, ysl=ysl, sm=sm: e.scalar_tensor_tensor(
                        ysl, ysl, sm[:, 2:3], fg[:], ALU.mult, ALU.mult), yk + [(smk, 2), "fg"], yk)
                P.dma(D[xout][r * 128:(r + 1) * 128, :], ysl, yk, [(xout, r)])
            P.flush()


GELU_C = 0.7978845608028654


def l1_tiles():
    return [(i, 0) for i in range(16)] + [(16, 1), (17, 1)]


def phase_l1a(nc, P, D, banks):
    with contextlib.ExitStack() as st:
        sb = lambda name, shape, dt=F32: st.enter_context(nc.sbuf_tensor("c_" + name, shape, dt))
        nP = bank_rot(banks, 0, 8)
        ident = sb("ident", [128, 128], BF16)
        ident32 = sb("ident32", [128, 128])
        consts = sb("consts", [128, 4])
        gbc = sb("gbc", [128, 1024])
        w_in = sb("w_in", [128, 8, 2592], BF16)
        Abc = load_mod_rows(P, nc, st, D["mods"], 1, SC1, "c_A")
        Bbc = load_mod_rows(P, nc, st, D["mods"], 1, SH1, "c_B")
        W2 = sb("W2", [32, 512])
        bg = sb("bg", [128, 512])
        gm = sb("gm", [128, 3, 128])
        wsT = sb("wsT", [128, 4, 128], BF16)
        bsT = sb("bsT", [128, 4])
        lng = sb("lng", [128, 512]); lnb = sb("lnb", [128, 512])
        xt = Rot("cxt", [sb("xt%d" % i, [128, 1024]) for i in range(2)])
        junk = sb("junk", [128, 1024])
        t1r = Rot("t1", [sb("t1_%d" % i, [128, 1024]) for i in range(2)])
        hbr = Rot("hb", [sb("hb_%d" % i, [128, 1024], BF16) for i in range(2)])
        hTr = Rot("hT", [sb("hT_%d" % i, [128, 8, 128], BF16) for i in range(2)])
        small = Rot("csmall", [sb("small%d" % i, [128, 16]) for i in range(3)])
        qkr = Rot("qk", [sb("qk%d" % i, [128, 512]) for i in range(2)])
        g32r = Rot("g32", [sb("g32_%d" % i, [128, 32]) for i in range(2)])
        uvr = Rot("uv", [sb("uv%d" % i, [128, 2, 512]) for i in range(2)])
        g32T = sb("g32T", [32, 128])
        zs = sb("zs", [128, 512]); la = sb("la", [128, 512])
        bsb = sb("bsb", [128, 2, 256])
        ex = sb("ex", [128, 256]); tmp = sb("tmp", [128, 256])
        gl = sb("gl", [128, 6, 256], BF16)
        glT = Rot("glT", [sb("glT%d" % i, [128, 8, 128], BF16) for i in range(2)])
        dec = Rot("dec", [sb("dec%d" % i, [128, 4]) for i in range(2)])
        vb = Rot("cvb", [sb("vb%d" % i, [128, 512], BF16) for i in range(2)])
        rsb = Rot("rsb", [sb("rsb%d" % i, [128, 512], BF16) for i in range(2)])
        ge = sb("ge", [128, 2, 512])
        gt_ = sb("gt_", [128, 512]); gt2_ = sb("gt2_", [128, 512])
        vgn = sb("vgn", [128, 512], BF16)
        dlb = Rot("dlb", [sb("dlb%d" % i, [128, 512], BF16) for i in range(2)])

        P.dma(ident[:], D["identbf"][:, :], [], ["ident"])
        P.dma(ident32[:], D["ident32"][:, :], [], ["ident32"])
        P.v(lambda e: e.memset(consts[:, 0:1], -0.5), [], ["consts"])
        neghalf = consts[:, 0:1]
        P.dma(gbc[:], D["norm_mix_g"][1:2, :].partition_broadcast(128), [], ["gbc"])
        for c in range(3):
            lo, hi = c * 864, (c + 1) * 864
            P.dma(w_in[:, :, lo:hi], D["odd_w_in"][0].rearrange("(kc p) n -> p kc n", p=128)[:, :, lo:hi], [],
                  [("w_in", c)], eng="pool")
        wkeys = [("w_in", c) for c in range(3)]
        P.v(lambda e: e.memset(W2[:], 0.0), [], ["W2"])
        P.dma(W2[0:16, 0:256], D["gla_w_g2"][0, 0], ["W2"], ["W2"])
        P.dma(W2[16:32, 256:512], D["gla_w_g2"][0, 1], ["W2"], ["W2"])
        P.dma(bg[:], D["gla_b_g"][0:1].rearrange("o a n -> o (a n)").partition_broadcast(128), [], ["bg"])
        P.dma(gm[:], D["gmask"][0:3].rearrange("a p n -> p a n"), [], ["gm"])
        P.dma(wsT[:], D["sg_w_sT"].rearrange("g s t -> s g t"), [], ["wsT"], eng="pool")
        P.dma(bsT[:], D["sg_b_sT"][:, :], [], ["bsT"])
        P.dma(lng[:], D["sg_ln_g"][0:1, :].partition_broadcast(128), [], ["lng"])
        P.dma(lnb[:], D["sg_ln_b"][0:1, :].partition_broadcast(128), [], ["lnb"])
        for v in range(2):
            a, ak = Abc[v]
            P.v(lambda e, a=a: e.scalar_tensor_tensor(a[:], a[:], 1.0, gbc[:], ALU.add, ALU.mult), [ak, "gbc"], [ak])

        def gelu(src, src_key, dst, dst_key):
            P.v(lambda e: e.tensor_tensor(gt2_[:], src, src, ALU.mult), [src_key], ["gt2_"])
            P.v(lambda e: e.tensor_scalar(gt2_[:], gt2_[:], 0.044715, 1.0, ALU.mult, ALU.add), ["gt2_"], ["gt2_"])
            P.v(lambda e: e.tensor_tensor(gt2_[:], gt2_[:], src, ALU.mult), ["gt2_", src_key], ["gt2_"], eng="pool")
            P.act(gt2_[:], gt2_[:], AF.Sigmoid, ["gt2_"], ["gt2_"], scale=2.0 * GELU_C)
            P.v(lambda e: e.tensor_tensor(dst, src, gt2_[:], ALU.mult), [src_key, "gt2_"], [dst_key])

        def stage_a(r, var):
            lat = var == 0
            A, Ak = Abc[var]; B, Bk = Bbc[var]
            rs = slice(r * 128, (r + 1) * 128)
            x_ap, xk = xt.next()
            P.dma(x_ap[:], D["x2"][rs, :], [], [xk])
            sm, smk = small.next()
            P.act(junk[:], x_ap[:], AF.Square, [xk], ["junk", (smk, 0)], accum_out=sm[:, 0:1])
            rms_rstd(P, sm[:, 0:1], (smk, 0), sm[:, 2:3], (smk, 2), neghalf, 1024, sm[:, 1:2], (smk, 1))
            t1, t1k = t1r.next(); hb, hbk = hbr.next(); hT, hTk = hTr.next()
            P.v(lambda e, x_ap=x_ap, sm=sm, A=A, t1=t1: e.scalar_tensor_tensor(
                t1[:], x_ap[:], sm[:, 2:3], A[:], ALU.mult, ALU.mult), [xk, (smk, 2), Ak], [t1k])
            P.v(lambda e, B=B, t1=t1, hb=hb: e.tensor_tensor(hb[:], t1[:], B[:], ALU.add), [t1k, Bk], [hbk])
            bk, bkey = nP()
            bkb = bk[:].bitcast(BF16)
            for kc in range(8):
                P.tr(bkb[:, kc * 128:(kc + 1) * 128], hb[:, kc * 128:(kc + 1) * 128], ident[:], [hbk, "ident"], [bkey])
            P.act(hT[:].rearrange("p a b -> p (a b)"), bkb, AF.Copy, [bkey], [hTk])

            def proj(c0, c1):
                bk, bkey = nP()
                for kc in range(8):
                    P.mm(bk[:, 0:c1 - c0], hT[:, kc, :], w_in[:, kc, c0:c1], kc == 0, kc == 7, [hTk] + wkeys, [bkey])
                return bk, bkey
            zqk, zqkk = proj(0, 512)
            qk, qkk = qkr.next()
            P.act(qk[:], zqk[:, :], AF.Copy, [zqkk], [qkk])
            zv, zvk = proj(512, 1024)
            v_ap, vk = vb.next()
            P.act(v_ap[:], zv[:, :], AF.Copy, [zvk], [vk])
            P.dma(D["g_v"][rs, :], v_ap[:], [vk], [("g_v", r)])
            zg, zgk = proj(1024, 1056)
            g32, g32k = g32r.next()
            P.v(lambda e, zg=zg, g32=g32: e.tensor_copy(g32[:], zg[:, 0:32]), [zgk], [g32k])
            uv, uvk = uvr.next()
            if lat:
                zr, zrk = proj(1056, 1568)
                r_ap, rk = rsb.next()
                P.act(r_ap[:], zr[:, :], AF.Silu, [zrk], [rk])
                P.dma(D["rsilu"][rs, :], r_ap[:], [rk], [("rsilu", r)])
                zu, zuk = proj(1568, 2080)
                P.act(uv[:, 0, :], zu[:, :], AF.Copy, [zuk], [(uvk, 0)])
                zvg, zvgk = proj(2080, 2592)
                P.v(lambda e, uv=uv, zvg=zvg: e.tensor_copy(uv[:, 1, :], zvg[:, :]), [zvgk], [(uvk, 1)])
            return dict(r=r, lat=lat, rs=rs, sm=sm, smk=smk, qk=qk, qkk=qkk, g32=g32, g32k=g32k, uv=uv, uvk=uvk)

        def stage_b(c):
            r, lat, rs, sm, smk, qk, qkk, g32, g32k, uv, uvk = (c[k] for k in (
                "r", "lat", "rs", "sm", "smk", "qk", "qkk", "g32", "g32k", "uv", "uvk"))
            bk, bkey = nP()
            P.tr(bk[0:32, 0:128], g32[:, :], ident32[:], [g32k, "ident32"], [bkey])
            P.v(lambda e, bk=bk: e.tensor_copy(g32T[:], bk[0:32, 0:128]), [bkey], ["g32T"])
            zz, zzk = nP()
            P.mm(zz[:, :], g32T[0:32, :], W2[0:32, :], True, True, ["g32T", "W2"], [zzk])
            P.v(lambda e, zz=zz: e.tensor_tensor(zs[:], zz[:, :], bg[:], ALU.add), [zzk, "bg"], ["zs"])
            P.act(zs[:], zs[:], AF.Exp, ["zs"], ["zs"], scale=-1.0)
            P.act(zs[:], zs[:], AF.Ln, ["zs"], ["zs"], bias=1.0)
            P.v(lambda e: e.tensor_scalar(la[:], zs[:], -1.0 / 16.0, None, ALU.mult), ["zs"], ["la"])
            cA, cAk = nP()
            P.mm(cA[:, 0:256], gm[:, 0, :], la[:, 0:256], True, True, ["gm", "la"], [cAk])
            P.mm(cA[:, 256:512], gm[:, 1, :], la[:, 256:512], True, True, ["gm", "la"], [cAk])
            cL, cLk = nP()
            P.mm(cL[:, :], gm[:, 2, :], la[:, :], True, True, ["gm", "la"], [cLk])
            P.act(bsb[:].rearrange("p a b -> p (a b)"), cA[:, :], AF.Copy, [cAk], ["bsb"])
            cT, cTk = nP()
            for j in range(4):
                P.mm(cT[:, j:j + 1], la[:, j * 128:(j + 1) * 128], gm[:, 2, 0:1], True, True, ["gm", "la"], [cTk])
            d_ap, dk = dec.next()
            P.act(d_ap[:], cT[:, 0:4], AF.Exp, [cTk], [dk])
            P.dma(D["g_dec"][:, r, :], d_ap[:], [dk], [("g_dec", r)])
            for p in range(2):
                if p == 1 and not lat:
                    continue
                bp = bsb[:, p, :]
                if lat:
                    P.act(ex[:], bp, AF.Exp, ["bsb"], ["ex"])
                    P.v(lambda e, p=p: e.scalar_tensor_tensor(gl[:, 2 * p, :], qk[:, 0:256], 0.125, ex[:], ALU.mult, ALU.mult),
                        [qkk, "ex"], [("gl", 2 * p)])
                P.act(ex[:], bp, AF.Exp, ["bsb"], ["ex"], scale=-1.0)
                P.v(lambda e, p=p: e.tensor_tensor(gl[:, 2 * p + 1, :], qk[:, 256:512], ex[:], ALU.mult),
                    [qkk, "ex"], [("gl", 2 * p + 1)])
                P.v(lambda e, p=p, bp=bp, cL=cL: e.tensor_tensor(tmp[:], cL[:, p * 256:(p + 1) * 256], bp, ALU.subtract),
                    [cLk, "bsb"], ["tmp"])
                P.act(ex[:], tmp[:], AF.Exp, ["tmp"], ["ex"])
                P.v(lambda e, p=p: e.tensor_tensor(gl[:, 4 + p, :], qk[:, 256:512], ex[:], ALU.mult),
                    [qkk, "ex"], [("gl", 4 + p)])
                P.dma(D["g_kd"][p, rs, :], gl[:, 4 + p, :], [("gl", 4 + p)], [("g_kd", p, r)])
            bk, bkey = nP()
            bkb = bk[:].bitcast(BF16)
            arrs = [0, 1, 2, 3] if lat else [1]
            for a_ in arrs:
                for j in range(2):
                    P.tr(bkb[:, (a_ * 2 + j) * 128:(a_ * 2 + j + 1) * 128], gl[:, a_, j * 128:(j + 1) * 128], ident[:],
                         [("gl", a_), "ident"], [bkey])
            gT, gTk = glT.next()
            if lat:
                P.act(gT[:].rearrange("p a b -> p (a b)"), bkb, AF.Copy, [bkey], [gTk])
                P.dma(D["g_T"][:, r, :, :], gT[:], [gTk], [("g_T", r)])
            else:
                P.act(gT[:, 2:4, :].rearrange("p a b -> p (a b)"), bkb[:, 256:512], AF.Copy, [bkey], [gTk])
                P.dma(D["g_T"][:, r, 2:4, :], gT[:, 2:4, :], [gTk], [("g_T", r)])
            if not lat:
                return
            gelu(uv[:, 0, :], (uvk, 0), ge[:, 0, :], ("ge", 0))
            gelu(uv[:, 1, :], (uvk, 1), ge[:, 1, :], ("ge", 1))
            P.v(lambda e, sm=sm: e.reduce_sum(sm[:, 8:9], ge[:, 1, :], AX.X), [("ge", 1)], [(smk, 8)])
            P.act(junk[:, 0:512], ge[:, 1, :], AF.Square, [("ge", 1)], ["junk", (smk, 9)], accum_out=sm[:, 9:10])
            P.v(lambda e, sm=sm: e.tensor_scalar(sm[:, 10:11], sm[:, 8:9], 1.0 / 512, None, ALU.mult), [(smk, 8)], [(smk, 10)])
            P.v(lambda e, sm=sm: e.tensor_tensor(sm[:, 11:12], sm[:, 10:11], sm[:, 10:11], ALU.mult), [(smk, 10)], [(smk, 11)])
            P.v(lambda e, sm=sm: e.scalar_tensor_tensor(sm[:, 12:13], sm[:, 9:10], 1.0 / 512, sm[:, 11:12], ALU.mult, ALU.subtract),
                [(smk, 9), (smk, 11)], [(smk, 12)])
            P.v(lambda e, sm=sm: e.tensor_scalar(sm[:, 12:13], sm[:, 12:13], EPS, None, ALU.add), [(smk, 12)], [(smk, 12)])
            P.v(lambda e, sm=sm: e.tensor_tensor(sm[:, 13:14], sm[:, 12:13], neghalf, ALU.pow), [(smk, 12), "consts"],
                [(smk, 13)], eng="pool")
            P.v(lambda e, sm=sm: e.tensor_scalar(gt_[:], ge[:, 1, :], sm[:, 10:11], sm[:, 13:14], ALU.subtract, ALU.mult),
                [("ge", 1), (smk, 10), (smk, 13)], ["gt_"])
            P.v(lambda e: e.tensor_tensor(gt_[:], gt_[:], lng[:], ALU.mult), ["gt_", "lng"], ["gt_"])
            P.v(lambda e: e.tensor_tensor(vgn[:], gt_[:], lnb[:], ALU.add), ["gt_", "lnb"], ["vgn"])
            sp_, spk = nP()
            for gi in range(4):
                P.mm(sp_[:, gi * 128:(gi + 1) * 128], wsT[:, gi, :], vgn[:, gi * 128:(gi + 1) * 128], True, True,
                     ["wsT", "vgn"], [spk])
            dl_ap, dlk = dlb.next()
            for gi in range(4):
                P.v(lambda e, gi=gi, sp_=sp_, dl_ap=dl_ap: e.scalar_tensor_tensor(
                    dl_ap[:, gi * 128:(gi + 1) * 128], sp_[:, gi * 128:(gi + 1) * 128], bsT[:, gi:gi + 1],
                    ge[:, 0, gi * 128:(gi + 1) * 128], ALU.add, ALU.mult), [spk, "bsT", ("ge", 0)], [dlk])
            P.dma(D["dl"][rs, :], dl_ap[:], [dlk], [("dl", r)])

        pend = None
        for (r, var) in l1_tiles():
            cur = stage_a(r, var)
            if pend is not None:
                stage_b(pend)
            pend = cur
        stage_b(pend)
        P.flush()


def _gla_pass(nc, P, D, banks, st, sb, pidx, order, S, Sb, on_out):
    nAT, nO, nU = bank_rot(banks, 0, 2), bank_rot(banks, 2, 4), bank_rot(banks, 4, 6)
    gT = Rot("gTl", [sb("gTl%d" % i, [128, 4, 128], BF16) for i in range(2)])
    kd = Rot("kdl", [sb("kdl%d" % i, [128, 256], BF16) for i in range(2)])
    vv = Rot("vl", [sb("vl%d" % i, [128, 512], BF16) for i in range(2)])
    dc = Rot("dcl", [sb("dcl%d" % i, [128, 2]) for i in range(2)])
    ATm = Rot("ATm", [sb("ATm%d" % i, [128, 128], BF16) for i in range(2)])
    mask = sb("gmask_sb", [128, 128])
    P.dma(mask[:], D["gmask"][pidx], [], ["gmask"])
    for r in order:
        lat = r < 16
        rs = slice(r * 128, (r + 1) * 128)
        g_ap, gk = gT.next()
        if lat:
            P.dma(g_ap[:], D["g_T"][:, r, 4 * pidx:4 * pidx + 4, :], [], [gk])
        else:
            P.dma(g_ap[:, 2:4, :], D["g_T"][:, r, 4 * pidx + 2:4 * pidx + 4, :], [], [gk])
        k_ap, kk = kd.next()
        P.dma(k_ap[:], D["g_kd"][pidx, rs, :], [], [kk])
        v_ap, vk = vv.next()
        P.dma(v_ap[:], D["g_v"][rs, :], [], [vk])
        d_ap, dk = dc.next()
        P.dma(d_ap[:], D["g_dec"][:, r, 2 * pidx:2 * pidx + 2], [], [dk])
        if lat:
            O, Ok = nO()
            for h in range(4):
                j, po = h // 2, (h % 2) * 64
                AT, ATk = nAT()
                P.mm(AT[:, 0:128], g_ap[po:po + 64, 2 + j, :], g_ap[po:po + 64, j, :], True, True, [gk], [ATk])
                am, amk = ATm.next()
                P.v(lambda e, am=am, AT=AT: e.tensor_tensor(am[:], AT[:, 0:128], mask[:], ALU.mult), [ATk, "gmask"], [amk])
                P.mm(O[:, h * 128:(h + 1) * 128], am[:], v_ap[:, h * 128:(h + 1) * 128], True, False, [amk, vk], [Ok])
                P.mm(O[:, h * 128:(h + 1) * 128], g_ap[po:po + 64, j, :], Sb[po:po + 64, j, :], False, True,
                     [gk, ("Sb", j, h % 2)], [Ok])
            on_out(r, O, Ok)
        for j in range(2):
            for hh in range(2):
                po = hh * 64
                U, Uk = nU()
                P.mm(U[:, 0:128], k_ap[:, j * 128:(j + 1) * 128], v_ap[:, (2 * j + hh) * 128:(2 * j + hh + 1) * 128],
                     True, True, [kk, vk], [Uk])
                P.v(lambda e, U=U, j=j, po=po, d_ap=d_ap: e.scalar_tensor_tensor(
                    S[po:po + 64, j, :], S[po:po + 64, j, :], d_ap[po:po + 64, j:j + 1], U[po:po + 64, 0:128],
                    ALU.mult, ALU.add), [Uk, dk, ("S", j, hh)], [("S", j, hh)])
                P.act(Sb[po:po + 64, j, :], S[po:po + 64, j, :], AF.Copy, [("S", j, hh)], [("Sb", j, hh)])


def phase_l1b_a(nc, P, D, banks):
    with contextlib.ExitStack() as st:
        sb = lambda name, shape, dt=F32: st.enter_context(nc.sbuf_tensor("d_" + name, shape, dt))
        S = sb("S", [128, 2, 128]); Sb = sb("Sb", [128, 2, 128], BF16)
        oa = Rot("oa", [sb("oa%d" % i, [128, 512]) for i in range(2)])
        P.v(lambda e: e.memset(S[:], 0.0), [], [("S", j, hh) for j in range(2) for hh in range(2)])
        P.v(lambda e: e.memset(Sb[:], 0.0), [], [("Sb", j, hh) for j in range(2) for hh in range(2)])

        def on_out(r, O, Ok):
            o_ap, ok_ = oa.next()
            P.act(o_ap[:], O[:, :], AF.Copy, [Ok], [ok_])
            P.dma(D["OA"][r * 128:(r + 1) * 128, :], o_ap[:], [ok_], [("OA", r)])
        _gla_pass(nc, P, D, banks, st, sb, 0, [16, 17] + list(range(16)), S, Sb, on_out)
        P.dma(D["cc_in"].rearrange("(a p) n -> p a n", p=128), S[:], [("S", j, hh) for j in range(2) for hh in range(2)],
              ["cc_in"])
        P.cc(lambda e: e.collective_compute("AllGather", ALU.bypass, replica_groups=[[0, 1], [2, 3], [4, 5], [6, 7]],
                                            ins=[D["cc_in"].opt()], outs=[D["cc_out"].opt()]), ["cc_in"], ["cc_out"])
        P.flush()


def phase_l1b_b(nc, P, D, banks):
    with contextlib.ExitStack() as st:
        sb = lambda name, shape, dt=F32: st.enter_context(nc.sbuf_tensor("e_" + name, shape, dt))
        nT, nW = bank_rot(banks, 0, 2), bank_rot(banks, 2, 8)
        S = sb("S", [128, 2, 128]); Sb = sb("Sb", [128, 2, 128], BF16)
        skeys = [("S", j, hh) for j in range(2) for hh in range(2)]
        both = sb("both", [128, 4, 128])
        sel = sb("sel", [128, 2])
        P.dma(both[:], D["cc_out"].rearrange("(a p) n -> p a n", p=128), [], ["both"])
        P.dma(sel[:], D["sel"][:, :], [], ["sel"])
        P.v(lambda e: e.tensor_scalar(S[:].rearrange("p a b -> p (a b)"), both[:, 0:2, :].rearrange("p a b -> p (a b)"),
                                      sel[:, 0:1], None, ALU.mult), ["both", "sel"], skeys)
        P.v(lambda e: e.scalar_tensor_tensor(S[:].rearrange("p a b -> p (a b)"),
                                             both[:, 2:4, :].rearrange("p a b -> p (a b)"), sel[:, 1:2],
                                             S[:].rearrange("p a b -> p (a b)"), ALU.mult, ALU.add),
            ["both", "sel"] + skeys, skeys)
        P.act(Sb[:], S[:], AF.Copy, skeys, [("Sb", j, hh) for j in range(2) for hh in range(2)])
        ident = sb("ident", [128, 128], BF16)
        P.dma(ident[:], D["identbf"][:, :], [], ["ident"])
        consts = sb("consts", [128, 4])
        P.v(lambda e: e.memset(consts[:], -0.5), [], ["consts"])
        gng = sb("gng", [128, 512])
        P.dma(gng[:], D["gla_norm_g"][0:1, :].partition_broadcast(128), [], ["gng"])
        w_out = sb("w_out", [128, 8, 1024], BF16)
        P.dma(w_out[:], D["odd_w_out"][0].rearrange("(kc p) n -> p kc n", p=128), [], ["w_out"], eng="pool")
        G = load_mod_rows(P, nc, st, D["mods"], 1, GT1, "e_G")
        oa = Rot("eoa", [sb("oa%d" % i, [128, 512]) for i in range(2)])
        rsl = Rot("ersl", [sb("rsl%d" % i, [128, 512], BF16) for i in range(2)])
        gr = sb("gr", [128, 512])
        junk = sb("junk", [128, 128])
        small = Rot("esmall", [sb("small%d" % i, [128, 8]) for i in range(2)])
        mix_all = sb("mix_all", [128, 16, 1024], BF16)
        mixTr = Rot("emixT", [sb("mixT%d" % i, [128, 8, 128], BF16) for i in range(2)])
        xt = Rot("ext", [sb("xt%d" % i, [128, 1024]) for i in range(2)])
        ot = Rot("eot", [sb("ot%d" % i, [128, 1024]) for i in range(2)])

        def on_out(r, O, Ok):
            rs = slice(r * 128, (r + 1) * 128)
            o_ap, ok_ = oa.next()
            P.dma(o_ap[:], D["OA"][rs, :], [], [ok_])
            P.v(lambda e, o_ap=o_ap, O=O: e.tensor_tensor(o_ap[:], O[:, :], o_ap[:], ALU.add), [Ok, ok_], [ok_])
            r_ap, rk = rsl.next()
            P.dma(r_ap[:], D["rsilu"][rs, :], [], [rk])
            m_ap, mk = mix_all[:, r, :], ("mix", r)
            P.dma(m_ap[:, 512:1024], D["dl"][rs, :], [], [(mk, 1)])
            sm, smk = small.next()
            for h in range(4):
                P.act(junk[:], o_ap[:, h * 128:(h + 1) * 128], AF.Square, [ok_], ["junk", (smk, h)], accum_out=sm[:, h:h + 1])
            hk = [(smk, h) for h in range(4)]
            P.v(lambda e, sm=sm: e.tensor_scalar(sm[:, 0:4], sm[:, 0:4], 1.0 / 128, EPS, ALU.mult, ALU.add), hk, hk)
            P.v(lambda e, sm=sm: e.tensor_tensor(sm[:, 4:8], sm[:, 0:4], consts[:, 0:4], ALU.pow), hk + ["consts"],
                [(smk, 4)], eng="pool")
            P.v(lambda e, r_ap=r_ap: e.tensor_tensor(gr[:], gng[:], r_ap[:], ALU.mult), ["gng", rk], ["gr"])
            for h in range(4):
                P.v(lambda e, h=h, o_ap=o_ap, sm=sm, m_ap=m_ap: e.scalar_tensor_tensor(
                    m_ap[:, h * 128:(h + 1) * 128], o_ap[:, h * 128:(h + 1) * 128], sm[:, 4 + h:5 + h],
                    gr[:, h * 128:(h + 1) * 128], ALU.mult, ALU.mult), [ok_, (smk, 4), "gr"], [(mk, 0)])

        def out_proj(r):
            rs = slice(r * 128, (r + 1) * 128)
            m_ap, mk = mix_all[:, r, :], ("mix", r)
            bk, bkey = nT()
            bkb = bk[:].bitcast(BF16)
            for kc in range(8):
                P.tr(bkb[:, kc * 128:(kc + 1) * 128], m_ap[:, kc * 128:(kc + 1) * 128], ident[:],
                     [(mk, 0), (mk, 1), "ident"], [bkey])
            mT, mTk = mixTr.next()
            P.act(mT[:].rearrange("p a b -> p (a b)"), bkb, AF.Copy, [bkey], [mTk])
            x_ap, xk = xt.next()
            P.dma(x_ap[:], D["x2"][rs, :], [], [xk])
            t_ap, tk = ot.next()
            for dh in range(2):
                W, Wk = nW()
                for kc in range(8):
                    P.mm(W[:, :], mT[:, kc, :], w_out[:, kc, dh * 512:(dh + 1) * 512], kc == 0, kc == 7,
                         [mTk, "w_out"], [Wk])
                P.v(lambda e, t_ap=t_ap, W=W, dh=dh: e.tensor_tensor(
                    t_ap[:, dh * 512:(dh + 1) * 512], W[:, :], G[0][0][:, dh * 512:(dh + 1) * 512], ALU.mult),
                    [Wk, G[0][1]], [tk])
            P.v(lambda e, t_ap=t_ap, x_ap=x_ap: e.tensor_tensor(t_ap[:], t_ap[:], x_ap[:], ALU.add), [tk, xk], [tk],
                eng="pool")
            P.dma(D["x3"][rs, :], t_ap[:], [tk], [("x3", r)])
        _gla_pass(nc, P, D, banks, st, sb, 1, list(range(15, -1, -1)), S, Sb, on_out)
        for r in range(15, -1, -1):
            out_proj(r)
        P.flush()


I32 = mybir.dt.int32
STILE = 256
NB = STILE // 128


def n_stiles(T):
    return (2 * T + 32 * (STILE - 1) + STILE - 1) // STILE


def phase_moe_sparse(nc, P, D, banks, layer, xin, xout, tiles, tag, final_norm=False):
    NTl = len(tiles)
    T = NTl * 128
    NST = n_stiles(T)
    NSLOT = NST * STILE
    Xs, Ys = D["Xs"], D["Ys"]
    wgv = D["moe_w_gate%d" % layer].rearrange("e (p a kc) f -> (e p a) (kc f)", p=128, a=2)
    wuv = D["moe_w_up%d" % layer].rearrange("e (p a kc) f -> (e p a) (kc f)", p=128, a=2)
    wdv = D["moe_w_down%d" % layer].rearrange("e f d -> (e f) d")
    with contextlib.ExitStack() as st0:
        sb0 = lambda name, shape, dt=F32: st0.enter_context(nc.sbuf_tensor(tag + name, shape, dt))
        consts = sb0("consts", [128, 4])
        P.v(lambda e: e.memset(consts[:, 0:1], -0.5), [], ["consts"])
        neghalf = consts[:, 0:1]
        idxA = sb0("idxA", [128, NTl], I32); idxB = sb0("idxB", [128, NTl], I32)
        wAB = sb0("wAB", [128, 2, NTl])
        widx = sb0("widx", [128, NST, 6], I32)
        with contextlib.ExitStack() as st:
            sb = lambda name, shape, dt=F32: st.enter_context(nc.sbuf_tensor(tag + name, shape, dt))
            nA, nB = bank_rot(banks, 0, 4), bank_rot(banks, 4, 7)
            cntb, cntk = banks[7], ("ps", 7)
            ident = sb("ident32", [128, 128])
            gbc = sb("gbc", [128, 1024])
            w_r = sb("w_r", [128, 8, 36])
            gm = sb("gm", [128, 2, 128])
            eidrow = sb("eidrow", [128, 32])
            pc2 = sb("pc2", [128, 6])
            Abc = load_mod_rows(P, nc, st, D["mods"], layer, SC2, tag + "A")
            Bbc = load_mod_rows(P, nc, st, D["mods"], layer, SH2, tag + "B")
            xt = Rot("mxt", [sb("xt%d" % i, [128, 1024]) for i in range(2)])
            junk = sb("junk", [128, 1024])
            t1r = Rot("t1", [sb("t1_%d" % i, [128, 1024]) for i in range(2)])
            h2r = Rot("h2", [sb("h2_%d" % i, [128, 1024]) for i in range(2)])
            h2b = sb("h2b", [128, NTl, 1024], BF16)
            h2Tr = Rot("h2T32", [sb("h2T32_%d" % i, [128, 8, 128]) for i in range(2)])
            small = Rot("msmall", [sb("small%d" % i, [128, 16]) for i in range(2)])
            rt = Rot("mrt", [sb("rt%d" % i, [128, 96]) for i in range(2)])
            selA = sb("selA", [128, NTl, 32]); selB = sb("selB", [128, NTl, 32]); selm = sb("selm", [128, NTl, 32])
            LG = sb("LG", [128, NTl, 36])
            rs_ = sb("rs_", [128, 8, NTl])
            goh = sb("goh", [128, NTl, 4]); gex = sb("gex", [128, NTl, 4])
            etmp = sb("etmp", [128, NTl, 4, 8])
            ein = sb("ein", [128, NTl, 8]); oh1 = sb("oh1", [128, NTl, 8]); e2 = sb("e2", [128, NTl, 8]); oh2 = sb("oh2", [128, NTl, 8])
            zt = sb("zt", [128, 8, 1024], BF16)
            P.v(lambda e: e.memset(zt[:], 0.0), [], ["zt"], eng="pool")
            zkeys = []
            for z0 in range(0, NSLOT, 1024):
                nrow = min(1024, NSLOT - z0)
                P.dma(Xs[z0:z0 + nrow, :].rearrange("(a p) c -> p a c", p=128), zt[:, 0:nrow // 128, :], ["zt"], [("Xsz", z0)])
                zkeys.append(("Xsz", z0))
            P.dma(ident[:], D["ident32"][:, :], [], ["ident"])
            P.dma(gbc[:], D["norm_ffn_g"][layer:layer + 1, :].partition_broadcast(128), [], ["gbc"])
            P.dma(w_r[:, :, 0:4], D["moe_w_rg"][layer].rearrange("(kc p) n -> p kc n", p=128), [], [("w_r", 0)])
            P.dma(w_r[:, :, 4:36], D["moe_w_re"][layer].rearrange("(kc p) n -> p kc n", p=128), [], [("w_r", 1)])
            P.dma(gm[:], D["gmask"][3:5].rearrange("a p n -> p a n"), [], ["gm"])
            P.dma(eidrow[:], D["eidrow"][:, :], [], ["eidrow"])
            P.dma(pc2[:], D["pc2"][:, :], [], ["pc2"])
            for v in range(2):
                a, ak = Abc[v]
                P.v(lambda e, a=a: e.scalar_tensor_tensor(a[:], a[:], 1.0, gbc[:], ALU.add, ALU.mult), [ak, "gbc"], [ak])
            for ti, (r, var) in enumerate(tiles):
                A, Ak = Abc[var]; B, Bk = Bbc[var]
                x_ap, xk = xt.next()
                P.dma(x_ap[:], D[xin][r * 128:(r + 1) * 128, :], [], [xk])
                sm, smk = small.next()
                P.act(junk[:], x_ap[:], AF.Square, [xk], ["junk", (smk, 0)], accum_out=sm[:, 0:1])
                rms_rstd(P, sm[:, 0:1], (smk, 0), sm[:, 2:3], (smk, 2), neghalf, 1024, sm[:, 1:2], (smk, 1))
                t1, t1k = t1r.next(); h2, h2k = h2r.next(); h2T32, hTk = h2Tr.next()
                P.v(lambda e, x_ap=x_ap, sm=sm, A=A, t1=t1: e.scalar_tensor_tensor(
                    t1[:], x_ap[:], sm[:, 2:3], A[:], ALU.mult, ALU.mult), [xk, (smk, 2), Ak], [t1k])
                P.v(lambda e, B=B, t1=t1, h2=h2: e.tensor_tensor(h2[:], t1[:], B[:], ALU.add), [t1k, Bk], [h2k])
                P.act(h2b[:, ti, :], h2[:], AF.Copy, [h2k], [("h2b", ti)])
                for half in range(2):
                    bk, bkey = nA()
                    for j in range(4):
                        kc = half * 4 + j
                        P.tr(bk[:, j * 128:(j + 1) * 128], h2[:, kc * 128:(kc + 1) * 128], ident[:], [h2k, "ident"], [bkey])
                    P.v(lambda e, bk=bk, half=half, h2T32=h2T32: e.tensor_copy(
                        h2T32[:, half * 4:(half + 1) * 4, :], bk[:, :].rearrange("p (a b) -> p a b", a=4)),
                        [bkey], [(hTk, half)])
                lg, lgk = nB()
                for kc in range(8):
                    P.mm(lg[:, 0:36], h2T32[:, kc, :], w_r[:, kc, :], kc == 0, kc == 7,
                         [(hTk, kc // 4), ("w_r", 0), ("w_r", 1)], [lgk])
                P.v(lambda e, lg=lg, ti=ti: e.tensor_copy(LG[:, ti, :], lg[:, 0:36]), [lgk], [("LG", ti)])
            lgk_all = [("LG", ti) for ti in range(NTl)]
            NT = NTl
            G = LG[:, :, 0:4]
            E4 = LG[:, :, 4:36].rearrange("p t (g e) -> p t g e", g=4)
            bc = lambda ap2, n: ap2.unsqueeze(2).to_broadcast([128, NT, n])
            P.v(lambda e: e.tensor_reduce(rs_[:, 0, :], G, AX.X, ALU.max), lgk_all, ["gmax"])
            P.v(lambda e: e.tensor_tensor(goh[:], G, bc(rs_[:, 0, :], 4), ALU.is_equal), lgk_all + ["gmax"], ["goh"])
            P.v(lambda e: e.tensor_tensor(gex[:], G, bc(rs_[:, 0, :], 4), ALU.subtract), lgk_all + ["gmax"], ["gex"])
            P.act(gex[:], gex[:], AF.Exp, ["gex"], ["gex"])
            P.v(lambda e: e.tensor_reduce(rs_[:, 1, :], gex[:], AX.X, ALU.add), ["gex"], ["gsum"])
            P.v(lambda e: e.reciprocal(rs_[:, 2, :], rs_[:, 1, :]), ["gsum"], ["pmax"])
            P.v(lambda e: e.tensor_tensor(etmp[:], E4, goh[:].unsqueeze(3).to_broadcast([128, NT, 4, 8]), ALU.mult),
                lgk_all + ["goh"], ["etmp"])
            P.v(lambda e: e.tensor_tensor(ein[:], etmp[:, :, 0, :], etmp[:, :, 1, :], ALU.add), ["etmp"], ["ein"])
            P.v(lambda e: e.tensor_tensor(ein[:], ein[:], etmp[:, :, 2, :], ALU.add), ["etmp", "ein"], ["ein"])
            P.v(lambda e: e.tensor_tensor(ein[:], ein[:], etmp[:, :, 3, :], ALU.add), ["etmp", "ein"], ["ein"])
            P.v(lambda e: e.tensor_reduce(rs_[:, 3, :], ein[:], AX.X, ALU.max), ["ein"], ["m1"])
            P.v(lambda e: e.tensor_tensor(oh1[:], ein[:], bc(rs_[:, 3, :], 8), ALU.is_equal), ["ein", "m1"], ["oh1"])
            P.v(lambda e: e.scalar_tensor_tensor(e2[:], oh1[:], -1e30, ein[:], ALU.mult, ALU.add), ["oh1", "ein"], ["e2"])
            P.v(lambda e: e.tensor_reduce(rs_[:, 4, :], e2[:], AX.X, ALU.max), ["e2"], ["m2"])
            P.v(lambda e: e.tensor_tensor(oh2[:], e2[:], bc(rs_[:, 4, :], 8), ALU.is_equal), ["e2", "m2"], ["oh2"])
            P.v(lambda e: e.tensor_tensor(rs_[:, 5, :], rs_[:, 4, :], rs_[:, 3, :], ALU.subtract), ["m1", "m2"], ["dd"])
            P.act(rs_[:, 6, :], rs_[:, 5, :], AF.Exp, ["dd"], ["ed"])
            P.v(lambda e: e.tensor_scalar(rs_[:, 7, :], rs_[:, 6, :], 1.0, None, ALU.add), ["ed"], ["w1"])
            P.v(lambda e: e.reciprocal(rs_[:, 7, :], rs_[:, 7, :]), ["w1"], ["w1"])
            P.v(lambda e: e.tensor_tensor(wAB[:, 0, :], rs_[:, 7, :], rs_[:, 2, :], ALU.mult), ["w1", "pmax"], ["wA"])
            P.v(lambda e: e.tensor_tensor(wAB[:, 1, :], wAB[:, 0, :], rs_[:, 6, :], ALU.mult), ["wA", "ed"], ["wB"])
            s4 = lambda ap3: ap3[:].rearrange("p t (g e) -> p t g e", g=4)
            P.v(lambda e: e.tensor_tensor(s4(selA), oh1[:].unsqueeze(2).to_broadcast([128, NT, 4, 8]),
                                          goh[:].unsqueeze(3).to_broadcast([128, NT, 4, 8]), ALU.mult), ["oh1", "goh"], ["selA"])
            P.v(lambda e: e.tensor_tensor(s4(selB), oh2[:].unsqueeze(2).to_broadcast([128, NT, 4, 8]),
                                          goh[:].unsqueeze(3).to_broadcast([128, NT, 4, 8]), ALU.mult), ["oh2", "goh"], ["selB"])
            P.v(lambda e: e.tensor_tensor(selm[:], selA[:], selB[:], ALU.add), ["selA", "selB"], ["selm"])
            for ti in range(NTl):
                P.mm(cntb[:, 0:32], gm[:, 1, :], selm[:, ti, :], ti == 0, ti == NTl - 1, ["gm", "selm"], [cntk])
            seg = sb("seg", [128, 8, 32])
            segT = sb("segT", [32, 128])
            ecol = sb("ecol", [128, NST])
            sti = sb("sti", [128, 2, NST, 32])
            stc = sb("stc", [128, 64])
            P.dma(stc[:], D["stile_c"][:, :], [], ["stc"])
            widxf = sb("widxf", [128, NST, 6])
            slotf = sb("slotf", [128, 2, NTl])
            P.v(lambda e: e.tensor_copy(seg[:, 0, :], cntb[:, 0:32]), [cntk], ["cnt"])
            P.v(lambda e: e.tensor_scalar(seg[:, 1, :], seg[:, 0, :], 0.0, None, ALU.is_gt), ["cnt"], ["nst"])
            for k in range(1, (T + STILE - 1) // STILE + 1):
                P.v(lambda e, k=k: e.scalar_tensor_tensor(seg[:, 1, :], seg[:, 0, :], float(STILE * k), seg[:, 1, :],
                                                          ALU.is_gt, ALU.add), ["cnt", "nst"], ["nst"])
            P.v(lambda e: e.tensor_scalar(seg[:, 2, :], seg[:, 1, :], float(STILE), None, ALU.mult), ["nst"], ["pc"])
            bk, bkey = nA()
            P.tr(bk[0:32, 0:128], seg[:, 2, :], ident[:], ["pc", "ident"], [bkey])
            P.v(lambda e, bk=bk: e.tensor_copy(segT[:], bk[0:32, 0:128]), [bkey], ["segT"])
            bk2, bkey2 = nA()
            P.mm(bk2[:, 0:32], segT[0:32, :], gm[0:32, 0, 0:32], True, True, ["segT", "gm"], [bkey2])
            P.v(lambda e, bk2=bk2: e.tensor_copy(seg[:, 3, :], bk2[:, 0:32]), [bkey2], ["start"])
            P.v(lambda e: e.tensor_tensor(seg[:, 4, :], seg[:, 3, :], seg[:, 2, :], ALU.add), ["start", "pc"], ["end"])
            P.v(lambda e: e.tensor_copy(seg[:, 5, :], seg[:, 3, :]), ["start"], ["base"])
            bci = lambda ap2: ap2.unsqueeze(1).to_broadcast([128, NST, 32])
            cI = stc[:, 0:NST].unsqueeze(2).to_broadcast([128, NST, 32])
            P.v(lambda e: e.tensor_tensor(sti[:, 0, :, :], bci(seg[:, 3, :]), cI, ALU.is_le), ["start", "stc"], ["sti0"])
            P.v(lambda e: e.tensor_tensor(sti[:, 1, :, :], bci(seg[:, 4, :]), cI, ALU.is_gt), ["end", "stc"], ["sti1"])
            P.v(lambda e: e.tensor_tensor(sti[:, 0, :, :], sti[:, 0, :, :], sti[:, 1, :, :], ALU.mult), ["sti0", "sti1"], ["sti0"])
            P.v(lambda e: e.tensor_tensor(sti[:, 0, :, :], sti[:, 0, :, :], bci(eidrow[:]), ALU.mult), ["sti0", "eidrow"], ["sti0"])
            P.v(lambda e: e.tensor_reduce(ecol[:, :], sti[:, 0, :, :], AX.X, ALU.add), ["sti0"], ["ecol"])
            ek = ["ecol"]
            for a_ in range(2):
                P.v(lambda e, a_=a_: e.tensor_scalar(widxf[:, :, a_], ecol[:, :], 256.0, pc2[:, a_:a_ + 1], ALU.mult, ALU.add),
                    ek + ["pc2"], [("widxf", a_)])
            for fc in range(4):
                P.v(lambda e, fc=fc: e.tensor_scalar(widxf[:, :, 2 + fc], ecol[:, :], 512.0, pc2[:, 2 + fc:3 + fc], ALU.mult, ALU.add),
                    ek + ["pc2"], [("widxf", 2 + fc)])
            P.v(lambda e: e.tensor_copy(widx[:].rearrange("p a b -> p (a b)"), widxf[:].rearrange("p a b -> p (a b)")),
                [("widxf", j) for j in range(6)], ["widx"])
            for ti in range(NTl):
                wi, wik = nB()
                P.mm(wi[:, 0:32], gm[:, 0, :], selm[:, ti, :], True, True, ["gm", "selm"], [wik])
                P.mm(wi[:, 32:64], gm[:, 1, :], selm[:, ti, :], True, True, ["gm", "selm"], [wik])
                P.v(lambda e, wi=wi: e.tensor_tensor(seg[:, 6, :], wi[:, 0:32], seg[:, 5, :], ALU.add), [wik, "base"], ["segtmp"])
                P.v(lambda e, ti=ti: e.scalar_tensor_tensor(seg[:, 7, :], seg[:, 6, :], 1.0, selA[:, ti, :], ALU.mult, ALU.mult,
                                                            accum_out=slotf[:, 0, ti:ti + 1]), ["segtmp", "selA"],
                    ["segtmp2", ("slotA", ti)])
                P.v(lambda e, ti=ti: e.scalar_tensor_tensor(seg[:, 7, :], seg[:, 6, :], 1.0, selB[:, ti, :], ALU.mult, ALU.mult,
                                                            accum_out=slotf[:, 1, ti:ti + 1]), ["segtmp", "selB"],
                    ["segtmp2", ("slotB", ti)])
                P.v(lambda e, wi=wi: e.tensor_tensor(seg[:, 5, :], wi[:, 32:64], seg[:, 5, :], ALU.add), [wik, "base"], ["base"])
            P.v(lambda e: e.tensor_copy(idxA[:], slotf[:, 0, :]), [("slotA", ti) for ti in range(NTl)], ["idxA"])
            P.v(lambda e: e.tensor_copy(idxB[:], slotf[:, 1, :]), [("slotB", ti) for ti in range(NTl)], ["idxB"])
            for ti in range(NTl):
                for (ix, ixk) in ((idxA, "idxA"), (idxB, "idxB")):
                    P.op("pool", lambda e, ix=ix, ti=ti: e.indirect_dma_start(
                        out=Xs[0:NSLOT, :], out_offset=bass.IndirectOffsetOnAxis(ap=ix[:, ti:ti + 1], axis=0),
                        in_=h2b[:, ti, :], in_offset=None, bounds_check=None),
                        [("h2b", ti), ixk] + zkeys, [("Xs", ti, ixk)], dma=True, kind="dma")
            P.flush()
        with contextlib.ExitStack() as st:
            sb = lambda name, shape, dt=F32: st.enter_context(nc.sbuf_tensor(tag + name, shape, dt))
            nT, nGU, nY = bank_rot(banks, 0, 2), bank_rot(banks, 2, 6), bank_rot(banks, 6, 8)
            ident = sb("identb", [128, 128], BF16)
            P.dma(ident[:], D["identbf"][:, :], [], ["ident"])
            wg = Rot("wg", [sb("wg%d" % i, [128, 8, 512], BF16) for i in range(2)])
            wu = Rot("wu", [sb("wu%d" % i, [128, 8, 512], BF16) for i in range(2)])
            wd = Rot("wd", [sb("wd%d" % i, [128, 4, 1024], BF16) for i in range(2)])
            xr = Rot("xr", [sb("xr%d" % i, [128, 1024], BF16) for i in range(4)])
            XT = Rot("XT", [sb("XT%d" % i, [128, 8, STILE], BF16) for i in range(2)])
            hg = Rot("hg", [sb("hg%d" % i, [128, 4, STILE], BF16) for i in range(2)])
            sg = Rot("sg", [sb("sg%d" % i, [128, STILE]) for i in range(2)])
            yb = Rot("yb", [sb("yb%d" % i, [128, 1024]) for i in range(3)])

            def gather(dst2d, src, col, i, key):
                P.op("pool", lambda e: e.indirect_dma_start(
                    out=dst2d, out_offset=None, in_=src,
                    in_offset=bass.IndirectOffsetOnAxis(ap=widx[:, i, col:col + 1], axis=0),
                    bounds_check=None), [], [key], dma=True, kind="dma")

            def emit_gu(i):
                g_ap, gk = wg.next(); u_ap, uk = wu.next(); d_ap, dk = wd.next()
                for a_ in range(2):
                    gather(g_ap[:, 4 * a_:4 * a_ + 4, :].rearrange("p a f -> p (a f)"), wgv, a_, i, (gk, a_))
                    gather(u_ap[:, 4 * a_:4 * a_ + 4, :].rearrange("p a f -> p (a f)"), wuv, a_, i, (uk, a_))
                for fc in range(4):
                    gather(d_ap[:, fc, :], wdv, 2 + fc, i, (dk, fc))
                xT, xTk = XT.next()
                for b in range(NB):
                    x_ap, xk = xr.next()
                    P.dma(x_ap[:], Xs[i * STILE + b * 128:i * STILE + (b + 1) * 128, :], [], [xk])
                    bk, bkey = nT()
                    bkb = bk[:].bitcast(BF16)
                    xv = x_ap[:].rearrange("p (m kc) -> p kc m", kc=8)
                    for kc in range(8):
                        P.tr(bkb[:, kc * 128:(kc + 1) * 128], xv[:, kc, :], ident[:], [xk, "ident"], [bkey])
                    P.act(xT[:, :, b * 128:(b + 1) * 128], bkb.rearrange("p (a b) -> p a b", a=8), AF.Copy, [bkey], [(xTk, b)])
                xkeys = [(xTk, b) for b in range(NB)]
                hgt, hgk = hg.next()
                for fc in range(4):
                    Gp, Gk = nGU(); Up, Uk = nGU()
                    for kc in range(8):
                        P.mm(Gp[:, 0:STILE], g_ap[:, kc, fc * 128:(fc + 1) * 128], xT[:, kc, :], kc == 0, kc == 7,
                             [(gk, kc // 4)] + xkeys, [Gk])
                    for kc in range(8):
                        P.mm(Up[:, 0:STILE], u_ap[:, kc, fc * 128:(fc + 1) * 128], xT[:, kc, :], kc == 0, kc == 7,
                             [(uk, kc // 4)] + xkeys, [Uk])
                    s_ap, sk = sg.next()
                    P.act(s_ap[:, :], Gp[:, 0:STILE], AF.Silu, [Gk], [sk])
                    P.v(lambda e, hgt=hgt, Up=Up, s_ap=s_ap, fc=fc: e.tensor_tensor(
                        hgt[:, fc, :], Up[:, 0:STILE], s_ap[:, :], ALU.mult), [Uk, sk], [(hgk, fc)])
                return (i, hgt, hgk, d_ap, dk)

            def emit_down(i, hgt, hgk, d_ap, dk):
                for b in range(NB):
                    y_ap, yk = yb.next()
                    for dh in range(2):
                        Yp, Yk = nY()
                        for fc in range(4):
                            P.mm(Yp[:, :], hgt[:, fc, b * 128:(b + 1) * 128], d_ap[:, fc, dh * 512:(dh + 1) * 512],
                                 fc == 0, fc == 3, [(hgk, f) for f in range(4)] + [(dk, fc)], [Yk])
                        if dh == 0:
                            P.act(y_ap[:, 0:512], Yp[:, :], AF.Copy, [Yk], [(yk, 0)])
                        else:
                            P.v(lambda e, y_ap=y_ap, Yp=Yp: e.tensor_copy(y_ap[:, 512:1024], Yp[:, :]), [Yk], [(yk, 1)])
                    r0 = i * STILE + b * 128
                    P.dma(Ys[r0:r0 + 128, :], y_ap[:], [(yk, 0), (yk, 1)], [("Ys", r0)])

            pend = None
            for i in range(NST):
                cur = emit_gu(i)
                if pend is not None:
                    emit_down(*pend)
                pend = cur
            emit_down(*pend)
            P.flush()
        with contextlib.ExitStack() as st:
            sb = lambda name, shape, dt=F32: st.enter_context(nc.sbuf_tensor(tag + name, shape, dt))
            G = load_mod_rows(P, nc, st, D["mods"], layer, GT2, tag + "G")
            xt = Rot("mxt2", [sb("xt2_%d" % i, [128, 1024]) for i in range(4)])
            ya = Rot("ya", [sb("ya%d" % i, [128, 1024]) for i in range(4)])
            ybb = Rot("ybb", [sb("ybb%d" % i, [128, 1024]) for i in range(4)])
            if final_norm:
                fg = sb("fg", [128, 1024])
                P.dma(fg[:], D["final_norm_g"][0:1, :].partition_broadcast(128), [], ["fg"])
                junk = sb("junk3", [128, 1024])
                small = Rot("fsmall", [sb("fsmall%d" % i, [128, 4]) for i in range(2)])
            for ti, (r, var) in enumerate(tiles):
                x_ap, xk = xt.next()
                P.dma(x_ap[:], D[xin][r * 128:(r + 1) * 128, :], [], [xk])
                a_ap, ak = ya.next(); b_ap, bk_ = ybb.next()
                for (dst, dkey, ix) in ((a_ap, ak, idxA), (b_ap, bk_, idxB)):
                    P.op("pool", lambda e, dst=dst, ix=ix, ti=ti: e.indirect_dma_start(
                        out=dst[:, :], out_offset=None, in_=Ys[0:NSLOT, :],
                        in_offset=bass.IndirectOffsetOnAxis(ap=ix[:, ti:ti + 1], axis=0),
                        bounds_check=None), [], [dkey], dma=True, kind="dma")
                P.v(lambda e, a_ap=a_ap, ti=ti: e.tensor_scalar(a_ap[:], a_ap[:], wAB[:, 0, ti:ti + 1], None, ALU.mult), [ak], [ak])
                P.v(lambda e, a_ap=a_ap, b_ap=b_ap, ti=ti: e.scalar_tensor_tensor(
                    a_ap[:], b_ap[:], wAB[:, 1, ti:ti + 1], a_ap[:], ALU.mult, ALU.add), [ak, bk_], [ak])
                P.v(lambda e, a_ap=a_ap, var=var: e.tensor_tensor(a_ap[:], a_ap[:], G[var][0][:], ALU.mult), [ak, G[var][1]], [ak])
                P.v(lambda e, a_ap=a_ap, x_ap=x_ap: e.tensor_tensor(a_ap[:], a_ap[:], x_ap[:], ALU.add), [ak, xk], [ak])
                if final_norm:
                    sm, smk = small.next()
                    P.act(junk[:], a_ap[:], AF.Square, [ak], ["junk3", (smk, 0)], accum_out=sm[:, 0:1])
                    rms_rstd(P, sm[:, 0:1], (smk, 0), sm[:, 2:3], (smk, 2), neghalf, 1024, sm[:, 1:2], (smk, 1))
                    P.v(lambda e, a_ap=a_ap, sm=sm: e.scalar_tensor_tensor(
                        a_ap[:], a_ap[:], sm[:, 2:3], fg[:], ALU.mult, ALU.mult), [ak, (smk, 2), "fg"], [ak])
                P.dma(D[xout][r * 128:(r + 1) * 128, :], a_ap[:], [ak], [(xout, r)])
            P.flush()

import numpy as np
import ml_dtypes

BF = ml_dtypes.bfloat16
GRID_W = 64

W_SMALL = {
    "ada_w": [2, 1024, 6144], "ada_b": [2, 6144], "norm_mix_g": [2, 1024], "norm_ffn_g": [2, 1024],
    "even_w_in": [1, 1024, 1184], "mla_q_norm_g": [1, 256], "mla_w_uq": [1, 256, 768], "mla_kv_norm_g": [1, 128],
    "mla_w_ukv": [1, 128, 1024], "win_sink": [1, 8], "even_w_out": [1, 1024, 1024],
    "odd_w_in": [1, 1024, 2592], "gla_w_g2": [1, 2, 16, 256], "gla_b_g": [1, 2, 256], "gla_norm_g": [1, 512],
    "sg_ln_g": [1, 512], "sg_ln_b": [1, 512], "odd_w_out": [1, 1024, 1024],
    "moe_w_rg": [2, 1024, 4], "moe_w_re": [2, 1024, 32], "final_norm_g": [1, 1024],
}
W_MOE = {"moe_w_gate": [32, 1024, 512], "moe_w_up": [32, 1024, 512], "moe_w_down": [32, 512, 1024]}
CONSTS = {"wmask": ([2, 128, 512], BF16), "ident32": ([128, 128], F32), "identbf": ([128, 128], BF16),
          "gmask": ([5, 128, 128], F32), "eidrow": ([128, 32], F32), "pc2": ([128, 6], F32), "stile_c": ([128, 64], F32)}
PERCORE = {"xtok": [NTOK, 1024], "cvec": [2, 1024], "cA": [NTOK, 256], "sA": [NTOK, 256], "cB": [NTOK, 512],
           "sB": [NTOK, 512], "sg_w_sT": [4, 128, 128], "sg_b_sT": [128, 4], "sel": [128, 2]}
SCRATCH = {"mods": ([2, 2, 6144], F32), "QAT": ([96, 8, NTOK], BF16), "KAT": ([96, 8, NTOK], BF16),
           "VA": ([NTOK, 520], BF16), "QBT": ([64, 8, NTOK], BF16), "KBT": ([64, 2, NTOK], BF16),
           "VB": ([NTOK, 130], BF16), "x1": ([NOWN, 1024], F32), "x2": ([NOWN, 1024], F32),
           "g_T": ([128, 18, 8, 128], BF16), "g_kd": ([2, NOWN, 256], BF16), "g_v": ([NOWN, 512], BF16),
           "g_dec": ([128, 18, 4], F32), "rsilu": ([2048, 512], BF16), "dl": ([2048, 512], BF16),
           "OA": ([2048, 512], F32), "cc_in": ([256, 128], F32), "cc_out": ([512, 128], F32),
           "x3": ([2048, 1024], F32), "out": ([2048, 1024], F32),
           "Xs": ([n_stiles(NOWN) * STILE, 1024], BF16), "Ys": ([n_stiles(NOWN) * STILE, 1024], F32)}
HANDOFF = ["mods", "x2", "g_T", "g_kd", "g_v", "g_dec", "rsilu", "dl", "OA"]


def rope_tables(pos, dim, nheads):
    pos = np.asarray(pos)
    half = dim // 2
    inv = np.power(np.float32(10000.0), -np.arange(0, half, 2, dtype=np.float32) / np.float32(half)).astype(np.float32)
    row = (pos // GRID_W).astype(np.float32)
    col = (pos % GRID_W).astype(np.float32)
    ar = row[:, None] * inv[None, :]
    ac = col[:, None] * inv[None, :]
    ang = np.concatenate([ar, ar, ac, ac], axis=-1).astype(np.float32)
    cos = np.cos(ang).astype(np.float32)
    sin = np.sin(ang).astype(np.float32)
    blk = dim // 4
    sign = np.concatenate([-np.ones(blk), np.ones(blk), -np.ones(blk), np.ones(blk)]).astype(np.float32)
    ssin = sin * sign[None, :]
    no = pos < 0
    cos[no] = 1.0
    ssin[no] = 0.0
    return np.tile(cos, (1, nheads)), np.tile(ssin, (1, nheads))


_CONST = {}


def const_inputs():
    if not _CONST:
        j = np.arange(128)[:, None]
        i = np.arange(128)[None, :]
        m0 = np.tile((j >= i).astype(np.float32), (1, 4))
        m1 = np.tile((j <= i).astype(np.float32), (1, 4))
        _CONST["wmask"] = np.stack([m0, m1]).astype(BF)
        _CONST["ident32"] = np.eye(128, dtype=np.float32)
        _CONST["identbf"] = np.eye(128, dtype=np.float32).astype(BF)
        one = np.ones((128, 128), bool)
        _CONST["gmask"] = np.stack([(j <= i), (j >= i), one, (j < i), one]).astype(np.float32)
        _CONST["eidrow"] = np.tile(np.arange(32, dtype=np.float32)[None, :], (128, 1))
        p = np.arange(128, dtype=np.float32)
        _CONST["stile_c"] = np.tile((np.arange(64, dtype=np.float32) * STILE)[None, :], (128, 1))
        _CONST["pc2"] = np.stack([2 * p, 2 * p + 1, p, 128 + p, 256 + p, 384 + p], 1).astype(np.float32)
    return _CONST


def local_order(hf):
    own = np.arange(hf * 2048, (hf + 1) * 2048)
    oth = np.arange((1 - hf) * 2048, (2 - hf) * 2048)
    cidx = np.arange(256)
    if hf == 1:
        own, oth, cidx = own[::-1], oth[::-1], cidx[::-1]
    return own, oth, cidx


def weights_for(core, inp, layers=(0, 1)):
    hf = core % 2
    m = {}
    for k, shp in W_SMALL.items():
        m[k] = np.ascontiguousarray(np.asarray(inp[k]).reshape(shp))
    for l in layers:
        for k in W_MOE:
            m["%s%d" % (k, l)] = np.asarray(inp[k][l])
    ws = np.asarray(inp["sg_w_s"][0])
    bs = np.asarray(inp["sg_b_s"][0])
    if hf == 1:
        m["gla_w_g2"] = np.ascontiguousarray(m["gla_w_g2"][:, ::-1])
        m["gla_b_g"] = np.ascontiguousarray(m["gla_b_g"][:, ::-1])
        w = m["odd_w_in"].copy()
        w[:, :, 1024:1040] = m["odd_w_in"][:, :, 1040:1056]
        w[:, :, 1040:1056] = m["odd_w_in"][:, :, 1024:1040]
        m["odd_w_in"] = w
        ws = ws[:, ::-1, ::-1]
        bs = bs[:, ::-1]
    m["sg_w_sT"] = np.ascontiguousarray(ws.transpose(0, 2, 1))
    m["sg_b_sT"] = np.ascontiguousarray(bs.T)
    return m


def core_inputs(core, inp):
    b, hf = core // 2, core % 2
    own, oth, cidx = local_order(hf)
    pos = np.concatenate([own, oth, -np.ones(256, dtype=np.int64)])
    xtok = np.concatenate([inp["x"][b][own], inp["x"][b][oth], inp["ctx"][b][cidx]], 0)
    cA, sA = rope_tables(pos, 32, 8)
    cB, sB = rope_tables(pos, 64, 8)
    m = {"xtok": np.ascontiguousarray(xtok), "cvec": np.stack([inp["c"][b], inp["c_ctx"]]).astype(np.float32),
         "cA": cA, "sA": sA, "cB": cB, "sB": sB}
    sel = np.zeros((128, 2), np.float32)
    sel[:, 1 - hf] = 1.0
    m["sel"] = sel
    m.update(const_inputs())
    return m


def declare(nc, ext_in, ext_out, moe_layers=(0, 1)):
    D = {}
    dr = lambda n, s, dt=F32, k="ExternalInput": nc.dram_tensor(n, s, dt, kind=k).ap()
    for k, shp in PERCORE.items():
        D[k] = dr(k, shp)
    for k, (shp, dt) in CONSTS.items():
        D[k] = dr(k, shp, dt)
    for k, shp in W_SMALL.items():
        D[k] = dr(k, shp)
    for l in moe_layers:
        for k, shp in W_MOE.items():
            D["%s%d" % (k, l)] = dr("%s%d" % (k, l), shp)
    for k, (shp, dt) in SCRATCH.items():
        kind = "ExternalOutput" if k in ext_out else ("ExternalInput" if k in ext_in else "Internal")
        D[k] = dr(k, shp, dt, kind)
    return D


MOE0_TILES = [(i, 0) for i in range(16)] + [(16, 1), (17, 1)]
MOE1_TILES = [(i, 0) for i in range(16)]


def build_fused(extra_out=()):
    nc = bass.Bass("TRN2", target_bir_lowering=False)
    D = declare(nc, (), ["out"] + list(extra_out), moe_layers=(0, 1))
    banks = [nc.alloc_psum_tensor("bank%d" % i, [128, 512], F32) for i in range(8)]
    P = Prog(nc)
    phase_ada(nc, P, D, banks)
    phase_l0a(nc, P, D, banks)
    phase_l0b(nc, P, D, banks)
    phase_moe_sparse(nc, P, D, banks, 0, "x1", "x2", MOE0_TILES, "m0_")
    phase_l1a(nc, P, D, banks)
    phase_l1b_a(nc, P, D, banks)
    phase_l1b_b(nc, P, D, banks)
    phase_moe_sparse(nc, P, D, banks, 1, "x3", "out", MOE1_TILES, "m1_", final_norm=True)
    P.flush(final=True)
    return nc


def kernel(**inputs):
    inp = {k: np.asarray(v) for k, v in inputs.items()}
    n = 8
    nc = build_fused()
    in_maps = []
    for c in range(n):
        m = core_inputs(c, inp)
        m.update(weights_for(c, inp, layers=(0, 1)))
        in_maps.append(m)
    res = run_bass_kernel_spmd(nc, in_maps, core_ids=list(range(n))).results
    out = np.zeros((4, 4096, 1024), np.float32)
    for c in range(n):
        b, hf = c // 2, c % 2
        own = local_order(hf)[0]
        out[b][own] = np.asarray(res[c]["out"], dtype=np.float32)
    return out
```

```python
import contextlib
import numpy as np
import concourse.bass as bass
import concourse.mybir as mybir
from concourse.bass_utils import run_bass_kernel_spmd

F32 = mybir.dt.float32
BF16 = mybir.dt.bfloat16
AF = mybir.ActivationFunctionType
ALU = mybir.AluOpType
AX = mybir.AxisListType
N_DMA_SEMS = 10


class Op:
    __slots__ = ("eng", "fn", "deps", "signal", "sem", "val", "is_dma", "idx", "kind", "sem_eng")

    def __init__(self, eng, fn, is_dma, kind):
        self.eng = eng
        self.fn = fn
        self.deps = set()
        self.signal = False
        self.sem = None
        self.val = 0
        self.is_dma = is_dma
        self.kind = kind
        self.sem_eng = None


class Rot:
    def __init__(self, name, aps):
        self.name = name
        self.aps = aps
        self.i = 0

    def next(self):
        k = self.i % len(self.aps)
        self.i += 1
        return self.aps[k], (self.name, k)


class Prog:
    ENGS = ("pe", "act", "dve", "pool", "sp")

    def __init__(self, nc):
        self.nc = nc
        self.st = contextlib.ExitStack()
        st = self.st
        self.esem = {e: st.enter_context(nc.semaphore("s_" + e)) for e in ("pe", "act", "dve", "pool", "cc")}
        self.dsem = {e: [st.enter_context(nc.semaphore("d_%s%d" % (e, i))) for i in range(N_DMA_SEMS)]
                     for e in ("sp", "act", "pool")}
        self.cnt = {e: 0 for e in self.esem}
        self.dcnt = {e: [0] * N_DMA_SEMS for e in self.dsem}
        self.drr = {e: 0 for e in self.dsem}
        self.nflush = 0
        self.total_ops = 0
        self._reset()

    def _reset(self):
        self.ops = []
        self.last_w = {}
        self.readers = {}

    def op(self, eng, fn, reads=(), writes=(), dma=False, kind=""):
        o = Op(eng, fn, dma, kind)
        o.idx = len(self.ops)
        ex = [r for r in reads if isinstance(r, tuple) and r[0] == "ps"]
        if ex and eng != "pe":
            reads = [r for r in reads if r not in ex]
            writes = list(writes) + ex
        for r in reads:
            w = self.last_w.get(r)
            if w is not None:
                o.deps.add(w)
        for wkey in writes:
            w = self.last_w.get(wkey)
            if w is not None:
                o.deps.add(w)
            for rd in self.readers.get(wkey, ()):
                o.deps.add(rd)
        for r in reads:
            self.readers.setdefault(r, []).append(o.idx)
        for wkey in writes:
            self.last_w[wkey] = o.idx
            self.readers[wkey] = []
        o.deps.discard(o.idx)
        self.ops.append(o)
        return o

    def dma(self, out, in_, reads, writes, eng="sp", **kw):
        return self.op(eng, lambda e: e.dma_start(out=out, in_=in_, **kw), reads, writes, dma=True, kind="dma")

    def mm(self, out, lhsT, rhs, start, stop, reads, writes, **kw):
        return self.op("pe", lambda e: e.matmul(out, lhsT, rhs, start=start, stop=stop, **kw),
                       reads, writes, kind="mm")

    def tr(self, out, in_, ident, reads, writes):
        return self.op("pe", lambda e: e.transpose(out, in_, ident), reads, writes, kind="mm")

    def act(self, out, in_, func, reads, writes, **kw):
        return self.op("act", lambda e: e.activation(out, in_, func, **kw), reads, writes, kind="act")

    def cc(self, fn, reads, writes):
        o = self.op("pool", fn, reads, writes, kind="cc")
        o.sem_eng = "cc"
        o.signal = True
        return o

    def v(self, fn, reads, writes, eng="dve"):
        return self.op(eng, fn, reads, writes, kind="v")

    def flush(self, final=False):
        nc = self.nc
        ops = self.ops
        self.total_ops += len(ops)
        for o in ops:
            if o.eng == "pe":
                o.deps = {d for d in o.deps if ops[d].eng != "pe"}
        for o in ops:
            for d in o.deps:
                ops[d].signal = True
        base_cnt = dict(self.cnt)
        base_dcnt = {e: list(v) for e, v in self.dcnt.items()}
        dprev = {e: [None] * N_DMA_SEMS for e in self.dsem}
        for o in ops:
            if o.is_dma:
                o.signal = True
                k = self.drr[o.eng]
                self.drr[o.eng] = (k + 1) % N_DMA_SEMS
                self.dcnt[o.eng][k] += 16
                o.sem = self.dsem[o.eng][k]
                o.val = self.dcnt[o.eng][k]
                p = dprev[o.eng][k]
                if p is not None:
                    o.deps.add(p)
                dprev[o.eng][k] = o.idx
            elif o.signal:
                se = o.sem_eng or o.eng
                self.cnt[se] += 1
                o.sem = self.esem[se]
                o.val = self.cnt[se]
        first = self.nflush == 0
        self.nflush += 1
        with nc.Block() as blk:
            getters = {"pe": blk.tensor, "act": blk.scalar, "dve": blk.vector, "pool": blk.gpsimd, "sp": blk.sync}
            for ename in self.ENGS:
                mine = [o for o in ops if o.eng == ename]

                def body(e, mine=mine, ename=ename):
                    waited = {}
                    if not first:
                        for en, sem in self.esem.items():
                            if base_cnt[en] > 0 and en != ename:
                                e.wait_ge(sem, base_cnt[en])
                                waited[sem.num] = base_cnt[en]
                        for en, sems in self.dsem.items():
                            for k, sem in enumerate(sems):
                                if base_dcnt[en][k] > 0:
                                    e.wait_ge(sem, base_dcnt[en][k])
                                    waited[sem.num] = base_dcnt[en][k]
                    for o in mine:
                        need = {}
                        for d in o.deps:
                            do = ops[d]
                            if need.get(do.sem.num, (None, 0))[1] < do.val:
                                need[do.sem.num] = (do.sem, do.val)
                        for num, (sem, val) in need.items():
                            if waited.get(num, 0) >= val:
                                continue
                            e.wait_ge(sem, val)
                            waited[num] = val
                        ins = o.fn(e)
                        if o.signal:
                            if o.kind == "cc":
                                ins.then_inc(o.sem)
                            else:
                                ins.then_inc(o.sem, 16 if o.is_dma else 1)
                    if final and ename == "sp":
                        for en, sems in self.dsem.items():
                            for k, sem in enumerate(sems):
                                if self.dcnt[en][k] > 0:
                                    e.wait_ge(sem, self.dcnt[en][k])
                        for en, sem in self.esem.items():
                            if self.cnt[en] > 0:
                                e.wait_ge(sem, self.cnt[en])

                getters[ename](body)
        self._reset()
        if final:
            self.st.close()


def bank_rot(banks, lo, hi):
    state = {"i": 0}

    def nxt():
        k = lo + state["i"] % (hi - lo)
        state["i"] += 1
        return banks[k], ("ps", k)
    return nxt


EPS = 1e-6
NTOK = 4352
NOWN = 2304
SH1, SC1, GT1, SH2, SC2, GT2 = range(6)


def own_tiles():
    return [(i, i, 0) for i in range(16)] + [(16, 32, 1), (17, 33, 1)]


def rms_rstd(P, ss_ap, ss_key, out_ap, out_key, neghalf, D, tmp_ap, tmp_key):
    P.v(lambda e: e.tensor_scalar(tmp_ap, ss_ap, 1.0 / D, EPS, ALU.mult, ALU.add), [ss_key], [tmp_key])
    P.v(lambda e: e.tensor_tensor(out_ap, tmp_ap, neghalf, ALU.pow), [tmp_key, "consts"], [out_key], eng="pool")


def load_mod_rows(P, nc, st, mods_d, layer, which, names):
    out = []
    for v in range(2):
        t = st.enter_context(nc.sbuf_tensor("%s%d" % (names, v), [128, 1024], F32))
        P.dma(t[:], mods_d[layer, v:v + 1, which * 1024:(which + 1) * 1024].partition_broadcast(128), ["mods"],
              [(names, v)])
        out.append((t, (names, v)))
    return out


def phase_ada(nc, P, D, banks):
    with contextlib.ExitStack() as st:
        sb = lambda name, shape, dt=F32: st.enter_context(nc.sbuf_tensor(name, shape, dt))
        PS = Rot("ps", banks)
        ident = sb("ada_ident", [128, 128])
        cb = sb("ada_cb", [128, 2, 1024])
        screp = sb("ada_screp", [128, 2, 8, 128])
        bias = sb("ada_bias", [96, 2, 128])
        wbuf = Rot("ada_w", [sb("ada_wbuf%d" % i, [128, 8, 512]) for i in range(4)])
        accs = sb("ada_accs", [128, 2, 96])
        outT = sb("ada_outT", [96, 2, 128])
        P.dma(ident[:], D["ident32"][:, :], [], ["ident"])
        for v in range(2):
            P.dma(cb[:, v, :], D["cvec"][v:v + 1, :].partition_broadcast(128), [], [("cb", v)])
        for l in range(2):
            for v in range(2):
                P.dma(bias[48 * v:48 * v + 48, l, :], D["ada_b"][l].rearrange("(c p) -> c p", p=128), [], [("bias", l, v)])
        for v in range(2):
            P.act(cb[:, v, :], cb[:, v, :], AF.Silu, [("cb", v)], [("cb", v)])
            for half in range(2):
                p, pk = PS.next()
                for j in range(4):
                    kc = half * 4 + j
                    P.tr(p[:, j * 128:(j + 1) * 128], cb[:, v, kc * 128:(kc + 1) * 128], ident[:],
                         [("cb", v), "ident"], [pk])
                P.v(lambda e, p=p, v=v, half=half: e.tensor_copy(
                    screp[:, v, half * 4:(half + 1) * 4, :], p[:, :].rearrange("p (a b) -> p a b", a=4)),
                    [pk], [("screp", v)])
        for l in range(2):
            wl = D["ada_w"][l].rearrange("(kc p) n -> p kc n", p=128)
            acc, acck = PS.next()
            accv = acc[:, 0:96].rearrange("p (v c) -> p v c", v=2)
            for cblk in range(12):
                wb, wk = wbuf.next()
                P.dma(wb[:], wl[:, :, cblk * 512:(cblk + 1) * 512], [], [wk])
                for j in range(4):
                    c = cblk * 4 + j
                    for kc in range(8):
                        P.mm(accv[:, :, c], wb[:, kc, j * 128:(j + 1) * 128], screp[:, :, kc, 0], kc == 0, kc == 7,
                             [("screp", 0), ("screp", 1), wk], [acck])
            P.v(lambda e, acc=acc, l=l: e.tensor_copy(accs[:, l, :], acc[:, 0:96]), [acck], [("accs", l)])
            tp, tpk = PS.next()
            P.tr(tp[0:96, 0:128], accs[:, l, :], ident[:], [("accs", l), "ident"], [tpk])
            P.v(lambda e, tp=tp, l=l: e.tensor_tensor(outT[:, l, :], tp[0:96, 0:128], bias[:, l, :], ALU.add),
                [tpk, ("bias", l, 0), ("bias", l, 1)], [("outT", l)])
            P.dma(D["mods"][l].rearrange("v (c p) -> (v c) p", p=128), outT[:, l, :], [("outT", l)], ["mods"])
        P.flush()


def phase_l0a(nc, P, D, banks):
    NT = NTOK // 128
    with contextlib.ExitStack() as st:
        sb = lambda name, shape, dt=F32: st.enter_context(nc.sbuf_tensor(name, shape, dt))
        PS = Rot("ps", banks)
        ident = sb("a_ident", [128, 128], BF16)
        consts = sb("a_consts", [128, 4])
        gbc = sb("a_gbc", [128, 1024]); qg = sb("a_qg", [128, 256]); kvg = sb("a_kvg", [128, 128])
        w_in = sb("a_w_in", [128, 8, 1184], BF16)
        w_uq = sb("a_w_uq", [128, 2, 768], BF16)
        w_ukv = sb("a_w_ukv", [128, 1024], BF16)
        xt = Rot("xt", [sb("a_xt%d" % i, [128, 1024]) for i in range(2)])
        tabs = Rot("tabs", [sb("a_tabs%d" % i, [128, 1536]) for i in range(3)])
        junk = sb("a_junk", [128, 1024])
        t1r = Rot("t1", [sb("a_t1_%d" % i, [128, 1024]) for i in range(2)])
        hbr = Rot("hb", [sb("a_hb_%d" % i, [128, 1024], BF16) for i in range(2)])
        hTr = Rot("hT", [sb("a_hT_%d" % i, [128, 8, 128], BF16) for i in range(2)])
        small = Rot("small", [sb("a_small%d" % i, [128, 8]) for i in range(3)])
        zsr = Rot("zs", [sb("a_zs%d" % i, [128, 1184]) for i in range(2)])
        cqn = sb("a_cqn", [128, 384], BF16)
        cqnT = sb("a_cqnT", [128, 3, 128], BF16)
        krr = sb("a_krr", [128, 32], BF16)
        ropet = sb("a_ropet", [128, 2, 512])
        QA = sb("a_QA", [128, 8, 96], BF16); KA = sb("a_KA", [128, 8, 96], BF16)
        QB = sb("a_QB", [128, 8, 64], BF16); KB = sb("a_KB", [128, 2, 64], BF16)
        VAs = Rot("VAs", [sb("a_VAs%d" % i, [128, 8, 65], BF16) for i in range(2)])
        VBs = Rot("VBs", [sb("a_VBs%d" % i, [128, 2, 65], BF16) for i in range(2)])
        oQA = Rot("oQA", [sb("a_oQA%d" % i, [96, 8, 128], BF16) for i in range(2)])
        oKA = Rot("oKA", [sb("a_oKA%d" % i, [96, 8, 128], BF16) for i in range(2)])
        oQB = Rot("oQB", [sb("a_oQB%d" % i, [64, 8, 128], BF16) for i in range(2)])
        oKB = Rot("oKB", [sb("a_oKB%d" % i, [64, 2, 128], BF16) for i in range(2)])
        Abc = load_mod_rows(P, nc, st, D["mods"], 0, SC1, "a_A")
        Bbc = load_mod_rows(P, nc, st, D["mods"], 0, SH1, "a_B")

        P.dma(ident[:], D["identbf"][:, :], [], ["ident"])
        P.v(lambda e: e.memset(consts[:, 0:1], -0.5), [], ["consts"])
        P.dma(gbc[:], D["norm_mix_g"][0:1, :].partition_broadcast(128), [], ["gbc"])
        P.dma(qg[:], D["mla_q_norm_g"][0:1, :].partition_broadcast(128), [], ["qg"])
        P.dma(kvg[:], D["mla_kv_norm_g"][0:1, :].partition_broadcast(128), [], ["kvg"])
        P.dma(w_in[:], D["even_w_in"][0].rearrange("(kc p) n -> p kc n", p=128), [], ["w_in"], eng="pool")
        P.dma(w_uq[:], D["mla_w_uq"][0].rearrange("(kc p) n -> p kc n", p=128), [], ["w_uq"], eng="pool")
        P.dma(w_ukv[:], D["mla_w_ukv"][0], [], ["w_ukv"], eng="pool")
        for i, r in enumerate(VAs.aps):
            P.v(lambda e, r=r: e.memset(r[:, :, 64:65], 1.0), [], [("VAs", i)])
        for i, r in enumerate(VBs.aps):
            P.v(lambda e, r=r: e.memset(r[:, :, 64:65], 1.0), [], [("VBs", i)])
        for v in range(2):
            a, ak = Abc[v]
            P.v(lambda e, a=a: e.scalar_tensor_tensor(a[:], a[:], 1.0, gbc[:], ALU.add, ALU.mult), [ak, "gbc"], [ak])
        neghalf = consts[:, 0:1]
        v3 = lambda ap, h: ap.rearrange("p (h w) -> p h w", h=h)

        def rope(src4, dst4, cos4, ssin4, nh, blk, rkeys, wkeys):
            W = 4 * blk
            tmpa = ropet[:, 0, 0:nh * W].rearrange("p (h w) -> p h w", h=nh)
            tmpb = ropet[:, 1, 0:nh * W].rearrange("p (h w) -> p h w", h=nh)
            P.v(lambda e: e.tensor_tensor(tmpa, src4, cos4, ALU.mult), rkeys, ["ropeA"])
            v5 = lambda a: a.rearrange("p h (q w b) -> p h q w b", q=2, w=2)
            for w in range(2):
                P.v(lambda e, w=w: e.tensor_tensor(
                    v5(tmpb)[:, :, :, w, :], v5(src4)[:, :, :, 1 - w, :], v5(ssin4)[:, :, :, w, :], ALU.mult),
                    rkeys, ["ropeB%d" % w])
            P.v(lambda e: e.tensor_tensor(dst4, tmpa, tmpb, ALU.add), ["ropeA", "ropeB0", "ropeB1"], wkeys)

        def stage_a(t):
            rs = slice(t * 128, (t + 1) * 128)
            var = 1 if t >= 32 else 0
            need_q = (t < 16) or (t >= 32)
            A, Ak = Abc[var]; B, Bk = Bbc[var]
            x_ap, x_key = xt.next()
            P.dma(x_ap[:], D["xtok"][rs, :], [], [x_key])
            tb_ap, tb_key = tabs.next()
            P.dma(tb_ap[:, 0:256], D["cA"][rs, :], [], [(tb_key, 0)])
            P.dma(tb_ap[:, 256:512], D["sA"][rs, :], [], [(tb_key, 1)])
            P.dma(tb_ap[:, 512:1024], D["cB"][rs, :], [], [(tb_key, 2)])
            P.dma(tb_ap[:, 1024:1536], D["sB"][rs, :], [], [(tb_key, 3)])
            tkeys = [(tb_key, i) for i in range(4)]
            cA = tb_ap[:, 0:256]; sA = tb_ap[:, 256:512]; cB = tb_ap[:, 512:1024]; sB = tb_ap[:, 1024:1536]
            sm, sm_key = small.next()
            P.act(junk[:], x_ap[:], AF.Square, [x_key], ["junk", (sm_key, 0)], accum_out=sm[:, 0:1])
            rms_rstd(P, sm[:, 0:1], (sm_key, 0), sm[:, 2:3], (sm_key, 2), neghalf, 1024, sm[:, 1:2], (sm_key, 1))
            t1, t1k = t1r.next(); hb, hbk = hbr.next(); hT, hTk = hTr.next()
            P.v(lambda e, x_ap=x_ap, sm=sm, A=A, t1=t1: e.scalar_tensor_tensor(
                t1[:], x_ap[:], sm[:, 2:3], A[:], ALU.mult, ALU.mult), [x_key, (sm_key, 2), Ak], [t1k])
            P.v(lambda e, B=B, t1=t1, hb=hb: e.tensor_tensor(hb[:], t1[:], B[:], ALU.add), [t1k, Bk], [hbk])
            bk, bkey = PS.next()
            bkb = bk[:].bitcast(BF16)
            for kc in range(8):
                P.tr(bkb[:, kc * 128:(kc + 1) * 128], hb[:, kc * 128:(kc + 1) * 128], ident[:], [hbk, "ident"], [bkey])
            P.act(hT[:].rearrange("p a b -> p (a b)"), bkb, AF.Copy, [bkey], [hTk])
            blocks = [(0, 416), (416, 928), (928, 1184)]
            zb = []
            for bi, (c0, c1) in enumerate(blocks):
                if bi == 1 and not need_q:
                    zb.append((None, None))
                    continue
                bk, bkey = PS.next()
                for kc in range(8):
                    P.mm(bk[:, 0:c1 - c0], hT[:, kc, :], w_in[:, kc, c0:c1], kc == 0, kc == 7, [hTk, "w_in"], [bkey])
                zb.append((bk, bkey))
            zs, zsk = zsr.next()
            for bi, (c0, c1) in enumerate(blocks):
                if zb[bi][0] is None:
                    continue
                P.act(zs[:, c0:c1], zb[bi][0][:, 0:c1 - c0], AF.Copy, [zb[bi][1]], [(zsk, bi)])
            return dict(t=t, rs=rs, need_q=need_q, zs=zs, zsk=zsk, tb_ap=tb_ap, tkeys=tkeys, sm=sm, sm_key=sm_key)

        def stage_b(c):
            t, rs, need_q, zs, zsk, tb_ap, tkeys, sm, sm_key = (c[k] for k in ("t", "rs", "need_q", "zs", "zsk", "tb_ap", "tkeys", "sm", "sm_key"))
            cA = tb_ap[:, 0:256]; sA = tb_ap[:, 256:512]; cB = tb_ap[:, 512:1024]; sB = tb_ap[:, 1024:1536]
            z0, z0k = zs[:, 0:416], (zsk, 0)
            z1, z1k = zs[:, 416:928], (zsk, 1)
            z2, z2k = zs[:, 928:1184], (zsk, 2)
            if need_q:
                P.act(junk[:, 0:256], z0[:, 0:256], AF.Square, [z0k], ["junk", (sm_key, 3)], accum_out=sm[:, 3:4])
                rms_rstd(P, sm[:, 3:4], (sm_key, 3), sm[:, 4:5], (sm_key, 4), neghalf, 256, sm[:, 1:2], (sm_key, 1))
                P.v(lambda e, z0=z0, sm=sm: e.scalar_tensor_tensor(
                    cqn[:, 0:256], z0[:, 0:256], sm[:, 4:5], qg[:], ALU.mult, ALU.mult),
                    [z0k, (sm_key, 4), "qg"], ["cqn"])
            P.act(junk[:, 0:128], z0[:, 256:384], AF.Square, [z0k], ["junk", (sm_key, 5)], accum_out=sm[:, 5:6])
            rms_rstd(P, sm[:, 5:6], (sm_key, 5), sm[:, 6:7], (sm_key, 6), neghalf, 128, sm[:, 1:2], (sm_key, 1))
            P.v(lambda e, z0=z0, sm=sm: e.scalar_tensor_tensor(
                cqn[:, 256:384], z0[:, 256:384], sm[:, 6:7], kvg[:], ALU.mult, ALU.mult),
                [z0k, (sm_key, 6), "kvg"], ["cqn"])
            bk, bkey = PS.next()
            bkb = bk[:].bitcast(BF16)
            for j in (range(3) if need_q else [2]):
                P.tr(bkb[:, j * 128:(j + 1) * 128], cqn[:, j * 128:(j + 1) * 128], ident[:], ["cqn", "ident"], [bkey])
            P.act(cqnT[:].rearrange("p a b -> p (a b)"), bkb[:, 0:384], AF.Copy, [bkey], ["cqnT"])
            rope(v3(z0[:, 384:416], 1), v3(krr[:, :], 1), v3(cA[:, 0:32], 1), v3(sA[:, 0:32], 1), 1, 8,
                 tkeys + [z0k], ["krr"])
            if need_q:
                for hh in range(2):
                    bk, bkey = PS.next()
                    for kc in range(2):
                        P.mm(bk[:, 0:384], cqnT[:, kc, :], w_uq[:, kc, hh * 384:(hh + 1) * 384], kc == 0, kc == 1,
                             ["cqnT", "w_uq"], [bkey])
                    q4 = bk[:, 0:384].rearrange("p (h w) -> p h w", h=4)
                    P.act(QA[:, hh * 4:(hh + 1) * 4, 0:64], q4[:, :, 0:64], AF.Copy, [bkey], [("QA", hh, 0)])
                    rope(q4[:, :, 64:96], QA[:, hh * 4:(hh + 1) * 4, 64:96],
                         v3(cA[:, hh * 128:(hh + 1) * 128], 4), v3(sA[:, hh * 128:(hh + 1) * 128], 4), 4, 8,
                         tkeys + [bkey], [("QA", hh, 1)])
            va, va_key = VAs.next()
            for hh in range(2):
                bk, bkey = PS.next()
                P.mm(bk[:, :], cqnT[:, 2, :], w_ukv[:, hh * 512:(hh + 1) * 512], True, True, ["cqnT", "w_ukv"], [bkey])
                k4 = bk[:, :].rearrange("p (h w) -> p h w", h=4)
                P.act(KA[:, hh * 4:(hh + 1) * 4, 0:64], k4[:, :, 0:64], AF.Copy, [bkey], [("KA", hh, 0)])
                P.v(lambda e, va=va, k4=k4, hh=hh: e.tensor_copy(va[:, hh * 4:(hh + 1) * 4, 0:64], k4[:, :, 64:128]),
                    [bkey], [(va_key, hh)])
            for h in range(8):
                P.v(lambda e, h=h: e.tensor_copy(KA[:, h, 64:96], krr[:, :]), ["krr"], [("KA", h, 1)], eng="pool")
            P.dma(D["VA"][rs, :], va[:].rearrange("p h w -> p (h w)"), [(va_key, 0), (va_key, 1)], [("VA", t)], eng="act")
            if need_q:
                q8 = z1[:, :].rearrange("p (h w) -> p h w", h=8)
                rope(q8, QB[:, :, :], v3(cB[:, :], 8), v3(sB[:, :], 8), 8, 16, tkeys + [z1k], ["QB"])
            k2 = z2[:, 0:128].rearrange("p (h w) -> p h w", h=2)
            rope(k2, KB[:, :, :], v3(cB[:, 0:128], 2), v3(sB[:, 0:128], 2), 2, 16, tkeys + [z2k], ["KB"])
            vb, vb_key = VBs.next()
            P.act(vb[:, :, 0:64], z2[:, 128:256].rearrange("p (h w) -> p h w", h=2), AF.Copy, [z2k], [vb_key])
            P.dma(D["VB"][rs, :], vb[:].rearrange("p h w -> p (h w)"), [vb_key], [("VB", t)], eng="act")
            QAk = [("QA", hh, j) for hh in range(2) for j in range(2)]
            KAk = [("KA", hh, 0) for hh in range(2)] + [("KA", h, 1) for h in range(8)]
            jobs = [(KA, KAk, 8, 96, oKA, "KAT"), (KB, ["KB"], 2, 64, oKB, "KBT")]
            if need_q:
                jobs += [(QA, QAk, 8, 96, oQA, "QAT"), (QB, ["QB"], 8, 64, oQB, "QBT")]
            for (src, skeys, nh, Dh, pool, dname) in jobs:
                bk, bkey = PS.next()
                bkb = bk[:].bitcast(BF16)
                for h in range(nh):
                    P.tr(bkb[0:Dh, h * 128:(h + 1) * 128], src[:, h, :], ident[:], skeys + ["ident"], [bkey])
                ob, okey = pool.next()
                P.act(ob[:].rearrange("p a b -> p (a b)"), bkb[0:Dh, 0:nh * 128], AF.Copy, [bkey], [okey])
                P.dma(D[dname][:, :, rs], ob[:], [okey], [(dname, t)], eng="act")

        pend = None
        for t in range(NT):
            cur = stage_a(t)
            if pend is not None:
                stage_b(pend)
            pend = cur
        stage_b(pend)
        P.flush()


def phase_l0b(nc, P, D, banks):
    with contextlib.ExitStack() as st:
        sb = lambda name, shape, dt=F32: st.enter_context(nc.sbuf_tensor(name, shape, dt))
        nS, nO, nR = bank_rot(banks, 0, 4), bank_rot(banks, 4, 6), bank_rot(banks, 6, 8)
        nOP = bank_rot(banks, 0, 4)
        KBT = sb("b_KBT", [64, 2, NTOK], BF16)
        VB = sb("b_VB", [128, 34, 130], BF16)
        VA = sb("b_VA", [128, 34, 520], BF16)
        w_out = sb("b_wout", [128, 16, 1024], BF16)
        sel = sb("b_sel", [65, 64])
        es = sb("b_es", [64, 8])
        masks = sb("b_masks", [128, 2, 512], BF16)
        G = load_mod_rows(P, nc, st, D["mods"], 0, GT1, "b_G")
        KAh = Rot("KAh", [sb("b_KAh%d" % i, [96, NTOK], BF16) for i in range(2)])
        QAg = Rot("QAg", [sb("b_QAg%d" % i, [96, 8, 512], BF16) for i in range(2)])
        QBg = Rot("QBg", [sb("b_QBg%d" % i, [64, 8, 512], BF16) for i in range(2)])
        LA = 2
        PT = Rot("PT", [sb("b_PT%d" % i, [128, 512], BF16) for i in range(LA + 2)])
        Osb = Rot("Osb", [sb("b_Osb%d" % i, [65, 512]) for i in range(2)])
        rec = Rot("rec", [sb("b_rec%d" % i, [64, 512]) for i in range(2)])
        mixT = sb("b_mixT", [128, 16, 512], BF16)
        xt = Rot("bxt", [sb("b_xt%d" % i, [128, 1024]) for i in range(2)])
        ot = Rot("bot", [sb("b_ot%d" % i, [128, 1024]) for i in range(2)])

        P.dma(KBT[:], D["KBT"][:, :, :], [], ["KBT"])
        P.dma(VB[:], D["VB"].rearrange("(c p) w -> p c w", p=128), [], ["VB"])
        P.dma(VA[:], D["VA"].rearrange("(c p) w -> p c w", p=128), [], ["VA"])
        P.v(lambda e: e.memset(w_out[64:128, :, :], 0.0), [], ["w_out_z"], eng="pool")
        P.v(lambda e: e.memset(mixT[64:128, :, :], 0.0), [], ["mixT_z"], eng="pool")
        P.dma(w_out[0:64, :, :], D["even_w_out"][0].rearrange("(j p) n -> p j n", p=64), [], ["w_out"], eng="pool")
        P.v(lambda e: e.memset(sel[:], 0.0), [], ["sel"])
        P.v(lambda e: e.memset(sel[64:65, :], 1.0), ["sel"], ["sel"])
        P.dma(es[:], D["win_sink"][0:1, :].partition_broadcast(64), [], ["es"])
        P.act(es[:], es[:], AF.Exp, ["es"], ["es"])
        P.dma(masks[:], D["wmask"].rearrange("a p n -> p a n"), [], ["masks"])

        groups = [(g * 512, 512, g * 512, 0) for g in range(4)] + [(4096, 256, 2048, 1)]
        SC_A = 96.0 ** -0.5
        SC_B = 64.0 ** -0.5
        for (tok0, N, row0, var) in groups:
            qa, qak = QAg.next()
            P.dma(qa[:, :, 0:N], D["QAT"][:, :, tok0:tok0 + N], [], [qak])
            qb, qbk = QBg.next()
            P.dma(qb[:, :, 0:N], D["QBT"][:, :, tok0:tok0 + N], [], [qbk])
            chunks = list(range(34)) if var == 0 else [32, 33]
            def mla_norm(h, O, Ok, N=N):
                osb, osk = Osb.next()
                P.v(lambda e: e.tensor_copy(osb[0:65, 0:N], O[0:65, 0:N]), [Ok], [osk])
                R, Rk = nR()
                P.mm(R[0:64, 0:N], sel[0:65, 0:64], osb[0:65, 0:N], True, True, ["sel", osk], [Rk])
                rc, rck = rec.next()
                P.v(lambda e: e.reciprocal(rc[0:64, 0:N], R[0:64, 0:N]), [Rk], [rck])
                P.v(lambda e: e.tensor_tensor(mixT[0:64, h, 0:N], osb[0:64, 0:N], rc[0:64, 0:N], ALU.mult),
                    [osk, rck], [("mixT", h)])

            defer = None
            for h in range(8):
                ka, kak = KAh.next()
                P.dma(ka[:], D["KAT"][:, h, :], [], [kak])
                O, Ok = nO()
                pend = []
                for ci, kc in enumerate(chunks + [None] * LA):
                    if kc is not None:
                        S, Sk = nS()
                        P.mm(S[:, 0:N], ka[0:96, kc * 128:(kc + 1) * 128], qa[0:96, h, 0:N], True, True, [kak, qak], [Sk])
                        pt, ptk = PT.next()
                        P.act(pt[:, 0:N], S[:, 0:N], AF.Exp, [Sk], [ptk], scale=SC_A)
                        pend.append((ci, kc, pt, ptk))
                    if defer is not None and (ci == LA or kc is None):
                        mla_norm(*defer)
                        defer = None
                    if len(pend) > LA or (kc is None and pend):
                        pci, pkc, ppt, pptk = pend.pop(0)
                        P.mm(O[0:65, 0:N], VA[:, pkc, h * 65:(h + 1) * 65], ppt[:, 0:N], pci == 0, pci == len(chunks) - 1,
                             ["VA", pptk], [Ok])
                defer = (h, O, Ok)
            mla_norm(*defer)
            nb = N // 128

            def win_norm(g, b, O, Ok):
                osb, osk = Osb.next()
                P.v(lambda e: e.tensor_copy(osb[0:65, :], O[0:65, :]), [Ok], [osk])
                R, Rk = nR()
                P.mm(R[0:64, :], sel[0:65, 0:64], osb[0:65, :], True, True, ["sel", osk], [Rk])
                rc, rck = rec.next()
                for j in range(4):
                    P.v(lambda e, j=j: e.tensor_scalar(
                        rc[0:64, j * 128:(j + 1) * 128], R[0:64, j * 128:(j + 1) * 128],
                        es[0:64, g * 4 + j:g * 4 + j + 1], None, ALU.add), [Rk, "es"], [rck])
                P.v(lambda e: e.reciprocal(rc[0:64, :], rc[0:64, :]), [rck], [rck])
                P.v(lambda e: e.tensor_tensor(
                    mixT[0:64, 8 + g * 4:8 + g * 4 + 4, b * 128:(b + 1) * 128],
                    osb[0:64, :].rearrange("p (h q) -> p h q", h=4),
                    rc[0:64, :].rearrange("p (h q) -> p h q", h=4), ALU.mult),
                    [osk, rck], [("mixT", 8 + g * 4 + j) for j in range(4)])

            deferw = None
            for b in range(nb):
                tt = tok0 // 128 + b
                if var == 0:
                    cl = []
                    if tt > 0:
                        cl.append((tt - 1, 0))
                    cl.append((tt, None))
                    cl.append((tt + 1, 1))
                    cl += [(32, None), (33, None)]
                else:
                    cl = [(32, None), (33, None)]
                for g in range(2):
                    O, Ok = nO()
                    Q4 = qb[0:64, g * 4:(g + 1) * 4, b * 128:(b + 1) * 128]
                    pend = []
                    for ci, item in enumerate(cl + [None] * LA):
                        if item is not None:
                            kc, mk_ = item
                            S, Sk = nS()
                            P.mm(S[:, :].rearrange("p (h q) -> p h q", h=4), KBT[0:64, g, kc * 128:(kc + 1) * 128], Q4,
                                 True, True, ["KBT", qbk], [Sk])
                            pt, ptk = PT.next()
                            P.act(pt[:, :], S[:, :], AF.Exp, [Sk], [ptk], scale=SC_B)
                            if mk_ is not None:
                                P.v(lambda e, pt=pt, mk_=mk_: e.tensor_tensor(pt[:, :], pt[:, :], masks[:, mk_, :], ALU.mult),
                                    [ptk, "masks"], [ptk], eng="pool")
                            pend.append((ci, kc, pt, ptk))
                        if deferw is not None and (ci == min(LA, len(cl) - 1)):
                            win_norm(*deferw)
                            deferw = None
                        if len(pend) > LA or (item is None and pend):
                            pci, pkc, ppt, pptk = pend.pop(0)
                            P.mm(O[0:65, :], VB[:, pkc, g * 65:(g + 1) * 65], ppt[:, :], pci == 0, pci == len(cl) - 1,
                                 ["VB", pptk], [Ok])
                    deferw = (g, b, O, Ok)
            if deferw is not None:
                win_norm(*deferw)
                deferw = None
            mkeys = [("mixT", j) for j in range(16)]
            for b in range(nb):
                x_ap, xk = xt.next()
                P.dma(x_ap[:], D["xtok"][tok0 + b * 128:tok0 + (b + 1) * 128, :], [], [xk])
                o_ap, ok_ = ot.next()
                for dh in range(2):
                    Ob, Obk = nOP()
                    for j in range(16):
                        P.mm(Ob[:, :], mixT[0:128, j, b * 128:(b + 1) * 128], w_out[0:128, j, dh * 512:(dh + 1) * 512],
                             j == 0, j == 15, mkeys + ["w_out", "w_out_z", "mixT_z"], [Obk])
                    P.v(lambda e, o_ap=o_ap, Ob=Ob, dh=dh, var=var: e.tensor_tensor(
                        o_ap[:, dh * 512:(dh + 1) * 512], Ob[:, :], G[var][0][:, dh * 512:(dh + 1) * 512], ALU.mult),
                        [Obk, G[var][1]], [ok_])
                P.v(lambda e, o_ap=o_ap, x_ap=x_ap: e.tensor_tensor(o_ap[:], o_ap[:], x_ap[:], ALU.add),
                    [ok_, xk], [ok_], eng="pool")
                r0 = row0 + b * 128
                P.dma(D["x1"][r0:r0 + 128, :], o_ap[:], [ok_], [("x1", r0)])
        P.flush()


def phase_moe(nc, P, D, banks, layer, xin, xout, tiles, tag, final_norm=False):
    NTl = len(tiles)
    T = NTl * 128
    with contextlib.ExitStack() as st0:
        sb0 = lambda name, shape, dt=F32: st0.enter_context(nc.sbuf_tensor(tag + name, shape, dt))
        h2T = sb0("h2T", [128, 8, T], BF16)
        comb = sb0("comb", [128, NTl, 32])
        consts = sb0("consts", [128, 4])
        P.v(lambda e: e.memset(consts[:, 0:1], -0.5), [], ["consts"])
        neghalf = consts[:, 0:1]
        with contextlib.ExitStack() as st:
            sb = lambda name, shape, dt=F32: st.enter_context(nc.sbuf_tensor(tag + name, shape, dt))
            nA, nB = bank_rot(banks, 0, 4), bank_rot(banks, 4, 8)
            ident = sb("ident32", [128, 128])
            gbc = sb("gbc", [128, 1024])
            w_r = sb("w_r", [128, 8, 36])
            Abc = load_mod_rows(P, nc, st, D["mods"], layer, SC2, tag + "A")
            Bbc = load_mod_rows(P, nc, st, D["mods"], layer, SH2, tag + "B")
            xt = Rot("mxt", [sb("xt%d" % i, [128, 1024]) for i in range(2)])
            junk = sb("junk", [128, 1024])
            t1 = sb("t1", [128, 1024])
            h2 = sb("h2", [128, 1024])
            h2T32 = sb("h2T32", [128, 8, 128])
            small = Rot("msmall", [sb("small%d" % i, [128, 16]) for i in range(2)])
            rt = Rot("mrt", [sb("rt%d" % i, [128, 96]) for i in range(2)])
            P.dma(ident[:], D["ident32"][:, :], [], ["ident"])
            P.dma(gbc[:], D["norm_ffn_g"][layer:layer + 1, :].partition_broadcast(128), [], ["gbc"])
            P.dma(w_r[:, :, 0:4], D["moe_w_rg"][layer].rearrange("(kc p) n -> p kc n", p=128), [], [("w_r", 0)])
            P.dma(w_r[:, :, 4:36], D["moe_w_re"][layer].rearrange("(kc p) n -> p kc n", p=128), [], [("w_r", 1)])
            for v in range(2):
                a, ak = Abc[v]
                P.v(lambda e, a=a: e.scalar_tensor_tensor(a[:], a[:], 1.0, gbc[:], ALU.add, ALU.mult), [ak, "gbc"], [ak])
            for ti, (r, var) in enumerate(tiles):
                A, Ak = Abc[var]; B, Bk = Bbc[var]
                x_ap, xk = xt.next()
                P.dma(x_ap[:], D[xin][r * 128:(r + 1) * 128, :], [], [xk])
                sm, smk = small.next()
                P.act(junk[:], x_ap[:], AF.Square, [xk], ["junk", (smk, 0)], accum_out=sm[:, 0:1])
                rms_rstd(P, sm[:, 0:1], (smk, 0), sm[:, 2:3], (smk, 2), neghalf, 1024, sm[:, 1:2], (smk, 1))
                P.v(lambda e, x_ap=x_ap, sm=sm, A=A: e.scalar_tensor_tensor(
                    t1[:], x_ap[:], sm[:, 2:3], A[:], ALU.mult, ALU.mult), [xk, (smk, 2), Ak], ["t1"])
                P.v(lambda e, B=B: e.tensor_tensor(h2[:], t1[:], B[:], ALU.add), ["t1", Bk], ["h2"])
                for half in range(2):
                    bk, bkey = nA()
                    for j in range(4):
                        kc = half * 4 + j
                        P.tr(bk[:, j * 128:(j + 1) * 128], h2[:, kc * 128:(kc + 1) * 128], ident[:], ["h2", "ident"], [bkey])
                    P.act(h2T[:, half * 4:(half + 1) * 4, ti * 128:(ti + 1) * 128],
                          bk[:, :].rearrange("p (a b) -> p a b", a=4), AF.Copy, [bkey], [("h2T", ti, half)])
                    P.v(lambda e, bk=bk, half=half: e.tensor_copy(
                        h2T32[:, half * 4:(half + 1) * 4, :], bk[:, :].rearrange("p (a b) -> p a b", a=4)),
                        [bkey], [("h2T32", half)])
                lg, lgk = nB()
                for kc in range(8):
                    P.mm(lg[:, 0:36], h2T32[:, kc, :], w_r[:, kc, :], kc == 0, kc == 7,
                         [("h2T32", kc // 4), ("w_r", 0), ("w_r", 1)], [lgk])
                R, Rk = rt.next()
                P.v(lambda e, R=R, lg=lg: e.tensor_copy(R[:, 0:36], lg[:, 0:36]), [lgk], [(Rk, "lg")])
                s_ = lambda c: sm[:, c:c + 1]
                P.v(lambda e, R=R, sm=sm: e.reduce_max(sm[:, 3:4], R[:, 0:4], AX.X), [(Rk, "lg")], [(smk, 3)])
                P.v(lambda e, R=R, sm=sm: e.tensor_scalar(R[:, 36:40], R[:, 0:4], sm[:, 3:4], None, ALU.is_equal),
                    [(Rk, "lg"), (smk, 3)], [(Rk, "goh")])
                P.v(lambda e, sm=sm: e.tensor_scalar(sm[:, 4:5], sm[:, 3:4], -1.0, None, ALU.mult), [(smk, 3)], [(smk, 4)])
                P.act(R[:, 80:84], R[:, 0:4], AF.Exp, [(Rk, "lg"), (smk, 4)], [(Rk, "gexp"), (smk, 5)],
                      bias=sm[:, 4:5], accum_out=sm[:, 5:6])
                P.v(lambda e, sm=sm: e.reciprocal(sm[:, 6:7], sm[:, 5:6]), [(smk, 5)], [(smk, 6)])
                P.v(lambda e, R=R: e.tensor_scalar(R[:, 40:48], R[:, 4:12], R[:, 36:37], None, ALU.mult),
                    [(Rk, "lg"), (Rk, "goh")], [(Rk, "ein")])
                for g in range(1, 4):
                    P.v(lambda e, R=R, g=g: e.scalar_tensor_tensor(
                        R[:, 40:48], R[:, 4 + 8 * g:12 + 8 * g], R[:, 36 + g:37 + g], R[:, 40:48], ALU.mult, ALU.add),
                        [(Rk, "lg"), (Rk, "goh"), (Rk, "ein")], [(Rk, "ein")])
                P.v(lambda e, R=R, sm=sm: e.reduce_max(sm[:, 7:8], R[:, 40:48], AX.X), [(Rk, "ein")], [(smk, 7)])
                P.v(lambda e, R=R, sm=sm: e.tensor_scalar(R[:, 48:56], R[:, 40:48], sm[:, 7:8], None, ALU.is_equal),
                    [(Rk, "ein"), (smk, 7)], [(Rk, "oh1")])
                P.v(lambda e, R=R: e.scalar_tensor_tensor(R[:, 56:64], R[:, 48:56], -1e30, R[:, 40:48], ALU.mult, ALU.add),
                    [(Rk, "oh1"), (Rk, "ein")], [(Rk, "e2")])
                P.v(lambda e, R=R, sm=sm: e.reduce_max(sm[:, 8:9], R[:, 56:64], AX.X), [(Rk, "e2")], [(smk, 8)])
                P.v(lambda e, R=R, sm=sm: e.tensor_scalar(R[:, 64:72], R[:, 56:64], sm[:, 8:9], None, ALU.is_equal),
                    [(Rk, "e2"), (smk, 8)], [(Rk, "oh2")])
                P.v(lambda e, sm=sm: e.tensor_tensor(sm[:, 9:10], sm[:, 8:9], sm[:, 7:8], ALU.subtract),
                    [(smk, 7), (smk, 8)], [(smk, 9)])
                P.act(sm[:, 10:11], sm[:, 9:10], AF.Exp, [(smk, 9)], [(smk, 10)])
                P.v(lambda e, sm=sm: e.tensor_scalar(sm[:, 11:12], sm[:, 10:11], 1.0, None, ALU.add), [(smk, 10)], [(smk, 11)])
                P.v(lambda e, sm=sm: e.reciprocal(sm[:, 11:12], sm[:, 11:12]), [(smk, 11)], [(smk, 11)])
                P.v(lambda e, sm=sm: e.tensor_tensor(sm[:, 12:13], sm[:, 11:12], sm[:, 6:7], ALU.mult),
                    [(smk, 11), (smk, 6)], [(smk, 12)])
                P.v(lambda e, sm=sm: e.tensor_tensor(sm[:, 13:14], sm[:, 12:13], sm[:, 10:11], ALU.mult),
                    [(smk, 12), (smk, 10)], [(smk, 13)])
                P.v(lambda e, R=R, sm=sm: e.tensor_scalar(R[:, 72:80], R[:, 48:56], sm[:, 12:13], None, ALU.mult),
                    [(Rk, "oh1"), (smk, 12)], [(Rk, "loc")])
                P.v(lambda e, R=R, sm=sm: e.scalar_tensor_tensor(
                    R[:, 72:80], R[:, 64:72], sm[:, 13:14], R[:, 72:80], ALU.mult, ALU.add),
                    [(Rk, "oh2"), (smk, 13), (Rk, "loc")], [(Rk, "loc")])
                for g in range(4):
                    P.v(lambda e, R=R, g=g, ti=ti: e.tensor_scalar(
                        comb[:, ti, g * 8:(g + 1) * 8], R[:, 72:80], R[:, 36 + g:37 + g], None, ALU.mult),
                        [(Rk, "loc"), (Rk, "goh")], [("comb", ti)])
            P.flush()
        with contextlib.ExitStack() as st:
            sb = lambda name, shape, dt=F32: st.enter_context(nc.sbuf_tensor(tag + name, shape, dt))
            nGU, nY = bank_rot(banks, 0, 4), bank_rot(banks, 4, 8)
            yacc = sb("yacc", [128, NTl, 1024])
            wg = Rot("wg", [sb("wg%d" % i, [128, 8, 512], BF16) for i in range(2)])
            wu = Rot("wu", [sb("wu%d" % i, [128, 8, 512], BF16) for i in range(2)])
            wd = Rot("wd", [sb("wd%d" % i, [128, 4, 1024], BF16) for i in range(2)])
            hg = Rot("hg", [sb("hg%d" % i, [128, 4, 512], BF16) for i in range(2)])
            sg = Rot("sg", [sb("sg%d" % i, [128, 512]) for i in range(2)])
            G = load_mod_rows(P, nc, st, D["mods"], layer, GT2, tag + "G")
            xt = Rot("mxt2", [sb("xt2_%d" % i, [128, 1024]) for i in range(2)])
            groups = []
            t0 = 0
            while t0 < NTl:
                n = min(4, NTl - t0)
                groups.append((t0, n))
                t0 += n
            def emit_gu(ex, t0, n, g_ap, gk, u_ap, uk):
                N = n * 128
                ts = slice(t0 * 128, t0 * 128 + N)
                hgt, hgk = hg.next()
                for fc in range(4):
                    Gp, Gk = nGU(); Up, Uk = nGU()
                    for kc in range(8):
                        P.mm(Gp[:, 0:N], g_ap[:, kc, fc * 128:(fc + 1) * 128], h2T[:, kc, ts], kc == 0, kc == 7,
                             [gk, "h2T"], [Gk])
                    for kc in range(8):
                        P.mm(Up[:, 0:N], u_ap[:, kc, fc * 128:(fc + 1) * 128], h2T[:, kc, ts], kc == 0, kc == 7,
                             [uk, "h2T"], [Uk])
                    s_ap, sk = sg.next()
                    P.act(s_ap[:, 0:N], Gp[:, 0:N], AF.Silu, [Gk], [sk])
                    P.v(lambda e, hgt=hgt, Up=Up, s_ap=s_ap, fc=fc, N=N: e.tensor_tensor(
                        hgt[:, fc, 0:N], Up[:, 0:N], s_ap[:, 0:N], ALU.mult), [Uk, sk], [(hgk, fc)])
                return hgt, hgk

            def emit_down(ex, t0, n, hgt, hgk, d_ap, dk):
                for b in range(n):
                    ti = t0 + b
                    for dh in range(2):
                        Yp, Yk = nY()
                        for fc in range(4):
                            P.mm(Yp[:, :], hgt[:, fc, b * 128:(b + 1) * 128], d_ap[:, fc, dh * 512:(dh + 1) * 512],
                                 fc == 0, fc == 3, [(hgk, f) for f in range(4)] + [dk], [Yk])
                        ysl = yacc[:, ti, dh * 512:(dh + 1) * 512]
                        if ex == 0:
                            P.v(lambda e, ysl=ysl, Yp=Yp, ti=ti, ex=ex: e.tensor_scalar(
                                ysl, Yp[:, :], comb[:, ti, ex:ex + 1], None, ALU.mult), [Yk], [("yacc", ti, dh)])
                        else:
                            P.v(lambda e, ysl=ysl, Yp=Yp, ti=ti, ex=ex: e.scalar_tensor_tensor(
                                ysl, Yp[:, :], comb[:, ti, ex:ex + 1], ysl, ALU.mult, ALU.add),
                                [Yk, ("yacc", ti, dh)], [("yacc", ti, dh)])

            pend = None
            for ex in range(32):
                g_ap, gk = wg.next(); u_ap, uk = wu.next(); d_ap, dk = wd.next()
                P.dma(g_ap[:], D["moe_w_gate%d" % layer][ex].rearrange("(kc p) f -> p kc f", p=128), [], [gk], eng="pool")
                P.dma(u_ap[:], D["moe_w_up%d" % layer][ex].rearrange("(kc p) f -> p kc f", p=128), [], [uk], eng="pool")
                P.dma(d_ap[:], D["moe_w_down%d" % layer][ex].rearrange("(kc p) f -> p kc f", p=128), [], [dk], eng="pool")
                for (t0, n) in groups:
                    hgt, hgk = emit_gu(ex, t0, n, g_ap, gk, u_ap, uk)
                    if pend is not None:
                        emit_down(*pend)
                    pend = (ex, t0, n, hgt, hgk, d_ap, dk)
            emit_down(*pend)
            if final_norm:
                fg = sb("fg", [128, 1024])
                P.dma(fg[:], D["final_norm_g"][0:1, :].partition_broadcast(128), [], ["fg"])
                junk = sb("junk3", [128, 1024])
                small = Rot("fsmall", [sb("fsmall%d" % i, [128, 4]) for i in range(2)])
            for ti, (r, var) in enumerate(tiles):
                x_ap, xk = xt.next()
                P.dma(x_ap[:], D[xin][r * 128:(r + 1) * 128, :], [], [xk])
                ysl = yacc[:, ti, :]
                yk = [("yacc", ti, 0), ("yacc", ti, 1)]
                P.v(lambda e, ysl=ysl, var=var: e.tensor_tensor(ysl, ysl, G[var][0][:], ALU.mult), yk + [G[var][1]], yk)
                P.v(lambda e, ysl=ysl, x_ap=x_ap: e.tensor_tensor(ysl, ysl, x_ap[:], ALU.add), yk + [xk], yk, eng="pool")
                if final_norm:
                    sm, smk = small.next()
                    P.act(junk[:], ysl, AF.Square, yk, ["junk3", (smk, 0)], accum_out=sm[:, 0:1])
                    rms_rstd(P, sm[:, 0:1], (smk, 0), sm[:, 2:3], (smk, 2), neghalf, 1024, sm[:, 1:2], (smk, 1))
                    P.v(lambda e, ysl=ysl, sm=sm: e.scalar_tensor_tensor(
                        ysl, ysl, sm[:, 2:3], fg[:], ALU.mult, ALU.mult), yk + [(smk, 2), "fg"], yk)
                P.dma(D[xout][r * 128:(r + 1) * 128, :], ysl, yk, [(xout, r)])
            P.flush()


GELU_C = 0.7978845608028654


def l1_tiles():
    return [(i, 0) for i in range(16)] + [(16, 1), (17, 1)]


def phase_l1a(nc, P, D, banks):
    with contextlib.ExitStack() as st:
        sb = lambda name, shape, dt=F32: st.enter_context(nc.sbuf_tensor("c_" + name, shape, dt))
        nP = bank_rot(banks, 0, 8)
        ident = sb("ident", [128, 128], BF16)
        ident32 = sb("ident32", [128, 128])
        consts = sb("consts", [128, 4])
        gbc = sb("gbc", [128, 1024])
        w_in = sb("w_in", [128, 8, 2592], BF16)
        Abc = load_mod_rows(P, nc, st, D["mods"], 1, SC1, "c_A")
        Bbc = load_mod_rows(P, nc, st, D["mods"], 1, SH1, "c_B")
        W2 = sb("W2", [32, 512])
        bg = sb("bg", [128, 512])
        gm = sb("gm", [128, 3, 128])
        wsT = sb("wsT", [128, 4, 128], BF16)
        bsT = sb("bsT", [128, 4])
        lng = sb("lng", [128, 512]); lnb = sb("lnb", [128, 512])
        xt = Rot("cxt", [sb("xt%d" % i, [128, 1024]) for i in range(2)])
        junk = sb("junk", [128, 1024])
        t1r = Rot("t1", [sb("t1_%d" % i, [128, 1024]) for i in range(2)])
        hbr = Rot("hb", [sb("hb_%d" % i, [128, 1024], BF16) for i in range(2)])
        hTr = Rot("hT", [sb("hT_%d" % i, [128, 8, 128], BF16) for i in range(2)])
        small = Rot("csmall", [sb("small%d" % i, [128, 16]) for i in range(3)])
        qkr = Rot("qk", [sb("qk%d" % i, [128, 512]) for i in range(2)])
        g32r = Rot("g32", [sb("g32_%d" % i, [128, 32]) for i in range(2)])
        uvr = Rot("uv", [sb("uv%d" % i, [128, 2, 512]) for i in range(2)])
        g32T = sb("g32T", [32, 128])
        zs = sb("zs", [128, 512]); la = sb("la", [128, 512])
        bsb = sb("bsb", [128, 2, 256])
        ex = sb("ex", [128, 256]); tmp = sb("tmp", [128, 256])
        gl = sb("gl", [128, 6, 256], BF16)
        glT = Rot("glT", [sb("glT%d" % i, [128, 8, 128], BF16) for i in range(2)])
        dec = Rot("dec", [sb("dec%d" % i, [128, 4]) for i in range(2)])
        vb = Rot("cvb", [sb("vb%d" % i, [128, 512], BF16) for i in range(2)])
        rsb = Rot("rsb", [sb("rsb%d" % i, [128, 512], BF16) for i in range(2)])
        ge = sb("ge", [128, 2, 512])
        gt_ = sb("gt_", [128, 512]); gt2_ = sb("gt2_", [128, 512])
        vgn = sb("vgn", [128, 512], BF16)
        dlb = Rot("dlb", [sb("dlb%d" % i, [128, 512], BF16) for i in range(2)])

        P.dma(ident[:], D["identbf"][:, :], [], ["ident"])
        P.dma(ident32[:], D["ident32"][:, :], [], ["ident32"])
        P.v(lambda e: e.memset(consts[:, 0:1], -0.5), [], ["consts"])
        neghalf = consts[:, 0:1]
        P.dma(gbc[:], D["norm_mix_g"][1:2, :].partition_broadcast(128), [], ["gbc"])
        for c in range(3):
            lo, hi = c * 864, (c + 1) * 864
            P.dma(w_in[:, :, lo:hi], D["odd_w_in"][0].rearrange("(kc p) n -> p kc n", p=128)[:, :, lo:hi], [],
                  [("w_in", c)], eng="pool")
        wkeys = [("w_in", c) for c in range(3)]
        P.v(lambda e: e.memset(W2[:], 0.0), [], ["W2"])
        P.dma(W2[0:16, 0:256], D["gla_w_g2"][0, 0], ["W2"], ["W2"])
        P.dma(W2[16:32, 256:512], D["gla_w_g2"][0, 1], ["W2"], ["W2"])
        P.dma(bg[:], D["gla_b_g"][0:1].rearrange("o a n -> o (a n)").partition_broadcast(128), [], ["bg"])
        P.dma(gm[:], D["gmask"][0:3].rearrange("a p n -> p a n"), [], ["gm"])
        P.dma(wsT[:], D["sg_w_sT"].rearrange("g s t -> s g t"), [], ["wsT"], eng="pool")
        P.dma(bsT[:], D["sg_b_sT"][:, :], [], ["bsT"])
        P.dma(lng[:], D["sg_ln_g"][0:1, :].partition_broadcast(128), [], ["lng"])
        P.dma(lnb[:], D["sg_ln_b"][0:1, :].partition_broadcast(128), [], ["lnb"])
        for v in range(2):
            a, ak = Abc[v]
            P.v(lambda e, a=a: e.scalar_tensor_tensor(a[:], a[:], 1.0, gbc[:], ALU.add, ALU.mult), [ak, "gbc"], [ak])

        def gelu(src, src_key, dst, dst_key):
            P.v(lambda e: e.tensor_tensor(gt2_[:], src, src, ALU.mult), [src_key], ["gt2_"])
            P.v(lambda e: e.tensor_scalar(gt2_[:], gt2_[:], 0.044715, 1.0, ALU.mult, ALU.add), ["gt2_"], ["gt2_"])
            P.v(lambda e: e.tensor_tensor(gt2_[:], gt2_[:], src, ALU.mult), ["gt2_", src_key], ["gt2_"], eng="pool")
            P.act(gt2_[:], gt2_[:], AF.Sigmoid, ["gt2_"], ["gt2_"], scale=2.0 * GELU_C)
            P.v(lambda e: e.tensor_tensor(dst, src, gt2_[:], ALU.mult), [src_key, "gt2_"], [dst_key])

        def stage_a(r, var):
            lat = var == 0
            A, Ak = Abc[var]; B, Bk = Bbc[var]
            rs = slice(r * 128, (r + 1) * 128)
            x_ap, xk = xt.next()
            P.dma(x_ap[:], D["x2"][rs, :], [], [xk])
            sm, smk = small.next()
            P.act(junk[:], x_ap[:], AF.Square, [xk], ["junk", (smk, 0)], accum_out=sm[:, 0:1])
            rms_rstd(P, sm[:, 0:1], (smk, 0), sm[:, 2:3], (smk, 2), neghalf, 1024, sm[:, 1:2], (smk, 1))
            t1, t1k = t1r.next(); hb, hbk = hbr.next(); hT, hTk = hTr.next()
            P.v(lambda e, x_ap=x_ap, sm=sm, A=A, t1=t1: e.scalar_tensor_tensor(
                t1[:], x_ap[:], sm[:, 2:3], A[:], ALU.mult, ALU.mult), [xk, (smk, 2), Ak], [t1k])
            P.v(lambda e, B=B, t1=t1, hb=hb: e.tensor_tensor(hb[:], t1[:], B[:], ALU.add), [t1k, Bk], [hbk])
            bk, bkey = nP()
            bkb = bk[:].bitcast(BF16)
            for kc in range(8):
                P.tr(bkb[:, kc * 128:(kc + 1) * 128], hb[:, kc * 128:(kc + 1) * 128], ident[:], [hbk, "ident"], [bkey])
            P.act(hT[:].rearrange("p a b -> p (a b)"), bkb, AF.Copy, [bkey], [hTk])

            def proj(c0, c1):
                bk, bkey = nP()
                for kc in range(8):
                    P.mm(bk[:, 0:c1 - c0], hT[:, kc, :], w_in[:, kc, c0:c1], kc == 0, kc == 7, [hTk] + wkeys, [bkey])
                return bk, bkey
            zqk, zqkk = proj(0, 512)
            qk, qkk = qkr.next()
            P.act(qk[:], zqk[:, :], AF.Copy, [zqkk], [qkk])
            zv, zvk = proj(512, 1024)
            v_ap, vk = vb.next()
            P.act(v_ap[:], zv[:, :], AF.Copy, [zvk], [vk])
            P.dma(D["g_v"][rs, :], v_ap[:], [vk], [("g_v", r)], eng="act")
            zg, zgk = proj(1024, 1056)
            g32, g32k = g32r.next()
            P.v(lambda e, zg=zg, g32=g32: e.tensor_copy(g32[:], zg[:, 0:32]), [zgk], [g32k])
            uv, uvk = uvr.next()
            if lat:
                zr, zrk = proj(1056, 1568)
                r_ap, rk = rsb.next()
                P.act(r_ap[:], zr[:, :], AF.Silu, [zrk], [rk])
                P.dma(D["rsilu"][rs, :], r_ap[:], [rk], [("rsilu", r)], eng="act")
                zu, zuk = proj(1568, 2080)
                P.act(uv[:, 0, :], zu[:, :], AF.Copy, [zuk], [(uvk, 0)])
                zvg, zvgk = proj(2080, 2592)
                P.v(lambda e, uv=uv, zvg=zvg: e.tensor_copy(uv[:, 1, :], zvg[:, :]), [zvgk], [(uvk, 1)])
            return dict(r=r, lat=lat, rs=rs, sm=sm, smk=smk, qk=qk, qkk=qkk, g32=g32, g32k=g32k, uv=uv, uvk=uvk)

        def stage_b(c):
            r, lat, rs, sm, smk, qk, qkk, g32, g32k, uv, uvk = (c[k] for k in (
                "r", "lat", "rs", "sm", "smk", "qk", "qkk", "g32", "g32k", "uv", "uvk"))
            bk, bkey = nP()
            P.tr(bk[0:32, 0:128], g32[:, :], ident32[:], [g32k, "ident32"], [bkey])
            P.v(lambda e, bk=bk: e.tensor_copy(g32T[:], bk[0:32, 0:128]), [bkey], ["g32T"])
            zz, zzk = nP()
            P.mm(zz[:, :], g32T[0:32, :], W2[0:32, :], True, True, ["g32T", "W2"], [zzk])
            P.v(lambda e, zz=zz: e.tensor_tensor(zs[:], zz[:, :], bg[:], ALU.add), [zzk, "bg"], ["zs"])
            P.act(zs[:], zs[:], AF.Exp, ["zs"], ["zs"], scale=-1.0)
            P.act(zs[:], zs[:], AF.Ln, ["zs"], ["zs"], bias=1.0)
            P.v(lambda e: e.tensor_scalar(la[:], zs[:], -1.0 / 16.0, None, ALU.mult), ["zs"], ["la"])
            cA, cAk = nP()
            P.mm(cA[:, 0:256], gm[:, 0, :], la[:, 0:256], True, True, ["gm", "la"], [cAk])
            P.mm(cA[:, 256:512], gm[:, 1, :], la[:, 256:512], True, True, ["gm", "la"], [cAk])
            cL, cLk = nP()
            P.mm(cL[:, :], gm[:, 2, :], la[:, :], True, True, ["gm", "la"], [cLk])
            P.act(bsb[:].rearrange("p a b -> p (a b)"), cA[:, :], AF.Copy, [cAk], ["bsb"])
            cT, cTk = nP()
            for j in range(4):
                P.mm(cT[:, j:j + 1], la[:, j * 128:(j + 1) * 128], gm[:, 2, 0:1], True, True, ["gm", "la"], [cTk])
            d_ap, dk = dec.next()
            P.act(d_ap[:], cT[:, 0:4], AF.Exp, [cTk], [dk])
            P.dma(D["g_dec"][:, r, :], d_ap[:], [dk], [("g_dec", r)], eng="act")
            for p in range(2):
                if p == 1 and not lat:
                    continue
                bp = bsb[:, p, :]
                if lat:
                    P.act(ex[:], bp, AF.Exp, ["bsb"], ["ex"])
                    P.v(lambda e, p=p: e.scalar_tensor_tensor(gl[:, 2 * p, :], qk[:, 0:256], 0.125, ex[:], ALU.mult, ALU.mult),
                        [qkk, "ex"], [("gl", 2 * p)])
                P.act(ex[:], bp, AF.Exp, ["bsb"], ["ex"], scale=-1.0)
                P.v(lambda e, p=p: e.tensor_tensor(gl[:, 2 * p + 1, :], qk[:, 256:512], ex[:], ALU.mult),
                    [qkk, "ex"], [("gl", 2 * p + 1)])
                P.v(lambda e, p=p, bp=bp, cL=cL: e.tensor_tensor(tmp[:], cL[:, p * 256:(p + 1) * 256], bp, ALU.subtract),
                    [cLk, "bsb"], ["tmp"])
                P.act(ex[:], tmp[:], AF.Exp, ["tmp"], ["ex"])
                P.v(lambda e, p=p: e.tensor_tensor(gl[:, 4 + p, :], qk[:, 256:512], ex[:], ALU.mult),
                    [qkk, "ex"], [("gl", 4 + p)])
                P.dma(D["g_kd"][p, rs, :], gl[:, 4 + p, :], [("gl", 4 + p)], [("g_kd", p, r)], eng="act")
            bk, bkey = nP()
            bkb = bk[:].bitcast(BF16)
            arrs = [0, 1, 2, 3] if lat else [1]
            for a_ in arrs:
                for j in range(2):
                    P.tr(bkb[:, (a_ * 2 + j) * 128:(a_ * 2 + j + 1) * 128], gl[:, a_, j * 128:(j + 1) * 128], ident[:],
                         [("gl", a_), "ident"], [bkey])
            gT, gTk = glT.next()
            if lat:
                P.act(gT[:].rearrange("p a b -> p (a b)"), bkb, AF.Copy, [bkey], [gTk])
                P.dma(D["g_T"][:, r, :, :], gT[:], [gTk], [("g_T", r)], eng="act")
            else:
                P.act(gT[:, 2:4, :].rearrange("p a b -> p (a b)"), bkb[:, 256:512], AF.Copy, [bkey], [gTk])
                P.dma(D["g_T"][:, r, 2:4, :], gT[:, 2:4, :], [gTk], [("g_T", r)], eng="act")
            if not lat:
                return
            gelu(uv[:, 0, :], (uvk, 0), ge[:, 0, :], ("ge", 0))
            gelu(uv[:, 1, :], (uvk, 1), ge[:, 1, :], ("ge", 1))
            P.v(lambda e, sm=sm: e.reduce_sum(sm[:, 8:9], ge[:, 1, :], AX.X), [("ge", 1)], [(smk, 8)])
            P.act(junk[:, 0:512], ge[:, 1, :], AF.Square, [("ge", 1)], ["junk", (smk, 9)], accum_out=sm[:, 9:10])
            P.v(lambda e, sm=sm: e.tensor_scalar(sm[:, 10:11], sm[:, 8:9], 1.0 / 512, None, ALU.mult), [(smk, 8)], [(smk, 10)])
            P.v(lambda e, sm=sm: e.tensor_tensor(sm[:, 11:12], sm[:, 10:11], sm[:, 10:11], ALU.mult), [(smk, 10)], [(smk, 11)])
            P.v(lambda e, sm=sm: e.scalar_tensor_tensor(sm[:, 12:13], sm[:, 9:10], 1.0 / 512, sm[:, 11:12], ALU.mult, ALU.subtract),
                [(smk, 9), (smk, 11)], [(smk, 12)])
            P.v(lambda e, sm=sm: e.tensor_scalar(sm[:, 12:13], sm[:, 12:13], EPS, None, ALU.add), [(smk, 12)], [(smk, 12)])
            P.v(lambda e, sm=sm: e.tensor_tensor(sm[:, 13:14], sm[:, 12:13], neghalf, ALU.pow), [(smk, 12), "consts"],
                [(smk, 13)], eng="pool")
            P.v(lambda e, sm=sm: e.tensor_scalar(gt_[:], ge[:, 1, :], sm[:, 10:11], sm[:, 13:14], ALU.subtract, ALU.mult),
                [("ge", 1), (smk, 10), (smk, 13)], ["gt_"])
            P.v(lambda e: e.tensor_tensor(gt_[:], gt_[:], lng[:], ALU.mult), ["gt_", "lng"], ["gt_"])
            P.v(lambda e: e.tensor_tensor(vgn[:], gt_[:], lnb[:], ALU.add), ["gt_", "lnb"], ["vgn"])
            sp_, spk = nP()
            for gi in range(4):
                P.mm(sp_[:, gi * 128:(gi + 1) * 128], wsT[:, gi, :], vgn[:, gi * 128:(gi + 1) * 128], True, True,
                     ["wsT", "vgn"], [spk])
            dl_ap, dlk = dlb.next()
            for gi in range(4):
                P.v(lambda e, gi=gi, sp_=sp_, dl_ap=dl_ap: e.scalar_tensor_tensor(
                    dl_ap[:, gi * 128:(gi + 1) * 128], sp_[:, gi * 128:(gi + 1) * 128], bsT[:, gi:gi + 1],
                    ge[:, 0, gi * 128:(gi + 1) * 128], ALU.add, ALU.mult), [spk, "bsT", ("ge", 0)], [dlk])
            P.dma(D["dl"][rs, :], dl_ap[:], [dlk], [("dl", r)], eng="act")

        pend = None
        for (r, var) in l1_tiles():
            cur = stage_a(r, var)
            if pend is not None:
                stage_b(pend)
            pend = cur
        stage_b(pend)
        P.flush()


def _gla_pass(nc, P, D, banks, st, sb, pidx, order, S, Sb, on_out):
    nAT, nO, nU = bank_rot(banks, 0, 2), bank_rot(banks, 2, 4), bank_rot(banks, 4, 6)
    gT = Rot("gTl", [sb("gTl%d" % i, [128, 4, 128], BF16) for i in range(2)])
    kd = Rot("kdl", [sb("kdl%d" % i, [128, 256], BF16) for i in range(2)])
    vv = Rot("vl", [sb("vl%d" % i, [128, 512], BF16) for i in range(2)])
    dc = Rot("dcl", [sb("dcl%d" % i, [128, 2]) for i in range(2)])
    ATm = Rot("ATm", [sb("ATm%d" % i, [128, 128], BF16) for i in range(2)])
    mask = sb("gmask_sb", [128, 128])
    P.dma(mask[:], D["gmask"][pidx], [], ["gmask"])
    for r in order:
        lat = r < 16
        rs = slice(r * 128, (r + 1) * 128)
        g_ap, gk = gT.next()
        if lat:
            P.dma(g_ap[:], D["g_T"][:, r, 4 * pidx:4 * pidx + 4, :], [], [gk])
        else:
            P.dma(g_ap[:, 2:4, :], D["g_T"][:, r, 4 * pidx + 2:4 * pidx + 4, :], [], [gk])
        k_ap, kk = kd.next()
        P.dma(k_ap[:], D["g_kd"][pidx, rs, :], [], [kk])
        v_ap, vk = vv.next()
        P.dma(v_ap[:], D["g_v"][rs, :], [], [vk])
        d_ap, dk = dc.next()
        P.dma(d_ap[:], D["g_dec"][:, r, 2 * pidx:2 * pidx + 2], [], [dk])
        if lat:
            O, Ok = nO()
            for h in range(4):
                j, po = h // 2, (h % 2) * 64
                AT, ATk = nAT()
                P.mm(AT[:, 0:128], g_ap[po:po + 64, 2 + j, :], g_ap[po:po + 64, j, :], True, True, [gk], [ATk])
                am, amk = ATm.next()
                P.v(lambda e, am=am, AT=AT: e.tensor_tensor(am[:], AT[:, 0:128], mask[:], ALU.mult), [ATk, "gmask"], [amk])
                P.mm(O[:, h * 128:(h + 1) * 128], am[:], v_ap[:, h * 128:(h + 1) * 128], True, False, [amk, vk], [Ok])
                P.mm(O[:, h * 128:(h + 1) * 128], g_ap[po:po + 64, j, :], Sb[po:po + 64, j, :], False, True,
                     [gk, ("Sb", j, h % 2)], [Ok])
            on_out(r, O, Ok)
        for j in range(2):
            for hh in range(2):
                po = hh * 64
                U, Uk = nU()
                P.mm(U[:, 0:128], k_ap[:, j * 128:(j + 1) * 128], v_ap[:, (2 * j + hh) * 128:(2 * j + hh + 1) * 128],
                     True, True, [kk, vk], [Uk])
                P.v(lambda e, U=U, j=j, po=po, d_ap=d_ap: e.scalar_tensor_tensor(
                    S[po:po + 64, j, :], S[po:po + 64, j, :], d_ap[po:po + 64, j:j + 1], U[po:po + 64, 0:128],
                    ALU.mult, ALU.add), [Uk, dk, ("S", j, hh)], [("S", j, hh)])
                P.act(Sb[po:po + 64, j, :], S[po:po + 64, j, :], AF.Copy, [("S", j, hh)], [("Sb", j, hh)])


def phase_l1b_a(nc, P, D, banks):
    with contextlib.ExitStack() as st:
        sb = lambda name, shape, dt=F32: st.enter_context(nc.sbuf_tensor("d_" + name, shape, dt))
        S = sb("S", [128, 2, 128]); Sb = sb("Sb", [128, 2, 128], BF16)
        oa = Rot("oa", [sb("oa%d" % i, [128, 512]) for i in range(2)])
        P.v(lambda e: e.memset(S[:], 0.0), [], [("S", j, hh) for j in range(2) for hh in range(2)])
        P.v(lambda e: e.memset(Sb[:], 0.0), [], [("Sb", j, hh) for j in range(2) for hh in range(2)])

        def on_out(r, O, Ok):
            o_ap, ok_ = oa.next()
            P.act(o_ap[:], O[:, :], AF.Copy, [Ok], [ok_])
            P.dma(D["OA"][r * 128:(r + 1) * 128, :], o_ap[:], [ok_], [("OA", r)])
        _gla_pass(nc, P, D, banks, st, sb, 0, [16, 17] + list(range(16)), S, Sb, on_out)
        P.dma(D["cc_in"].rearrange("(a p) n -> p a n", p=128), S[:], [("S", j, hh) for j in range(2) for hh in range(2)],
              ["cc_in"])
        P.cc(lambda e: e.collective_compute("AllGather", ALU.bypass, replica_groups=[[0, 1], [2, 3], [4, 5], [6, 7]],
                                            ins=[D["cc_in"].opt()], outs=[D["cc_out"].opt()]), ["cc_in"], ["cc_out"])
        P.flush()


def phase_l1b_b(nc, P, D, banks):
    with contextlib.ExitStack() as st:
        sb = lambda name, shape, dt=F32: st.enter_context(nc.sbuf_tensor("e_" + name, shape, dt))
        nT, nW = bank_rot(banks, 0, 2), bank_rot(banks, 2, 8)
        S = sb("S", [128, 2, 128]); Sb = sb("Sb", [128, 2, 128], BF16)
        skeys = [("S", j, hh) for j in range(2) for hh in range(2)]
        both = sb("both", [128, 4, 128])
        sel = sb("sel", [128, 2])
        P.dma(both[:], D["cc_out"].rearrange("(a p) n -> p a n", p=128), [], ["both"])
        P.dma(sel[:], D["sel"][:, :], [], ["sel"])
        P.v(lambda e: e.tensor_scalar(S[:].rearrange("p a b -> p (a b)"), both[:, 0:2, :].rearrange("p a b -> p (a b)"),
                                      sel[:, 0:1], None, ALU.mult), ["both", "sel"], skeys)
        P.v(lambda e: e.scalar_tensor_tensor(S[:].rearrange("p a b -> p (a b)"),
                                             both[:, 2:4, :].rearrange("p a b -> p (a b)"), sel[:, 1:2],
                                             S[:].rearrange("p a b -> p (a b)"), ALU.mult, ALU.add),
            ["both", "sel"] + skeys, skeys)
        P.act(Sb[:], S[:], AF.Copy, skeys, [("Sb", j, hh) for j in range(2) for hh in range(2)])
        ident = sb("ident", [128, 128], BF16)
        P.dma(ident[:], D["identbf"][:, :], [], ["ident"])
        consts = sb("consts", [128, 4])
        P.v(lambda e: e.memset(consts[:], -0.5), [], ["consts"])
        gng = sb("gng", [128, 512])
        P.dma(gng[:], D["gla_norm_g"][0:1, :].partition_broadcast(128), [], ["gng"])
        w_out = sb("w_out", [128, 8, 1024], BF16)
        P.dma(w_out[:], D["odd_w_out"][0].rearrange("(kc p) n -> p kc n", p=128), [], ["w_out"], eng="pool")
        G = load_mod_rows(P, nc, st, D["mods"], 1, GT1, "e_G")
        oa = Rot("eoa", [sb("oa%d" % i, [128, 512]) for i in range(2)])
        rsl = Rot("ersl", [sb("rsl%d" % i, [128, 512], BF16) for i in range(2)])
        gr = sb("gr", [128, 512])
        junk = sb("junk", [128, 128])
        small = Rot("esmall", [sb("small%d" % i, [128, 8]) for i in range(2)])
        mix_all = sb("mix_all", [128, 16, 1024], BF16)
        mixTr = Rot("emixT", [sb("mixT%d" % i, [128, 8, 128], BF16) for i in range(2)])
        xt = Rot("ext", [sb("xt%d" % i, [128, 1024]) for i in range(2)])
        ot = Rot("eot", [sb("ot%d" % i, [128, 1024]) for i in range(2)])

        def on_out(r, O, Ok):
            rs = slice(r * 128, (r + 1) * 128)
            o_ap, ok_ = oa.next()
            P.dma(o_ap[:], D["OA"][rs, :], [], [ok_])
            P.v(lambda e, o_ap=o_ap, O=O: e.tensor_tensor(o_ap[:], O[:, :], o_ap[:], ALU.add), [Ok, ok_], [ok_])
            r_ap, rk = rsl.next()
            P.dma(r_ap[:], D["rsilu"][rs, :], [], [rk])
            m_ap, mk = mix_all[:, r, :], ("mix", r)
            P.dma(m_ap[:, 512:1024], D["dl"][rs, :], [], [(mk, 1)])
            sm, smk = small.next()
            for h in range(4):
                P.act(junk[:], o_ap[:, h * 128:(h + 1) * 128], AF.Square, [ok_], ["junk", (smk, h)], accum_out=sm[:, h:h + 1])
            hk = [(smk, h) for h in range(4)]
            P.v(lambda e, sm=sm: e.tensor_scalar(sm[:, 0:4], sm[:, 0:4], 1.0 / 128, EPS, ALU.mult, ALU.add), hk, hk)
            P.v(lambda e, sm=sm: e.tensor_tensor(sm[:, 4:8], sm[:, 0:4], consts[:, 0:4], ALU.pow), hk + ["consts"],
                [(smk, 4)], eng="pool")
            P.v(lambda e, r_ap=r_ap: e.tensor_tensor(gr[:], gng[:], r_ap[:], ALU.mult), ["gng", rk], ["gr"])
            for h in range(4):
                P.v(lambda e, h=h, o_ap=o_ap, sm=sm, m_ap=m_ap: e.scalar_tensor_tensor(
                    m_ap[:, h * 128:(h + 1) * 128], o_ap[:, h * 128:(h + 1) * 128], sm[:, 4 + h:5 + h],
                    gr[:, h * 128:(h + 1) * 128], ALU.mult, ALU.mult), [ok_, (smk, 4), "gr"], [(mk, 0)])

        def out_proj(r):
            rs = slice(r * 128, (r + 1) * 128)
            m_ap, mk = mix_all[:, r, :], ("mix", r)
            bk, bkey = nT()
            bkb = bk[:].bitcast(BF16)
            for kc in range(8):
                P.tr(bkb[:, kc * 128:(kc + 1) * 128], m_ap[:, kc * 128:(kc + 1) * 128], ident[:],
                     [(mk, 0), (mk, 1), "ident"], [bkey])
            mT, mTk = mixTr.next()
            P.act(mT[:].rearrange("p a b -> p (a b)"), bkb, AF.Copy, [bkey], [mTk])
            x_ap, xk = xt.next()
            P.dma(x_ap[:], D["x2"][rs, :], [], [xk])
            t_ap, tk = ot.next()
            for dh in range(2):
                W, Wk = nW()
                for kc in range(8):
                    P.mm(W[:, :], mT[:, kc, :], w_out[:, kc, dh * 512:(dh + 1) * 512], kc == 0, kc == 7,
                         [mTk, "w_out"], [Wk])
                P.v(lambda e, t_ap=t_ap, W=W, dh=dh: e.tensor_tensor(
                    t_ap[:, dh * 512:(dh + 1) * 512], W[:, :], G[0][0][:, dh * 512:(dh + 1) * 512], ALU.mult),
                    [Wk, G[0][1]], [tk])
            P.v(lambda e, t_ap=t_ap, x_ap=x_ap: e.tensor_tensor(t_ap[:], t_ap[:], x_ap[:], ALU.add), [tk, xk], [tk],
                eng="pool")
            P.dma(D["x3"][rs, :], t_ap[:], [tk], [("x3", r)])
        _gla_pass(nc, P, D, banks, st, sb, 1, list(range(15, -1, -1)), S, Sb, on_out)
        for r in range(15, -1, -1):
            out_proj(r)
        P.flush()


I32 = mybir.dt.int32
STILE = 256
NB = STILE // 128


def n_stiles(T):
    return (2 * T + 32 * (STILE - 1) + STILE - 1) // STILE


def phase_moe_sparse(nc, P, D, banks, layer, xin, xout, tiles, tag, final_norm=False):
    NTl = len(tiles)
    T = NTl * 128
    NST = n_stiles(T)
    NSLOT = NST * STILE
    Xs, Ys = D["Xs"], D["Ys"]
    wgv = D["moe_w_gate%d" % layer].rearrange("e (p a kc) f -> (e p a) (kc f)", p=128, a=2)
    wuv = D["moe_w_up%d" % layer].rearrange("e (p a kc) f -> (e p a) (kc f)", p=128, a=2)
    wdv = D["moe_w_down%d" % layer].rearrange("e f d -> (e f) d")
    with contextlib.ExitStack() as st0:
        sb0 = lambda name, shape, dt=F32: st0.enter_context(nc.sbuf_tensor(tag + name, shape, dt))
        consts = sb0("consts", [128, 4])
        P.v(lambda e: e.memset(consts[:, 0:1], -0.5), [], ["consts"])
        neghalf = consts[:, 0:1]
        idxA = sb0("idxA", [128, NTl], I32); idxB = sb0("idxB", [128, NTl], I32)
        wAB = sb0("wAB", [128, 2, NTl])
        widx = sb0("widx", [128, NST, 6], I32)
        with contextlib.ExitStack() as st:
            sb = lambda name, shape, dt=F32: st.enter_context(nc.sbuf_tensor(tag + name, shape, dt))
            nA, nB = bank_rot(banks, 0, 4), bank_rot(banks, 4, 7)
            cntb, cntk = banks[7], ("ps", 7)
            ident = sb("ident32", [128, 128])
            gbc = sb("gbc", [128, 1024])
            w_r = sb("w_r", [128, 8, 36])
            gm = sb("gm", [128, 2, 128])
            eidrow = sb("eidrow", [128, 32])
            pc2 = sb("pc2", [128, 6])
            Abc = load_mod_rows(P, nc, st, D["mods"], layer, SC2, tag + "A")
            Bbc = load_mod_rows(P, nc, st, D["mods"], layer, SH2, tag + "B")
            xt = Rot("mxt", [sb("xt%d" % i, [128, 1024]) for i in range(2)])
            junk = sb("junk", [128, 1024])
            t1r = Rot("t1", [sb("t1_%d" % i, [128, 1024]) for i in range(2)])
            h2r = Rot("h2", [sb("h2_%d" % i, [128, 1024]) for i in range(2)])
            h2b = sb("h2b", [128, NTl, 1024], BF16)
            h2Tr = Rot("h2T32", [sb("h2T32_%d" % i, [128, 8, 128]) for i in range(2)])
            small = Rot("msmall", [sb("small%d" % i, [128, 16]) for i in range(2)])
            rt = Rot("mrt", [sb("rt%d" % i, [128, 96]) for i in range(2)])
            selA = sb("selA", [128, NTl, 32]); selB = sb("selB", [128, NTl, 32]); selm = sb("selm", [128, NTl, 32])
            LG = sb("LG", [128, NTl, 36])
            rs_ = sb("rs_", [128, 8, NTl])
            goh = sb("goh", [128, NTl, 4]); gex = sb("gex", [128, NTl, 4])
            etmp = sb("etmp", [128, NTl, 4, 8])
            ein = sb("ein", [128, NTl, 8]); oh1 = sb("oh1", [128, NTl, 8]); e2 = sb("e2", [128, NTl, 8]); oh2 = sb("oh2", [128, NTl, 8])
            zt = sb("zt", [128, 8, 1024], BF16)
            P.v(lambda e: e.memset(zt[:], 0.0), [], ["zt"], eng="pool")
            zkeys = []
            for z0 in range(0, NSLOT, 1024):
                nrow = min(1024, NSLOT - z0)
                P.dma(Xs[z0:z0 + nrow, :].rearrange("(a p) c -> p a c", p=128), zt[:, 0:nrow // 128, :], ["zt"], [("Xsz", z0)])
                zkeys.append(("Xsz", z0))
            P.dma(ident[:], D["ident32"][:, :], [], ["ident"])
            P.dma(gbc[:], D["norm_ffn_g"][layer:layer + 1, :].partition_broadcast(128), [], ["gbc"])
            P.dma(w_r[:, :, 0:4], D["moe_w_rg"][layer].rearrange("(kc p) n -> p kc n", p=128), [], [("w_r", 0)])
            P.dma(w_r[:, :, 4:36], D["moe_w_re"][layer].rearrange("(kc p) n -> p kc n", p=128), [], [("w_r", 1)])
            P.dma(gm[:], D["gmask"][3:5].rearrange("a p n -> p a n"), [], ["gm"])
            P.dma(eidrow[:], D["eidrow"][:, :], [], ["eidrow"])
            P.dma(pc2[:], D["pc2"][:, :], [], ["pc2"])
            for v in range(2):
                a, ak = Abc[v]
                P.v(lambda e, a=a: e.scalar_tensor_tensor(a[:], a[:], 1.0, gbc[:], ALU.add, ALU.mult), [ak, "gbc"], [ak])
            for ti, (r, var) in enumerate(tiles):
                A, Ak = Abc[var]; B, Bk = Bbc[var]
                x_ap, xk = xt.next()
                P.dma(x_ap[:], D[xin][r * 128:(r + 1) * 128, :], [], [xk])
                sm, smk = small.next()
                P.act(junk[:], x_ap[:], AF.Square, [xk], ["junk", (smk, 0)], accum_out=sm[:, 0:1])
                rms_rstd(P, sm[:, 0:1], (smk, 0), sm[:, 2:3], (smk, 2), neghalf, 1024, sm[:, 1:2], (smk, 1))
                t1, t1k = t1r.next(); h2, h2k = h2r.next(); h2T32, hTk = h2Tr.next()
                P.v(lambda e, x_ap=x_ap, sm=sm, A=A, t1=t1: e.scalar_tensor_tensor(
                    t1[:], x_ap[:], sm[:, 2:3], A[:], ALU.mult, ALU.mult), [xk, (smk, 2), Ak], [t1k])
                P.v(lambda e, B=B, t1=t1, h2=h2: e.tensor_tensor(h2[:], t1[:], B[:], ALU.add), [t1k, Bk], [h2k])
                P.act(h2b[:, ti, :], h2[:], AF.Copy, [h2k], [("h2b", ti)])
                for half in range(2):
                    bk, bkey = nA()
                    for j in range(4):
                        kc = half * 4 + j
                        P.tr(bk[:, j * 128:(j + 1) * 128], h2[:, kc * 128:(kc + 1) * 128], ident[:], [h2k, "ident"], [bkey])
                    P.v(lambda e, bk=bk, half=half, h2T32=h2T32: e.tensor_copy(
                        h2T32[:, half * 4:(half + 1) * 4, :], bk[:, :].rearrange("p (a b) -> p a b", a=4)),
                        [bkey], [(hTk, half)])
                lg, lgk = nB()
                for kc in range(8):
                    P.mm(lg[:, 0:36], h2T32[:, kc, :], w_r[:, kc, :], kc == 0, kc == 7,
                         [(hTk, kc // 4), ("w_r", 0), ("w_r", 1)], [lgk])
                P.v(lambda e, lg=lg, ti=ti: e.tensor_copy(LG[:, ti, :], lg[:, 0:36]), [lgk], [("LG", ti)])
            lgk_all = [("LG", ti) for ti in range(NTl)]
            NT = NTl
            G = LG[:, :, 0:4]
            E4 = LG[:, :, 4:36].rearrange("p t (g e) -> p t g e", g=4)
            bc = lambda ap2, n: ap2.unsqueeze(2).to_broadcast([128, NT, n])
            P.v(lambda e: e.tensor_reduce(rs_[:, 0, :], G, AX.X, ALU.max), lgk_all, ["gmax"])
            P.v(lambda e: e.tensor_tensor(goh[:], G, bc(rs_[:, 0, :], 4), ALU.is_equal), lgk_all + ["gmax"], ["goh"])
            P.v(lambda e: e.tensor_tensor(gex[:], G, bc(rs_[:, 0, :], 4), ALU.subtract), lgk_all + ["gmax"], ["gex"])
            P.act(gex[:], gex[:], AF.Exp, ["gex"], ["gex"])
            P.v(lambda e: e.tensor_reduce(rs_[:, 1, :], gex[:], AX.X, ALU.add), ["gex"], ["gsum"])
            P.v(lambda e: e.reciprocal(rs_[:, 2, :], rs_[:, 1, :]), ["gsum"], ["pmax"])
            P.v(lambda e: e.tensor_tensor(etmp[:], E4, goh[:].unsqueeze(3).to_broadcast([128, NT, 4, 8]), ALU.mult),
                lgk_all + ["goh"], ["etmp"])
            P.v(lambda e: e.tensor_tensor(ein[:], etmp[:, :, 0, :], etmp[:, :, 1, :], ALU.add), ["etmp"], ["ein"])
            P.v(lambda e: e.tensor_tensor(ein[:], ein[:], etmp[:, :, 2, :], ALU.add), ["etmp", "ein"], ["ein"])
            P.v(lambda e: e.tensor_tensor(ein[:], ein[:], etmp[:, :, 3, :], ALU.add), ["etmp", "ein"], ["ein"])
            P.v(lambda e: e.tensor_reduce(rs_[:, 3, :], ein[:], AX.X, ALU.max), ["ein"], ["m1"])
            P.v(lambda e: e.tensor_tensor(oh1[:], ein[:], bc(rs_[:, 3, :], 8), ALU.is_equal), ["ein", "m1"], ["oh1"])
            P.v(lambda e: e.scalar_tensor_tensor(e2[:], oh1[:], -1e30, ein[:], ALU.mult, ALU.add), ["oh1", "ein"], ["e2"])
            P.v(lambda e: e.tensor_reduce(rs_[:, 4, :], e2[:], AX.X, ALU.max), ["e2"], ["m2"])
            P.v(lambda e: e.tensor_tensor(oh2[:], e2[:], bc(rs_[:, 4, :], 8), ALU.is_equal), ["e2", "m2"], ["oh2"])
            P.v(lambda e: e.tensor_tensor(rs_[:, 5, :], rs_[:, 4, :], rs_[:, 3, :], ALU.subtract), ["m1", "m2"], ["dd"])
            P.act(rs_[:, 6, :], rs_[:, 5, :], AF.Exp, ["dd"], ["ed"])
            P.v(lambda e: e.tensor_scalar(rs_[:, 7, :], rs_[:, 6, :], 1.0, None, ALU.add), ["ed"], ["w1"])
            P.v(lambda e: e.reciprocal(rs_[:, 7, :], rs_[:, 7, :]), ["w1"], ["w1"])
            P.v(lambda e: e.tensor_tensor(wAB[:, 0, :], rs_[:, 7, :], rs_[:, 2, :], ALU.mult), ["w1", "pmax"], ["wA"])
            P.v(lambda e: e.tensor_tensor(wAB[:, 1, :], wAB[:, 0, :], rs_[:, 6, :], ALU.mult), ["wA", "ed"], ["wB"])
            s4 = lambda ap3: ap3[:].rearrange("p t (g e) -> p t g e", g=4)
            P.v(lambda e: e.tensor_tensor(s4(selA), oh1[:].unsqueeze(2).to_broadcast([128, NT, 4, 8]),
                                          goh[:].unsqueeze(3).to_broadcast([128, NT, 4, 8]), ALU.mult), ["oh1", "goh"], ["selA"])
            P.v(lambda e: e.tensor_tensor(s4(selB), oh2[:].unsqueeze(2).to_broadcast([128, NT, 4, 8]),
                                          goh[:].unsqueeze(3).to_broadcast([128, NT, 4, 8]), ALU.mult), ["oh2", "goh"], ["selB"])
            P.v(lambda e: e.tensor_tensor(selm[:], selA[:], selB[:], ALU.add), ["selA", "selB"], ["selm"])
            for ti in range(NTl):
                P.mm(cntb[:, 0:32], gm[:, 1, :], selm[:, ti, :], ti == 0, ti == NTl - 1, ["gm", "selm"], [cntk])
            seg = sb("seg", [128, 8, 32])
            segT = sb("segT", [32, 128])
            ecol = sb("ecol", [128, NST])
            sti = sb("sti", [128, 2, NST, 32])
            stc = sb("stc", [128, 64])
            P.dma(stc[:], D["stile_c"][:, :], [], ["stc"])
            widxf = sb("widxf", [128, NST, 6])
            slotf = sb("slotf", [128, 2, NTl])
            P.v(lambda e: e.tensor_copy(seg[:, 0, :], cntb[:, 0:32]), [cntk], ["cnt"])
            P.v(lambda e: e.tensor_scalar(seg[:, 1, :], seg[:, 0, :], 0.0, None, ALU.is_gt), ["cnt"], ["nst"])
            for k in range(1, (T + STILE - 1) // STILE + 1):
                P.v(lambda e, k=k: e.scalar_tensor_tensor(seg[:, 1, :], seg[:, 0, :], float(STILE * k), seg[:, 1, :],
                                                          ALU.is_gt, ALU.add), ["cnt", "nst"], ["nst"])
            P.v(lambda e: e.tensor_scalar(seg[:, 2, :], seg[:, 1, :], float(STILE), None, ALU.mult), ["nst"], ["pc"])
            bk, bkey = nA()
            P.tr(bk[0:32, 0:128], seg[:, 2, :], ident[:], ["pc", "ident"], [bkey])
            P.v(lambda e, bk=bk: e.tensor_copy(segT[:], bk[0:32, 0:128]), [bkey], ["segT"])
            bk2, bkey2 = nA()
            P.mm(bk2[:, 0:32], segT[0:32, :], gm[0:32, 0, 0:32], True, True, ["segT", "gm"], [bkey2])
            P.v(lambda e, bk2=bk2: e.tensor_copy(seg[:, 3, :], bk2[:, 0:32]), [bkey2], ["start"])
            P.v(lambda e: e.tensor_tensor(seg[:, 4, :], seg[:, 3, :], seg[:, 2, :], ALU.add), ["start", "pc"], ["end"])
            P.v(lambda e: e.tensor_copy(seg[:, 5, :], seg[:, 3, :]), ["start"], ["base"])
            bci = lambda ap2: ap2.unsqueeze(1).to_broadcast([128, NST, 32])
            cI = stc[:, 0:NST].unsqueeze(2).to_broadcast([128, NST, 32])
            P.v(lambda e: e.tensor_tensor(sti[:, 0, :, :], bci(seg[:, 3, :]), cI, ALU.is_le), ["start", "stc"], ["sti0"])
            P.v(lambda e: e.tensor_tensor(sti[:, 1, :, :], bci(seg[:, 4, :]), cI, ALU.is_gt), ["end", "stc"], ["sti1"])
            P.v(lambda e: e.tensor_tensor(sti[:, 0, :, :], sti[:, 0, :, :], sti[:, 1, :, :], ALU.mult), ["sti0", "sti1"], ["sti0"])
            P.v(lambda e: e.tensor_tensor(sti[:, 0, :, :], sti[:, 0, :, :], bci(eidrow[:]), ALU.mult), ["sti0", "eidrow"], ["sti0"])
            P.v(lambda e: e.tensor_reduce(ecol[:, :], sti[:, 0, :, :], AX.X, ALU.add), ["sti0"], ["ecol"])
            ek = ["ecol"]
            for a_ in range(2):
                P.v(lambda e, a_=a_: e.tensor_scalar(widxf[:, :, a_], ecol[:, :], 256.0, pc2[:, a_:a_ + 1], ALU.mult, ALU.add),
                    ek + ["pc2"], [("widxf", a_)])
            for fc in range(4):
                P.v(lambda e, fc=fc: e.tensor_scalar(widxf[:, :, 2 + fc], ecol[:, :], 512.0, pc2[:, 2 + fc:3 + fc], ALU.mult, ALU.add),
                    ek + ["pc2"], [("widxf", 2 + fc)])
            P.v(lambda e: e.tensor_copy(widx[:].rearrange("p a b -> p (a b)"), widxf[:].rearrange("p a b -> p (a b)")),
                [("widxf", j) for j in range(6)], ["widx"])
            for ti in range(NTl):
                wi, wik = nB()
                P.mm(wi[:, 0:32], gm[:, 0, :], selm[:, ti, :], True, True, ["gm", "selm"], [wik])
                P.mm(wi[:, 32:64], gm[:, 1, :], selm[:, ti, :], True, True, ["gm", "selm"], [wik])
                P.v(lambda e, wi=wi: e.tensor_tensor(seg[:, 6, :], wi[:, 0:32], seg[:, 5, :], ALU.add), [wik, "base"], ["segtmp"])
                P.v(lambda e, ti=ti: e.scalar_tensor_tensor(seg[:, 7, :], seg[:, 6, :], 1.0, selA[:, ti, :], ALU.mult, ALU.mult,
                                                            accum_out=slotf[:, 0, ti:ti + 1]), ["segtmp", "selA"],
                    ["segtmp2", ("slotA", ti)])
                P.v(lambda e, ti=ti: e.scalar_tensor_tensor(seg[:, 7, :], seg[:, 6, :], 1.0, selB[:, ti, :], ALU.mult, ALU.mult,
                                                            accum_out=slotf[:, 1, ti:ti + 1]), ["segtmp", "selB"],
                    ["segtmp2", ("slotB", ti)])
                P.v(lambda e, wi=wi: e.tensor_tensor(seg[:, 5, :], wi[:, 32:64], seg[:, 5, :], ALU.add), [wik, "base"], ["base"])
            P.v(lambda e: e.tensor_copy(idxA[:], slotf[:, 0, :]), [("slotA", ti) for ti in range(NTl)], ["idxA"])
            P.v(lambda e: e.tensor_copy(idxB[:], slotf[:, 1, :]), [("slotB", ti) for ti in range(NTl)], ["idxB"])
            for ti in range(NTl):
                for (ix, ixk) in ((idxA, "idxA"), (idxB, "idxB")):
                    P.op("pool", lambda e, ix=ix, ti=ti: e.indirect_dma_start(
                        out=Xs[0:NSLOT, :], out_offset=bass.IndirectOffsetOnAxis(ap=ix[:, ti:ti + 1], axis=0),
                        in_=h2b[:, ti, :], in_offset=None, bounds_check=None),
                        [("h2b", ti), ixk] + zkeys, [("Xs", ti, ixk)], dma=True, kind="dma")
            P.flush()
        with contextlib.ExitStack() as st:
            sb = lambda name, shape, dt=F32: st.enter_context(nc.sbuf_tensor(tag + name, shape, dt))
            nT, nGU, nY = bank_rot(banks, 0, 2), bank_rot(banks, 2, 6), bank_rot(banks, 6, 8)
            ident = sb("identb", [128, 128], BF16)
            P.dma(ident[:], D["identbf"][:, :], [], ["ident"])
            wg = Rot("wg", [sb("wg%d" % i, [128, 8, 512], BF16) for i in range(2)])
            wu = Rot("wu", [sb("wu%d" % i, [128, 8, 512], BF16) for i in range(2)])
            wd = Rot("wd", [sb("wd%d" % i, [128, 4, 1024], BF16) for i in range(2)])
            xr = Rot("xr", [sb("xr%d" % i, [128, 1024], BF16) for i in range(4)])
            XT = Rot("XT", [sb("XT%d" % i, [128, 8, STILE], BF16) for i in range(2)])
            hg = Rot("hg", [sb("hg%d" % i, [128, 4, STILE], BF16) for i in range(2)])
            sg = Rot("sg", [sb("sg%d" % i, [128, STILE]) for i in range(2)])
            yb = Rot("yb", [sb("yb%d" % i, [128, 1024]) for i in range(3)])

            def gather(dst2d, src, col, i, key):
                P.op("pool", lambda e: e.indirect_dma_start(
                    out=dst2d, out_offset=None, in_=src,
                    in_offset=bass.IndirectOffsetOnAxis(ap=widx[:, i, col:col + 1], axis=0),
                    bounds_check=None), [], [key], dma=True, kind="dma")

            def emit_gu(i):
                g_ap, gk = wg.next(); u_ap, uk = wu.next(); d_ap, dk = wd.next()
                for a_ in range(2):
                    gather(g_ap[:, 4 * a_:4 * a_ + 4, :].rearrange("p a f -> p (a f)"), wgv, a_, i, (gk, a_))
                    gather(u_ap[:, 4 * a_:4 * a_ + 4, :].rearrange("p a f -> p (a f)"), wuv, a_, i, (uk, a_))
                for fc in range(4):
                    gather(d_ap[:, fc, :], wdv, 2 + fc, i, (dk, fc))
                xT, xTk = XT.next()
                for b in range(NB):
                    x_ap, xk = xr.next()
                    P.dma(x_ap[:], Xs[i * STILE + b * 128:i * STILE + (b + 1) * 128, :], [], [xk])
                    bk, bkey = nT()
                    bkb = bk[:].bitcast(BF16)
                    xv = x_ap[:].rearrange("p (m kc) -> p kc m", kc=8)
                    for kc in range(8):
                        P.tr(bkb[:, kc * 128:(kc + 1) * 128], xv[:, kc, :], ident[:], [xk, "ident"], [bkey])
                    P.act(xT[:, :, b * 128:(b + 1) * 128], bkb.rearrange("p (a b) -> p a b", a=8), AF.Copy, [bkey], [(xTk, b)])
                xkeys = [(xTk, b) for b in range(NB)]
                hgt, hgk = hg.next()
                for fc in range(4):
                    Gp, Gk = nGU(); Up, Uk = nGU()
                    for kc in range(8):
                        P.mm(Gp[:, 0:STILE], g_ap[:, kc, fc * 128:(fc + 1) * 128], xT[:, kc, :], kc == 0, kc == 7,
                             [(gk, kc // 4)] + xkeys, [Gk])
                    for kc in range(8):
                        P.mm(Up[:, 0:STILE], u_ap[:, kc, fc * 128:(fc + 1) * 128], xT[:, kc, :], kc == 0, kc == 7,
                             [(uk, kc // 4)] + xkeys, [Uk])
                    s_ap, sk = sg.next()
                    P.act(s_ap[:, :], Gp[:, 0:STILE], AF.Silu, [Gk], [sk])
                    P.v(lambda e, hgt=hgt, Up=Up, s_ap=s_ap, fc=fc: e.tensor_tensor(
                        hgt[:, fc, :], Up[:, 0:STILE], s_ap[:, :], ALU.mult), [Uk, sk], [(hgk, fc)])
                return (i, hgt, hgk, d_ap, dk)

            def emit_down(i, hgt, hgk, d_ap, dk):
                for b in range(NB):
                    y_ap, yk = yb.next()
                    for dh in range(2):
                        Yp, Yk = nY()
                        for fc in range(4):
                            P.mm(Yp[:, :], hgt[:, fc, b * 128:(b + 1) * 128], d_ap[:, fc, dh * 512:(dh + 1) * 512],
                                 fc == 0, fc == 3, [(hgk, f) for f in range(4)] + [(dk, fc)], [Yk])
                        if dh == 0:
                            P.act(y_ap[:, 0:512], Yp[:, :], AF.Copy, [Yk], [(yk, 0)])
                        else:
                            P.v(lambda e, y_ap=y_ap, Yp=Yp: e.tensor_copy(y_ap[:, 512:1024], Yp[:, :]), [Yk], [(yk, 1)])
                    r0 = i * STILE + b * 128
                    P.dma(Ys[r0:r0 + 128, :], y_ap[:], [(yk, 0), (yk, 1)], [("Ys", r0)])

            pend = None
            for i in range(NST):
                cur = emit_gu(i)
                if pend is not None:
                    emit_down(*pend)
                pend = cur
            emit_down(*pend)
            P.flush()
        with contextlib.ExitStack() as st:
            sb = lambda name, shape, dt=F32: st.enter_context(nc.sbuf_tensor(tag + name, shape, dt))
            G = load_mod_rows(P, nc, st, D["mods"], layer, GT2, tag + "G")
            xt = Rot("mxt2", [sb("xt2_%d" % i, [128, 1024]) for i in range(4)])
            ya = Rot("ya", [sb("ya%d" % i, [128, 1024]) for i in range(4)])
            ybb = Rot("ybb", [sb("ybb%d" % i, [128, 1024]) for i in range(4)])
            if final_norm:
                fg = sb("fg", [128, 1024])
                P.dma(fg[:], D["final_norm_g"][0:1, :].partition_broadcast(128), [], ["fg"])
                junk = sb("junk3", [128, 1024])
                small = Rot("fsmall", [sb("fsmall%d" % i, [128, 4]) for i in range(2)])
            for ti, (r, var) in enumerate(tiles):
                x_ap, xk = xt.next()
                P.dma(x_ap[:], D[xin][r * 128:(r + 1) * 128, :], [], [xk])
                a_ap, ak = ya.next(); b_ap, bk_ = ybb.next()
                for (dst, dkey, ix) in ((a_ap, ak, idxA), (b_ap, bk_, idxB)):
                    P.op("pool", lambda e, dst=dst, ix=ix, ti=ti: e.indirect_dma_start(
                        out=dst[:, :], out_offset=None, in_=Ys[0:NSLOT, :],
                        in_offset=bass.IndirectOffsetOnAxis(ap=ix[:, ti:ti + 1], axis=0),
                        bounds_check=None), [], [dkey], dma=True, kind="dma")
                P.v(lambda e, a_ap=a_ap, ti=ti: e.tensor_scalar(a_ap[:], a_ap[:], wAB[:, 0, ti:ti + 1], None, ALU.mult), [ak], [ak])
                P.v(lambda e, a_ap=a_ap, b_ap=b_ap, ti=ti: e.scalar_tensor_tensor(
                    a_ap[:], b_ap[:], wAB[:, 1, ti:ti + 1], a_ap[:], ALU.mult, ALU.add), [ak, bk_], [ak])
                P.v(lambda e, a_ap=a_ap, var=var: e.tensor_tensor(a_ap[:], a_ap[:], G[var][0][:], ALU.mult), [ak, G[var][1]], [ak])
                P.v(lambda e, a_ap=a_ap, x_ap=x_ap: e.tensor_tensor(a_ap[:], a_ap[:], x_ap[:], ALU.add), [ak, xk], [ak])
                if final_norm:
                    sm, smk = small.next()
                    P.act(junk[:], a_ap[:], AF.Square, [ak], ["junk3", (smk, 0)], accum_out=sm[:, 0:1])
                    rms_rstd(P, sm[:, 0:1], (smk, 0), sm[:, 2:3], (smk, 2), neghalf, 1024, sm[:, 1:2], (smk, 1))
                    P.v(lambda e, a_ap=a_ap, sm=sm: e.scalar_tensor_tensor(
                        a_ap[:], a_ap[:], sm[:, 2:3], fg[:], ALU.mult, ALU.mult), [ak, (smk, 2), "fg"], [ak])
                P.dma(D[xout][r * 128:(r + 1) * 128, :], a_ap[:], [ak], [(xout, r)])
            P.flush()

import numpy as np
import ml_dtypes

BF = ml_dtypes.bfloat16
GRID_W = 64

W_SMALL = {
    "ada_w": [2, 1024, 6144], "ada_b": [2, 6144], "norm_mix_g": [2, 1024], "norm_ffn_g": [2, 1024],
    "even_w_in": [1, 1024, 1184], "mla_q_norm_g": [1, 256], "mla_w_uq": [1, 256, 768], "mla_kv_norm_g": [1, 128],
    "mla_w_ukv": [1, 128, 1024], "win_sink": [1, 8], "even_w_out": [1, 1024, 1024],
    "odd_w_in": [1, 1024, 2592], "gla_w_g2": [1, 2, 16, 256], "gla_b_g": [1, 2, 256], "gla_norm_g": [1, 512],
    "sg_ln_g": [1, 512], "sg_ln_b": [1, 512], "odd_w_out": [1, 1024, 1024],
    "moe_w_rg": [2, 1024, 4], "moe_w_re": [2, 1024, 32], "final_norm_g": [1, 1024],
}
W_MOE = {"moe_w_gate": [32, 1024, 512], "moe_w_up": [32, 1024, 512], "moe_w_down": [32, 512, 1024]}
CONSTS = {"wmask": ([2, 128, 512], BF16), "ident32": ([128, 128], F32), "identbf": ([128, 128], BF16),
          "gmask": ([5, 128, 128], F32), "eidrow": ([128, 32], F32), "pc2": ([128, 6], F32), "stile_c": ([128, 64], F32)}
PERCORE = {"xtok": [NTOK, 1024], "cvec": [2, 1024], "cA": [NTOK, 256], "sA": [NTOK, 256], "cB": [NTOK, 512],
           "sB": [NTOK, 512], "sg_w_sT": [4, 128, 128], "sg_b_sT": [128, 4], "sel": [128, 2]}
SCRATCH = {"mods": ([2, 2, 6144], F32), "QAT": ([96, 8, NTOK], BF16), "KAT": ([96, 8, NTOK], BF16),
           "VA": ([NTOK, 520], BF16), "QBT": ([64, 8, NTOK], BF16), "KBT": ([64, 2, NTOK], BF16),
           "VB": ([NTOK, 130], BF16), "x1": ([NOWN, 1024], F32), "x2": ([NOWN, 1024], F32),
           "g_T": ([128, 18, 8, 128], BF16), "g_kd": ([2, NOWN, 256], BF16), "g_v": ([NOWN, 512], BF16),
           "g_dec": ([128, 18, 4], F32), "rsilu": ([2048, 512], BF16), "dl": ([2048, 512], BF16),
           "OA": ([2048, 512], F32), "cc_in": ([256, 128], F32), "cc_out": ([512, 128], F32),
           "x3": ([2048, 1024], F32), "out": ([2048, 1024], F32),
           "Xs": ([n_stiles(NOWN) * STILE, 1024], BF16), "Ys": ([n_stiles(NOWN) * STILE, 1024], F32)}
HANDOFF = ["mods", "x2", "g_T", "g_kd", "g_v", "g_dec", "rsilu", "dl", "OA"]


def rope_tables(pos, dim, nheads):
    pos = np.asarray(pos)
    half = dim // 2
    inv = np.power(np.float32(10000.0), -np.arange(0, half, 2, dtype=np.float32) / np.float32(half)).astype(np.float32)
    row = (pos // GRID_W).astype(np.float32)
    col = (pos % GRID_W).astype(np.float32)
    ar = row[:, None] * inv[None, :]
    ac = col[:, None] * inv[None, :]
    ang = np.concatenate([ar, ar, ac, ac], axis=-1).astype(np.float32)
    cos = np.cos(ang).astype(np.float32)
    sin = np.sin(ang).astype(np.float32)
    blk = dim // 4
    sign = np.concatenate([-np.ones(blk), np.ones(blk), -np.ones(blk), np.ones(blk)]).astype(np.float32)
    ssin = sin * sign[None, :]
    no = pos < 0
    cos[no] = 1.0
    ssin[no] = 0.0
    return np.tile(cos, (1, nheads)), np.tile(ssin, (1, nheads))


_CONST = {}


def const_inputs():
    if not _CONST:
        j = np.arange(128)[:, None]
        i = np.arange(128)[None, :]
        m0 = np.tile((j >= i).astype(np.float32), (1, 4))
        m1 = np.tile((j <= i).astype(np.float32), (1, 4))
        _CONST["wmask"] = np.stack([m0, m1]).astype(BF)
        _CONST["ident32"] = np.eye(128, dtype=np.float32)
        _CONST["identbf"] = np.eye(128, dtype=np.float32).astype(BF)
        one = np.ones((128, 128), bool)
        _CONST["gmask"] = np.stack([(j <= i), (j >= i), one, (j < i), one]).astype(np.float32)
        _CONST["eidrow"] = np.tile(np.arange(32, dtype=np.float32)[None, :], (128, 1))
        p = np.arange(128, dtype=np.float32)
        _CONST["stile_c"] = np.tile((np.arange(64, dtype=np.float32) * STILE)[None, :], (128, 1))
        _CONST["pc2"] = np.stack([2 * p, 2 * p + 1, p, 128 + p, 256 + p, 384 + p], 1).astype(np.float32)
    return _CONST


def local_order(hf):
    own = np.arange(hf * 2048, (hf + 1) * 2048)
    oth = np.arange((1 - hf) * 2048, (2 - hf) * 2048)
    cidx = np.arange(256)
    if hf == 1:
        own, oth, cidx = own[::-1], oth[::-1], cidx[::-1]
    return own, oth, cidx


def weights_for(core, inp, layers=(0, 1)):
    hf = core % 2
    m = {}
    for k, shp in W_SMALL.items():
        m[k] = np.ascontiguousarray(np.asarray(inp[k]).reshape(shp))
    for l in layers:
        for k in W_MOE:
            m["%s%d" % (k, l)] = np.asarray(inp[k][l])
    ws = np.asarray(inp["sg_w_s"][0])
    bs = np.asarray(inp["sg_b_s"][0])
    if hf == 1:
        m["gla_w_g2"] = np.ascontiguousarray(m["gla_w_g2"][:, ::-1])
        m["gla_b_g"] = np.ascontiguousarray(m["gla_b_g"][:, ::-1])
        w = m["odd_w_in"].copy()
        w[:, :, 1024:1040] = m["odd_w_in"][:, :, 1040:1056]
        w[:, :, 1040:1056] = m["odd_w_in"][:, :, 1024:1040]
        m["odd_w_in"] = w
        ws = ws[:, ::-1, ::-1]
        bs = bs[:, ::-1]
    m["sg_w_sT"] = np.ascontiguousarray(ws.transpose(0, 2, 1))
    m["sg_b_sT"] = np.ascontiguousarray(bs.T)
    return m


def core_inputs(core, inp):
    b, hf = core // 2, core % 2
    own, oth, cidx = local_order(hf)
    pos = np.concatenate([own, oth, -np.ones(256, dtype=np.int64)])
    xtok = np.concatenate([inp["x"][b][own], inp["x"][b][oth], inp["ctx"][b][cidx]], 0)
    cA, sA = rope_tables(pos, 32, 8)
    cB, sB = rope_tables(pos, 64, 8)
    m = {"xtok": np.ascontiguousarray(xtok), "cvec": np.stack([inp["c"][b], inp["c_ctx"]]).astype(np.float32),
         "cA": cA, "sA": sA, "cB": cB, "sB": sB}
    sel = np.zeros((128, 2), np.float32)
    sel[:, 1 - hf] = 1.0
    m["sel"] = sel
    m.update(const_inputs())
    return m


def declare(nc, ext_in, ext_out, moe_layers=(0, 1)):
    D = {}
    dr = lambda n, s, dt=F32, k="ExternalInput": nc.dram_tensor(n, s, dt, kind=k).ap()
    for k, shp in PERCORE.items():
        D[k] = dr(k, shp)
    for k, (shp, dt) in CONSTS.items():
        D[k] = dr(k, shp, dt)
    for k, shp in W_SMALL.items():
        D[k] = dr(k, shp)
    for l in moe_layers:
        for k, shp in W_MOE.items():
            D["%s%d" % (k, l)] = dr("%s%d" % (k, l), shp)
    for k, (shp, dt) in SCRATCH.items():
        kind = "ExternalOutput" if k in ext_out else ("ExternalInput" if k in ext_in else "Internal")
        D[k] = dr(k, shp, dt, kind)
    return D


MOE0_TILES = [(i, 0) for i in range(16)] + [(16, 1), (17, 1)]
MOE1_TILES = [(i, 0) for i in range(16)]


def build_fused(extra_out=()):
    nc = bass.Bass("TRN2", target_bir_lowering=False)
    D = declare(nc, (), ["out"] + list(extra_out), moe_layers=(0, 1))
    banks = [nc.alloc_psum_tensor("bank%d" % i, [128, 512], F32) for i in range(8)]
    P = Prog(nc)
    phase_ada(nc, P, D, banks)
    phase_l0a(nc, P, D, banks)
    phase_l0b(nc, P, D, banks)
    phase_moe_sparse(nc, P, D, banks, 0, "x1", "x2", MOE0_TILES, "m0_")
    phase_l1a(nc, P, D, banks)
    phase_l1b_a(nc, P, D, banks)
    phase_l1b_b(nc, P, D, banks)
    phase_moe_sparse(nc, P, D, banks, 1, "x3", "out", MOE1_TILES, "m1_", final_norm=True)
    P.flush(final=True)
    return nc


def kernel(**inputs):
    inp = {k: np.asarray(v) for k, v in inputs.items()}
    n = 8
    nc = build_fused()
    in_maps = []
    for c in range(n):
        m = core_inputs(c, inp)
        m.update(weights_for(c, inp, layers=(0, 1)))
        in_maps.append(m)
    res = run_bass_kernel_spmd(nc, in_maps, core_ids=list(range(n))).results
    out = np.zeros((4, 4096, 1024), np.float32)
    for c in range(n):
        b, hf = c // 2, c % 2
        own = local_order(hf)[0]
        out[b][own] = np.asarray(res[c]["out"], dtype=np.float32)
    return out
```

```python
import contextlib
import numpy as np
import concourse.bass as bass
import concourse.mybir as mybir
from concourse.bass_utils import run_bass_kernel_spmd

F32 = mybir.dt.float32
BF16 = mybir.dt.bfloat16
AF = mybir.ActivationFunctionType
ALU = mybir.AluOpType
AX = mybir.AxisListType
N_DMA_SEMS = 10


class Op:
    __slots__ = ("eng", "fn", "deps", "signal", "sem", "val", "is_dma", "idx", "kind", "sem_eng")

    def __init__(self, eng, fn, is_dma, kind):
        self.eng = eng
        self.fn = fn
        self.deps = set()
        self.signal = False
        self.sem = None
        self.val = 0
        self.is_dma = is_dma
        self.kind = kind
        self.sem_eng = None


class Rot:
    def __init__(self, name, aps):
        self.name = name
        self.aps = aps
        self.i = 0

    def next(self):
        k = self.i % len(self.aps)
        self.i += 1
        return self.aps[k], (self.name, k)


class Prog:
    ENGS = ("pe", "act", "dve", "pool", "sp")

    def __init__(self, nc):
        self.nc = nc
        self.st = contextlib.ExitStack()
        st = self.st
        self.esem = {e: st.enter_context(nc.semaphore("s_" + e)) for e in ("pe", "act", "dve", "pool", "cc")}
        self.dsem = {e: [st.enter_context(nc.semaphore("d_%s%d" % (e, i))) for i in range(N_DMA_SEMS)]
                     for e in ("sp", "act", "pool")}
        self.cnt = {e: 0 for e in self.esem}
        self.dcnt = {e: [0] * N_DMA_SEMS for e in self.dsem}
        self.drr = {e: 0 for e in self.dsem}
        self.nflush = 0
        self.total_ops = 0
        self._reset()

    def _reset(self):
        self.ops = []
        self.last_w = {}
        self.readers = {}

    def op(self, eng, fn, reads=(), writes=(), dma=False, kind=""):
        o = Op(eng, fn, dma, kind)
        o.idx = len(self.ops)
        ex = [r for r in reads if isinstance(r, tuple) and r[0] == "ps"]
        if ex and eng != "pe":
            reads = [r for r in reads if r not in ex]
            writes = list(writes) + ex
        for r in reads:
            w = self.last_w.get(r)
            if w is not None:
                o.deps.add(w)
        for wkey in writes:
            w = self.last_w.get(wkey)
            if w is not None:
                o.deps.add(w)
            for rd in self.readers.get(wkey, ()):
                o.deps.add(rd)
        for r in reads:
            self.readers.setdefault(r, []).append(o.idx)
        for wkey in writes:
            self.last_w[wkey] = o.idx
            self.readers[wkey] = []
        o.deps.discard(o.idx)
        self.ops.append(o)
        return o

    def dma(self, out, in_, reads, writes, eng="sp", **kw):
        return self.op(eng, lambda e: e.dma_start(out=out, in_=in_, **kw), reads, writes, dma=True, kind="dma")

    def mm(self, out, lhsT, rhs, start, stop, reads, writes, **kw):
        return self.op("pe", lambda e: e.matmul(out, lhsT, rhs, start=start, stop=stop, **kw),
                       reads, writes, kind="mm")

    def tr(self, out, in_, ident, reads, writes):
        return self.op("pe", lambda e: e.transpose(out, in_, ident), reads, writes, kind="mm")

    def act(self, out, in_, func, reads, writes, **kw):
        return self.op("act", lambda e: e.activation(out, in_, func, **kw), reads, writes, kind="act")

    def cc(self, fn, reads, writes):
        o = self.op("pool", fn, reads, writes, kind="cc")
        o.sem_eng = "cc"
        o.signal = True
        return o

    def v(self, fn, reads, writes, eng="dve"):
        return self.op(eng, fn, reads, writes, kind="v")

    def flush(self, final=False):
        nc = self.nc
        ops = self.ops
        self.total_ops += len(ops)
        for o in ops:
            if o.eng == "pe":
                o.deps = {d for d in o.deps if ops[d].eng != "pe"}
        for o in ops:
            for d in o.deps:
                ops[d].signal = True
        base_cnt = dict(self.cnt)
        base_dcnt = {e: list(v) for e, v in self.dcnt.items()}
        dprev = {e: [None] * N_DMA_SEMS for e in self.dsem}
        for o in ops:
            if o.is_dma:
                o.signal = True
                k = self.drr[o.eng]
                self.drr[o.eng] = (k + 1) % N_DMA_SEMS
                self.dcnt[o.eng][k] += 16
                o.sem = self.dsem[o.eng][k]
                o.val = self.dcnt[o.eng][k]
                p = dprev[o.eng][k]
                if p is not None:
                    o.deps.add(p)
                dprev[o.eng][k] = o.idx
            elif o.signal:
                se = o.sem_eng or o.eng
                self.cnt[se] += 1
                o.sem = self.esem[se]
                o.val = self.cnt[se]
        first = self.nflush == 0
        self.nflush += 1
        with nc.Block() as blk:
            getters = {"pe": blk.tensor, "act": blk.scalar, "dve": blk.vector, "pool": blk.gpsimd, "sp": blk.sync}
            for ename in self.ENGS:
                mine = [o for o in ops if o.eng == ename]

                def body(e, mine=mine, ename=ename):
                    waited = {}
                    if not first:
                        for en, sem in self.esem.items():
                            if base_cnt[en] > 0 and en != ename:
                                e.wait_ge(sem, base_cnt[en])
                                waited[sem.num] = base_cnt[en]
                        for en, sems in self.dsem.items():
                            for k, sem in enumerate(sems):
                                if base_dcnt[en][k] > 0:
                                    e.wait_ge(sem, base_dcnt[en][k])
                                    waited[sem.num] = base_dcnt[en][k]
                    for o in mine:
                        need = {}
                        for d in o.deps:
                            do = ops[d]
                            if need.get(do.sem.num, (None, 0))[1] < do.val:
                                need[do.sem.num] = (do.sem, do.val)
                        for num, (sem, val) in need.items():
                            if waited.get(num, 0) >= val:
                                continue
                            e.wait_ge(sem, val)
                            waited[num] = val
                        ins = o.fn(e)
                        if o.signal:
                            if o.kind == "cc":
                                ins.then_inc(o.sem)
                            else:
                                ins.then_inc(o.sem, 16 if o.is_dma else 1)
                    if final and ename == "sp":
                        for en, sems in self.dsem.items():
                            for k, sem in enumerate(sems):
                                if self.dcnt[en][k] > 0:
                                    e.wait_ge(sem, self.dcnt[en][k])
                        for en, sem in self.esem.items():
                            if self.cnt[en] > 0:
                                e.wait_ge(sem, self.cnt[en])

                getters[ename](body)
        self._reset()
        if final:
            self.st.close()


def bank_rot(banks, lo, hi):
    state = {"i": 0}

    def nxt():
        k = lo + state["i"] % (hi - lo)
        state["i"] += 1
        return banks[k], ("ps", k)
    return nxt


EPS = 1e-6
NTOK = 4352
NOWN = 2304
SH1, SC1, GT1, SH2, SC2, GT2 = range(6)


def own_tiles():
    return [(i, i, 0) for i in range(16)] + [(16, 32, 1), (17, 33, 1)]


def rms_rstd(P, ss_ap, ss_key, out_ap, out_key, neghalf, D, tmp_ap, tmp_key):
    P.v(lambda e: e.tensor_scalar(tmp_ap, ss_ap, 1.0 / D, EPS, ALU.mult, ALU.add), [ss_key], [tmp_key])
    P.v(lambda e: e.tensor_tensor(out_ap, tmp_ap, neghalf, ALU.pow), [tmp_key, "consts"], [out_key], eng="pool")


def load_mod_rows(P, nc, st, mods_d, layer, which, names):
    out = []
    for v in range(2):
        t = st.enter_context(nc.sbuf_tensor("%s%d" % (names, v), [128, 1024], F32))
        P.dma(t[:], mods_d[layer, v:v + 1, which * 1024:(which + 1) * 1024].partition_broadcast(128), ["mods"],
              [(names, v)])
        out.append((t, (names, v)))
    return out


def phase_ada(nc, P, D, banks):
    with contextlib.ExitStack() as st:
        sb = lambda name, shape, dt=F32: st.enter_context(nc.sbuf_tensor(name, shape, dt))
        PS = Rot("ps", banks)
        ident = sb("ada_ident", [128, 128])
        cb = sb("ada_cb", [128, 2, 1024])
        screp = sb("ada_screp", [128, 2, 8, 128])
        bias = sb("ada_bias", [96, 2, 128])
        wbuf = Rot("ada_w", [sb("ada_wbuf%d" % i, [128, 8, 512]) for i in range(4)])
        accs = sb("ada_accs", [128, 2, 96])
        outT = sb("ada_outT", [96, 2, 128])
        P.dma(ident[:], D["ident32"][:, :], [], ["ident"])
        for v in range(2):
            P.dma(cb[:, v, :], D["cvec"][v:v + 1, :].partition_broadcast(128), [], [("cb", v)])
        for l in range(2):
            for v in range(2):
                P.dma(bias[48 * v:48 * v + 48, l, :], D["ada_b"][l].rearrange("(c p) -> c p", p=128), [], [("bias", l, v)])
        for v in range(2):
            P.act(cb[:, v, :], cb[:, v, :], AF.Silu, [("cb", v)], [("cb", v)])
            for half in range(2):
                p, pk = PS.next()
                for j in range(4):
                    kc = half * 4 + j
                    P.tr(p[:, j * 128:(j + 1) * 128], cb[:, v, kc * 128:(kc + 1) * 128], ident[:],
                         [("cb", v), "ident"], [pk])
                P.v(lambda e, p=p, v=v, half=half: e.tensor_copy(
                    screp[:, v, half * 4:(half + 1) * 4, :], p[:, :].rearrange("p (a b) -> p a b", a=4)),
                    [pk], [("screp", v)])
        for l in range(2):
            wl = D["ada_w"][l].rearrange("(kc p) n -> p kc n", p=128)
            acc, acck = PS.next()
            accv = acc[:, 0:96].rearrange("p (v c) -> p v c", v=2)
            for cblk in range(12):
                wb, wk = wbuf.next()
                P.dma(wb[:], wl[:, :, cblk * 512:(cblk + 1) * 512], [], [wk])
                for j in range(4):
                    c = cblk * 4 + j
                    for kc in range(8):
                        P.mm(accv[:, :, c], wb[:, kc, j * 128:(j + 1) * 128], screp[:, :, kc, 0], kc == 0, kc == 7,
                             [("screp", 0), ("screp", 1), wk], [acck])
            P.v(lambda e, acc=acc, l=l: e.tensor_copy(accs[:, l, :], acc[:, 0:96]), [acck], [("accs", l)])
            tp, tpk = PS.next()
            P.tr(tp[0:96, 0:128], accs[:, l, :], ident[:], [("accs", l), "ident"], [tpk])
            P.v(lambda e, tp=tp, l=l: e.tensor_tensor(outT[:, l, :], tp[0:96, 0:128], bias[:, l, :], ALU.add),
                [tpk, ("bias", l, 0), ("bias", l, 1)], [("outT", l)])
            P.dma(D["mods"][l].rearrange("v (c p) -> (v c) p", p=128), outT[:, l, :], [("outT", l)], ["mods"])
        P.flush()


def phase_l0a(nc, P, D, banks):
    NT = NTOK // 128
    with contextlib.ExitStack() as st:
        sb = lambda name, shape, dt=F32: st.enter_context(nc.sbuf_tensor(name, shape, dt))
        PS = Rot("ps", banks)
        ident = sb("a_ident", [128, 128], BF16)
        consts = sb("a_consts", [128, 4])
        gbc = sb("a_gbc", [128, 1024]); qg = sb("a_qg", [128, 256]); kvg = sb("a_kvg", [128, 128])
        w_in = sb("a_w_in", [128, 8, 1184], BF16)
        w_uq = sb("a_w_uq", [128, 2, 768], BF16)
        w_ukv = sb("a_w_ukv", [128, 1024], BF16)
        xt = Rot("xt", [sb("a_xt%d" % i, [128, 1024]) for i in range(2)])
        tabs = Rot("tabs", [sb("a_tabs%d" % i, [128, 1536]) for i in range(3)])
        junk = sb("a_junk", [128, 1024])
        t1r = Rot("t1", [sb("a_t1_%d" % i, [128, 1024]) for i in range(2)])
        hbr = Rot("hb", [sb("a_hb_%d" % i, [128, 1024], BF16) for i in range(2)])
        hTr = Rot("hT", [sb("a_hT_%d" % i, [128, 8, 128], BF16) for i in range(2)])
        small = Rot("small", [sb("a_small%d" % i, [128, 8]) for i in range(3)])
        zsr = Rot("zs", [sb("a_zs%d" % i, [128, 1184]) for i in range(2)])
        cqn = sb("a_cqn", [128, 384], BF16)
        cqnT = sb("a_cqnT", [128, 3, 128], BF16)
        krr = sb("a_krr", [128, 32], BF16)
        ropet = sb("a_ropet", [128, 2, 512])
        QA = sb("a_QA", [128, 8, 96], BF16); KA = sb("a_KA", [128, 8, 96], BF16)
        QB = sb("a_QB", [128, 8, 64], BF16); KB = sb("a_KB", [128, 2, 64], BF16)
        VAs = Rot("VAs", [sb("a_VAs%d" % i, [128, 8, 65], BF16) for i in range(2)])
        VBs = Rot("VBs", [sb("a_VBs%d" % i, [128, 2, 65], BF16) for i in range(2)])
        oQA = Rot("oQA", [sb("a_oQA%d" % i, [96, 8, 128], BF16) for i in range(2)])
        oKA = Rot("oKA", [sb("a_oKA%d" % i, [96, 8, 128], BF16) for i in range(2)])
        oQB = Rot("oQB", [sb("a_oQB%d" % i, [64, 8, 128], BF16) for i in range(2)])
        oKB = Rot("oKB", [sb("a_oKB%d" % i, [64, 2, 128], BF16) for i in range(2)])
        Abc = load_mod_rows(P, nc, st, D["mods"], 0, SC1, "a_A")
        Bbc = load_mod_rows(P, nc, st, D["mods"], 0, SH1, "a_B")

        P.dma(ident[:], D["identbf"][:, :], [], ["ident"])
        P.v(lambda e: e.memset(consts[:, 0:1], -0.5), [], ["consts"])
        P.dma(gbc[:], D["norm_mix_g"][0:1, :].partition_broadcast(128), [], ["gbc"])
        P.dma(qg[:], D["mla_q_norm_g"][0:1, :].partition_broadcast(128), [], ["qg"])
        P.dma(kvg[:], D["mla_kv_norm_g"][0:1, :].partition_broadcast(128), [], ["kvg"])
        P.dma(w_in[:], D["even_w_in"][0].rearrange("(kc p) n -> p kc n", p=128), [], ["w_in"], eng="pool")
        P.dma(w_uq[:], D["mla_w_uq"][0].rearrange("(kc p) n -> p kc n", p=128), [], ["w_uq"], eng="pool")
        P.dma(w_ukv[:], D["mla_w_ukv"][0], [], ["w_ukv"], eng="pool")
        for i, r in enumerate(VAs.aps):
            P.v(lambda e, r=r: e.memset(r[:, :, 64:65], 1.0), [], [("VAs", i)])
        for i, r in enumerate(VBs.aps):
            P.v(lambda e, r=r: e.memset(r[:, :, 64:65], 1.0), [], [("VBs", i)])
        for v in range(2):
            a, ak = Abc[v]
            P.v(lambda e, a=a: e.scalar_tensor_tensor(a[:], a[:], 1.0, gbc[:], ALU.add, ALU.mult), [ak, "gbc"], [ak])
        neghalf = consts[:, 0:1]
        v3 = lambda ap, h: ap.rearrange("p (h w) -> p h w", h=h)

        def rope(src4, dst4, cos4, ssin4, nh, blk, rkeys, wkeys):
            W = 4 * blk
            tmpa = ropet[:, 0, 0:nh * W].rearrange("p (h w) -> p h w", h=nh)
            tmpb = ropet[:, 1, 0:nh * W].rearrange("p (h w) -> p h w", h=nh)
            P.v(lambda e: e.tensor_tensor(tmpa, src4, cos4, ALU.mult), rkeys, ["ropeA"])
            v5 = lambda a: a.rearrange("p h (q w b) -> p h q w b", q=2, w=2)
            for w in range(2):
                P.v(lambda e, w=w: e.tensor_tensor(
                    v5(tmpb)[:, :, :, w, :], v5(src4)[:, :, :, 1 - w, :], v5(ssin4)[:, :, :, w, :], ALU.mult),
                    rkeys, ["ropeB%d" % w])
            P.v(lambda e: e.tensor_tensor(dst4, tmpa, tmpb, ALU.add), ["ropeA", "ropeB0", "ropeB1"], wkeys)

        def stage_a(t):
            rs = slice(t * 128, (t + 1) * 128)
            var = 1 if t >= 32 else 0
            need_q = (t < 16) or (t >= 32)
            A, Ak = Abc[var]; B, Bk = Bbc[var]
            x_ap, x_key = xt.next()
            P.dma(x_ap[:], D["xtok"][rs, :], [], [x_key])
            tb_ap, tb_key = tabs.next()
            P.dma(tb_ap[:, 0:256], D["cA"][rs, :], [], [(tb_key, 0)])
            P.dma(tb_ap[:, 256:512], D["sA"][rs, :], [], [(tb_key, 1)])
            P.dma(tb_ap[:, 512:1024], D["cB"][rs, :], [], [(tb_key, 2)])
            P.dma(tb_ap[:, 1024:1536], D["sB"][rs, :], [], [(tb_key, 3)])
            tkeys = [(tb_key, i) for i in range(4)]
            cA = tb_ap[:, 0:256]; sA = tb_ap[:, 256:512]; cB = tb_ap[:, 512:1024]; sB = tb_ap[:, 1024:1536]
            sm, sm_key = small.next()
            P.act(junk[:], x_ap[:], AF.Square, [x_key], ["junk", (sm_key, 0)], accum_out=sm[:, 0:1])
            rms_rstd(P, sm[:, 0:1], (sm_key, 0), sm[:, 2:3], (sm_key, 2), neghalf, 1024, sm[:, 1:2], (sm_key, 1))
            t1, t1k = t1r.next(); hb, hbk = hbr.next(); hT, hTk = hTr.next()
            P.v(lambda e, x_ap=x_ap, sm=sm, A=A, t1=t1: e.scalar_tensor_tensor(
                t1[:], x_ap[:], sm[:, 2:3], A[:], ALU.mult, ALU.mult), [x_key, (sm_key, 2), Ak], [t1k])
            P.v(lambda e, B=B, t1=t1, hb=hb: e.tensor_tensor(hb[:], t1[:], B[:], ALU.add), [t1k, Bk], [hbk])
            bk, bkey = PS.next()
            bkb = bk[:].bitcast(BF16)
            for kc in range(8):
                P.tr(bkb[:, kc * 128:(kc + 1) * 128], hb[:, kc * 128:(kc + 1) * 128], ident[:], [hbk, "ident"], [bkey])
            P.act(hT[:].rearrange("p a b -> p (a b)"), bkb, AF.Copy, [bkey], [hTk])
            blocks = [(0, 416), (416, 928), (928, 1184)]
            zb = []
            for bi, (c0, c1) in enumerate(blocks):
                if bi == 1 and not need_q:
                    zb.append((None, None))
                    continue
                bk, bkey = PS.next()
                for kc in range(8):
                    P.mm(bk[:, 0:c1 - c0], hT[:, kc, :], w_in[:, kc, c0:c1], kc == 0, kc == 7, [hTk, "w_in"], [bkey])
                zb.append((bk, bkey))
            zs, zsk = zsr.next()
            for bi, (c0, c1) in enumerate(blocks):
                if zb[bi][0] is None:
                    continue
                P.act(zs[:, c0:c1], zb[bi][0][:, 0:c1 - c0], AF.Copy, [zb[bi][1]], [(zsk, bi)])
            return dict(t=t, rs=rs, need_q=need_q, zs=zs, zsk=zsk, tb_ap=tb_ap, tkeys=tkeys, sm=sm, sm_key=sm_key)

        def stage_b(c):
            t, rs, need_q, zs, zsk, tb_ap, tkeys, sm, sm_key = (c[k] for k in ("t", "rs", "need_q", "zs", "zsk", "tb_ap", "tkeys", "sm", "sm_key"))
            cA = tb_ap[:, 0:256]; sA = tb_ap[:, 256:512]; cB = tb_ap[:, 512:1024]; sB = tb_ap[:, 1024:1536]
            z0, z0k = zs[:, 0:416], (zsk, 0)
            z1, z1k = zs[:, 416:928], (zsk, 1)
            z2, z2k = zs[:, 928:1184], (zsk, 2)
            if need_q:
                P.act(junk[:, 0:256], z0[:, 0:256], AF.Square, [z0k], ["junk", (sm_key, 3)], accum_out=sm[:, 3:4])
                rms_rstd(P, sm[:, 3:4], (sm_key, 3), sm[:, 4:5], (sm_key, 4), neghalf, 256, sm[:, 1:2], (sm_key, 1))
                P.v(lambda e, z0=z0, sm=sm: e.scalar_tensor_tensor(
                    cqn[:, 0:256], z0[:, 0:256], sm[:, 4:5], qg[:], ALU.mult, ALU.mult),
                    [z0k, (sm_key, 4), "qg"], ["cqn"])
            P.act(junk[:, 0:128], z0[:, 256:384], AF.Square, [z0k], ["junk", (sm_key, 5)], accum_out=sm[:, 5:6])
            rms_rstd(P, sm[:, 5:6], (sm_key, 5), sm[:, 6:7], (sm_key, 6), neghalf, 128, sm[:, 1:2], (sm_key, 1))
            P.v(lambda e, z0=z0, sm=sm: e.scalar_tensor_tensor(
                cqn[:, 256:384], z0[:, 256:384], sm[:, 6:7], kvg[:], ALU.mult, ALU.mult),
                [z0k, (sm_key, 6), "kvg"], ["cqn"])
            bk, bkey = PS.next()
            bkb = bk[:].bitcast(BF16)
            for j in (range(3) if need_q else [2]):
                P.tr(bkb[:, j * 128:(j + 1) * 128], cqn[:, j * 128:(j + 1) * 128], ident[:], ["cqn", "ident"], [bkey])
            P.act(cqnT[:].rearrange("p a b -> p (a b)"), bkb[:, 0:384], AF.Copy, [bkey], ["cqnT"])
            rope(v3(z0[:, 384:416], 1), v3(krr[:, :], 1), v3(cA[:, 0:32], 1), v3(sA[:, 0:32], 1), 1, 8,
                 tkeys + [z0k], ["krr"])
            if need_q:
                for hh in range(2):
                    bk, bkey = PS.next()
                    for kc in range(2):
                        P.mm(bk[:, 0:384], cqnT[:, kc, :], w_uq[:, kc, hh * 384:(hh + 1) * 384], kc == 0, kc == 1,
                             ["cqnT", "w_uq"], [bkey])
                    q4 = bk[:, 0:384].rearrange("p (h w) -> p h w", h=4)
                    P.act(QA[:, hh * 4:(hh + 1) * 4, 0:64], q4[:, :, 0:64], AF.Copy, [bkey], [("QA", hh, 0)])
                    rope(q4[:, :, 64:96], QA[:, hh * 4:(hh + 1) * 4, 64:96],
                         v3(cA[:, hh * 128:(hh + 1) * 128], 4), v3(sA[:, hh * 128:(hh + 1) * 128], 4), 4, 8,
                         tkeys + [bkey], [("QA", hh, 1)])
            va, va_key = VAs.next()
            for hh in range(2):
                bk, bkey = PS.next()
                P.mm(bk[:, :], cqnT[:, 2, :], w_ukv[:, hh * 512:(hh + 1) * 512], True, True, ["cqnT", "w_ukv"], [bkey])
                k4 = bk[:, :].rearrange("p (h w) -> p h w", h=4)
                P.act(KA[:, hh * 4:(hh + 1) * 4, 0:64], k4[:, :, 0:64], AF.Copy, [bkey], [("KA", hh, 0)])
                P.v(lambda e, va=va, k4=k4, hh=hh: e.tensor_copy(va[:, hh * 4:(hh + 1) * 4, 0:64], k4[:, :, 64:128]),
                    [bkey], [(va_key, hh)])
            for h in range(8):
                P.v(lambda e, h=h: e.tensor_copy(KA[:, h, 64:96], krr[:, :]), ["krr"], [("KA", h, 1)], eng="pool")
            P.dma(D["VA"][rs, :], va[:].rearrange("p h w -> p (h w)"), [(va_key, 0), (va_key, 1)], [("VA", t)], eng="act")
            if need_q:
                q8 = z1[:, :].rearrange("p (h w) -> p h w", h=8)
                rope(q8, QB[:, :, :], v3(cB[:, :], 8), v3(sB[:, :], 8), 8, 16, tkeys + [z1k], ["QB"])
            k2 = z2[:, 0:128].rearrange("p (h w) -> p h w", h=2)
            rope(k2, KB[:, :, :], v3(cB[:, 0:128], 2), v3(sB[:, 0:128], 2), 2, 16, tkeys + [z2k], ["KB"])
            vb, vb_key = VBs.next()
            P.act(vb[:, :, 0:64], z2[:, 128:256].rearrange("p (h w) -> p h w", h=2), AF.Copy, [z2k], [vb_key])
            P.dma(D["VB"][rs, :], vb[:].rearrange("p h w -> p (h w)"), [vb_key], [("VB", t)], eng="act")
            QAk = [("QA", hh, j) for hh in range(2) for j in range(2)]
            KAk = [("KA", hh, 0) for hh in range(2)] + [("KA", h, 1) for h in range(8)]
            jobs = [(KA, KAk, 8, 96, oKA, "KAT"), (KB, ["KB"], 2, 64, oKB, "KBT")]
            if need_q:
                jobs += [(QA, QAk, 8, 96, oQA, "QAT"), (QB, ["QB"], 8, 64, oQB, "QBT")]
            for (src, skeys, nh, Dh, pool, dname) in jobs:
                bk, bkey = PS.next()
                bkb = bk[:].bitcast(BF16)
                for h in range(nh):
                    P.tr(bkb[0:Dh, h * 128:(h + 1) * 128], src[:, h, :], ident[:], skeys + ["ident"], [bkey])
                ob, okey = pool.next()
                P.act(ob[:].rearrange("p a b -> p (a b)"), bkb[0:Dh, 0:nh * 128], AF.Copy, [bkey], [okey])
                P.dma(D[dname][:, :, rs], ob[:], [okey], [(dname, t)], eng="act")

        pend = None
        for t in range(NT):
            cur = stage_a(t)
            if pend is not None:
                stage_b(pend)
            pend = cur
        stage_b(pend)
        P.flush()


def phase_l0b(nc, P, D, banks):
    with contextlib.ExitStack() as st:
        sb = lambda name, shape, dt=F32: st.enter_context(nc.sbuf_tensor(name, shape, dt))
        nS, nO, nR = bank_rot(banks, 0, 4), bank_rot(banks, 4, 6), bank_rot(banks, 6, 8)
        nOP = bank_rot(banks, 0, 4)
        KBT = sb("b_KBT", [64, 2, NTOK], BF16)
        VB = sb("b_VB", [128, 34, 130], BF16)
        VA = sb("b_VA", [128, 34, 520], BF16)
        w_out = sb("b_wout", [128, 16, 1024], BF16)
        sel = sb("b_sel", [65, 64])
        es = sb("b_es", [64, 8])
        masks = sb("b_masks", [128, 2, 512], BF16)
        G = load_mod_rows(P, nc, st, D["mods"], 0, GT1, "b_G")
        KAh = Rot("KAh", [sb("b_KAh%d" % i, [96, NTOK], BF16) for i in range(2)])
        QAg = Rot("QAg", [sb("b_QAg%d" % i, [96, 8, 512], BF16) for i in range(2)])
        QBg = Rot("QBg", [sb("b_QBg%d" % i, [64, 8, 512], BF16) for i in range(2)])
        LA = 2
        PT = Rot("PT", [sb("b_PT%d" % i, [128, 512], BF16) for i in range(LA + 2)])
        Osb = Rot("Osb", [sb("b_Osb%d" % i, [65, 512]) for i in range(2)])
        rec = Rot("rec", [sb("b_rec%d" % i, [64, 512]) for i in range(2)])
        mixT = sb("b_mixT", [128, 16, 512], BF16)
        xt = Rot("bxt", [sb("b_xt%d" % i, [128, 1024]) for i in range(2)])
        ot = Rot("bot", [sb("b_ot%d" % i, [128, 1024]) for i in range(2)])

        P.dma(KBT[:], D["KBT"][:, :, :], [], ["KBT"])
        P.dma(VB[:], D["VB"].rearrange("(c p) w -> p c w", p=128), [], ["VB"])
        P.dma(VA[:], D["VA"].rearrange("(c p) w -> p c w", p=128), [], ["VA"])
        P.v(lambda e: e.memset(w_out[64:128, :, :], 0.0), [], ["w_out_z"], eng="pool")
        P.v(lambda e: e.memset(mixT[64:128, :, :], 0.0), [], ["mixT_z"], eng="pool")
        P.dma(w_out[0:64, :, :], D["even_w_out"][0].rearrange("(j p) n -> p j n", p=64), [], ["w_out"], eng="pool")
        P.v(lambda e: e.memset(sel[:], 0.0), [], ["sel"])
        P.v(lambda e: e.memset(sel[64:65, :], 1.0), ["sel"], ["sel"])
        P.dma(es[:], D["win_sink"][0:1, :].partition_broadcast(64), [], ["es"])
        P.act(es[:], es[:], AF.Exp, ["es"], ["es"])
        P.dma(masks[:], D["wmask"].rearrange("a p n -> p a n"), [], ["masks"])

        groups = [(g * 512, 512, g * 512, 0) for g in range(4)] + [(4096, 256, 2048, 1)]
        SC_A = 96.0 ** -0.5
        SC_B = 64.0 ** -0.5
        for (tok0, N, row0, var) in groups:
            qa, qak = QAg.next()
            P.dma(qa[:, :, 0:N], D["QAT"][:, :, tok0:tok0 + N], [], [qak])
            qb, qbk = QBg.next()
            P.dma(qb[:, :, 0:N], D["QBT"][:, :, tok0:tok0 + N], [], [qbk])
            chunks = list(range(34)) if var == 0 else [32, 33]
            def mla_norm(h, O, Ok, N=N):
                osb, osk = Osb.next()
                P.v(lambda e: e.tensor_copy(osb[0:65, 0:N], O[0:65, 0:N]), [Ok], [osk])
                R, Rk = nR()
                P.mm(R[0:64, 0:N], sel[0:65, 0:64], osb[0:65, 0:N], True, True, ["sel", osk], [Rk])
                rc, rck = rec.next()
                P.v(lambda e: e.reciprocal(rc[0:64, 0:N], R[0:64, 0:N]), [Rk], [rck])
                P.v(lambda e: e.tensor_tensor(mixT[0:64, h, 0:N], osb[0:64, 0:N], rc[0:64, 0:N], ALU.mult),
                    [osk, rck], [("mixT", h)])

            defer = None
            for h in range(8):
                ka, kak = KAh.next()
                P.dma(ka[:], D["KAT"][:, h, :], [], [kak])
                O, Ok = nO()
                pend = []
                for ci, kc in enumerate(chunks + [None] * LA):
                    if kc is not None:
                        S, Sk = nS()
                        P.mm(S[:, 0:N], ka[0:96, kc * 128:(kc + 1) * 128], qa[0:96, h, 0:N], True, True, [kak, qak], [Sk])
                        pt, ptk = PT.next()
                        P.act(pt[:, 0:N], S[:, 0:N], AF.Exp, [Sk], [ptk], scale=SC_A)
                        pend.append((ci, kc, pt, ptk))
                    if defer is not None and (ci == LA or kc is None):
                        mla_norm(*defer)
                        defer = None
                    if len(pend) > LA or (kc is None and pend):
                        pci, pkc, ppt, pptk = pend.pop(0)
                        P.mm(O[0:65, 0:N], VA[:, pkc, h * 65:(h + 1) * 65], ppt[:, 0:N], pci == 0, pci == len(chunks) - 1,
                             ["VA", pptk], [Ok])
                defer = (h, O, Ok)
            mla_norm(*defer)
            nb = N // 128

            def win_norm(g, b, O, Ok):
                osb, osk = Osb.next()
                P.v(lambda e: e.tensor_copy(osb[0:65, :], O[0:65, :]), [Ok], [osk])
                R, Rk = nR()
                P.mm(R[0:64, :], sel[0:65, 0:64], osb[0:65, :], True, True, ["sel", osk], [Rk])
                rc, rck = rec.next()
                for j in range(4):
                    P.v(lambda e, j=j: e.tensor_scalar(
                        rc[0:64, j * 128:(j + 1) * 128], R[0:64, j * 128:(j + 1) * 128],
                        es[0:64, g * 4 + j:g * 4 + j + 1], None, ALU.add), [Rk, "es"], [rck])
                P.v(lambda e: e.reciprocal(rc[0:64, :], rc[0:64, :]), [rck], [rck])
                P.v(lambda e: e.tensor_tensor(
                    mixT[0:64, 8 + g * 4:8 + g * 4 + 4, b * 128:(b + 1) * 128],
                    osb[0:64, :].rearrange("p (h q) -> p h q", h=4),
                    rc[0:64, :].rearrange("p (h q) -> p h q", h=4), ALU.mult),
                    [osk, rck], [("mixT", 8 + g * 4 + j) for j in range(4)])

            deferw = None
            for b in range(nb):
                tt = tok0 // 128 + b
                if var == 0:
                    cl = []
                    if tt > 0:
                        cl.append((tt - 1, 0))
                    cl.append((tt, None))
                    cl.append((tt + 1, 1))
                    cl += [(32, None), (33, None)]
                else:
                    cl = [(32, None), (33, None)]
                for g in range(2):
                    O, Ok = nO()
                    Q4 = qb[0:64, g * 4:(g + 1) * 4, b * 128:(b + 1) * 128]
                    pend = []
                    for ci, item in enumerate(cl + [None] * LA):
                        if item is not None:
                            kc, mk_ = item
                            S, Sk = nS()
                            P.mm(S[:, :].rearrange("p (h q) -> p h q", h=4), KBT[0:64, g, kc * 128:(kc + 1) * 128], Q4,
                                 True, True, ["KBT", qbk], [Sk])
                            pt, ptk = PT.next()
                            P.act(pt[:, :], S[:, :], AF.Exp, [Sk], [ptk], scale=SC_B)
                            if mk_ is not None:
                                P.v(lambda e, pt=pt, mk_=mk_: e.tensor_tensor(pt[:, :], pt[:, :], masks[:, mk_, :], ALU.mult),
                                    [ptk, "masks"], [ptk], eng="pool")
                            pend.append((ci, kc, pt, ptk))
                        if deferw is not None and (ci == min(LA, len(cl) - 1)):
                            win_norm(*deferw)
                            deferw = None
                        if len(pend) > LA or (item is None and pend):
                            pci, pkc, ppt, pptk = pend.pop(0)
                            P.mm(O[0:65, :], VB[:, pkc, g * 65:(g + 1) * 65], ppt[:, :], pci == 0, pci == len(cl) - 1,
                                 ["VB", pptk], [Ok])
                    deferw = (g, b, O, Ok)
            if deferw is not None:
                win_norm(*deferw)
                deferw = None
            mkeys = [("mixT", j) for j in range(16)]
            for b in range(nb):
                x_ap, xk = xt.next()
                P.dma(x_ap[:], D["xtok"][tok0 + b * 128:tok0 + (b + 1) * 128, :], [], [xk])
                o_ap, ok_ = ot.next()
                for dh in range(2):
                    Ob, Obk = nOP()
                    for j in range(16):
                        P.mm(Ob[:, :], mixT[0:128, j, b * 128:(b + 1) * 128], w_out[0:128, j, dh * 512:(dh + 1) * 512],
                             j == 0, j == 15, mkeys + ["w_out", "w_out_z", "mixT_z"], [Obk])
                    P.v(lambda e, o_ap=o_ap, Ob=Ob, dh=dh, var=var: e.tensor_tensor(
                        o_ap[:, dh * 512:(dh + 1) * 512], Ob[:, :], G[var][0][:, dh * 512:(dh + 1) * 512], ALU.mult),
                        [Obk, G[var][1]], [ok_])
                P.v(lambda e, o_ap=o_ap, x_ap=x_ap: e.tensor_tensor(o_ap[:], o_ap[:], x_ap[:], ALU.add),
                    [ok_, xk], [ok_], eng="pool")
                r0 = row0 + b * 128
                P.dma(D["x1"][r0:r0 + 128, :], o_ap[:], [ok_], [("x1", r0)], eng="pool")
        P.flush()


def phase_moe(nc, P, D, banks, layer, xin, xout, tiles, tag, final_norm=False):
    NTl = len(tiles)
    T = NTl * 128
    with contextlib.ExitStack() as st0:
        sb0 = lambda name, shape, dt=F32: st0.enter_context(nc.sbuf_tensor(tag + name, shape, dt))
        h2T = sb0("h2T", [128, 8, T], BF16)
        comb = sb0("comb", [128, NTl, 32])
        consts = sb0("consts", [128, 4])
        P.v(lambda e: e.memset(consts[:, 0:1], -0.5), [], ["consts"])
        neghalf = consts[:, 0:1]
        with contextlib.ExitStack() as st:
            sb = lambda name, shape, dt=F32: st.enter_context(nc.sbuf_tensor(tag + name, shape, dt))
            nA, nB = bank_rot(banks, 0, 4), bank_rot(banks, 4, 8)
            ident = sb("ident32", [128, 128])
            gbc = sb("gbc", [128, 1024])
            w_r = sb("w_r", [128, 8, 36])
            Abc = load_mod_rows(P, nc, st, D["mods"], layer, SC2, tag + "A")
            Bbc = load_mod_rows(P, nc, st, D["mods"], layer, SH2, tag + "B")
            xt = Rot("mxt", [sb("xt%d" % i, [128, 1024]) for i in range(2)])
            junk = sb("junk", [128, 1024])
            t1 = sb("t1", [128, 1024])
            h2 = sb("h2", [128, 1024])
            h2T32 = sb("h2T32", [128, 8, 128])
            small = Rot("msmall", [sb("small%d" % i, [128, 16]) for i in range(2)])
            rt = Rot("mrt", [sb("rt%d" % i, [128, 96]) for i in range(2)])
            P.dma(ident[:], D["ident32"][:, :], [], ["ident"])
            P.dma(gbc[:], D["norm_ffn_g"][layer:layer + 1, :].partition_broadcast(128), [], ["gbc"])
            P.dma(w_r[:, :, 0:4], D["moe_w_rg"][layer].rearrange("(kc p) n -> p kc n", p=128), [], [("w_r", 0)])
            P.dma(w_r[:, :, 4:36], D["moe_w_re"][layer].rearrange("(kc p) n -> p kc n", p=128), [], [("w_r", 1)])
            for v in range(2):
                a, ak = Abc[v]
                P.v(lambda e, a=a: e.scalar_tensor_tensor(a[:], a[:], 1.0, gbc[:], ALU.add, ALU.mult), [ak, "gbc"], [ak])
            for ti, (r, var) in enumerate(tiles):
                A, Ak = Abc[var]; B, Bk = Bbc[var]
                x_ap, xk = xt.next()
                P.dma(x_ap[:], D[xin][r * 128:(r + 1) * 128, :], [], [xk])
                sm, smk = small.next()
                P.act(junk[:], x_ap[:], AF.Square, [xk], ["junk", (smk, 0)], accum_out=sm[:, 0:1])
                rms_rstd(P, sm[:, 0:1], (smk, 0), sm[:, 2:3], (smk, 2), neghalf, 1024, sm[:, 1:2], (smk, 1))
                P.v(lambda e, x_ap=x_ap, sm=sm, A=A: e.scalar_tensor_tensor(
                    t1[:], x_ap[:], sm[:, 2:3], A[:], ALU.mult, ALU.mult), [xk, (smk, 2), Ak], ["t1"])
                P.v(lambda e, B=B: e.tensor_tensor(h2[:], t1[:], B[:], ALU.add), ["t1", Bk], ["h2"])
                for half in range(2):
                    bk, bkey = nA()
                    for j in range(4):
                        kc = half * 4 + j
                        P.tr(bk[:, j * 128:(j + 1) * 128], h2[:, kc * 128:(kc + 1) * 128], ident[:], ["h2", "ident"], [bkey])
                    P.act(h2T[:, half * 4:(half + 1) * 4, ti * 128:(ti + 1) * 128],
                          bk[:, :].rearrange("p (a b) -> p a b", a=4), AF.Copy, [bkey], [("h2T", ti, half)])
                    P.v(lambda e, bk=bk, half=half: e.tensor_copy(
                        h2T32[:, half * 4:(half + 1) * 4, :], bk[:, :].rearrange("p (a b) -> p a b", a=4)),
                        [bkey], [("h2T32", half)])
                lg, lgk = nB()
                for kc in range(8):
                    P.mm(lg[:, 0:36], h2T32[:, kc, :], w_r[:, kc, :], kc == 0, kc == 7,
                         [("h2T32", kc // 4), ("w_r", 0), ("w_r", 1)], [lgk])
                R, Rk = rt.next()
                P.v(lambda e, R=R, lg=lg: e.tensor_copy(R[:, 0:36], lg[:, 0:36]), [lgk], [(Rk, "lg")])
                s_ = lambda c: sm[:, c:c + 1]
                P.v(lambda e, R=R, sm=sm: e.reduce_max(sm[:, 3:4], R[:, 0:4], AX.X), [(Rk, "lg")], [(smk, 3)])
                P.v(lambda e, R=R, sm=sm: e.tensor_scalar(R[:, 36:40], R[:, 0:4], sm[:, 3:4], None, ALU.is_equal),
                    [(Rk, "lg"), (smk, 3)], [(Rk, "goh")])
                P.v(lambda e, sm=sm: e.tensor_scalar(sm[:, 4:5], sm[:, 3:4], -1.0, None, ALU.mult), [(smk, 3)], [(smk, 4)])
                P.act(R[:, 80:84], R[:, 0:4], AF.Exp, [(Rk, "lg"), (smk, 4)], [(Rk, "gexp"), (smk, 5)],
                      bias=sm[:, 4:5], accum_out=sm[:, 5:6])
                P.v(lambda e, sm=sm: e.reciprocal(sm[:, 6:7], sm[:, 5:6]), [(smk, 5)], [(smk, 6)])
                P.v(lambda e, R=R: e.tensor_scalar(R[:, 40:48], R[:, 4:12], R[:, 36:37], None, ALU.mult),
                    [(Rk, "lg"), (Rk, "goh")], [(Rk, "ein")])
                for g in range(1, 4):
                    P.v(lambda e, R=R, g=g: e.scalar_tensor_tensor(
                        R[:, 40:48], R[:, 4 + 8 * g:12 + 8 * g], R[:, 36 + g:37 + g], R[:, 40:48], ALU.mult, ALU.add),
                        [(Rk, "lg"), (Rk, "goh"), (Rk, "ein")], [(Rk, "ein")])
                P.v(lambda e, R=R, sm=sm: e.reduce_max(sm[:, 7:8], R[:, 40:48], AX.X), [(Rk, "ein")], [(smk, 7)])
                P.v(lambda e, R=R, sm=sm: e.tensor_scalar(R[:, 48:56], R[:, 40:48], sm[:, 7:8], None, ALU.is_equal),
                    [(Rk, "ein"), (smk, 7)], [(Rk, "oh1")])
                P.v(lambda e, R=R: e.scalar_tensor_tensor(R[:, 56:64], R[:, 48:56], -1e30, R[:, 40:48], ALU.mult, ALU.add),
                    [(Rk, "oh1"), (Rk, "ein")], [(Rk, "e2")])
                P.v(lambda e, R=R, sm=sm: e.reduce_max(sm[:, 8:9], R[:, 56:64], AX.X), [(Rk, "e2")], [(smk, 8)])
                P.v(lambda e, R=R, sm=sm: e.tensor_scalar(R[:, 64:72], R[:, 56:64], sm[:, 8:9], None, ALU.is_equal),
                    [(Rk, "e2"), (smk, 8)], [(Rk, "oh2")])
                P.v(lambda e, sm=sm: e.tensor_tensor(sm[:, 9:10], sm[:, 8:9], sm[:, 7:8], ALU.subtract),
                    [(smk, 7), (smk, 8)], [(smk, 9)])
                P.act(sm[:, 10:11], sm[:, 9:10], AF.Exp, [(smk, 9)], [(smk, 10)])
                P.v(lambda e, sm=sm: e.tensor_scalar(sm[:, 11:12], sm[:, 10:11], 1.0, None, ALU.add), [(smk, 10)], [(smk, 11)])
                P.v(lambda e, sm=sm: e.reciprocal(sm[:, 11:12], sm[:, 11:12]), [(smk, 11)], [(smk, 11)])
                P.v(lambda e, sm=sm: e.tensor_tensor(sm[:, 12:13], sm[:, 11:12], sm[:, 6:7], ALU.mult),
                    [(smk, 11), (smk, 6)], [(smk, 12)])
                P.v(lambda e, sm=sm: e.tensor_tensor(sm[:, 13:14], sm[:, 12:13], sm[:, 10:11], ALU.mult),
                    [(smk, 12), (smk, 10)], [(smk, 13)])
                P.v(lambda e, R=R, sm=sm: e.tensor_scalar(R[:, 72:80], R[:, 48:56], sm[:, 12:13], None, ALU.mult),
                    [(Rk, "oh1"), (smk, 12)], [(Rk, "loc")])
                P.v(lambda e, R=R, sm=sm: e.scalar_tensor_tensor(
                    R[:, 72:80], R[:, 64:72], sm[:, 13:14], R[:, 72:80], ALU.mult, ALU.add),
                    [(Rk, "oh2"), (smk, 13), (Rk, "loc")], [(Rk, "loc")])
                for g in range(4):
                    P.v(lambda e, R=R, g=g, ti=ti: e.tensor_scalar(
                        comb[:, ti, g * 8:(g + 1) * 8], R[:, 72:80], R[:, 36 + g:37 + g], None, ALU.mult),
                        [(Rk, "loc"), (Rk, "goh")], [("comb", ti)])
            P.flush()
        with contextlib.ExitStack() as st:
            sb = lambda name, shape, dt=F32: st.enter_context(nc.sbuf_tensor(tag + name, shape, dt))
            nGU, nY = bank_rot(banks, 0, 4), bank_rot(banks, 4, 8)
            yacc = sb("yacc", [128, NTl, 1024])
            wg = Rot("wg", [sb("wg%d" % i, [128, 8, 512], BF16) for i in range(2)])
            wu = Rot("wu", [sb("wu%d" % i, [128, 8, 512], BF16) for i in range(2)])
            wd = Rot("wd", [sb("wd%d" % i, [128, 4, 1024], BF16) for i in range(2)])
            hg = Rot("hg", [sb("hg%d" % i, [128, 4, 512], BF16) for i in range(2)])
            sg = Rot("sg", [sb("sg%d" % i, [128, 512]) for i in range(2)])
            G = load_mod_rows(P, nc, st, D["mods"], layer, GT2, tag + "G")
            xt = Rot("mxt2", [sb("xt2_%d" % i, [128, 1024]) for i in range(2)])
            groups = []
            t0 = 0
            while t0 < NTl:
                n = min(4, NTl - t0)
                groups.append((t0, n))
                t0 += n
            def emit_gu(ex, t0, n, g_ap, gk, u_ap, uk):
                N = n * 128
                ts = slice(t0 * 128, t0 * 128 + N)
                hgt, hgk = hg.next()
                for fc in range(4):
                    Gp, Gk = nGU(); Up, Uk = nGU()
                    for kc in range(8):
                        P.mm(Gp[:, 0:N], g_ap[:, kc, fc * 128:(fc + 1) * 128], h2T[:, kc, ts], kc == 0, kc == 7,
                             [gk, "h2T"], [Gk])
                    for kc in range(8):
                        P.mm(Up[:, 0:N], u_ap[:, kc, fc * 128:(fc + 1) * 128], h2T[:, kc, ts], kc == 0, kc == 7,
                             [uk, "h2T"], [Uk])
                    s_ap, sk = sg.next()
                    P.act(s_ap[:, 0:N], Gp[:, 0:N], AF.Silu, [Gk], [sk])
                    P.v(lambda e, hgt=hgt, Up=Up, s_ap=s_ap, fc=fc, N=N: e.tensor_tensor(
                        hgt[:, fc, 0:N], Up[:, 0:N], s_ap[:, 0:N], ALU.mult), [Uk, sk], [(hgk, fc)])
                return hgt, hgk

            def emit_down(ex, t0, n, hgt, hgk, d_ap, dk):
                for b in range(n):
                    ti = t0 + b
                    for dh in range(2):
                        Yp, Yk = nY()
                        for fc in range(4):
                            P.mm(Yp[:, :], hgt[:, fc, b * 128:(b + 1) * 128], d_ap[:, fc, dh * 512:(dh + 1) * 512],
                                 fc == 0, fc == 3, [(hgk, f) for f in range(4)] + [dk], [Yk])
                        ysl = yacc[:, ti, dh * 512:(dh + 1) * 512]
                        if ex == 0:
                            P.v(lambda e, ysl=ysl, Yp=Yp, ti=ti, ex=ex: e.tensor_scalar(
                                ysl, Yp[:, :], comb[:, ti, ex:ex + 1], None, ALU.mult), [Yk], [("yacc", ti, dh)])
                        else:
                            P.v(lambda e, ysl=ysl, Yp=Yp, ti=ti, ex=ex: e.scalar_tensor_tensor(
                                ysl, Yp[:, :], comb[:, ti, ex:ex + 1], ysl, ALU.mult, ALU.add),
                                [Yk, ("yacc", ti, dh)], [("yacc", ti, dh)])

            pend = None
            for ex in range(32):
                g_ap, gk = wg.next(); u_ap, uk = wu.next(); d_ap, dk = wd.next()
                P.dma(g_ap[:], D["moe_w_gate%d" % layer][ex].rearrange("(kc p) f -> p kc f", p=128), [], [gk], eng="pool")
                P.dma(u_ap[:], D["moe_w_up%d" % layer][ex].rearrange("(kc p) f -> p kc f", p=128), [], [uk], eng="pool")
                P.dma(d_ap[:], D["moe_w_down%d" % layer][ex].rearrange("(kc p) f -> p kc f", p=128), [], [dk], eng="pool")
                for (t0, n) in groups:
                    hgt, hgk = emit_gu(ex, t0, n, g_ap, gk, u_ap, uk)
                    if pend is not None:
                        emit_down(*pend)
                    pend = (ex, t0, n, hgt, hgk, d_ap, dk)
            emit_down(*pend)
            if final_norm:
                fg = sb("fg", [128, 1024])
                P.dma(fg[:], D["final_norm_g"][0:1, :].partition_broadcast(128), [], ["fg"])
                junk = sb("junk3", [128, 1024])
                small = Rot("fsmall", [sb("fsmall%d" % i, [128, 4]) for i in range(2)])
            for ti, (r, var) in enumerate(tiles):
                x_ap, xk = xt.next()
                P.dma(x_ap[:], D[xin][r * 128:(r + 1) * 128, :], [], [xk])
                ysl = yacc[:, ti, :]
                yk = [("yacc", ti, 0), ("yacc", ti, 1)]
                P.v(lambda e, ysl=ysl, var=var: e.tensor_tensor(ysl, ysl, G[var][0][:], ALU.mult), yk + [G[var][1]], yk)
                P.v(lambda e, ysl=ysl, x_ap=x_ap: e.tensor_tensor(ysl, ysl, x_ap[:], ALU.add), yk + [xk], yk, eng="pool")
                if final_norm:
                    sm, smk = small.next()
                    P.act(junk[:], ysl, AF.Square, yk, ["junk3", (smk, 0)], accum_out=sm[:, 0:1])
                    rms_rstd(P, sm[:, 0:1], (smk, 0), sm[:, 2:3], (smk, 2), neghalf, 1024, sm[:, 1:2], (smk, 1))
                    P.v(lambda e, ysl=ysl, sm=sm: e.scalar_tensor_tensor(
                        ysl, ysl, sm[:, 2:3], fg[:], ALU.mult, ALU.mult), yk + [(smk, 2), "fg"], yk)
                P.dma(D[xout][r * 128:(r + 1) * 128, :], ysl, yk, [(xout, r)])
            P.flush()


GELU_C = 0.7978845608028654


def l1_tiles():
    return [(i, 0) for i in range(16)] + [(16, 1), (17, 1)]


def phase_l1a(nc, P, D, banks):
    with contextlib.ExitStack() as st:
        sb = lambda name, shape, dt=F32: st.enter_context(nc.sbuf_tensor("c_" + name, shape, dt))
        nP = bank_rot(banks, 0, 8)
        ident = sb("ident", [128, 128], BF16)
        ident32 = sb("ident32", [128, 128])
        consts = sb("consts", [128, 4])
        gbc = sb("gbc", [128, 1024])
        w_in = sb("w_in", [128, 8, 2592], BF16)
        Abc = load_mod_rows(P, nc, st, D["mods"], 1, SC1, "c_A")
        Bbc = load_mod_rows(P, nc, st, D["mods"], 1, SH1, "c_B")
        W2 = sb("W2", [32, 512])
        bg = sb("bg", [128, 512])
        gm = sb("gm", [128, 3, 128])
        wsT = sb("wsT", [128, 4, 128], BF16)
        bsT = sb("bsT", [128, 4])
        lng = sb("lng", [128, 512]); lnb = sb("lnb", [128, 512])
        xt = Rot("cxt", [sb("xt%d" % i, [128, 1024]) for i in range(2)])
        junk = sb("junk", [128, 1024])
        t1r = Rot("t1", [sb("t1_%d" % i, [128, 1024]) for i in range(2)])
        hbr = Rot("hb", [sb("hb_%d" % i, [128, 1024], BF16) for i in range(2)])
        hTr = Rot("hT", [sb("hT_%d" % i, [128, 8, 128], BF16) for i in range(2)])
        small = Rot("csmall", [sb("small%d" % i, [128, 16]) for i in range(3)])
        qkr = Rot("qk", [sb("qk%d" % i, [128, 512]) for i in range(2)])
        g32r = Rot("g32", [sb("g32_%d" % i, [128, 32]) for i in range(2)])
        uvr = Rot("uv", [sb("uv%d" % i, [128, 2, 512]) for i in range(2)])
        g32T = sb("g32T", [32, 128])
        zs = sb("zs", [128, 512]); la = sb("la", [128, 512])
        bsb = sb("bsb", [128, 2, 256])
        ex = sb("ex", [128, 256]); tmp = sb("tmp", [128, 256])
        gl = sb("gl", [128, 6, 256], BF16)
        glT = Rot("glT", [sb("glT%d" % i, [128, 8, 128], BF16) for i in range(2)])
        dec = Rot("dec", [sb("dec%d" % i, [128, 4]) for i in range(2)])
        vb = Rot("cvb", [sb("vb%d" % i, [128, 512], BF16) for i in range(2)])
        rsb = Rot("rsb", [sb("rsb%d" % i, [128, 512], BF16) for i in range(2)])
        ge = sb("ge", [128, 2, 512])
        gt_ = sb("gt_", [128, 512]); gt2_ = sb("gt2_", [128, 512])
        vgn = sb("vgn", [128, 512], BF16)
        dlb = Rot("dlb", [sb("dlb%d" % i, [128, 512], BF16) for i in range(2)])

        P.dma(ident[:], D["identbf"][:, :], [], ["ident"])
        P.dma(ident32[:], D["ident32"][:, :], [], ["ident32"])
        P.v(lambda e: e.memset(consts[:, 0:1], -0.5), [], ["consts"])
        neghalf = consts[:, 0:1]
        P.dma(gbc[:], D["norm_mix_g"][1:2, :].partition_broadcast(128), [], ["gbc"])
        for c in range(3):
            lo, hi = c * 864, (c + 1) * 864
            P.dma(w_in[:, :, lo:hi], D["odd_w_in"][0].rearrange("(kc p) n -> p kc n", p=128)[:, :, lo:hi], [],
                  [("w_in", c)], eng="pool")
        wkeys = [("w_in", c) for c in range(3)]
        P.v(lambda e: e.memset(W2[:], 0.0), [], ["W2"])
        P.dma(W2[0:16, 0:256], D["gla_w_g2"][0, 0], ["W2"], ["W2"])
        P.dma(W2[16:32, 256:512], D["gla_w_g2"][0, 1], ["W2"], ["W2"])
        P.dma(bg[:], D["gla_b_g"][0:1].rearrange("o a n -> o (a n)").partition_broadcast(128), [], ["bg"])
        P.dma(gm[:], D["gmask"][0:3].rearrange("a p n -> p a n"), [], ["gm"])
        P.dma(wsT[:], D["sg_w_sT"].rearrange("g s t -> s g t"), [], ["wsT"], eng="pool")
        P.dma(bsT[:], D["sg_b_sT"][:, :], [], ["bsT"])
        P.dma(lng[:], D["sg_ln_g"][0:1, :].partition_broadcast(128), [], ["lng"])
        P.dma(lnb[:], D["sg_ln_b"][0:1, :].partition_broadcast(128), [], ["lnb"])
        for v in range(2):
            a, ak = Abc[v]
            P.v(lambda e, a=a: e.scalar_tensor_tensor(a[:], a[:], 1.0, gbc[:], ALU.add, ALU.mult), [ak, "gbc"], [ak])

        def gelu(src, src_key, dst, dst_key):
            P.v(lambda e: e.tensor_tensor(gt2_[:], src, src, ALU.mult), [src_key], ["gt2_"])
            P.v(lambda e: e.tensor_scalar(gt2_[:], gt2_[:], 0.044715, 1.0, ALU.mult, ALU.add), ["gt2_"], ["gt2_"])
            P.v(lambda e: e.tensor_tensor(gt2_[:], gt2_[:], src, ALU.mult), ["gt2_", src_key], ["gt2_"], eng="pool")
            P.act(gt2_[:], gt2_[:], AF.Sigmoid, ["gt2_"], ["gt2_"], scale=2.0 * GELU_C)
            P.v(lambda e: e.tensor_tensor(dst, src, gt2_[:], ALU.mult), [src_key, "gt2_"], [dst_key])

        def stage_a(r, var):
            lat = var == 0
            A, Ak = Abc[var]; B, Bk = Bbc[var]
            rs = slice(r * 128, (r + 1) * 128)
            x_ap, xk = xt.next()
            P.dma(x_ap[:], D["x2"][rs, :], [], [xk])
            sm, smk = small.next()
            P.act(junk[:], x_ap[:], AF.Square, [xk], ["junk", (smk, 0)], accum_out=sm[:, 0:1])
            rms_rstd(P, sm[:, 0:1], (smk, 0), sm[:, 2:3], (smk, 2), neghalf, 1024, sm[:, 1:2], (smk, 1))
            t1, t1k = t1r.next(); hb, hbk = hbr.next(); hT, hTk = hTr.next()
            P.v(lambda e, x_ap=x_ap, sm=sm, A=A, t1=t1: e.scalar_tensor_tensor(
                t1[:], x_ap[:], sm[:, 2:3], A[:], ALU.mult, ALU.mult), [xk, (smk, 2), Ak], [t1k])
            P.v(lambda e, B=B, t1=t1, hb=hb: e.tensor_tensor(hb[:], t1[:], B[:], ALU.add), [t1k, Bk], [hbk])
            bk, bkey = nP()
            bkb = bk[:].bitcast(BF16)
            for kc in range(8):
                P.tr(bkb[:, kc * 128:(kc + 1) * 128], hb[:, kc * 128:(kc + 1) * 128], ident[:], [hbk, "ident"], [bkey])
            P.act(hT[:].rearrange("p a b -> p (a b)"), bkb, AF.Copy, [bkey], [hTk])

            def proj(c0, c1):
                bk, bkey = nP()
                for kc in range(8):
                    P.mm(bk[:, 0:c1 - c0], hT[:, kc, :], w_in[:, kc, c0:c1], kc == 0, kc == 7, [hTk] + wkeys, [bkey])
                return bk, bkey
            zqk, zqkk = proj(0, 512)
            qk, qkk = qkr.next()
            P.act(qk[:], zqk[:, :], AF.Copy, [zqkk], [qkk])
            zv, zvk = proj(512, 1024)
            v_ap, vk = vb.next()
            P.act(v_ap[:], zv[:, :], AF.Copy, [zvk], [vk])
            P.dma(D["g_v"][rs, :], v_ap[:], [vk], [("g_v", r)], eng="act")
            zg, zgk = proj(1024, 1056)
            g32, g32k = g32r.next()
            P.v(lambda e, zg=zg, g32=g32: e.tensor_copy(g32[:], zg[:, 0:32]), [zgk], [g32k])
            uv, uvk = uvr.next()
            if lat:
                zr, zrk = proj(1056, 1568)
                r_ap, rk = rsb.next()
                P.act(r_ap[:], zr[:, :], AF.Silu, [zrk], [rk])
                P.dma(D["rsilu"][rs, :], r_ap[:], [rk], [("rsilu", r)], eng="act")
                zu, zuk = proj(1568, 2080)
                P.act(uv[:, 0, :], zu[:, :], AF.Copy, [zuk], [(uvk, 0)])
                zvg, zvgk = proj(2080, 2592)
                P.v(lambda e, uv=uv, zvg=zvg: e.tensor_copy(uv[:, 1, :], zvg[:, :]), [zvgk], [(uvk, 1)])
            return dict(r=r, lat=lat, rs=rs, sm=sm, smk=smk, qk=qk, qkk=qkk, g32=g32, g32k=g32k, uv=uv, uvk=uvk)

        def stage_b(c):
            r, lat, rs, sm, smk, qk, qkk, g32, g32k, uv, uvk = (c[k] for k in (
                "r", "lat", "rs", "sm", "smk", "qk", "qkk", "g32", "g32k", "uv", "uvk"))
            bk, bkey = nP()
            P.tr(bk[0:32, 0:128], g32[:, :], ident32[:], [g32k, "ident32"], [bkey])
            P.v(lambda e, bk=bk: e.tensor_copy(g32T[:], bk[0:32, 0:128]), [bkey], ["g32T"])
            zz, zzk = nP()
            P.mm(zz[:, :], g32T[0:32, :], W2[0:32, :], True, True, ["g32T", "W2"], [zzk])
            P.v(lambda e, zz=zz: e.tensor_tensor(zs[:], zz[:, :], bg[:], ALU.add), [zzk, "bg"], ["zs"])
            P.act(zs[:], zs[:], AF.Exp, ["zs"], ["zs"], scale=-1.0)
            P.act(zs[:], zs[:], AF.Ln, ["zs"], ["zs"], bias=1.0)
            P.v(lambda e: e.tensor_scalar(la[:], zs[:], -1.0 / 16.0, None, ALU.mult), ["zs"], ["la"])
            cA, cAk = nP()
            P.mm(cA[:, 0:256], gm[:, 0, :], la[:, 0:256], True, True, ["gm", "la"], [cAk])
            P.mm(cA[:, 256:512], gm[:, 1, :], la[:, 256:512], True, True, ["gm", "la"], [cAk])
            cL, cLk = nP()
            P.mm(cL[:, :], gm[:, 2, :], la[:, :], True, True, ["gm", "la"], [cLk])
            P.act(bsb[:].rearrange("p a b -> p (a b)"), cA[:, :], AF.Copy, [cAk], ["bsb"])
            cT, cTk = nP()
            for j in range(4):
                P.mm(cT[:, j:j + 1], la[:, j * 128:(j + 1) * 128], gm[:, 2, 0:1], True, True, ["gm", "la"], [cTk])
            d_ap, dk = dec.next()
            P.act(d_ap[:], cT[:, 0:4], AF.Exp, [cTk], [dk])
            P.dma(D["g_dec"][:, r, :], d_ap[:], [dk], [("g_dec", r)], eng="act")
            for p in range(2):
                if p == 1 and not lat:
                    continue
                bp = bsb[:, p, :]
                if lat:
                    P.act(ex[:], bp, AF.Exp, ["bsb"], ["ex"])
                    P.v(lambda e, p=p: e.scalar_tensor_tensor(gl[:, 2 * p, :], qk[:, 0:256], 0.125, ex[:], ALU.mult, ALU.mult),
                        [qkk, "ex"], [("gl", 2 * p)])
                P.act(ex[:], bp, AF.Exp, ["bsb"], ["ex"], scale=-1.0)
                P.v(lambda e, p=p: e.tensor_tensor(gl[:, 2 * p + 1, :], qk[:, 256:512], ex[:], ALU.mult),
                    [qkk, "ex"], [("gl", 2 * p + 1)])
                P.v(lambda e, p=p, bp=bp, cL=cL: e.tensor_tensor(tmp[:], cL[:, p * 256:(p + 1) * 256], bp, ALU.subtract),
                    [cLk, "bsb"], ["tmp"])
                P.act(ex[:], tmp[:], AF.Exp, ["tmp"], ["ex"])
                P.v(lambda e, p=p: e.tensor_tensor(gl[:, 4 + p, :], qk[:, 256:512], ex[:], ALU.mult),
                    [qkk, "ex"], [("gl", 4 + p)])
                P.dma(D["g_kd"][p, rs, :], gl[:, 4 + p, :], [("gl", 4 + p)], [("g_kd", p, r)], eng="act")
            bk, bkey = nP()
            bkb = bk[:].bitcast(BF16)
            arrs = [0, 1, 2, 3] if lat else [1]
            for a_ in arrs:
                for j in range(2):
                    P.tr(bkb[:, (a_ * 2 + j) * 128:(a_ * 2 + j + 1) * 128], gl[:, a_, j * 128:(j + 1) * 128], ident[:],
                         [("gl", a_), "ident"], [bkey])
            gT, gTk = glT.next()
            if lat:
                P.act(gT[:].rearrange("p a b -> p (a b)"), bkb, AF.Copy, [bkey], [gTk])
                P.dma(D["g_T"][:, r, :, :], gT[:], [gTk], [("g_T", r)], eng="act")
            else:
                P.act(gT[:, 2:4, :].rearrange("p a b -> p (a b)"), bkb[:, 256:512], AF.Copy, [bkey], [gTk])
                P.dma(D["g_T"][:, r, 2:4, :], gT[:, 2:4, :], [gTk], [("g_T", r)], eng="act")
            if not lat:
                return
            gelu(uv[:, 0, :], (uvk, 0), ge[:, 0, :], ("ge", 0))
            gelu(uv[:, 1, :], (uvk, 1), ge[:, 1, :], ("ge", 1))
            P.v(lambda e, sm=sm: e.reduce_sum(sm[:, 8:9], ge[:, 1, :], AX.X), [("ge", 1)], [(smk, 8)])
            P.act(junk[:, 0:512], ge[:, 1, :], AF.Square, [("ge", 1)], ["junk", (smk, 9)], accum_out=sm[:, 9:10])
            P.v(lambda e, sm=sm: e.tensor_scalar(sm[:, 10:11], sm[:, 8:9], 1.0 / 512, None, ALU.mult), [(smk, 8)], [(smk, 10)])
            P.v(lambda e, sm=sm: e.tensor_tensor(sm[:, 11:12], sm[:, 10:11], sm[:, 10:11], ALU.mult), [(smk, 10)], [(smk, 11)])
            P.v(lambda e, sm=sm: e.scalar_tensor_tensor(sm[:, 12:13], sm[:, 9:10], 1.0 / 512, sm[:, 11:12], ALU.mult, ALU.subtract),
                [(smk, 9), (smk, 11)], [(smk, 12)])
            P.v(lambda e, sm=sm: e.tensor_scalar(sm[:, 12:13], sm[:, 12:13], EPS, None, ALU.add), [(smk, 12)], [(smk, 12)])
            P.v(lambda e, sm=sm: e.tensor_tensor(sm[:, 13:14], sm[:, 12:13], neghalf, ALU.pow), [(smk, 12), "consts"],
                [(smk, 13)], eng="pool")
            P.v(lambda e, sm=sm: e.tensor_scalar(gt_[:], ge[:, 1, :], sm[:, 10:11], sm[:, 13:14], ALU.subtract, ALU.mult),
                [("ge", 1), (smk, 10), (smk, 13)], ["gt_"])
            P.v(lambda e: e.tensor_tensor(gt_[:], gt_[:], lng[:], ALU.mult), ["gt_", "lng"], ["gt_"])
            P.v(lambda e: e.tensor_tensor(vgn[:], gt_[:], lnb[:], ALU.add), ["gt_", "lnb"], ["vgn"])
            sp_, spk = nP()
            for gi in range(4):
                P.mm(sp_[:, gi * 128:(gi + 1) * 128], wsT[:, gi, :], vgn[:, gi * 128:(gi + 1) * 128], True, True,
                     ["wsT", "vgn"], [spk])
            dl_ap, dlk = dlb.next()
            for gi in range(4):
                P.v(lambda e, gi=gi, sp_=sp_, dl_ap=dl_ap: e.scalar_tensor_tensor(
                    dl_ap[:, gi * 128:(gi + 1) * 128], sp_[:, gi * 128:(gi + 1) * 128], bsT[:, gi:gi + 1],
                    ge[:, 0, gi * 128:(gi + 1) * 128], ALU.add, ALU.mult), [spk, "bsT", ("ge", 0)], [dlk])
            P.dma(D["dl"][rs, :], dl_ap[:], [dlk], [("dl", r)], eng="act")

        pend = None
        for (r, var) in l1_tiles():
            cur = stage_a(r, var)
            if pend is not None:
                stage_b(pend)
            pend = cur
        stage_b(pend)
        P.flush()


def _gla_pass(nc, P, D, banks, st, sb, pidx, order, S, Sb, on_out):
    nAT, nO, nU = bank_rot(banks, 0, 2), bank_rot(banks, 2, 4), bank_rot(banks, 4, 6)
    gT = Rot("gTl", [sb("gTl%d" % i, [128, 4, 128], BF16) for i in range(2)])
    kd = Rot("kdl", [sb("kdl%d" % i, [128, 256], BF16) for i in range(2)])
    vv = Rot("vl", [sb("vl%d" % i, [128, 512], BF16) for i in range(2)])
    dc = Rot("dcl", [sb("dcl%d" % i, [128, 2]) for i in range(2)])
    ATm = Rot("ATm", [sb("ATm%d" % i, [128, 128], BF16) for i in range(2)])
    mask = sb("gmask_sb", [128, 128])
    P.dma(mask[:], D["gmask"][pidx], [], ["gmask"])
    for r in order:
        lat = r < 16
        rs = slice(r * 128, (r + 1) * 128)
        g_ap, gk = gT.next()
        if lat:
            P.dma(g_ap[:], D["g_T"][:, r, 4 * pidx:4 * pidx + 4, :], [], [gk])
        else:
            P.dma(g_ap[:, 2:4, :], D["g_T"][:, r, 4 * pidx + 2:4 * pidx + 4, :], [], [gk])
        k_ap, kk = kd.next()
        P.dma(k_ap[:], D["g_kd"][pidx, rs, :], [], [kk])
        v_ap, vk = vv.next()
        P.dma(v_ap[:], D["g_v"][rs, :], [], [vk])
        d_ap, dk = dc.next()
        P.dma(d_ap[:], D["g_dec"][:, r, 2 * pidx:2 * pidx + 2], [], [dk])
        if lat:
            O, Ok = nO()
            for h in range(4):
                j, po = h // 2, (h % 2) * 64
                AT, ATk = nAT()
                P.mm(AT[:, 0:128], g_ap[po:po + 64, 2 + j, :], g_ap[po:po + 64, j, :], True, True, [gk], [ATk])
                am, amk = ATm.next()
                P.v(lambda e, am=am, AT=AT: e.tensor_tensor(am[:], AT[:, 0:128], mask[:], ALU.mult), [ATk, "gmask"], [amk])
                P.mm(O[:, h * 128:(h + 1) * 128], am[:], v_ap[:, h * 128:(h + 1) * 128], True, False, [amk, vk], [Ok])
                P.mm(O[:, h * 128:(h + 1) * 128], g_ap[po:po + 64, j, :], Sb[po:po + 64, j, :], False, True,
                     [gk, ("Sb", j, h % 2)], [Ok])
            on_out(r, O, Ok)
        for j in range(2):
            for hh in range(2):
                po = hh * 64
                U, Uk = nU()
                P.mm(U[:, 0:128], k_ap[:, j * 128:(j + 1) * 128], v_ap[:, (2 * j + hh) * 128:(2 * j + hh + 1) * 128],
                     True, True, [kk, vk], [Uk])
                P.v(lambda e, U=U, j=j, po=po, d_ap=d_ap: e.scalar_tensor_tensor(
                    S[po:po + 64, j, :], S[po:po + 64, j, :], d_ap[po:po + 64, j:j + 1], U[po:po + 64, 0:128],
                    ALU.mult, ALU.add), [Uk, dk, ("S", j, hh)], [("S", j, hh)])
                P.act(Sb[po:po + 64, j, :], S[po:po + 64, j, :], AF.Copy, [("S", j, hh)], [("Sb", j, hh)])


def phase_l1b_a(nc, P, D, banks):
    with contextlib.ExitStack() as st:
        sb = lambda name, shape, dt=F32: st.enter_context(nc.sbuf_tensor("d_" + name, shape, dt))
        S = sb("S", [128, 2, 128]); Sb = sb("Sb", [128, 2, 128], BF16)
        oa = Rot("oa", [sb("oa%d" % i, [128, 512]) for i in range(2)])
        P.v(lambda e: e.memset(S[:], 0.0), [], [("S", j, hh) for j in range(2) for hh in range(2)])
        P.v(lambda e: e.memset(Sb[:], 0.0), [], [("Sb", j, hh) for j in range(2) for hh in range(2)])

        def on_out(r, O, Ok):
            o_ap, ok_ = oa.next()
            P.act(o_ap[:], O[:, :], AF.Copy, [Ok], [ok_])
            P.dma(D["OA"][r * 128:(r + 1) * 128, :], o_ap[:], [ok_], [("OA", r)], eng="act")
        _gla_pass(nc, P, D, banks, st, sb, 0, [16, 17] + list(range(16)), S, Sb, on_out)
        P.dma(D["cc_in"].rearrange("(a p) n -> p a n", p=128), S[:], [("S", j, hh) for j in range(2) for hh in range(2)],
              ["cc_in"])
        P.cc(lambda e: e.collective_compute("AllGather", ALU.bypass, replica_groups=[[0, 1], [2, 3], [4, 5], [6, 7]],
                                            ins=[D["cc_in"].opt()], outs=[D["cc_out"].opt()]), ["cc_in"], ["cc_out"])
        P.flush()


def phase_l1b_b(nc, P, D, banks):
    with contextlib.ExitStack() as st:
        sb = lambda name, shape, dt=F32: st.enter_context(nc.sbuf_tensor("e_" + name, shape, dt))
        nT, nW = bank_rot(banks, 0, 2), bank_rot(banks, 2, 8)
        S = sb("S", [128, 2, 128]); Sb = sb("Sb", [128, 2, 128], BF16)
        skeys = [("S", j, hh) for j in range(2) for hh in range(2)]
        both = sb("both", [128, 4, 128])
        sel = sb("sel", [128, 2])
        P.dma(both[:], D["cc_out"].rearrange("(a p) n -> p a n", p=128), [], ["both"])
        P.dma(sel[:], D["sel"][:, :], [], ["sel"])
        P.v(lambda e: e.tensor_scalar(S[:].rearrange("p a b -> p (a b)"), both[:, 0:2, :].rearrange("p a b -> p (a b)"),
                                      sel[:, 0:1], None, ALU.mult), ["both", "sel"], skeys)
        P.v(lambda e: e.scalar_tensor_tensor(S[:].rearrange("p a b -> p (a b)"),
                                             both[:, 2:4, :].rearrange("p a b -> p (a b)"), sel[:, 1:2],
                                             S[:].rearrange("p a b -> p (a b)"), ALU.mult, ALU.add),
            ["both", "sel"] + skeys, skeys)
        P.act(Sb[:], S[:], AF.Copy, skeys, [("Sb", j, hh) for j in range(2) for hh in range(2)])
        ident = sb("ident", [128, 128], BF16)
        P.dma(ident[:], D["identbf"][:, :], [], ["ident"])
        consts = sb("consts", [128, 4])
        P.v(lambda e: e.memset(consts[:], -0.5), [], ["consts"])
        gng = sb("gng", [128, 512])
        P.dma(gng[:], D["gla_norm_g"][0:1, :].partition_broadcast(128), [], ["gng"])
        w_out = sb("w_out", [128, 8, 1024], BF16)
        P.dma(w_out[:], D["odd_w_out"][0].rearrange("(kc p) n -> p kc n", p=128), [], ["w_out"], eng="pool")
        G = load_mod_rows(P, nc, st, D["mods"], 1, GT1, "e_G")
        oa = Rot("eoa", [sb("oa%d" % i, [128, 512]) for i in range(2)])
        rsl = Rot("ersl", [sb("rsl%d" % i, [128, 512], BF16) for i in range(2)])
        gr = sb("gr", [128, 512])
        junk = sb("junk", [128, 128])
        small = Rot("esmall", [sb("small%d" % i, [128, 8]) for i in range(2)])
        mix_all = sb("mix_all", [128, 16, 1024], BF16)
        mixTr = Rot("emixT", [sb("mixT%d" % i, [128, 8, 128], BF16) for i in range(2)])
        xt = Rot("ext", [sb("xt%d" % i, [128, 1024]) for i in range(2)])
        ot = Rot("eot", [sb("ot%d" % i, [128, 1024]) for i in range(2)])

        def on_out(r, O, Ok):
            rs = slice(r * 128, (r + 1) * 128)
            o_ap, ok_ = oa.next()
            P.dma(o_ap[:], D["OA"][rs, :], [], [ok_])
            P.v(lambda e, o_ap=o_ap, O=O: e.tensor_tensor(o_ap[:], O[:, :], o_ap[:], ALU.add), [Ok, ok_], [ok_])
            r_ap, rk = rsl.next()
            P.dma(r_ap[:], D["rsilu"][rs, :], [], [rk])
            m_ap, mk = mix_all[:, r, :], ("mix", r)
            P.dma(m_ap[:, 512:1024], D["dl"][rs, :], [], [(mk, 1)])
            sm, smk = small.next()
            for h in range(4):
                P.act(junk[:], o_ap[:, h * 128:(h + 1) * 128], AF.Square, [ok_], ["junk", (smk, h)], accum_out=sm[:, h:h + 1])
            hk = [(smk, h) for h in range(4)]
            P.v(lambda e, sm=sm: e.tensor_scalar(sm[:, 0:4], sm[:, 0:4], 1.0 / 128, EPS, ALU.mult, ALU.add), hk, hk)
            P.v(lambda e, sm=sm: e.tensor_tensor(sm[:, 4:8], sm[:, 0:4], consts[:, 0:4], ALU.pow), hk + ["consts"],
                [(smk, 4)], eng="pool")
            P.v(lambda e, r_ap=r_ap: e.tensor_tensor(gr[:], gng[:], r_ap[:], ALU.mult), ["gng", rk], ["gr"])
            for h in range(4):
                P.v(lambda e, h=h, o_ap=o_ap, sm=sm, m_ap=m_ap: e.scalar_tensor_tensor(
                    m_ap[:, h * 128:(h + 1) * 128], o_ap[:, h * 128:(h + 1) * 128], sm[:, 4 + h:5 + h],
                    gr[:, h * 128:(h + 1) * 128], ALU.mult, ALU.mult), [ok_, (smk, 4), "gr"], [(mk, 0)])

        def out_proj(r):
            rs = slice(r * 128, (r + 1) * 128)
            m_ap, mk = mix_all[:, r, :], ("mix", r)
            bk, bkey = nT()
            bkb = bk[:].bitcast(BF16)
            for kc in range(8):
                P.tr(bkb[:, kc * 128:(kc + 1) * 128], m_ap[:, kc * 128:(kc + 1) * 128], ident[:],
                     [(mk, 0), (mk, 1), "ident"], [bkey])
            mT, mTk = mixTr.next()
            P.act(mT[:].rearrange("p a b -> p (a b)"), bkb, AF.Copy, [bkey], [mTk])
            x_ap, xk = xt.next()
            P.dma(x_ap[:], D["x2"][rs, :], [], [xk])
            t_ap, tk = ot.next()
            for dh in range(2):
                W, Wk = nW()
                for kc in range(8):
                    P.mm(W[:, :], mT[:, kc, :], w_out[:, kc, dh * 512:(dh + 1) * 512], kc == 0, kc == 7,
                         [mTk, "w_out"], [Wk])
                P.v(lambda e, t_ap=t_ap, W=W, dh=dh: e.tensor_tensor(
                    t_ap[:, dh * 512:(dh + 1) * 512], W[:, :], G[0][0][:, dh * 512:(dh + 1) * 512], ALU.mult),
                    [Wk, G[0][1]], [tk])
            P.v(lambda e, t_ap=t_ap, x_ap=x_ap: e.tensor_tensor(t_ap[:], t_ap[:], x_ap[:], ALU.add), [tk, xk], [tk],
                eng="pool")
            P.dma(D["x3"][rs, :], t_ap[:], [tk], [("x3", r)], eng="pool")
        _gla_pass(nc, P, D, banks, st, sb, 1, list(range(15, -1, -1)), S, Sb, on_out)
        for r in range(15, -1, -1):
            out_proj(r)
        P.flush()


I32 = mybir.dt.int32
STILE = 256
NB = STILE // 128


def n_stiles(T):
    return (2 * T + 32 * (STILE - 1) + STILE - 1) // STILE


def phase_moe_sparse(nc, P, D, banks, layer, xin, xout, tiles, tag, final_norm=False):
    NTl = len(tiles)
    T = NTl * 128
    NST = n_stiles(T)
    NSLOT = NST * STILE
    Xs, Ys = D["Xs"], D["Ys"]
    wgv = D["moe_w_gate%d" % layer].rearrange("e (p a kc) f -> (e p a) (kc f)", p=128, a=2)
    wuv = D["moe_w_up%d" % layer].rearrange("e (p a kc) f -> (e p a) (kc f)", p=128, a=2)
    wdv = D["moe_w_down%d" % layer].rearrange("e f d -> (e f) d")
    with contextlib.ExitStack() as st0:
        sb0 = lambda name, shape, dt=F32: st0.enter_context(nc.sbuf_tensor(tag + name, shape, dt))
        consts = sb0("consts", [128, 4])
        P.v(lambda e: e.memset(consts[:, 0:1], -0.5), [], ["consts"])
        neghalf = consts[:, 0:1]
        idxA = sb0("idxA", [128, NTl], I32); idxB = sb0("idxB", [128, NTl], I32)
        wAB = sb0("wAB", [128, 2, NTl])
        widx = sb0("widx", [128, NST, 6], I32)
        with contextlib.ExitStack() as st:
            sb = lambda name, shape, dt=F32: st.enter_context(nc.sbuf_tensor(tag + name, shape, dt))
            nA, nB = bank_rot(banks, 0, 4), bank_rot(banks, 4, 7)
            cntb, cntk = banks[7], ("ps", 7)
            ident = sb("ident32", [128, 128])
            gbc = sb("gbc", [128, 1024])
            w_r = sb("w_r", [128, 8, 36])
            gm = sb("gm", [128, 2, 128])
            eidrow = sb("eidrow", [128, 32])
            pc2 = sb("pc2", [128, 6])
            Abc = load_mod_rows(P, nc, st, D["mods"], layer, SC2, tag + "A")
            Bbc = load_mod_rows(P, nc, st, D["mods"], layer, SH2, tag + "B")
            xt = Rot("mxt", [sb("xt%d" % i, [128, 1024]) for i in range(2)])
            junk = sb("junk", [128, 1024])
            t1r = Rot("t1", [sb("t1_%d" % i, [128, 1024]) for i in range(2)])
            h2r = Rot("h2", [sb("h2_%d" % i, [128, 1024]) for i in range(2)])
            h2b = sb("h2b", [128, NTl, 1024], BF16)
            h2Tr = Rot("h2T32", [sb("h2T32_%d" % i, [128, 8, 128]) for i in range(2)])
            small = Rot("msmall", [sb("small%d" % i, [128, 16]) for i in range(2)])
            rt = Rot("mrt", [sb("rt%d" % i, [128, 96]) for i in range(2)])
            selA = sb("selA", [128, NTl, 32]); selB = sb("selB", [128, NTl, 32]); selm = sb("selm", [128, NTl, 32])
            LG = sb("LG", [128, NTl, 36])
            rs_ = sb("rs_", [128, 8, NTl])
            goh = sb("goh", [128, NTl, 4]); gex = sb("gex", [128, NTl, 4])
            etmp = sb("etmp", [128, NTl, 4, 8])
            ein = sb("ein", [128, NTl, 8]); oh1 = sb("oh1", [128, NTl, 8]); e2 = sb("e2", [128, NTl, 8]); oh2 = sb("oh2", [128, NTl, 8])
            zt = sb("zt", [128, 8, 1024], BF16)
            P.v(lambda e: e.memset(zt[:], 0.0), [], ["zt"], eng="pool")
            zkeys = []
            for z0 in range(0, NSLOT, 1024):
                nrow = min(1024, NSLOT - z0)
                P.dma(Xs[z0:z0 + nrow, :].rearrange("(a p) c -> p a c", p=128), zt[:, 0:nrow // 128, :], ["zt"], [("Xsz", z0)])
                zkeys.append(("Xsz", z0))
            P.dma(ident[:], D["ident32"][:, :], [], ["ident"])
            P.dma(gbc[:], D["norm_ffn_g"][layer:layer + 1, :].partition_broadcast(128), [], ["gbc"])
            P.dma(w_r[:, :, 0:4], D["moe_w_rg"][layer].rearrange("(kc p) n -> p kc n", p=128), [], [("w_r", 0)])
            P.dma(w_r[:, :, 4:36], D["moe_w_re"][layer].rearrange("(kc p) n -> p kc n", p=128), [], [("w_r", 1)])
            P.dma(gm[:], D["gmask"][3:5].rearrange("a p n -> p a n"), [], ["gm"])
            P.dma(eidrow[:], D["eidrow"][:, :], [], ["eidrow"])
            P.dma(pc2[:], D["pc2"][:, :], [], ["pc2"])
            for v in range(2):
                a, ak = Abc[v]
                P.v(lambda e, a=a: e.scalar_tensor_tensor(a[:], a[:], 1.0, gbc[:], ALU.add, ALU.mult), [ak, "gbc"], [ak])
            for ti, (r, var) in enumerate(tiles):
                A, Ak = Abc[var]; B, Bk = Bbc[var]
                x_ap, xk = xt.next()
                P.dma(x_ap[:], D[xin][r * 128:(r + 1) * 128, :], [], [xk])
                sm, smk = small.next()
                P.act(junk[:], x_ap[:], AF.Square, [xk], ["junk", (smk, 0)], accum_out=sm[:, 0:1])
                rms_rstd(P, sm[:, 0:1], (smk, 0), sm[:, 2:3], (smk, 2), neghalf, 1024, sm[:, 1:2], (smk, 1))
                t1, t1k = t1r.next(); h2, h2k = h2r.next(); h2T32, hTk = h2Tr.next()
                P.v(lambda e, x_ap=x_ap, sm=sm, A=A, t1=t1: e.scalar_tensor_tensor(
                    t1[:], x_ap[:], sm[:, 2:3], A[:], ALU.mult, ALU.mult), [xk, (smk, 2), Ak], [t1k])
                P.v(lambda e, B=B, t1=t1, h2=h2: e.tensor_tensor(h2[:], t1[:], B[:], ALU.add), [t1k, Bk], [h2k])
                P.act(h2b[:, ti, :], h2[:], AF.Copy, [h2k], [("h2b", ti)])
                for half in range(2):
                    bk, bkey = nA()
                    for j in range(4):
                        kc = half * 4 + j
                        P.tr(bk[:, j * 128:(j + 1) * 128], h2[:, kc * 128:(kc + 1) * 128], ident[:], [h2k, "ident"], [bkey])
                    P.v(lambda e, bk=bk, half=half, h2T32=h2T32: e.tensor_copy(
                        h2T32[:, half * 4:(half + 1) * 4, :], bk[:, :].rearrange("p (a b) -> p a b", a=4)),
                        [bkey], [(hTk, half)])
                lg, lgk = nB()
                for kc in range(8):
                    P.mm(lg[:, 0:36], h2T32[:, kc, :], w_r[:, kc, :], kc == 0, kc == 7,
                         [(hTk, kc // 4), ("w_r", 0), ("w_r", 1)], [lgk])
                P.v(lambda e, lg=lg, ti=ti: e.tensor_copy(LG[:, ti, :], lg[:, 0:36]), [lgk], [("LG", ti)])
            lgk_all = [("LG", ti) for ti in range(NTl)]
            NT = NTl
            G = LG[:, :, 0:4]
            E4 = LG[:, :, 4:36].rearrange("p t (g e) -> p t g e", g=4)
            bc = lambda ap2, n: ap2.unsqueeze(2).to_broadcast([128, NT, n])
            P.v(lambda e: e.tensor_reduce(rs_[:, 0, :], G, AX.X, ALU.max), lgk_all, ["gmax"])
            P.v(lambda e: e.tensor_tensor(goh[:], G, bc(rs_[:, 0, :], 4), ALU.is_equal), lgk_all + ["gmax"], ["goh"])
            P.v(lambda e: e.tensor_tensor(gex[:], G, bc(rs_[:, 0, :], 4), ALU.subtract), lgk_all + ["gmax"], ["gex"])
            P.act(gex[:], gex[:], AF.Exp, ["gex"], ["gex"])
            P.v(lambda e: e.tensor_reduce(rs_[:, 1, :], gex[:], AX.X, ALU.add), ["gex"], ["gsum"])
            P.v(lambda e: e.reciprocal(rs_[:, 2, :], rs_[:, 1, :]), ["gsum"], ["pmax"])
            P.v(lambda e: e.tensor_tensor(etmp[:], E4, goh[:].unsqueeze(3).to_broadcast([128, NT, 4, 8]), ALU.mult),
                lgk_all + ["goh"], ["etmp"])
            P.v(lambda e: e.tensor_tensor(ein[:], etmp[:, :, 0, :], etmp[:, :, 1, :], ALU.add), ["etmp"], ["ein"])
            P.v(lambda e: e.tensor_tensor(ein[:], ein[:], etmp[:, :, 2, :], ALU.add), ["etmp", "ein"], ["ein"])
            P.v(lambda e: e.tensor_tensor(ein[:], ein[:], etmp[:, :, 3, :], ALU.add), ["etmp", "ein"], ["ein"])
            P.v(lambda e: e.tensor_reduce(rs_[:, 3, :], ein[:], AX.X, ALU.max), ["ein"], ["m1"])
            P.v(lambda e: e.tensor_tensor(oh1[:], ein[:], bc(rs_[:, 3, :], 8), ALU.is_equal), ["ein", "m1"], ["oh1"])
            P.v(lambda e: e.scalar_tensor_tensor(e2[:], oh1[:], -1e30, ein[:], ALU.mult, ALU.add), ["oh1", "ein"], ["e2"])
            P.v(lambda e: e.tensor_reduce(rs_[:, 4, :], e2[:], AX.X, ALU.max), ["e2"], ["m2"])
            P.v(lambda e: e.tensor_tensor(oh2[:], e2[:], bc(rs_[:, 4, :], 8), ALU.is_equal), ["e2", "m2"], ["oh2"])
            P.v(lambda e: e.tensor_tensor(rs_[:, 5, :], rs_[:, 4, :], rs_[:, 3, :], ALU.subtract), ["m1", "m2"], ["dd"])
            P.act(rs_[:, 6, :], rs_[:, 5, :], AF.Exp, ["dd"], ["ed"])
            P.v(lambda e: e.tensor_scalar(rs_[:, 7, :], rs_[:, 6, :], 1.0, None, ALU.add), ["ed"], ["w1"])
            P.v(lambda e: e.reciprocal(rs_[:, 7, :], rs_[:, 7, :]), ["w1"], ["w1"])
            P.v(lambda e: e.tensor_tensor(wAB[:, 0, :], rs_[:, 7, :], rs_[:, 2, :], ALU.mult), ["w1", "pmax"], ["wA"])
            P.v(lambda e: e.tensor_tensor(wAB[:, 1, :], wAB[:, 0, :], rs_[:, 6, :], ALU.mult), ["wA", "ed"], ["wB"])
            s4 = lambda ap3: ap3[:].rearrange("p t (g e) -> p t g e", g=4)
            P.v(lambda e: e.tensor_tensor(s4(selA), oh1[:].unsqueeze(2).to_broadcast([128, NT, 4, 8]),
                                          goh[:].unsqueeze(3).to_broadcast([128, NT, 4, 8]), ALU.mult), ["oh1", "goh"], ["selA"])
            P.v(lambda e: e.tensor_tensor(s4(selB), oh2[:].unsqueeze(2).to_broadcast([128, NT, 4, 8]),
                                          goh[:].unsqueeze(3).to_broadcast([128, NT, 4, 8]), ALU.mult), ["oh2", "goh"], ["selB"])
            P.v(lambda e: e.tensor_tensor(selm[:], selA[:], selB[:], ALU.add), ["selA", "selB"], ["selm"])
            for ti in range(NTl):
                P.mm(cntb[:, 0:32], gm[:, 1, :], selm[:, ti, :], ti == 0, ti == NTl - 1, ["gm", "selm"], [cntk])
            seg = sb("seg", [128, 8, 32])
            segT = sb("segT", [32, 128])
            ecol = sb("ecol", [128, NST])
            sti = sb("sti", [128, 2, NST, 32])
            stc = sb("stc", [128, 64])
            P.dma(stc[:], D["stile_c"][:, :], [], ["stc"])
            widxf = sb("widxf", [128, NST, 6])
            slotf = sb("slotf", [128, 2, NTl])
            P.v(lambda e: e.tensor_copy(seg[:, 0, :], cntb[:, 0:32]), [cntk], ["cnt"])
            P.v(lambda e: e.tensor_scalar(seg[:, 1, :], seg[:, 0, :], 0.0, None, ALU.is_gt), ["cnt"], ["nst"])
            for k in range(1, (T + STILE - 1) // STILE + 1):
                P.v(lambda e, k=k: e.scalar_tensor_tensor(seg[:, 1, :], seg[:, 0, :], float(STILE * k), seg[:, 1, :],
                                                          ALU.is_gt, ALU.add), ["cnt", "nst"], ["nst"])
            P.v(lambda e: e.tensor_scalar(seg[:, 2, :], seg[:, 1, :], float(STILE), None, ALU.mult), ["nst"], ["pc"])
            bk, bkey = nA()
            P.tr(bk[0:32, 0:128], seg[:, 2, :], ident[:], ["pc", "ident"], [bkey])
            P.v(lambda e, bk=bk: e.tensor_copy(segT[:], bk[0:32, 0:128]), [bkey], ["segT"])
            bk2, bkey2 = nA()
            P.mm(bk2[:, 0:32], segT[0:32, :], gm[0:32, 0, 0:32], True, True, ["segT", "gm"], [bkey2])
            P.v(lambda e, bk2=bk2: e.tensor_copy(seg[:, 3, :], bk2[:, 0:32]), [bkey2], ["start"])
            P.v(lambda e: e.tensor_tensor(seg[:, 4, :], seg[:, 3, :], seg[:, 2, :], ALU.add), ["start", "pc"], ["end"])
            P.v(lambda e: e.tensor_copy(seg[:, 5, :], seg[:, 3, :]), ["start"], ["base"])
            bci = lambda ap2: ap2.unsqueeze(1).to_broadcast([128, NST, 32])
            cI = stc[:, 0:NST].unsqueeze(2).to_broadcast([128, NST, 32])
            P.v(lambda e: e.tensor_tensor(sti[:, 0, :, :], bci(seg[:, 3, :]), cI, ALU.is_le), ["start", "stc"], ["sti0"])
            P.v(lambda e: e.tensor_tensor(sti[:, 1, :, :], bci(seg[:, 4, :]), cI, ALU.is_gt), ["end", "stc"], ["sti1"])
            P.v(lambda e: e.tensor_tensor(sti[:, 0, :, :], sti[:, 0, :, :], sti[:, 1, :, :], ALU.mult), ["sti0", "sti1"], ["sti0"])
            P.v(lambda e: e.tensor_tensor(sti[:, 0, :, :], sti[:, 0, :, :], bci(eidrow[:]), ALU.mult), ["sti0", "eidrow"], ["sti0"])
            P.v(lambda e: e.tensor_reduce(ecol[:, :], sti[:, 0, :, :], AX.X, ALU.add), ["sti0"], ["ecol"])
            ek = ["ecol"]
            for a_ in range(2):
                P.v(lambda e, a_=a_: e.tensor_scalar(widxf[:, :, a_], ecol[:, :], 256.0, pc2[:, a_:a_ + 1], ALU.mult, ALU.add),
                    ek + ["pc2"], [("widxf", a_)])
            for fc in range(4):
                P.v(lambda e, fc=fc: e.tensor_scalar(widxf[:, :, 2 + fc], ecol[:, :], 512.0, pc2[:, 2 + fc:3 + fc], ALU.mult, ALU.add),
                    ek + ["pc2"], [("widxf", 2 + fc)])
            P.v(lambda e: e.tensor_copy(widx[:].rearrange("p a b -> p (a b)"), widxf[:].rearrange("p a b -> p (a b)")),
                [("widxf", j) for j in range(6)], ["widx"])
            for ti in range(NTl):
                wi, wik = nB()
                P.mm(wi[:, 0:32], gm[:, 0, :], selm[:, ti, :], True, True, ["gm", "selm"], [wik])
                P.mm(wi[:, 32:64], gm[:, 1, :], selm[:, ti, :], True, True, ["gm", "selm"], [wik])
                P.v(lambda e, wi=wi: e.tensor_tensor(seg[:, 6, :], wi[:, 0:32], seg[:, 5, :], ALU.add), [wik, "base"], ["segtmp"])
                P.v(lambda e, ti=ti: e.scalar_tensor_tensor(seg[:, 7, :], seg[:, 6, :], 1.0, selA[:, ti, :], ALU.mult, ALU.mult,
                                                            accum_out=slotf[:, 0, ti:ti + 1]), ["segtmp", "selA"],
                    ["segtmp2", ("slotA", ti)])
                P.v(lambda e, ti=ti: e.scalar_tensor_tensor(seg[:, 7, :], seg[:, 6, :], 1.0, selB[:, ti, :], ALU.mult, ALU.mult,
                                                            accum_out=slotf[:, 1, ti:ti + 1]), ["segtmp", "selB"],
                    ["segtmp2", ("slotB", ti)])
                P.v(lambda e, wi=wi: e.tensor_tensor(seg[:, 5, :], wi[:, 32:64], seg[:, 5, :], ALU.add), [wik, "base"], ["base"])
            P.v(lambda e: e.tensor_copy(idxA[:], slotf[:, 0, :]), [("slotA", ti) for ti in range(NTl)], ["idxA"])
            P.v(lambda e: e.tensor_copy(idxB[:], slotf[:, 1, :]), [("slotB", ti) for ti in range(NTl)], ["idxB"])
            for ti in range(NTl):
                for (ix, ixk) in ((idxA, "idxA"), (idxB, "idxB")):
                    P.op("pool", lambda e, ix=ix, ti=ti: e.indirect_dma_start(
                        out=Xs[0:NSLOT, :], out_offset=bass.IndirectOffsetOnAxis(ap=ix[:, ti:ti + 1], axis=0),
                        in_=h2b[:, ti, :], in_offset=None, bounds_check=None),
                        [("h2b", ti), ixk] + zkeys, [("Xs", ti, ixk)], dma=True, kind="dma")
            P.flush()
        with contextlib.ExitStack() as st:
            sb = lambda name, shape, dt=F32: st.enter_context(nc.sbuf_tensor(tag + name, shape, dt))
            nT, nGU, nY = bank_rot(banks, 0, 2), bank_rot(banks, 2, 6), bank_rot(banks, 6, 8)
            ident = sb("identb", [128, 128], BF16)
            P.dma(ident[:], D["identbf"][:, :], [], ["ident"])
            wg = Rot("wg", [sb("wg%d" % i, [128, 8, 512], BF16) for i in range(2)])
            wu = Rot("wu", [sb("wu%d" % i, [128, 8, 512], BF16) for i in range(2)])
            wd = Rot("wd", [sb("wd%d" % i, [128, 4, 1024], BF16) for i in range(2)])
            xr = Rot("xr", [sb("xr%d" % i, [128, 1024], BF16) for i in range(4)])
            XT = Rot("XT", [sb("XT%d" % i, [128, 8, STILE], BF16) for i in range(2)])
            hg = Rot("hg", [sb("hg%d" % i, [128, 4, STILE], BF16) for i in range(2)])
            sg = Rot("sg", [sb("sg%d" % i, [128, STILE]) for i in range(2)])
            yb = Rot("yb", [sb("yb%d" % i, [128, 1024]) for i in range(3)])

            def gather(dst2d, src, col, i, key):
                P.op("pool", lambda e: e.indirect_dma_start(
                    out=dst2d, out_offset=None, in_=src,
                    in_offset=bass.IndirectOffsetOnAxis(ap=widx[:, i, col:col + 1], axis=0),
                    bounds_check=None), [], [key], dma=True, kind="dma")

            def emit_gu(i):
                g_ap, gk = wg.next(); u_ap, uk = wu.next(); d_ap, dk = wd.next()
                for a_ in range(2):
                    gather(g_ap[:, 4 * a_:4 * a_ + 4, :].rearrange("p a f -> p (a f)"), wgv, a_, i, (gk, a_))
                    gather(u_ap[:, 4 * a_:4 * a_ + 4, :].rearrange("p a f -> p (a f)"), wuv, a_, i, (uk, a_))
                for fc in range(4):
                    gather(d_ap[:, fc, :], wdv, 2 + fc, i, (dk, fc))
                xT, xTk = XT.next()
                for b in range(NB):
                    x_ap, xk = xr.next()
                    P.dma(x_ap[:], Xs[i * STILE + b * 128:i * STILE + (b + 1) * 128, :], [], [xk])
                    bk, bkey = nT()
                    bkb = bk[:].bitcast(BF16)
                    xv = x_ap[:].rearrange("p (m kc) -> p kc m", kc=8)
                    for kc in range(8):
                        P.tr(bkb[:, kc * 128:(kc + 1) * 128], xv[:, kc, :], ident[:], [xk, "ident"], [bkey])
                    P.act(xT[:, :, b * 128:(b + 1) * 128], bkb.rearrange("p (a b) -> p a b", a=8), AF.Copy, [bkey], [(xTk, b)])
                xkeys = [(xTk, b) for b in range(NB)]
                hgt, hgk = hg.next()
                for fc in range(4):
                    Gp, Gk = nGU(); Up, Uk = nGU()
                    for kc in range(8):
                        P.mm(Gp[:, 0:STILE], g_ap[:, kc, fc * 128:(fc + 1) * 128], xT[:, kc, :], kc == 0, kc == 7,
                             [(gk, kc // 4)] + xkeys, [Gk])
                    for kc in range(8):
                        P.mm(Up[:, 0:STILE], u_ap[:, kc, fc * 128:(fc + 1) * 128], xT[:, kc, :], kc == 0, kc == 7,
                             [(uk, kc // 4)] + xkeys, [Uk])
                    s_ap, sk = sg.next()
                    P.act(s_ap[:, :], Gp[:, 0:STILE], AF.Silu, [Gk], [sk])
                    P.v(lambda e, hgt=hgt, Up=Up, s_ap=s_ap, fc=fc: e.tensor_tensor(
                        hgt[:, fc, :], Up[:, 0:STILE], s_ap[:, :], ALU.mult), [Uk, sk], [(hgk, fc)])
                return (i, hgt, hgk, d_ap, dk)

            def emit_down(i, hgt, hgk, d_ap, dk):
                for b in range(NB):
                    y_ap, yk = yb.next()
                    for dh in range(2):
                        Yp, Yk = nY()
                        for fc in range(4):
                            P.mm(Yp[:, :], hgt[:, fc, b * 128:(b + 1) * 128], d_ap[:, fc, dh * 512:(dh + 1) * 512],
                                 fc == 0, fc == 3, [(hgk, f) for f in range(4)] + [(dk, fc)], [Yk])
                        if dh == 0:
                            P.act(y_ap[:, 0:512], Yp[:, :], AF.Copy, [Yk], [(yk, 0)])
                        else:
                            P.v(lambda e, y_ap=y_ap, Yp=Yp: e.tensor_copy(y_ap[:, 512:1024], Yp[:, :]), [Yk], [(yk, 1)])
                    r0 = i * STILE + b * 128
                    P.dma(Ys[r0:r0 + 128, :], y_ap[:], [(yk, 0), (yk, 1)], [("Ys", r0)], eng="act")

            pend = None
            for i in range(NST):
                cur = emit_gu(i)
                if pend is not None:
                    emit_down(*pend)
                pend = cur
            emit_down(*pend)
            P.flush()
        with contextlib.ExitStack() as st:
            sb = lambda name, shape, dt=F32: st.enter_context(nc.sbuf_tensor(tag + name, shape, dt))
            G = load_mod_rows(P, nc, st, D["mods"], layer, GT2, tag + "G")
            xt = Rot("mxt2", [sb("xt2_%d" % i, [128, 1024]) for i in range(4)])
            ya = Rot("ya", [sb("ya%d" % i, [128, 1024]) for i in range(4)])
            ybb = Rot("ybb", [sb("ybb%d" % i, [128, 1024]) for i in range(4)])
            if final_norm:
                fg = sb("fg", [128, 1024])
                P.dma(fg[:], D["final_norm_g"][0:1, :].partition_broadcast(128), [], ["fg"])
                junk = sb("junk3", [128, 1024])
                small = Rot("fsmall", [sb("fsmall%d" % i, [128, 4]) for i in range(2)])
            for ti, (r, var) in enumerate(tiles):
                x_ap, xk = xt.next()
                P.dma(x_ap[:], D[xin][r * 128:(r + 1) * 128, :], [], [xk])
                a_ap, ak = ya.next(); b_ap, bk_ = ybb.next()
                for (dst, dkey, ix) in ((a_ap, ak, idxA), (b_ap, bk_, idxB)):
                    P.op("pool", lambda e, dst=dst, ix=ix, ti=ti: e.indirect_dma_start(
                        out=dst[:, :], out_offset=None, in_=Ys[0:NSLOT, :],
                        in_offset=bass.IndirectOffsetOnAxis(ap=ix[:, ti:ti + 1], axis=0),
                        bounds_check=None), [], [dkey], dma=True, kind="dma")
                P.v(lambda e, a_ap=a_ap, ti=ti: e.tensor_scalar(a_ap[:], a_ap[:], wAB[:, 0, ti:ti + 1], None, ALU.mult), [ak], [ak])
                P.v(lambda e, a_ap=a_ap, b_ap=b_ap, ti=ti: e.scalar_tensor_tensor(
                    a_ap[:], b_ap[:], wAB[:, 1, ti:ti + 1], a_ap[:], ALU.mult, ALU.add), [ak, bk_], [ak])
                P.v(lambda e, a_ap=a_ap, var=var: e.tensor_tensor(a_ap[:], a_ap[:], G[var][0][:], ALU.mult), [ak, G[var][1]], [ak])
                P.v(lambda e, a_ap=a_ap, x_ap=x_ap: e.tensor_tensor(a_ap[:], a_ap[:], x_ap[:], ALU.add), [ak, xk], [ak])
                if final_norm:
                    sm, smk = small.next()
                    P.act(junk[:], a_ap[:], AF.Square, [ak], ["junk3", (smk, 0)], accum_out=sm[:, 0:1])
                    rms_rstd(P, sm[:, 0:1], (smk, 0), sm[:, 2:3], (smk, 2), neghalf, 1024, sm[:, 1:2], (smk, 1))
                    P.v(lambda e, a_ap=a_ap, sm=sm: e.scalar_tensor_tensor(
                        a_ap[:], a_ap[:], sm[:, 2:3], fg[:], ALU.mult, ALU.mult), [ak, (smk, 2), "fg"], [ak])
                P.dma(D[xout][r * 128:(r + 1) * 128, :], a_ap[:], [ak], [(xout, r)], eng="act")
            P.flush()

import numpy as np
import ml_dtypes

BF = ml_dtypes.bfloat16
GRID_W = 64

W_SMALL = {
    "ada_w": [2, 1024, 6144], "ada_b": [2, 6144], "norm_mix_g": [2, 1024], "norm_ffn_g": [2, 1024],
    "even_w_in": [1, 1024, 1184], "mla_q_norm_g": [1, 256], "mla_w_uq": [1, 256, 768], "mla_kv_norm_g": [1, 128],
    "mla_w_ukv": [1, 128, 1024], "win_sink": [1, 8], "even_w_out": [1, 1024, 1024],
    "odd_w_in": [1, 1024, 2592], "gla_w_g2": [1, 2, 16, 256], "gla_b_g": [1, 2, 256], "gla_norm_g": [1, 512],
    "sg_ln_g": [1, 512], "sg_ln_b": [1, 512], "odd_w_out": [1, 1024, 1024],
    "moe_w_rg": [2, 1024, 4], "moe_w_re": [2, 1024, 32], "final_norm_g": [1, 1024],
}
W_MOE = {"moe_w_gate": [32, 1024, 512], "moe_w_up": [32, 1024, 512], "moe_w_down": [32, 512, 1024]}
CONSTS = {"wmask": ([2, 128, 512], BF16), "ident32": ([128, 128], F32), "identbf": ([128, 128], BF16),
          "gmask": ([5, 128, 128], F32), "eidrow": ([128, 32], F32), "pc2": ([128, 6], F32), "stile_c": ([128, 64], F32)}
PERCORE = {"xtok": [NTOK, 1024], "cvec": [2, 1024], "cA": [NTOK, 256], "sA": [NTOK, 256], "cB": [NTOK, 512],
           "sB": [NTOK, 512], "sg_w_sT": [4, 128, 128], "sg_b_sT": [128, 4], "sel": [128, 2]}
SCRATCH = {"mods": ([2, 2, 6144], F32), "QAT": ([96, 8, NTOK], BF16), "KAT": ([96, 8, NTOK], BF16),
           "VA": ([NTOK, 520], BF16), "QBT": ([64, 8, NTOK], BF16), "KBT": ([64, 2, NTOK], BF16),
           "VB": ([NTOK, 130], BF16), "x1": ([NOWN, 1024], F32), "x2": ([NOWN, 1024], F32),
           "g_T": ([128, 18, 8, 128], BF16), "g_kd": ([2, NOWN, 256], BF16), "g_v": ([NOWN, 512], BF16),
           "g_dec": ([128, 18, 4], F32), "rsilu": ([2048, 512], BF16), "dl": ([2048, 512], BF16),
           "OA": ([2048, 512], F32), "cc_in": ([256, 128], F32), "cc_out": ([512, 128], F32),
           "x3": ([2048, 1024], F32), "out": ([2048, 1024], F32),
           "Xs": ([n_stiles(NOWN) * STILE, 1024], BF16), "Ys": ([n_stiles(NOWN) * STILE, 1024], F32)}
HANDOFF = ["mods", "x2", "g_T", "g_kd", "g_v", "g_dec", "rsilu", "dl", "OA"]


def rope_tables(pos, dim, nheads):
    pos = np.asarray(pos)
    half = dim // 2
    inv = np.power(np.float32(10000.0), -np.arange(0, half, 2, dtype=np.float32) / np.float32(half)).astype(np.float32)
    row = (pos // GRID_W).astype(np.float32)
    col = (pos % GRID_W).astype(np.float32)
    ar = row[:, None] * inv[None, :]
    ac = col[:, None] * inv[None, :]
    ang = np.concatenate([ar, ar, ac, ac], axis=-1).astype(np.float32)
    cos = np.cos(ang).astype(np.float32)
    sin = np.sin(ang).astype(np.float32)
    blk = dim // 4
    sign = np.concatenate([-np.ones(blk), np.ones(blk), -np.ones(blk), np.ones(blk)]).astype(np.float32)
    ssin = sin * sign[None, :]
    no = pos < 0
    cos[no] = 1.0
    ssin[no] = 0.0
    return np.tile(cos, (1, nheads)), np.tile(ssin, (1, nheads))


_CONST = {}


def const_inputs():
    if not _CONST:
        j = np.arange(128)[:, None]
        i = np.arange(128)[None, :]
        m0 = np.tile((j >= i).astype(np.float32), (1, 4))
        m1 = np.tile((j <= i).astype(np.float32), (1, 4))
        _CONST["wmask"] = np.stack([m0, m1]).astype(BF)
        _CONST["ident32"] = np.eye(128, dtype=np.float32)
        _CONST["identbf"] = np.eye(128, dtype=np.float32).astype(BF)
        one = np.ones((128, 128), bool)
        _CONST["gmask"] = np.stack([(j <= i), (j >= i), one, (j < i), one]).astype(np.float32)
        _CONST["eidrow"] = np.tile(np.arange(32, dtype=np.float32)[None, :], (128, 1))
        p = np.arange(128, dtype=np.float32)
        _CONST["stile_c"] = np.tile((np.arange(64, dtype=np.float32) * STILE)[None, :], (128, 1))
        _CONST["pc2"] = np.stack([2 * p, 2 * p + 1, p, 128 + p, 256 + p, 384 + p], 1).astype(np.float32)
    return _CONST


def local_order(hf):
    own = np.arange(hf * 2048, (hf + 1) * 2048)
    oth = np.arange((1 - hf) * 2048, (2 - hf) * 2048)
    cidx = np.arange(256)
    if hf == 1:
        own, oth, cidx = own[::-1], oth[::-1], cidx[::-1]
    return own, oth, cidx


def weights_for(core, inp, layers=(0, 1)):
    hf = core % 2
    m = {}
    for k, shp in W_SMALL.items():
        m[k] = np.ascontiguousarray(np.asarray(inp[k]).reshape(shp))
    for l in layers:
        for k in W_MOE:
            m["%s%d" % (k, l)] = np.asarray(inp[k][l])
    ws = np.asarray(inp["sg_w_s"][0])
    bs = np.asarray(inp["sg_b_s"][0])
    if hf == 1:
        m["gla_w_g2"] = np.ascontiguousarray(m["gla_w_g2"][:, ::-1])
        m["gla_b_g"] = np.ascontiguousarray(m["gla_b_g"][:, ::-1])
        w = m["odd_w_in"].copy()
        w[:, :, 1024:1040] = m["odd_w_in"][:, :, 1040:1056]
        w[:, :, 1040:1056] = m["odd_w_in"][:, :, 1024:1040]
        m["odd_w_in"] = w
        ws = ws[:, ::-1, ::-1]
        bs = bs[:, ::-1]
    m["sg_w_sT"] = np.ascontiguousarray(ws.transpose(0, 2, 1))
    m["sg_b_sT"] = np.ascontiguousarray(bs.T)
    return m


def core_inputs(core, inp):
    b, hf = core // 2, core % 2
    own, oth, cidx = local_order(hf)
    pos = np.concatenate([own, oth, -np.ones(256, dtype=np.int64)])
    xtok = np.concatenate([inp["x"][b][own], inp["x"][b][oth], inp["ctx"][b][cidx]], 0)
    cA, sA = rope_tables(pos, 32, 8)
    cB, sB = rope_tables(pos, 64, 8)
    m = {"xtok": np.ascontiguousarray(xtok), "cvec": np.stack([inp["c"][b], inp["c_ctx"]]).astype(np.float32),
         "cA": cA, "sA": sA, "cB": cB, "sB": sB}
    sel = np.zeros((128, 2), np.float32)
    sel[:, 1 - hf] = 1.0
    m["sel"] = sel
    m.update(const_inputs())
    return m


def declare(nc, ext_in, ext_out, moe_layers=(0, 1)):
    D = {}
    dr = lambda n, s, dt=F32, k="ExternalInput": nc.dram_tensor(n, s, dt, kind=k).ap()
    for k, shp in PERCORE.items():
        D[k] = dr(k, shp)
    for k, (shp, dt) in CONSTS.items():
        D[k] = dr(k, shp, dt)
    for k, shp in W_SMALL.items():
        D[k] = dr(k, shp)
    for l in moe_layers:
        for k, shp in W_MOE.items():
            D["%s%d" % (k, l)] = dr("%s%d" % (k, l), shp)
    for k, (shp, dt) in SCRATCH.items():
        kind = "ExternalOutput" if k in ext_out else ("ExternalInput" if k in ext_in else "Internal")
        D[k] = dr(k, shp, dt, kind)
    return D


MOE0_TILES = [(i, 0) for i in range(16)] + [(16, 1), (17, 1)]
MOE1_TILES = [(i, 0) for i in range(16)]


def build_fused(extra_out=()):
    nc = bass.Bass("TRN2", target_bir_lowering=False)
    D = declare(nc, (), ["out"] + list(extra_out), moe_layers=(0, 1))
    banks = [nc.alloc_psum_tensor("bank%d" % i, [128, 512], F32) for i in range(8)]
    P = Prog(nc)
    phase_ada(nc, P, D, banks)
    phase_l0a(nc, P, D, banks)
    phase_l0b(nc, P, D, banks)
    phase_moe_sparse(nc, P, D, banks, 0, "x1", "x2", MOE0_TILES, "m0_")
    phase_l1a(nc, P, D, banks)
    phase_l1b_a(nc, P, D, banks)
    phase_l1b_b(nc, P, D, banks)
    phase_moe_sparse(nc, P, D, banks, 1, "x3", "out", MOE1_TILES, "m1_", final_norm=True)
    P.flush(final=True)
    return nc


def kernel(**inputs):
    inp = {k: np.asarray(v) for k, v in inputs.items()}
    n = 8
    nc = build_fused()
    in_maps = []
    for c in range(n):
        m = core_inputs(c, inp)
        m.update(weights_for(c, inp, layers=(0, 1)))
        in_maps.append(m)
    res = run_bass_kernel_spmd(nc, in_maps, core_ids=list(range(n))).results
    out = np.zeros((4, 4096, 1024), np.float32)
    for c in range(n):
        b, hf = c // 2, c % 2
        own = local_order(hf)[0]
        out[b][own] = np.asarray(res[c]["out"], dtype=np.float32)
    return out
```

```python
import contextlib
import numpy as np
import concourse.bass as bass
import concourse.mybir as mybir
from concourse.bass_utils import run_bass_kernel_spmd

F32 = mybir.dt.float32
BF16 = mybir.dt.bfloat16
AF = mybir.ActivationFunctionType
ALU = mybir.AluOpType
AX = mybir.AxisListType
N_DMA_SEMS = 10


class Op:
    __slots__ = ("eng", "fn", "deps", "signal", "sem", "val", "is_dma", "idx", "kind", "sem_eng")

    def __init__(self, eng, fn, is_dma, kind):
        self.eng = eng
        self.fn = fn
        self.deps = set()
        self.signal = False
        self.sem = None
        self.val = 0
        self.is_dma = is_dma
        self.kind = kind
        self.sem_eng = None


class Rot:
    def __init__(self, name, aps):
        self.name = name
        self.aps = aps
        self.i = 0

    def next(self):
        k = self.i % len(self.aps)
        self.i += 1
        return self.aps[k], (self.name, k)


class Prog:
    ENGS = ("pe", "act", "dve", "pool", "sp")

    def __init__(self, nc):
        self.nc = nc
        self.st = contextlib.ExitStack()
        st = self.st
        self.esem = {e: st.enter_context(nc.semaphore("s_" + e)) for e in ("pe", "act", "dve", "pool", "cc")}
        self.dsem = {e: [st.enter_context(nc.semaphore("d_%s%d" % (e, i))) for i in range(N_DMA_SEMS)]
                     for e in ("sp", "act", "pool")}
        self.cnt = {e: 0 for e in self.esem}
        self.dcnt = {e: [0] * N_DMA_SEMS for e in self.dsem}
        self.drr = {e: 0 for e in self.dsem}
        self.nflush = 0
        self.total_ops = 0
        self._reset()

    def _reset(self):
        self.ops = []
        self.last_w = {}
        self.readers = {}

    def op(self, eng, fn, reads=(), writes=(), dma=False, kind=""):
        o = Op(eng, fn, dma, kind)
        o.idx = len(self.ops)
        ex = [r for r in reads if isinstance(r, tuple) and r[0] == "ps"]
        if ex and eng != "pe":
            reads = [r for r in reads if r not in ex]
            writes = list(writes) + ex
        for r in reads:
            w = self.last_w.get(r)
            if w is not None:
                o.deps.add(w)
        for wkey in writes:
            w = self.last_w.get(wkey)
            if w is not None:
                o.deps.add(w)
            for rd in self.readers.get(wkey, ()):
                o.deps.add(rd)
        for r in reads:
            self.readers.setdefault(r, []).append(o.idx)
        for wkey in writes:
            self.last_w[wkey] = o.idx
            self.readers[wkey] = []
        o.deps.discard(o.idx)
        self.ops.append(o)
        return o

    def dma(self, out, in_, reads, writes, eng="sp", **kw):
        return self.op(eng, lambda e: e.dma_start(out=out, in_=in_, **kw), reads, writes, dma=True, kind="dma")

    def mm(self, out, lhsT, rhs, start, stop, reads, writes, **kw):
        return self.op("pe", lambda e: e.matmul(out, lhsT, rhs, start=start, stop=stop, **kw),
                       reads, writes, kind="mm")

    def tr(self, out, in_, ident, reads, writes):
        return self.op("pe", lambda e: e.transpose(out, in_, ident), reads, writes, kind="mm")

    def act(self, out, in_, func, reads, writes, **kw):
        return self.op("act", lambda e: e.activation(out, in_, func, **kw), reads, writes, kind="act")

    def cc(self, fn, reads, writes):
        o = self.op("pool", fn, reads, writes, kind="cc")
        o.sem_eng = "cc"
        o.signal = True
        return o

    def v(self, fn, reads, writes, eng="dve"):
        return self.op(eng, fn, reads, writes, kind="v")

    def flush(self, final=False):
        nc = self.nc
        ops = self.ops
        self.total_ops += len(ops)
        for o in ops:
            if o.eng == "pe":
                o.deps = {d for d in o.deps if ops[d].eng != "pe"}
        for o in ops:
            for d in o.deps:
                ops[d].signal = True
        base_cnt = dict(self.cnt)
        base_dcnt = {e: list(v) for e, v in self.dcnt.items()}
        dprev = {e: [None] * N_DMA_SEMS for e in self.dsem}
        for o in ops:
            if o.is_dma:
                o.signal = True
                k = self.drr[o.eng]
                self.drr[o.eng] = (k + 1) % N_DMA_SEMS
                self.dcnt[o.eng][k] += 16
                o.sem = self.dsem[o.eng][k]
                o.val = self.dcnt[o.eng][k]
                p = dprev[o.eng][k]
                if p is not None:
                    o.deps.add(p)
                dprev[o.eng][k] = o.idx
            elif o.signal:
                se = o.sem_eng or o.eng
                self.cnt[se] += 1
                o.sem = self.esem[se]
                o.val = self.cnt[se]
        first = self.nflush == 0
        self.nflush += 1
        with nc.Block() as blk:
            getters = {"pe": blk.tensor, "act": blk.scalar, "dve": blk.vector, "pool": blk.gpsimd, "sp": blk.sync}
            for ename in self.ENGS:
                mine = [o for o in ops if o.eng == ename]

                def body(e, mine=mine, ename=ename):
                    waited = {}
                    if not first:
                        for en, sem in self.esem.items():
                            if base_cnt[en] > 0 and en != ename:
                                e.wait_ge(sem, base_cnt[en])
                                waited[sem.num] = base_cnt[en]
                        for en, sems in self.dsem.items():
                            for k, sem in enumerate(sems):
                                if base_dcnt[en][k] > 0:
                                    e.wait_ge(sem, base_dcnt[en][k])
                                    waited[sem.num] = base_dcnt[en][k]
                    for o in mine:
                        need = {}
                        for d in o.deps:
                            do = ops[d]
                            if need.get(do.sem.num, (None, 0))[1] < do.val:
                                need[do.sem.num] = (do.sem, do.val)
                        for num, (sem, val) in need.items():
                            if waited.get(num, 0) >= val:
                                continue
                            e.wait_ge(sem, val)
                            waited[num] = val
                        ins = o.fn(e)
                        if o.signal:
                            if o.kind == "cc":
                                ins.then_inc(o.sem)
                            else:
                                ins.then_inc(o.sem, 16 if o.is_dma else 1)
                    if final and ename == "sp":
                        for en, sems in self.dsem.items():
                            for k, sem in enumerate(sems):
                                if self.dcnt[en][k] > 0:
                                    e.wait_ge(sem, self.dcnt[en][k])
                        for en, sem in self.esem.items():
                            if self.cnt[en] > 0:
                                e.wait_ge(sem, self.cnt[en])

                getters[ename](body)
        self._reset()
        if final:
            self.st.close()


def bank_rot(banks, lo, hi):
    state = {"i": 0}

    def nxt():
        k = lo + state["i"] % (hi - lo)
        state["i"] += 1
        return banks[k], ("ps", k)
    return nxt


EPS = 1e-6
NTOK = 4352
NOWN = 2304
SH1, SC1, GT1, SH2, SC2, GT2 = range(6)


def own_tiles():
    return [(i, i, 0) for i in range(16)] + [(16, 32, 1), (17, 33, 1)]


def rms_rstd(P, ss_ap, ss_key, out_ap, out_key, neghalf, D, tmp_ap, tmp_key):
    P.v(lambda e: e.tensor_scalar(tmp_ap, ss_ap, 1.0 / D, EPS, ALU.mult, ALU.add), [ss_key], [tmp_key])
    P.v(lambda e: e.tensor_tensor(out_ap, tmp_ap, neghalf, ALU.pow), [tmp_key, "consts"], [out_key], eng="pool")


def load_mod_rows(P, nc, st, mods_d, layer, which, names):
    out = []
    for v in range(2):
        t = st.enter_context(nc.sbuf_tensor("%s%d" % (names, v), [128, 1024], F32))
        P.dma(t[:], mods_d[layer, v:v + 1, which * 1024:(which + 1) * 1024].partition_broadcast(128), ["mods"],
              [(names, v)])
        out.append((t, (names, v)))
    return out


def phase_ada(nc, P, D, banks):
    with contextlib.ExitStack() as st:
        sb = lambda name, shape, dt=F32: st.enter_context(nc.sbuf_tensor(name, shape, dt))
        PS = Rot("ps", banks)
        ident = sb("ada_ident", [128, 128])
        cb = sb("ada_cb", [128, 2, 1024])
        screp = sb("ada_screp", [128, 2, 8, 128])
        bias = sb("ada_bias", [96, 2, 128])
        wbuf = Rot("ada_w", [sb("ada_wbuf%d" % i, [128, 8, 512]) for i in range(4)])
        accs = sb("ada_accs", [128, 2, 96])
        outT = sb("ada_outT", [96, 2, 128])
        P.dma(ident[:], D["ident32"][:, :], [], ["ident"])
        for v in range(2):
            P.dma(cb[:, v, :], D["cvec"][v:v + 1, :].partition_broadcast(128), [], [("cb", v)])
        for l in range(2):
            for v in range(2):
                P.dma(bias[48 * v:48 * v + 48, l, :], D["ada_b"][l].rearrange("(c p) -> c p", p=128), [], [("bias", l, v)])
        for v in range(2):
            P.act(cb[:, v, :], cb[:, v, :], AF.Silu, [("cb", v)], [("cb", v)])
            for half in range(2):
                p, pk = PS.next()
                for j in range(4):
                    kc = half * 4 + j
                    P.tr(p[:, j * 128:(j + 1) * 128], cb[:, v, kc * 128:(kc + 1) * 128], ident[:],
                         [("cb", v), "ident"], [pk])
                P.v(lambda e, p=p, v=v, half=half: e.tensor_copy(
                    screp[:, v, half * 4:(half + 1) * 4, :], p[:, :].rearrange("p (a b) -> p a b", a=4)),
                    [pk], [("screp", v)])
        for l in range(2):
            wl = D["ada_w"][l].rearrange("(kc p) n -> p kc n", p=128)
            acc, acck = PS.next()
            accv = acc[:, 0:96].rearrange("p (v c) -> p v c", v=2)
            for cblk in range(12):
                wb, wk = wbuf.next()
                P.dma(wb[:], wl[:, :, cblk * 512:(cblk + 1) * 512], [], [wk])
                for j in range(4):
                    c = cblk * 4 + j
                    for kc in range(8):
                        P.mm(accv[:, :, c], wb[:, kc, j * 128:(j + 1) * 128], screp[:, :, kc, 0], kc == 0, kc == 7,
                             [("screp", 0), ("screp", 1), wk], [acck])
            P.v(lambda e, acc=acc, l=l: e.tensor_copy(accs[:, l, :], acc[:, 0:96]), [acck], [("accs", l)])
            tp, tpk = PS.next()
            P.tr(tp[0:96, 0:128], accs[:, l, :], ident[:], [("accs", l), "ident"], [tpk])
            P.v(lambda e, tp=tp, l=l: e.tensor_tensor(outT[:, l, :], tp[0:96, 0:128], bias[:, l, :], ALU.add),
                [tpk, ("bias", l, 0), ("bias", l, 1)], [("outT", l)])
            P.dma(D["mods"][l].rearrange("v (c p) -> (v c) p", p=128), outT[:, l, :], [("outT", l)], ["mods"])
        P.flush()


def phase_l0a(nc, P, D, banks):
    NT = NTOK // 128
    with contextlib.ExitStack() as st:
        sb = lambda name, shape, dt=F32: st.enter_context(nc.sbuf_tensor(name, shape, dt))
        PS = Rot("ps", banks)
        ident = sb("a_ident", [128, 128], BF16)
        consts = sb("a_consts", [128, 4])
        gbc = sb("a_gbc", [128, 1024]); qg = sb("a_qg", [128, 256]); kvg = sb("a_kvg", [128, 128])
        w_in = sb("a_w_in", [128, 8, 1184], BF16)
        w_uq = sb("a_w_uq", [128, 2, 768], BF16)
        w_ukv = sb("a_w_ukv", [128, 1024], BF16)
        xt = Rot("xt", [sb("a_xt%d" % i, [128, 1024]) for i in range(3)])
        tabs = Rot("tabs", [sb("a_tabs%d" % i, [128, 1536]) for i in range(4)])
        junk = sb("a_junk", [128, 1024])
        t1r = Rot("t1", [sb("a_t1_%d" % i, [128, 1024]) for i in range(2)])
        hbr = Rot("hb", [sb("a_hb_%d" % i, [128, 1024], BF16) for i in range(2)])
        hTr = Rot("hT", [sb("a_hT_%d" % i, [128, 8, 128], BF16) for i in range(2)])
        small = Rot("small", [sb("a_small%d" % i, [128, 8]) for i in range(4)])
        zsr = Rot("zs", [sb("a_zs%d" % i, [128, 1184]) for i in range(2)])
        cqn = sb("a_cqn", [128, 384], BF16)
        cqnT = sb("a_cqnT", [128, 3, 128], BF16)
        krr = sb("a_krr", [128, 32], BF16)
        ropet = sb("a_ropet", [128, 2, 512])
        QA = sb("a_QA", [128, 8, 96], BF16); KA = sb("a_KA", [128, 8, 96], BF16)
        QB = sb("a_QB", [128, 8, 64], BF16); KB = sb("a_KB", [128, 2, 64], BF16)
        VAs = Rot("VAs", [sb("a_VAs%d" % i, [128, 8, 65], BF16) for i in range(2)])
        VBs = Rot("VBs", [sb("a_VBs%d" % i, [128, 2, 65], BF16) for i in range(2)])
        oQA = Rot("oQA", [sb("a_oQA%d" % i, [96, 8, 128], BF16) for i in range(2)])
        oKA = Rot("oKA", [sb("a_oKA%d" % i, [96, 8, 128], BF16) for i in range(2)])
        oQB = Rot("oQB", [sb("a_oQB%d" % i, [64, 8, 128], BF16) for i in range(2)])
        oKB = Rot("oKB", [sb("a_oKB%d" % i, [64, 2, 128], BF16) for i in range(2)])
        Abc = load_mod_rows(P, nc, st, D["mods"], 0, SC1, "a_A")
        Bbc = load_mod_rows(P, nc, st, D["mods"], 0, SH1, "a_B")

        P.dma(ident[:], D["identbf"][:, :], [], ["ident"])
        P.v(lambda e: e.memset(consts[:, 0:1], -0.5), [], ["consts"])
        P.dma(gbc[:], D["norm_mix_g"][0:1, :].partition_broadcast(128), [], ["gbc"])
        P.dma(qg[:], D["mla_q_norm_g"][0:1, :].partition_broadcast(128), [], ["qg"])
        P.dma(kvg[:], D["mla_kv_norm_g"][0:1, :].partition_broadcast(128), [], ["kvg"])
        P.dma(w_in[:], D["even_w_in"][0].rearrange("(kc p) n -> p kc n", p=128), [], ["w_in"], eng="pool")
        P.dma(w_uq[:], D["mla_w_uq"][0].rearrange("(kc p) n -> p kc n", p=128), [], ["w_uq"], eng="pool")
        P.dma(w_ukv[:], D["mla_w_ukv"][0], [], ["w_ukv"], eng="pool")
        for i, r in enumerate(VAs.aps):
            P.v(lambda e, r=r: e.memset(r[:, :, 64:65], 1.0), [], [("VAs", i)])
        for i, r in enumerate(VBs.aps):
            P.v(lambda e, r=r: e.memset(r[:, :, 64:65], 1.0), [], [("VBs", i)])
        for v in range(2):
            a, ak = Abc[v]
            P.v(lambda e, a=a: e.scalar_tensor_tensor(a[:], a[:], 1.0, gbc[:], ALU.add, ALU.mult), [ak, "gbc"], [ak])
        neghalf = consts[:, 0:1]
        v3 = lambda ap, h: ap.rearrange("p (h w) -> p h w", h=h)

        def rope(src4, dst4, cos4, ssin4, nh, blk, rkeys, wkeys):
            W = 4 * blk
            tmpa = ropet[:, 0, 0:nh * W].rearrange("p (h w) -> p h w", h=nh)
            tmpb = ropet[:, 1, 0:nh * W].rearrange("p (h w) -> p h w", h=nh)
            P.v(lambda e: e.tensor_tensor(tmpa, src4, cos4, ALU.mult), rkeys, ["ropeA"])
            v5 = lambda a: a.rearrange("p h (q w b) -> p h q w b", q=2, w=2)
            for w in range(2):
                P.v(lambda e, w=w: e.tensor_tensor(
                    v5(tmpb)[:, :, :, w, :], v5(src4)[:, :, :, 1 - w, :], v5(ssin4)[:, :, :, w, :], ALU.mult),
                    rkeys, ["ropeB%d" % w])
            P.v(lambda e: e.tensor_tensor(dst4, tmpa, tmpb, ALU.add), ["ropeA", "ropeB0", "ropeB1"], wkeys)

        def stage_a(t):
            rs = slice(t * 128, (t + 1) * 128)
            var = 1 if t >= 32 else 0
            need_q = (t < 16) or (t >= 32)
            A, Ak = Abc[var]; B, Bk = Bbc[var]
            x_ap, x_key = xt.next()
            P.dma(x_ap[:], D["xtok"][rs, :], [], [x_key])
            tb_ap, tb_key = tabs.next()
            P.dma(tb_ap[:, 0:256], D["cA"][rs, :], [], [(tb_key, 0)])
            P.dma(tb_ap[:, 256:512], D["sA"][rs, :], [], [(tb_key, 1)])
            P.dma(tb_ap[:, 512:1024], D["cB"][rs, :], [], [(tb_key, 2)])
            P.dma(tb_ap[:, 1024:1536], D["sB"][rs, :], [], [(tb_key, 3)])
            tkeys = [(tb_key, i) for i in range(4)]
            cA = tb_ap[:, 0:256]; sA = tb_ap[:, 256:512]; cB = tb_ap[:, 512:1024]; sB = tb_ap[:, 1024:1536]
            sm, sm_key = small.next()
            P.act(junk[:], x_ap[:], AF.Square, [x_key], ["junk", (sm_key, 0)], accum_out=sm[:, 0:1])
            rms_rstd(P, sm[:, 0:1], (sm_key, 0), sm[:, 2:3], (sm_key, 2), neghalf, 1024, sm[:, 1:2], (sm_key, 1))
            t1, t1k = t1r.next(); hb, hbk = hbr.next(); hT, hTk = hTr.next()
            P.v(lambda e, x_ap=x_ap, sm=sm, A=A, t1=t1: e.scalar_tensor_tensor(
                t1[:], x_ap[:], sm[:, 2:3], A[:], ALU.mult, ALU.mult), [x_key, (sm_key, 2), Ak], [t1k])
            P.v(lambda e, B=B, t1=t1, hb=hb: e.tensor_tensor(hb[:], t1[:], B[:], ALU.add), [t1k, Bk], [hbk])
            bk, bkey = PS.next()
            bkb = bk[:].bitcast(BF16)
            for kc in range(8):
                P.tr(bkb[:, kc * 128:(kc + 1) * 128], hb[:, kc * 128:(kc + 1) * 128], ident[:], [hbk, "ident"], [bkey])
            P.act(hT[:].rearrange("p a b -> p (a b)"), bkb, AF.Copy, [bkey], [hTk])
            blocks = [(0, 416), (416, 928), (928, 1184)]
            zb = []
            for bi, (c0, c1) in enumerate(blocks):
                if bi == 1 and not need_q:
                    zb.append((None, None))
                    continue
                bk, bkey = PS.next()
                for kc in range(8):
                    P.mm(bk[:, 0:c1 - c0], hT[:, kc, :], w_in[:, kc, c0:c1], kc == 0, kc == 7, [hTk, "w_in"], [bkey])
                zb.append((bk, bkey))
            zs, zsk = zsr.next()
            for bi, (c0, c1) in enumerate(blocks):
                if zb[bi][0] is None:
                    continue
                P.act(zs[:, c0:c1], zb[bi][0][:, 0:c1 - c0], AF.Copy, [zb[bi][1]], [(zsk, bi)])
            return dict(t=t, rs=rs, need_q=need_q, zs=zs, zsk=zsk, tb_ap=tb_ap, tkeys=tkeys, sm=sm, sm_key=sm_key)

        def stage_b(c):
            t, rs, need_q, zs, zsk, tb_ap, tkeys, sm, sm_key = (c[k] for k in ("t", "rs", "need_q", "zs", "zsk", "tb_ap", "tkeys", "sm", "sm_key"))
            cA = tb_ap[:, 0:256]; sA = tb_ap[:, 256:512]; cB = tb_ap[:, 512:1024]; sB = tb_ap[:, 1024:1536]
            z0, z0k = zs[:, 0:416], (zsk, 0)
            z1, z1k = zs[:, 416:928], (zsk, 1)
            z2, z2k = zs[:, 928:1184], (zsk, 2)
            if need_q:
                P.act(junk[:, 0:256], z0[:, 0:256], AF.Square, [z0k], ["junk", (sm_key, 3)], accum_out=sm[:, 3:4])
                rms_rstd(P, sm[:, 3:4], (sm_key, 3), sm[:, 4:5], (sm_key, 4), neghalf, 256, sm[:, 1:2], (sm_key, 1))
                P.v(lambda e, z0=z0, sm=sm: e.scalar_tensor_tensor(
                    cqn[:, 0:256], z0[:, 0:256], sm[:, 4:5], qg[:], ALU.mult, ALU.mult),
                    [z0k, (sm_key, 4), "qg"], ["cqn"])
            P.act(junk[:, 0:128], z0[:, 256:384], AF.Square, [z0k], ["junk", (sm_key, 5)], accum_out=sm[:, 5:6])
            rms_rstd(P, sm[:, 5:6], (sm_key, 5), sm[:, 6:7], (sm_key, 6), neghalf, 128, sm[:, 1:2], (sm_key, 1))
            P.v(lambda e, z0=z0, sm=sm: e.scalar_tensor_tensor(
                cqn[:, 256:384], z0[:, 256:384], sm[:, 6:7], kvg[:], ALU.mult, ALU.mult),
                [z0k, (sm_key, 6), "kvg"], ["cqn"])
            bk, bkey = PS.next()
            bkb = bk[:].bitcast(BF16)
            for j in (range(3) if need_q else [2]):
                P.tr(bkb[:, j * 128:(j + 1) * 128], cqn[:, j * 128:(j + 1) * 128], ident[:], ["cqn", "ident"], [bkey])
            P.act(cqnT[:].rearrange("p a b -> p (a b)"), bkb[:, 0:384], AF.Copy, [bkey], ["cqnT"])
            rope(v3(z0[:, 384:416], 1), v3(krr[:, :], 1), v3(cA[:, 0:32], 1), v3(sA[:, 0:32], 1), 1, 8,
                 tkeys + [z0k], ["krr"])
            if need_q:
                for hh in range(2):
                    bk, bkey = PS.next()
                    for kc in range(2):
                        P.mm(bk[:, 0:384], cqnT[:, kc, :], w_uq[:, kc, hh * 384:(hh + 1) * 384], kc == 0, kc == 1,
                             ["cqnT", "w_uq"], [bkey])
                    q4 = bk[:, 0:384].rearrange("p (h w) -> p h w", h=4)
                    P.act(QA[:, hh * 4:(hh + 1) * 4, 0:64], q4[:, :, 0:64], AF.Copy, [bkey], [("QA", hh, 0)])
                    rope(q4[:, :, 64:96], QA[:, hh * 4:(hh + 1) * 4, 64:96],
                         v3(cA[:, hh * 128:(hh + 1) * 128], 4), v3(sA[:, hh * 128:(hh + 1) * 128], 4), 4, 8,
                         tkeys + [bkey], [("QA", hh, 1)])
            va, va_key = VAs.next()
            for hh in range(2):
                bk, bkey = PS.next()
                P.mm(bk[:, :], cqnT[:, 2, :], w_ukv[:, hh * 512:(hh + 1) * 512], True, True, ["cqnT", "w_ukv"], [bkey])
                k4 = bk[:, :].rearrange("p (h w) -> p h w", h=4)
                P.act(KA[:, hh * 4:(hh + 1) * 4, 0:64], k4[:, :, 0:64], AF.Copy, [bkey], [("KA", hh, 0)])
                P.v(lambda e, va=va, k4=k4, hh=hh: e.tensor_copy(va[:, hh * 4:(hh + 1) * 4, 0:64], k4[:, :, 64:128]),
                    [bkey], [(va_key, hh)])
            for h in range(8):
                P.v(lambda e, h=h: e.tensor_copy(KA[:, h, 64:96], krr[:, :]), ["krr"], [("KA", h, 1)], eng="pool")
            P.dma(D["VA"][rs, :], va[:].rearrange("p h w -> p (h w)"), [(va_key, 0), (va_key, 1)], [("VA", t)], eng="act")
            if need_q:
                q8 = z1[:, :].rearrange("p (h w) -> p h w", h=8)
                rope(q8, QB[:, :, :], v3(cB[:, :], 8), v3(sB[:, :], 8), 8, 16, tkeys + [z1k], ["QB"])
            k2 = z2[:, 0:128].rearrange("p (h w) -> p h w", h=2)
            rope(k2, KB[:, :, :], v3(cB[:, 0:128], 2), v3(sB[:, 0:128], 2), 2, 16, tkeys + [z2k], ["KB"])
            vb, vb_key = VBs.next()
            P.act(vb[:, :, 0:64], z2[:, 128:256].rearrange("p (h w) -> p h w", h=2), AF.Copy, [z2k], [vb_key])
            P.dma(D["VB"][rs, :], vb[:].rearrange("p h w -> p (h w)"), [vb_key], [("VB", t)], eng="act")
            QAk = [("QA", hh, j) for hh in range(2) for j in range(2)]
            KAk = [("KA", hh, 0) for hh in range(2)] + [("KA", h, 1) for h in range(8)]
            jobs = [(KA, KAk, 8, 96, oKA, "KAT"), (KB, ["KB"], 2, 64, oKB, "KBT")]
            if need_q:
                jobs += [(QA, QAk, 8, 96, oQA, "QAT"), (QB, ["QB"], 8, 64, oQB, "QBT")]
            for (src, skeys, nh, Dh, pool, dname) in jobs:
                bk, bkey = PS.next()
                bkb = bk[:].bitcast(BF16)
                for h in range(nh):
                    P.tr(bkb[0:Dh, h * 128:(h + 1) * 128], src[:, h, :], ident[:], skeys + ["ident"], [bkey])
                ob, okey = pool.next()
                P.act(ob[:].rearrange("p a b -> p (a b)"), bkb[0:Dh, 0:nh * 128], AF.Copy, [bkey], [okey])
                P.dma(D[dname][:, :, rs], ob[:], [okey], [(dname, t)], eng="act")

        pend = None
        for t in range(NT):
            cur = stage_a(t)
            if pend is not None:
                stage_b(pend)
            pend = cur
        stage_b(pend)
        P.flush()


def phase_l0b(nc, P, D, banks):
    with contextlib.ExitStack() as st:
        sb = lambda name, shape, dt=F32: st.enter_context(nc.sbuf_tensor(name, shape, dt))
        nS, nO, nR = bank_rot(banks, 0, 4), bank_rot(banks, 4, 6), bank_rot(banks, 6, 8)
        nOP = bank_rot(banks, 0, 4)
        KBT = sb("b_KBT", [64, 2, NTOK], BF16)
        VB = sb("b_VB", [128, 34, 130], BF16)
        VA = sb("b_VA", [128, 34, 520], BF16)
        w_out = sb("b_wout", [128, 16, 1024], BF16)
        sel = sb("b_sel", [65, 64])
        es = sb("b_es", [64, 8])
        masks = sb("b_masks", [128, 2, 512], BF16)
        G = load_mod_rows(P, nc, st, D["mods"], 0, GT1, "b_G")
        KAh = Rot("KAh", [sb("b_KAh%d" % i, [96, NTOK], BF16) for i in range(2)])
        QAg = Rot("QAg", [sb("b_QAg%d" % i, [96, 8, 512], BF16) for i in range(2)])
        QBg = Rot("QBg", [sb("b_QBg%d" % i, [64, 8, 512], BF16) for i in range(2)])
        LA = 2
        PT = Rot("PT", [sb("b_PT%d" % i, [128, 512], BF16) for i in range(LA + 2)])
        Osb = Rot("Osb", [sb("b_Osb%d" % i, [65, 512]) for i in range(2)])
        rec = Rot("rec", [sb("b_rec%d" % i, [64, 512]) for i in range(2)])
        mixT = sb("b_mixT", [128, 16, 512], BF16)
        xt = Rot("bxt", [sb("b_xt%d" % i, [128, 1024]) for i in range(2)])
        ot = Rot("bot", [sb("b_ot%d" % i, [128, 1024]) for i in range(2)])

        P.dma(KBT[:], D["KBT"][:, :, :], [], ["KBT"])
        P.dma(VB[:], D["VB"].rearrange("(c p) w -> p c w", p=128), [], ["VB"])
        P.dma(VA[:], D["VA"].rearrange("(c p) w -> p c w", p=128), [], ["VA"])
        P.v(lambda e: e.memset(w_out[64:128, :, :], 0.0), [], ["w_out_z"], eng="pool")
        P.v(lambda e: e.memset(mixT[64:128, :, :], 0.0), [], ["mixT_z"], eng="pool")
        P.dma(w_out[0:64, :, :], D["even_w_out"][0].rearrange("(j p) n -> p j n", p=64), [], ["w_out"], eng="pool")
        P.v(lambda e: e.memset(sel[:], 0.0), [], ["sel"])
        P.v(lambda e: e.memset(sel[64:65, :], 1.0), ["sel"], ["sel"])
        P.dma(es[:], D["win_sink"][0:1, :].partition_broadcast(64), [], ["es"])
        P.act(es[:], es[:], AF.Exp, ["es"], ["es"])
        P.dma(masks[:], D["wmask"].rearrange("a p n -> p a n"), [], ["masks"])

        groups = [(g * 512, 512, g * 512, 0) for g in range(4)] + [(4096, 256, 2048, 1)]
        SC_A = 96.0 ** -0.5
        SC_B = 64.0 ** -0.5
        for (tok0, N, row0, var) in groups:
            qa, qak = QAg.next()
            P.dma(qa[:, :, 0:N], D["QAT"][:, :, tok0:tok0 + N], [], [qak])
            qb, qbk = QBg.next()
            P.dma(qb[:, :, 0:N], D["QBT"][:, :, tok0:tok0 + N], [], [qbk])
            chunks = list(range(34)) if var == 0 else [32, 33]
            def mla_norm(h, O, Ok, N=N):
                osb, osk = Osb.next()
                P.v(lambda e: e.tensor_copy(osb[0:65, 0:N], O[0:65, 0:N]), [Ok], [osk])
                R, Rk = nR()
                P.mm(R[0:64, 0:N], sel[0:65, 0:64], osb[0:65, 0:N], True, True, ["sel", osk], [Rk])
                rc, rck = rec.next()
                P.v(lambda e: e.reciprocal(rc[0:64, 0:N], R[0:64, 0:N]), [Rk], [rck])
                P.v(lambda e: e.tensor_tensor(mixT[0:64, h, 0:N], osb[0:64, 0:N], rc[0:64, 0:N], ALU.mult),
                    [osk, rck], [("mixT", h)])

            defer = None
            for h in range(8):
                ka, kak = KAh.next()
                P.dma(ka[:], D["KAT"][:, h, :], [], [kak])
                O, Ok = nO()
                pend = []
                for ci, kc in enumerate(chunks + [None] * LA):
                    if kc is not None:
                        S, Sk = nS()
                        P.mm(S[:, 0:N], ka[0:96, kc * 128:(kc + 1) * 128], qa[0:96, h, 0:N], True, True, [kak, qak], [Sk])
                        pt, ptk = PT.next()
                        P.act(pt[:, 0:N], S[:, 0:N], AF.Exp, [Sk], [ptk], scale=SC_A)
                        pend.append((ci, kc, pt, ptk))
                    if defer is not None and (ci == LA or kc is None):
                        mla_norm(*defer)
                        defer = None
                    if len(pend) > LA or (kc is None and pend):
                        pci, pkc, ppt, pptk = pend.pop(0)
                        P.mm(O[0:65, 0:N], VA[:, pkc, h * 65:(h + 1) * 65], ppt[:, 0:N], pci == 0, pci == len(chunks) - 1,
                             ["VA", pptk], [Ok])
                defer = (h, O, Ok)
            mla_norm(*defer)
            nb = N // 128

            def win_norm(g, b, O, Ok):
                osb, osk = Osb.next()
                P.v(lambda e: e.tensor_copy(osb[0:65, :], O[0:65, :]), [Ok], [osk])
                R, Rk = nR()
                P.mm(R[0:64, :], sel[0:65, 0:64], osb[0:65, :], True, True, ["sel", osk], [Rk])
                rc, rck = rec.next()
                for j in range(4):
                    P.v(lambda e, j=j: e.tensor_scalar(
                        rc[0:64, j * 128:(j + 1) * 128], R[0:64, j * 128:(j + 1) * 128],
                        es[0:64, g * 4 + j:g * 4 + j + 1], None, ALU.add), [Rk, "es"], [rck])
                P.v(lambda e: e.reciprocal(rc[0:64, :], rc[0:64, :]), [rck], [rck])
                P.v(lambda e: e.tensor_tensor(
                    mixT[0:64, 8 + g * 4:8 + g * 4 + 4, b * 128:(b + 1) * 128],
                    osb[0:64, :].rearrange("p (h q) -> p h q", h=4),
                    rc[0:64, :].rearrange("p (h q) -> p h q", h=4), ALU.mult),
                    [osk, rck], [("mixT", 8 + g * 4 + j) for j in range(4)])

            deferw = None
            for b in range(nb):
                tt = tok0 // 128 + b
                if var == 0:
                    cl = []
                    if tt > 0:
                        cl.append((tt - 1, 0))
                    cl.append((tt, None))
                    cl.append((tt + 1, 1))
                    cl += [(32, None), (33, None)]
                else:
                    cl = [(32, None), (33, None)]
                for g in range(2):
                    O, Ok = nO()
                    Q4 = qb[0:64, g * 4:(g + 1) * 4, b * 128:(b + 1) * 128]
                    pend = []
                    for ci, item in enumerate(cl + [None] * LA):
                        if item is not None:
                            kc, mk_ = item
                            S, Sk = nS()
                            P.mm(S[:, :].rearrange("p (h q) -> p h q", h=4), KBT[0:64, g, kc * 128:(kc + 1) * 128], Q4,
                                 True, True, ["KBT", qbk], [Sk])
                            pt, ptk = PT.next()
                            P.act(pt[:, :], S[:, :], AF.Exp, [Sk], [ptk], scale=SC_B)
                            if mk_ is not None:
                                P.v(lambda e, pt=pt, mk_=mk_: e.tensor_tensor(pt[:, :], pt[:, :], masks[:, mk_, :], ALU.mult),
                                    [ptk, "masks"], [ptk], eng="pool")
                            pend.append((ci, kc, pt, ptk))
                        if deferw is not None and (ci == min(LA, len(cl) - 1)):
                            win_norm(*deferw)
                            deferw = None
                        if len(pend) > LA or (item is None and pend):
                            pci, pkc, ppt, pptk = pend.pop(0)
                            P.mm(O[0:65, :], VB[:, pkc, g * 65:(g + 1) * 65], ppt[:, :], pci == 0, pci == len(cl) - 1,
                                 ["VB", pptk], [Ok])
                    deferw = (g, b, O, Ok)
            if deferw is not None:
                win_norm(*deferw)
                deferw = None
            mkeys = [("mixT", j) for j in range(16)]
            for b in range(nb):
                x_ap, xk = xt.next()
                P.dma(x_ap[:], D["xtok"][tok0 + b * 128:tok0 + (b + 1) * 128, :], [], [xk])
                o_ap, ok_ = ot.next()
                for dh in range(2):
                    Ob, Obk = nOP()
                    for j in range(16):
                        P.mm(Ob[:, :], mixT[0:128, j, b * 128:(b + 1) * 128], w_out[0:128, j, dh * 512:(dh + 1) * 512],
                             j == 0, j == 15, mkeys + ["w_out", "w_out_z", "mixT_z"], [Obk])
                    P.v(lambda e, o_ap=o_ap, Ob=Ob, dh=dh, var=var: e.tensor_tensor(
                        o_ap[:, dh * 512:(dh + 1) * 512], Ob[:, :], G[var][0][:, dh * 512:(dh + 1) * 512], ALU.mult),
                        [Obk, G[var][1]], [ok_])
                P.v(lambda e, o_ap=o_ap, x_ap=x_ap: e.tensor_tensor(o_ap[:], o_ap[:], x_ap[:], ALU.add),
                    [ok_, xk], [ok_], eng="pool")
                r0 = row0 + b * 128
                P.dma(D["x1"][r0:r0 + 128, :], o_ap[:], [ok_], [("x1", r0)], eng="pool")
        P.flush()


def phase_moe(nc, P, D, banks, layer, xin, xout, tiles, tag, final_norm=False):
    NTl = len(tiles)
    T = NTl * 128
    with contextlib.ExitStack() as st0:
        sb0 = lambda name, shape, dt=F32: st0.enter_context(nc.sbuf_tensor(tag + name, shape, dt))
        h2T = sb0("h2T", [128, 8, T], BF16)
        comb = sb0("comb", [128, NTl, 32])
        consts = sb0("consts", [128, 4])
        P.v(lambda e: e.memset(consts[:, 0:1], -0.5), [], ["consts"])
        neghalf = consts[:, 0:1]
        with contextlib.ExitStack() as st:
            sb = lambda name, shape, dt=F32: st.enter_context(nc.sbuf_tensor(tag + name, shape, dt))
            nA, nB = bank_rot(banks, 0, 4), bank_rot(banks, 4, 8)
            ident = sb("ident32", [128, 128])
            gbc = sb("gbc", [128, 1024])
            w_r = sb("w_r", [128, 8, 36])
            Abc = load_mod_rows(P, nc, st, D["mods"], layer, SC2, tag + "A")
            Bbc = load_mod_rows(P, nc, st, D["mods"], layer, SH2, tag + "B")
            xt = Rot("mxt", [sb("xt%d" % i, [128, 1024]) for i in range(2)])
            junk = sb("junk", [128, 1024])
            t1 = sb("t1", [128, 1024])
            h2 = sb("h2", [128, 1024])
            h2T32 = sb("h2T32", [128, 8, 128])
            small = Rot("msmall", [sb("small%d" % i, [128, 16]) for i in range(2)])
            rt = Rot("mrt", [sb("rt%d" % i, [128, 96]) for i in range(2)])
            P.dma(ident[:], D["ident32"][:, :], [], ["ident"])
            P.dma(gbc[:], D["norm_ffn_g"][layer:layer + 1, :].partition_broadcast(128), [], ["gbc"])
            P.dma(w_r[:, :, 0:4], D["moe_w_rg"][layer].rearrange("(kc p) n -> p kc n", p=128), [], [("w_r", 0)])
            P.dma(w_r[:, :, 4:36], D["moe_w_re"][layer].rearrange("(kc p) n -> p kc n", p=128), [], [("w_r", 1)])
            for v in range(2):
                a, ak = Abc[v]
                P.v(lambda e, a=a: e.scalar_tensor_tensor(a[:], a[:], 1.0, gbc[:], ALU.add, ALU.mult), [ak, "gbc"], [ak])
            for ti, (r, var) in enumerate(tiles):
                A, Ak = Abc[var]; B, Bk = Bbc[var]
                x_ap, xk = xt.next()
                P.dma(x_ap[:], D[xin][r * 128:(r + 1) * 128, :], [], [xk])
                sm, smk = small.next()
                P.act(junk[:], x_ap[:], AF.Square, [xk], ["junk", (smk, 0)], accum_out=sm[:, 0:1])
                rms_rstd(P, sm[:, 0:1], (smk, 0), sm[:, 2:3], (smk, 2), neghalf, 1024, sm[:, 1:2], (smk, 1))
                P.v(lambda e, x_ap=x_ap, sm=sm, A=A: e.scalar_tensor_tensor(
                    t1[:], x_ap[:], sm[:, 2:3], A[:], ALU.mult, ALU.mult), [xk, (smk, 2), Ak], ["t1"])
                P.v(lambda e, B=B: e.tensor_tensor(h2[:], t1[:], B[:], ALU.add), ["t1", Bk], ["h2"])
                for half in range(2):
                    bk, bkey = nA()
                    for j in range(4):
                        kc = half * 4 + j
                        P.tr(bk[:, j * 128:(j + 1) * 128], h2[:, kc * 128:(kc + 1) * 128], ident[:], ["h2", "ident"], [bkey])
                    P.act(h2T[:, half * 4:(half + 1) * 4, ti * 128:(ti + 1) * 128],
                          bk[:, :].rearrange("p (a b) -> p a b", a=4), AF.Copy, [bkey], [("h2T", ti, half)])
                    P.v(lambda e, bk=bk, half=half: e.tensor_copy(
                        h2T32[:, half * 4:(half + 1) * 4, :], bk[:, :].rearrange("p (a b) -> p a b", a=4)),
                        [bkey], [("h2T32", half)])
                lg, lgk = nB()
                for kc in range(8):
                    P.mm(lg[:, 0:36], h2T32[:, kc, :], w_r[:, kc, :], kc == 0, kc == 7,
                         [("h2T32", kc // 4), ("w_r", 0), ("w_r", 1)], [lgk])
                R, Rk = rt.next()
                P.v(lambda e, R=R, lg=lg: e.tensor_copy(R[:, 0:36], lg[:, 0:36]), [lgk], [(Rk, "lg")])
                s_ = lambda c: sm[:, c:c + 1]
                P.v(lambda e, R=R, sm=sm: e.reduce_max(sm[:, 3:4], R[:, 0:4], AX.X), [(Rk, "lg")], [(smk, 3)])
                P.v(lambda e, R=R, sm=sm: e.tensor_scalar(R[:, 36:40], R[:, 0:4], sm[:, 3:4], None, ALU.is_equal),
                    [(Rk, "lg"), (smk, 3)], [(Rk, "goh")])
                P.v(lambda e, sm=sm: e.tensor_scalar(sm[:, 4:5], sm[:, 3:4], -1.0, None, ALU.mult), [(smk, 3)], [(smk, 4)])
                P.act(R[:, 80:84], R[:, 0:4], AF.Exp, [(Rk, "lg"), (smk, 4)], [(Rk, "gexp"), (smk, 5)],
                      bias=sm[:, 4:5], accum_out=sm[:, 5:6])
                P.v(lambda e, sm=sm: e.reciprocal(sm[:, 6:7], sm[:, 5:6]), [(smk, 5)], [(smk, 6)])
                P.v(lambda e, R=R: e.tensor_scalar(R[:, 40:48], R[:, 4:12], R[:, 36:37], None, ALU.mult),
                    [(Rk, "lg"), (Rk, "goh")], [(Rk, "ein")])
                for g in range(1, 4):
                    P.v(lambda e, R=R, g=g: e.scalar_tensor_tensor(
                        R[:, 40:48], R[:, 4 + 8 * g:12 + 8 * g], R[:, 36 + g:37 + g], R[:, 40:48], ALU.mult, ALU.add),
                        [(Rk, "lg"), (Rk, "goh"), (Rk, "ein")], [(Rk, "ein")])
                P.v(lambda e, R=R, sm=sm: e.reduce_max(sm[:, 7:8], R[:, 40:48], AX.X), [(Rk, "ein")], [(smk, 7)])
                P.v(lambda e, R=R, sm=sm: e.tensor_scalar(R[:, 48:56], R[:, 40:48], sm[:, 7:8], None, ALU.is_equal),
                    [(Rk, "ein"), (smk, 7)], [(Rk, "oh1")])
                P.v(lambda e, R=R: e.scalar_tensor_tensor(R[:, 56:64], R[:, 48:56], -1e30, R[:, 40:48], ALU.mult, ALU.add),
                    [(Rk, "oh1"), (Rk, "ein")], [(Rk, "e2")])
                P.v(lambda e, R=R, sm=sm: e.reduce_max(sm[:, 8:9], R[:, 56:64], AX.X), [(Rk, "e2")], [(smk, 8)])
                P.v(lambda e, R=R, sm=sm: e.tensor_scalar(R[:, 64:72], R[:, 56:64], sm[:, 8:9], None, ALU.is_equal),
                    [(Rk, "e2"), (smk, 8)], [(Rk, "oh2")])
                P.v(lambda e, sm=sm: e.tensor_tensor(sm[:, 9:10], sm[:, 8:9], sm[:, 7:8], ALU.subtract),
                    [(smk, 7), (smk, 8)], [(smk, 9)])
                P.act(sm[:, 10:11], sm[:, 9:10], AF.Exp, [(smk, 9)], [(smk, 10)])
                P.v(lambda e, sm=sm: e.tensor_scalar(sm[:, 11:12], sm[:, 10:11], 1.0, None, ALU.add), [(smk, 10)], [(smk, 11)])
                P.v(lambda e, sm=sm: e.reciprocal(sm[:, 11:12], sm[:, 11:12]), [(smk, 11)], [(smk, 11)])
                P.v(lambda e, sm=sm: e.tensor_tensor(sm[:, 12:13], sm[:, 11:12], sm[:, 6:7], ALU.mult),
                    [(smk, 11), (smk, 6)], [(smk, 12)])
                P.v(lambda e, sm=sm: e.tensor_tensor(sm[:, 13:14], sm[:, 12:13], sm[:, 10:11], ALU.mult),
                    [(smk, 12), (smk, 10)], [(smk, 13)])
                P.v(lambda e, R=R, sm=sm: e.tensor_scalar(R[:, 72:80], R[:, 48:56], sm[:, 12:13], None, ALU.mult),
                    [(Rk, "oh1"), (smk, 12)], [(Rk, "loc")])
                P.v(lambda e, R=R, sm=sm: e.scalar_tensor_tensor(
                    R[:, 72:80], R[:, 64:72], sm[:, 13:14], R[:, 72:80], ALU.mult, ALU.add),
                    [(Rk, "oh2"), (smk, 13), (Rk, "loc")], [(Rk, "loc")])
                for g in range(4):
                    P.v(lambda e, R=R, g=g, ti=ti: e.tensor_scalar(
                        comb[:, ti, g * 8:(g + 1) * 8], R[:, 72:80], R[:, 36 + g:37 + g], None, ALU.mult),
                        [(Rk, "loc"), (Rk, "goh")], [("comb", ti)])
            P.flush()
        with contextlib.ExitStack() as st:
            sb = lambda name, shape, dt=F32: st.enter_context(nc.sbuf_tensor(tag + name, shape, dt))
            nGU, nY = bank_rot(banks, 0, 4), bank_rot(banks, 4, 8)
            yacc = sb("yacc", [128, NTl, 1024])
            wg = Rot("wg", [sb("wg%d" % i, [128, 8, 512], BF16) for i in range(2)])
            wu = Rot("wu", [sb("wu%d" % i, [128, 8, 512], BF16) for i in range(2)])
            wd = Rot("wd", [sb("wd%d" % i, [128, 4, 1024], BF16) for i in range(2)])
            hg = Rot("hg", [sb("hg%d" % i, [128, 4, 512], BF16) for i in range(2)])
            sg = Rot("sg", [sb("sg%d" % i, [128, 512]) for i in range(2)])
            G = load_mod_rows(P, nc, st, D["mods"], layer, GT2, tag + "G")
            xt = Rot("mxt2", [sb("xt2_%d" % i, [128, 1024]) for i in range(2)])
            groups = []
            t0 = 0
            while t0 < NTl:
                n = min(4, NTl - t0)
                groups.append((t0, n))
                t0 += n
            def emit_gu(ex, t0, n, g_ap, gk, u_ap, uk):
                N = n * 128
                ts = slice(t0 * 128, t0 * 128 + N)
                hgt, hgk = hg.next()
                for fc in range(4):
                    Gp, Gk = nGU(); Up, Uk = nGU()
                    for kc in range(8):
                        P.mm(Gp[:, 0:N], g_ap[:, kc, fc * 128:(fc + 1) * 128], h2T[:, kc, ts], kc == 0, kc == 7,
                             [gk, "h2T"], [Gk])
                    for kc in range(8):
                        P.mm(Up[:, 0:N], u_ap[:, kc, fc * 128:(fc + 1) * 128], h2T[:, kc, ts], kc == 0, kc == 7,
                             [uk, "h2T"], [Uk])
                    s_ap, sk = sg.next()
                    P.act(s_ap[:, 0:N], Gp[:, 0:N], AF.Silu, [Gk], [sk])
                    P.v(lambda e, hgt=hgt, Up=Up, s_ap=s_ap, fc=fc, N=N: e.tensor_tensor(
                        hgt[:, fc, 0:N], Up[:, 0:N], s_ap[:, 0:N], ALU.mult), [Uk, sk], [(hgk, fc)])
                return hgt, hgk

            def emit_down(ex, t0, n, hgt, hgk, d_ap, dk):
                for b in range(n):
                    ti = t0 + b
                    for dh in range(2):
                        Yp, Yk = nY()
                        for fc in range(4):
                            P.mm(Yp[:, :], hgt[:, fc, b * 128:(b + 1) * 128], d_ap[:, fc, dh * 512:(dh + 1) * 512],
                                 fc == 0, fc == 3, [(hgk, f) for f in range(4)] + [dk], [Yk])
                        ysl = yacc[:, ti, dh * 512:(dh + 1) * 512]
                        if ex == 0:
                            P.v(lambda e, ysl=ysl, Yp=Yp, ti=ti, ex=ex: e.tensor_scalar(
                                ysl, Yp[:, :], comb[:, ti, ex:ex + 1], None, ALU.mult), [Yk], [("yacc", ti, dh)])
                        else:
                            P.v(lambda e, ysl=ysl, Yp=Yp, ti=ti, ex=ex: e.scalar_tensor_tensor(
                                ysl, Yp[:, :], comb[:, ti, ex:ex + 1], ysl, ALU.mult, ALU.add),
                                [Yk, ("yacc", ti, dh)], [("yacc", ti, dh)])

            pend = None
            for ex in range(32):
                g_ap, gk = wg.next(); u_ap, uk = wu.next(); d_ap, dk = wd.next()
                P.dma(g_ap[:], D["moe_w_gate%d" % layer][ex].rearrange("(kc p) f -> p kc f", p=128), [], [gk], eng="pool")
                P.dma(u_ap[:], D["moe_w_up%d" % layer][ex].rearrange("(kc p) f -> p kc f", p=128), [], [uk], eng="pool")
                P.dma(d_ap[:], D["moe_w_down%d" % layer][ex].rearrange("(kc p) f -> p kc f", p=128), [], [dk], eng="pool")
                for (t0, n) in groups:
                    hgt, hgk = emit_gu(ex, t0, n, g_ap, gk, u_ap, uk)
                    if pend is not None:
                        emit_down(*pend)
                    pend = (ex, t0, n, hgt, hgk, d_ap, dk)
            emit_down(*pend)
            if final_norm:
                fg = sb("fg", [128, 1024])
                P.dma(fg[:], D["final_norm_g"][0:1, :].partition_broadcast(128), [], ["fg"])
                junk = sb("junk3", [128, 1024])
                small = Rot("fsmall", [sb("fsmall%d" % i, [128, 4]) for i in range(2)])
            for ti, (r, var) in enumerate(tiles):
                x_ap, xk = xt.next()
                P.dma(x_ap[:], D[xin][r * 128:(r + 1) * 128, :], [], [xk])
                ysl = yacc[:, ti, :]
                yk = [("yacc", ti, 0), ("yacc", ti, 1)]
                P.v(lambda e, ysl=ysl, var=var: e.tensor_tensor(ysl, ysl, G[var][0][:], ALU.mult), yk + [G[var][1]], yk)
                P.v(lambda e, ysl=ysl, x_ap=x_ap: e.tensor_tensor(ysl, ysl, x_ap[:], ALU.add), yk + [xk], yk, eng="pool")
                if final_norm:
                    sm, smk = small.next()
                    P.act(junk[:], ysl, AF.Square, yk, ["junk3", (smk, 0)], accum_out=sm[:, 0:1])
                    rms_rstd(P, sm[:, 0:1], (smk, 0), sm[:, 2:3], (smk, 2), neghalf, 1024, sm[:, 1:2], (smk, 1))
                    P.v(lambda e, ysl=ysl, sm=sm: e.scalar_tensor_tensor(
                        ysl, ysl, sm[:, 2:3], fg[:], ALU.mult, ALU.mult), yk + [(smk, 2), "fg"], yk)
                P.dma(D[xout][r * 128:(r + 1) * 128, :], ysl, yk, [(xout, r)])
            P.flush()


GELU_C = 0.7978845608028654


def l1_tiles():
    return [(i, 0) for i in range(16)] + [(16, 1), (17, 1)]


def phase_l1a(nc, P, D, banks):
    with contextlib.ExitStack() as st:
        sb = lambda name, shape, dt=F32: st.enter_context(nc.sbuf_tensor("c_" + name, shape, dt))
        nP = bank_rot(banks, 0, 8)
        ident = sb("ident", [128, 128], BF16)
        ident32 = sb("ident32", [128, 128])
        consts = sb("consts", [128, 4])
        gbc = sb("gbc", [128, 1024])
        w_in = sb("w_in", [128, 8, 2592], BF16)
        Abc = load_mod_rows(P, nc, st, D["mods"], 1, SC1, "c_A")
        Bbc = load_mod_rows(P, nc, st, D["mods"], 1, SH1, "c_B")
        W2 = sb("W2", [32, 512])
        bg = sb("bg", [128, 512])
        gm = sb("gm", [128, 3, 128])
        wsT = sb("wsT", [128, 4, 128], BF16)
        bsT = sb("bsT", [128, 4])
        lng = sb("lng", [128, 512]); lnb = sb("lnb", [128, 512])
        xt = Rot("cxt", [sb("xt%d" % i, [128, 1024]) for i in range(3)])
        junk = sb("junk", [128, 1024])
        t1r = Rot("t1", [sb("t1_%d" % i, [128, 1024]) for i in range(2)])
        hbr = Rot("hb", [sb("hb_%d" % i, [128, 1024], BF16) for i in range(2)])
        hTr = Rot("hT", [sb("hT_%d" % i, [128, 8, 128], BF16) for i in range(2)])
        small = Rot("csmall", [sb("small%d" % i, [128, 16]) for i in range(3)])
        qkr = Rot("qk", [sb("qk%d" % i, [128, 512]) for i in range(2)])
        g32r = Rot("g32", [sb("g32_%d" % i, [128, 32]) for i in range(2)])
        uvr = Rot("uv", [sb("uv%d" % i, [128, 2, 512]) for i in range(2)])
        g32T = sb("g32T", [32, 128])
        zs = sb("zs", [128, 512]); la = sb("la", [128, 512])
        bsb = sb("bsb", [128, 2, 256])
        ex = sb("ex", [128, 256]); tmp = sb("tmp", [128, 256])
        gl = sb("gl", [128, 6, 256], BF16)
        glT = Rot("glT", [sb("glT%d" % i, [128, 8, 128], BF16) for i in range(2)])
        dec = Rot("dec", [sb("dec%d" % i, [128, 4]) for i in range(2)])
        vb = Rot("cvb", [sb("vb%d" % i, [128, 512], BF16) for i in range(2)])
        rsb = Rot("rsb", [sb("rsb%d" % i, [128, 512], BF16) for i in range(2)])
        ge = sb("ge", [128, 2, 512])
        gt_ = sb("gt_", [128, 512]); gt2_ = sb("gt2_", [128, 512])
        vgn = sb("vgn", [128, 512], BF16)
        dlb = Rot("dlb", [sb("dlb%d" % i, [128, 512], BF16) for i in range(2)])

        P.dma(ident[:], D["identbf"][:, :], [], ["ident"])
        P.dma(ident32[:], D["ident32"][:, :], [], ["ident32"])
        P.v(lambda e: e.memset(consts[:, 0:1], -0.5), [], ["consts"])
        neghalf = consts[:, 0:1]
        P.dma(gbc[:], D["norm_mix_g"][1:2, :].partition_broadcast(128), [], ["gbc"])
        for c in range(3):
            lo, hi = c * 864, (c + 1) * 864
            P.dma(w_in[:, :, lo:hi], D["odd_w_in"][0].rearrange("(kc p) n -> p kc n", p=128)[:, :, lo:hi], [],
                  [("w_in", c)], eng="pool")
        wkeys = [("w_in", c) for c in range(3)]
        P.v(lambda e: e.memset(W2[:], 0.0), [], ["W2"])
        P.dma(W2[0:16, 0:256], D["gla_w_g2"][0, 0], ["W2"], ["W2"])
        P.dma(W2[16:32, 256:512], D["gla_w_g2"][0, 1], ["W2"], ["W2"])
        P.dma(bg[:], D["gla_b_g"][0:1].rearrange("o a n -> o (a n)").partition_broadcast(128), [], ["bg"])
        P.dma(gm[:], D["gmask"][0:3].rearrange("a p n -> p a n"), [], ["gm"])
        P.dma(wsT[:], D["sg_w_sT"].rearrange("g s t -> s g t"), [], ["wsT"], eng="pool")
        P.dma(bsT[:], D["sg_b_sT"][:, :], [], ["bsT"])
        P.dma(lng[:], D["sg_ln_g"][0:1, :].partition_broadcast(128), [], ["lng"])
        P.dma(lnb[:], D["sg_ln_b"][0:1, :].partition_broadcast(128), [], ["lnb"])
        for v in range(2):
            a, ak = Abc[v]
            P.v(lambda e, a=a: e.scalar_tensor_tensor(a[:], a[:], 1.0, gbc[:], ALU.add, ALU.mult), [ak, "gbc"], [ak])

        def gelu(src, src_key, dst, dst_key):
            P.v(lambda e: e.tensor_tensor(gt2_[:], src, src, ALU.mult), [src_key], ["gt2_"])
            P.v(lambda e: e.tensor_scalar(gt2_[:], gt2_[:], 0.044715, 1.0, ALU.mult, ALU.add), ["gt2_"], ["gt2_"])
            P.v(lambda e: e.tensor_tensor(gt2_[:], gt2_[:], src, ALU.mult), ["gt2_", src_key], ["gt2_"], eng="pool")
            P.act(gt2_[:], gt2_[:], AF.Sigmoid, ["gt2_"], ["gt2_"], scale=2.0 * GELU_C)
            P.v(lambda e: e.tensor_tensor(dst, src, gt2_[:], ALU.mult), [src_key, "gt2_"], [dst_key])

        def stage_a(r, var):
            lat = var == 0
            A, Ak = Abc[var]; B, Bk = Bbc[var]
            rs = slice(r * 128, (r + 1) * 128)
            x_ap, xk = xt.next()
            P.dma(x_ap[:], D["x2"][rs, :], [], [xk])
            sm, smk = small.next()
            P.act(junk[:], x_ap[:], AF.Square, [xk], ["junk", (smk, 0)], accum_out=sm[:, 0:1])
            rms_rstd(P, sm[:, 0:1], (smk, 0), sm[:, 2:3], (smk, 2), neghalf, 1024, sm[:, 1:2], (smk, 1))
            t1, t1k = t1r.next(); hb, hbk = hbr.next(); hT, hTk = hTr.next()
            P.v(lambda e, x_ap=x_ap, sm=sm, A=A, t1=t1: e.scalar_tensor_tensor(
                t1[:], x_ap[:], sm[:, 2:3], A[:], ALU.mult, ALU.mult), [xk, (smk, 2), Ak], [t1k])
            P.v(lambda e, B=B, t1=t1, hb=hb: e.tensor_tensor(hb[:], t1[:], B[:], ALU.add), [t1k, Bk], [hbk])
            bk, bkey = nP()
            bkb = bk[:].bitcast(BF16)
            for kc in range(8):
                P.tr(bkb[:, kc * 128:(kc + 1) * 128], hb[:, kc * 128:(kc + 1) * 128], ident[:], [hbk, "ident"], [bkey])
            P.act(hT[:].rearrange("p a b -> p (a b)"), bkb, AF.Copy, [bkey], [hTk])

            def proj(c0, c1):
                bk, bkey = nP()
                for kc in range(8):
                    P.mm(bk[:, 0:c1 - c0], hT[:, kc, :], w_in[:, kc, c0:c1], kc == 0, kc == 7, [hTk] + wkeys, [bkey])
                return bk, bkey
            zqk, zqkk = proj(0, 512)
            qk, qkk = qkr.next()
            P.act(qk[:], zqk[:, :], AF.Copy, [zqkk], [qkk])
            zv, zvk = proj(512, 1024)
            v_ap, vk = vb.next()
            P.act(v_ap[:], zv[:, :], AF.Copy, [zvk], [vk])
            P.dma(D["g_v"][rs, :], v_ap[:], [vk], [("g_v", r)], eng="act")
            zg, zgk = proj(1024, 1056)
            g32, g32k = g32r.next()
            P.v(lambda e, zg=zg, g32=g32: e.tensor_copy(g32[:], zg[:, 0:32]), [zgk], [g32k])
            uv, uvk = uvr.next()
            if lat:
                zr, zrk = proj(1056, 1568)
                r_ap, rk = rsb.next()
                P.act(r_ap[:], zr[:, :], AF.Silu, [zrk], [rk])
                P.dma(D["rsilu"][rs, :], r_ap[:], [rk], [("rsilu", r)], eng="act")
                zu, zuk = proj(1568, 2080)
                P.act(uv[:, 0, :], zu[:, :], AF.Copy, [zuk], [(uvk, 0)])
                zvg, zvgk = proj(2080, 2592)
                P.v(lambda e, uv=uv, zvg=zvg: e.tensor_copy(uv[:, 1, :], zvg[:, :]), [zvgk], [(uvk, 1)])
            return dict(r=r, lat=lat, rs=rs, sm=sm, smk=smk, qk=qk, qkk=qkk, g32=g32, g32k=g32k, uv=uv, uvk=uvk)

        def stage_b(c):
            r, lat, rs, sm, smk, qk, qkk, g32, g32k, uv, uvk = (c[k] for k in (
                "r", "lat", "rs", "sm", "smk", "qk", "qkk", "g32", "g32k", "uv", "uvk"))
            bk, bkey = nP()
            P.tr(bk[0:32, 0:128], g32[:, :], ident32[:], [g32k, "ident32"], [bkey])
            P.v(lambda e, bk=bk: e.tensor_copy(g32T[:], bk[0:32, 0:128]), [bkey], ["g32T"])
            zz, zzk = nP()
            P.mm(zz[:, :], g32T[0:32, :], W2[0:32, :], True, True, ["g32T", "W2"], [zzk])
            P.v(lambda e, zz=zz: e.tensor_tensor(zs[:], zz[:, :], bg[:], ALU.add), [zzk, "bg"], ["zs"])
            P.act(zs[:], zs[:], AF.Exp, ["zs"], ["zs"], scale=-1.0)
            P.act(zs[:], zs[:], AF.Ln, ["zs"], ["zs"], bias=1.0)
            P.v(lambda e: e.tensor_scalar(la[:], zs[:], -1.0 / 16.0, None, ALU.mult), ["zs"], ["la"])
            cA, cAk = nP()
            P.mm(cA[:, 0:256], gm[:, 0, :], la[:, 0:256], True, True, ["gm", "la"], [cAk])
            P.mm(cA[:, 256:512], gm[:, 1, :], la[:, 256:512], True, True, ["gm", "la"], [cAk])
            cL, cLk = nP()
            P.mm(cL[:, :], gm[:, 2, :], la[:, :], True, True, ["gm", "la"], [cLk])
            P.act(bsb[:].rearrange("p a b -> p (a b)"), cA[:, :], AF.Copy, [cAk], ["bsb"])
            cT, cTk = nP()
            for j in range(4):
                P.mm(cT[:, j:j + 1], la[:, j * 128:(j + 1) * 128], gm[:, 2, 0:1], True, True, ["gm", "la"], [cTk])
            d_ap, dk = dec.next()
            P.act(d_ap[:], cT[:, 0:4], AF.Exp, [cTk], [dk])
            P.dma(D["g_dec"][:, r, :], d_ap[:], [dk], [("g_dec", r)], eng="act")
            for p in range(2):
                if p == 1 and not lat:
                    continue
                bp = bsb[:, p, :]
                if lat:
                    P.act(ex[:], bp, AF.Exp, ["bsb"], ["ex"])
                    P.v(lambda e, p=p: e.scalar_tensor_tensor(gl[:, 2 * p, :], qk[:, 0:256], 0.125, ex[:], ALU.mult, ALU.mult),
                        [qkk, "ex"], [("gl", 2 * p)])
                P.act(ex[:], bp, AF.Exp, ["bsb"], ["ex"], scale=-1.0)
                P.v(lambda e, p=p: e.tensor_tensor(gl[:, 2 * p + 1, :], qk[:, 256:512], ex[:], ALU.mult),
                    [qkk, "ex"], [("gl", 2 * p + 1)])
                P.v(lambda e, p=p, bp=bp, cL=cL: e.tensor_tensor(tmp[:], cL[:, p * 256:(p + 1) * 256], bp, ALU.subtract),
                    [cLk, "bsb"], ["tmp"])
                P.act(ex[:], tmp[:], AF.Exp, ["tmp"], ["ex"])
                P.v(lambda e, p=p: e.tensor_tensor(gl[:, 4 + p, :], qk[:, 256:512], ex[:], ALU.mult),
                    [qkk, "ex"], [("gl", 4 + p)])
                P.dma(D["g_kd"][p, rs, :], gl[:, 4 + p, :], [("gl", 4 + p)], [("g_kd", p, r)], eng="act")
            bk, bkey = nP()
            bkb = bk[:].bitcast(BF16)
            arrs = [0, 1, 2, 3] if lat else [1]
            for a_ in arrs:
                for j in range(2):
                    P.tr(bkb[:, (a_ * 2 + j) * 128:(a_ * 2 + j + 1) * 128], gl[:, a_, j * 128:(j + 1) * 128], ident[:],
                         [("gl", a_), "ident"], [bkey])
            gT, gTk = glT.next()
            if lat:
                P.act(gT[:].rearrange("p a b -> p (a b)"), bkb, AF.Copy, [bkey], [gTk])
                P.dma(D["g_T"][:, r, :, :], gT[:], [gTk], [("g_T", r)], eng="act")
            else:
                P.act(gT[:, 2:4, :].rearrange("p a b -> p (a b)"), bkb[:, 256:512], AF.Copy, [bkey], [gTk])
                P.dma(D["g_T"][:, r, 2:4, :], gT[:, 2:4, :], [gTk], [("g_T", r)], eng="act")
            if not lat:
                return
            gelu(uv[:, 0, :], (uvk, 0), ge[:, 0, :], ("ge", 0))
            gelu(uv[:, 1, :], (uvk, 1), ge[:, 1, :], ("ge", 1))
            P.v(lambda e, sm=sm: e.reduce_sum(sm[:, 8:9], ge[:, 1, :], AX.X), [("ge", 1)], [(smk, 8)])
            P.act(junk[:, 0:512], ge[:, 1, :], AF.Square, [("ge", 1)], ["junk", (smk, 9)], accum_out=sm[:, 9:10])
            P.v(lambda e, sm=sm: e.tensor_scalar(sm[:, 10:11], sm[:, 8:9], 1.0 / 512, None, ALU.mult), [(smk, 8)], [(smk, 10)])
            P.v(lambda e, sm=sm: e.tensor_tensor(sm[:, 11:12], sm[:, 10:11], sm[:, 10:11], ALU.mult), [(smk, 10)], [(smk, 11)])
            P.v(lambda e, sm=sm: e.scalar_tensor_tensor(sm[:, 12:13], sm[:, 9:10], 1.0 / 512, sm[:, 11:12], ALU.mult, ALU.subtract),
                [(smk, 9), (smk, 11)], [(smk, 12)])
            P.v(lambda e, sm=sm: e.tensor_scalar(sm[:, 12:13], sm[:, 12:13], EPS, None, ALU.add), [(smk, 12)], [(smk, 12)])
            P.v(lambda e, sm=sm: e.tensor_tensor(sm[:, 13:14], sm[:, 12:13], neghalf, ALU.pow), [(smk, 12), "consts"],
                [(smk, 13)], eng="pool")
            P.v(lambda e, sm=sm: e.tensor_scalar(gt_[:], ge[:, 1, :], sm[:, 10:11], sm[:, 13:14], ALU.subtract, ALU.mult),
                [("ge", 1), (smk, 10), (smk, 13)], ["gt_"])
            P.v(lambda e: e.tensor_tensor(gt_[:], gt_[:], lng[:], ALU.mult), ["gt_", "lng"], ["gt_"])
            P.v(lambda e: e.tensor_tensor(vgn[:], gt_[:], lnb[:], ALU.add), ["gt_", "lnb"], ["vgn"])
            sp_, spk = nP()
            for gi in range(4):
                P.mm(sp_[:, gi * 128:(gi + 1) * 128], wsT[:, gi, :], vgn[:, gi * 128:(gi + 1) * 128], True, True,
                     ["wsT", "vgn"], [spk])
            dl_ap, dlk = dlb.next()
            for gi in range(4):
                P.v(lambda e, gi=gi, sp_=sp_, dl_ap=dl_ap: e.scalar_tensor_tensor(
                    dl_ap[:, gi * 128:(gi + 1) * 128], sp_[:, gi * 128:(gi + 1) * 128], bsT[:, gi:gi + 1],
                    ge[:, 0, gi * 128:(gi + 1) * 128], ALU.add, ALU.mult), [spk, "bsT", ("ge", 0)], [dlk])
            P.dma(D["dl"][rs, :], dl_ap[:], [dlk], [("dl", r)], eng="act")

        pend = None
        for (r, var) in l1_tiles():
            cur = stage_a(r, var)
            if pend is not None:
                stage_b(pend)
            pend = cur
        stage_b(pend)
        P.flush()


def _gla_pass(nc, P, D, banks, st, sb, pidx, order, S, Sb, on_out):
    nAT, nO, nU = bank_rot(banks, 0, 2), bank_rot(banks, 2, 4), bank_rot(banks, 4, 6)
    gT = Rot("gTl", [sb("gTl%d" % i, [128, 4, 128], BF16) for i in range(3)])
    kd = Rot("kdl", [sb("kdl%d" % i, [128, 256], BF16) for i in range(3)])
    vv = Rot("vl", [sb("vl%d" % i, [128, 512], BF16) for i in range(3)])
    dc = Rot("dcl", [sb("dcl%d" % i, [128, 2]) for i in range(3)])
    ATm = Rot("ATm", [sb("ATm%d" % i, [128, 128], BF16) for i in range(2)])
    mask = sb("gmask_sb", [128, 128])
    P.dma(mask[:], D["gmask"][pidx], [], ["gmask"])
    for r in order:
        lat = r < 16
        rs = slice(r * 128, (r + 1) * 128)
        g_ap, gk = gT.next()
        if lat:
            P.dma(g_ap[:], D["g_T"][:, r, 4 * pidx:4 * pidx + 4, :], [], [gk])
        else:
            P.dma(g_ap[:, 2:4, :], D["g_T"][:, r, 4 * pidx + 2:4 * pidx + 4, :], [], [gk])
        k_ap, kk = kd.next()
        P.dma(k_ap[:], D["g_kd"][pidx, rs, :], [], [kk])
        v_ap, vk = vv.next()
        P.dma(v_ap[:], D["g_v"][rs, :], [], [vk])
        d_ap, dk = dc.next()
        P.dma(d_ap[:], D["g_dec"][:, r, 2 * pidx:2 * pidx + 2], [], [dk])
        if lat:
            O, Ok = nO()
            for h in range(4):
                j, po = h // 2, (h % 2) * 64
                AT, ATk = nAT()
                P.mm(AT[:, 0:128], g_ap[po:po + 64, 2 + j, :], g_ap[po:po + 64, j, :], True, True, [gk], [ATk])
                am, amk = ATm.next()
                P.v(lambda e, am=am, AT=AT: e.tensor_tensor(am[:], AT[:, 0:128], mask[:], ALU.mult), [ATk, "gmask"], [amk])
                P.mm(O[:, h * 128:(h + 1) * 128], am[:], v_ap[:, h * 128:(h + 1) * 128], True, False, [amk, vk], [Ok])
                P.mm(O[:, h * 128:(h + 1) * 128], g_ap[po:po + 64, j, :], Sb[po:po + 64, j, :], False, True,
                     [gk, ("Sb", j, h % 2)], [Ok])
            on_out(r, O, Ok)
        for j in range(2):
            for hh in range(2):
                po = hh * 64
                U, Uk = nU()
                P.mm(U[:, 0:128], k_ap[:, j * 128:(j + 1) * 128], v_ap[:, (2 * j + hh) * 128:(2 * j + hh + 1) * 128],
                     True, True, [kk, vk], [Uk])
                P.v(lambda e, U=U, j=j, po=po, d_ap=d_ap: e.scalar_tensor_tensor(
                    S[po:po + 64, j, :], S[po:po + 64, j, :], d_ap[po:po + 64, j:j + 1], U[po:po + 64, 0:128],
                    ALU.mult, ALU.add), [Uk, dk, ("S", j, hh)], [("S", j, hh)])
                P.act(Sb[po:po + 64, j, :], S[po:po + 64, j, :], AF.Copy, [("S", j, hh)], [("Sb", j, hh)])


def phase_l1b_a(nc, P, D, banks):
    with contextlib.ExitStack() as st:
        sb = lambda name, shape, dt=F32: st.enter_context(nc.sbuf_tensor("d_" + name, shape, dt))
        S = sb("S", [128, 2, 128]); Sb = sb("Sb", [128, 2, 128], BF16)
        oa = Rot("oa", [sb("oa%d" % i, [128, 512]) for i in range(2)])
        P.v(lambda e: e.memset(S[:], 0.0), [], [("S", j, hh) for j in range(2) for hh in range(2)])
        P.v(lambda e: e.memset(Sb[:], 0.0), [], [("Sb", j, hh) for j in range(2) for hh in range(2)])

        def on_out(r, O, Ok):
            o_ap, ok_ = oa.next()
            P.act(o_ap[:], O[:, :], AF.Copy, [Ok], [ok_])
            P.dma(D["OA"][r * 128:(r + 1) * 128, :], o_ap[:], [ok_], [("OA", r)], eng="act")
        _gla_pass(nc, P, D, banks, st, sb, 0, [16, 17] + list(range(16)), S, Sb, on_out)
        P.dma(D["cc_in"].rearrange("(a p) n -> p a n", p=128), S[:], [("S", j, hh) for j in range(2) for hh in range(2)],
              ["cc_in"])
        P.cc(lambda e: e.collective_compute("AllGather", ALU.bypass, replica_groups=[[0, 1], [2, 3], [4, 5], [6, 7]],
                                            ins=[D["cc_in"].opt()], outs=[D["cc_out"].opt()]), ["cc_in"], ["cc_out"])
        P.flush()


def phase_l1b_b(nc, P, D, banks):
    with contextlib.ExitStack() as st:
        sb = lambda name, shape, dt=F32: st.enter_context(nc.sbuf_tensor("e_" + name, shape, dt))
        nT, nW = bank_rot(banks, 0, 2), bank_rot(banks, 2, 8)
        S = sb("S", [128, 2, 128]); Sb = sb("Sb", [128, 2, 128], BF16)
        skeys = [("S", j, hh) for j in range(2) for hh in range(2)]
        both = sb("both", [128, 4, 128])
        sel = sb("sel", [128, 2])
        P.dma(both[:], D["cc_out"].rearrange("(a p) n -> p a n", p=128), [], ["both"])
        P.dma(sel[:], D["sel"][:, :], [], ["sel"])
        P.v(lambda e: e.tensor_scalar(S[:].rearrange("p a b -> p (a b)"), both[:, 0:2, :].rearrange("p a b -> p (a b)"),
                                      sel[:, 0:1], None, ALU.mult), ["both", "sel"], skeys)
        P.v(lambda e: e.scalar_tensor_tensor(S[:].rearrange("p a b -> p (a b)"),
                                             both[:, 2:4, :].rearrange("p a b -> p (a b)"), sel[:, 1:2],
                                             S[:].rearrange("p a b -> p (a b)"), ALU.mult, ALU.add),
            ["both", "sel"] + skeys, skeys)
        P.act(Sb[:], S[:], AF.Copy, skeys, [("Sb", j, hh) for j in range(2) for hh in range(2)])
        ident = sb("ident", [128, 128], BF16)
        P.dma(ident[:], D["identbf"][:, :], [], ["ident"])
        consts = sb("consts", [128, 4])
        P.v(lambda e: e.memset(consts[:], -0.5), [], ["consts"])
        gng = sb("gng", [128, 512])
        P.dma(gng[:], D["gla_norm_g"][0:1, :].partition_broadcast(128), [], ["gng"])
        w_out = sb("w_out", [128, 8, 1024], BF16)
        P.dma(w_out[:], D["odd_w_out"][0].rearrange("(kc p) n -> p kc n", p=128), [], ["w_out"], eng="pool")
        G = load_mod_rows(P, nc, st, D["mods"], 1, GT1, "e_G")
        oa = Rot("eoa", [sb("oa%d" % i, [128, 512]) for i in range(2)])
        rsl = Rot("ersl", [sb("rsl%d" % i, [128, 512], BF16) for i in range(2)])
        gr = sb("gr", [128, 512])
        junk = sb("junk", [128, 128])
        small = Rot("esmall", [sb("small%d" % i, [128, 8]) for i in range(2)])
        mix_all = sb("mix_all", [128, 16, 1024], BF16)
        mixTr = Rot("emixT", [sb("mixT%d" % i, [128, 8, 128], BF16) for i in range(2)])
        xt = Rot("ext", [sb("xt%d" % i, [128, 1024]) for i in range(2)])
        ot = Rot("eot", [sb("ot%d" % i, [128, 1024]) for i in range(2)])

        def on_out(r, O, Ok):
            rs = slice(r * 128, (r + 1) * 128)
            o_ap, ok_ = oa.next()
            P.dma(o_ap[:], D["OA"][rs, :], [], [ok_])
            P.v(lambda e, o_ap=o_ap, O=O: e.tensor_tensor(o_ap[:], O[:, :], o_ap[:], ALU.add), [Ok, ok_], [ok_])
            r_ap, rk = rsl.next()
            P.dma(r_ap[:], D["rsilu"][rs, :], [], [rk])
            m_ap, mk = mix_all[:, r, :], ("mix", r)
            P.dma(m_ap[:, 512:1024], D["dl"][rs, :], [], [(mk, 1)])
            sm, smk = small.next()
            for h in range(4):
                P.act(junk[:], o_ap[:, h * 128:(h + 1) * 128], AF.Square, [ok_], ["junk", (smk, h)], accum_out=sm[:, h:h + 1])
            hk = [(smk, h) for h in range(4)]
            P.v(lambda e, sm=sm: e.tensor_scalar(sm[:, 0:4], sm[:, 0:4], 1.0 / 128, EPS, ALU.mult, ALU.add), hk, hk)
            P.v(lambda e, sm=sm: e.tensor_tensor(sm[:, 4:8], sm[:, 0:4], consts[:, 0:4], ALU.pow), hk + ["consts"],
                [(smk, 4)], eng="pool")
            P.v(lambda e, r_ap=r_ap: e.tensor_tensor(gr[:], gng[:], r_ap[:], ALU.mult), ["gng", rk], ["gr"])
            for h in range(4):
                P.v(lambda e, h=h, o_ap=o_ap, sm=sm, m_ap=m_ap: e.scalar_tensor_tensor(
                    m_ap[:, h * 128:(h + 1) * 128], o_ap[:, h * 128:(h + 1) * 128], sm[:, 4 + h:5 + h],
                    gr[:, h * 128:(h + 1) * 128], ALU.mult, ALU.mult), [ok_, (smk, 4), "gr"], [(mk, 0)])

        def out_proj(r):
            rs = slice(r * 128, (r + 1) * 128)
            m_ap, mk = mix_all[:, r, :], ("mix", r)
            bk, bkey = nT()
            bkb = bk[:].bitcast(BF16)
            for kc in range(8):
                P.tr(bkb[:, kc * 128:(kc + 1) * 128], m_ap[:, kc * 128:(kc + 1) * 128], ident[:],
                     [(mk, 0), (mk, 1), "ident"], [bkey])
            mT, mTk = mixTr.next()
            P.act(mT[:].rearrange("p a b -> p (a b)"), bkb, AF.Copy, [bkey], [mTk])
            x_ap, xk = xt.next()
            P.dma(x_ap[:], D["x2"][rs, :], [], [xk])
            t_ap, tk = ot.next()
            for dh in range(2):
                W, Wk = nW()
                for kc in range(8):
                    P.mm(W[:, :], mT[:, kc, :], w_out[:, kc, dh * 512:(dh + 1) * 512], kc == 0, kc == 7,
                         [mTk, "w_out"], [Wk])
                P.v(lambda e, t_ap=t_ap, W=W, dh=dh: e.tensor_tensor(
                    t_ap[:, dh * 512:(dh + 1) * 512], W[:, :], G[0][0][:, dh * 512:(dh + 1) * 512], ALU.mult),
                    [Wk, G[0][1]], [tk])
            P.v(lambda e, t_ap=t_ap, x_ap=x_ap: e.tensor_tensor(t_ap[:], t_ap[:], x_ap[:], ALU.add), [tk, xk], [tk],
                eng="pool")
            P.dma(D["x3"][rs, :], t_ap[:], [tk], [("x3", r)], eng="pool")
        _gla_pass(nc, P, D, banks, st, sb, 1, list(range(15, -1, -1)), S, Sb, on_out)
        for r in range(15, -1, -1):
            out_proj(r)
        P.flush()


I32 = mybir.dt.int32
STILE = 256
NB = STILE // 128


def n_stiles(T):
    return (2 * T + 32 * (STILE - 1) + STILE - 1) // STILE


def phase_moe_sparse(nc, P, D, banks, layer, xin, xout, tiles, tag, final_norm=False):
    NTl = len(tiles)
    T = NTl * 128
    NST = n_stiles(T)
    NSLOT = NST * STILE
    Xs, Ys = D["Xs"], D["Ys"]
    wgv = D["moe_w_gate%d" % layer].rearrange("e (p a kc) f -> (e p a) (kc f)", p=128, a=2)
    wuv = D["moe_w_up%d" % layer].rearrange("e (p a kc) f -> (e p a) (kc f)", p=128, a=2)
    wdv = D["moe_w_down%d" % layer].rearrange("e f d -> (e f) d")
    with contextlib.ExitStack() as st0:
        sb0 = lambda name, shape, dt=F32: st0.enter_context(nc.sbuf_tensor(tag + name, shape, dt))
        consts = sb0("consts", [128, 4])
        P.v(lambda e: e.memset(consts[:, 0:1], -0.5), [], ["consts"])
        neghalf = consts[:, 0:1]
        idxA = sb0("idxA", [128, NTl], I32); idxB = sb0("idxB", [128, NTl], I32)
        wAB = sb0("wAB", [128, 2, NTl])
        widx = sb0("widx", [128, NST, 6], I32)
        with contextlib.ExitStack() as st:
            sb = lambda name, shape, dt=F32: st.enter_context(nc.sbuf_tensor(tag + name, shape, dt))
            nA, nB = bank_rot(banks, 0, 4), bank_rot(banks, 4, 7)
            cntb, cntk = banks[7], ("ps", 7)
            ident = sb("ident32", [128, 128])
            gbc = sb("gbc", [128, 1024])
            w_r = sb("w_r", [128, 8, 36])
            gm = sb("gm", [128, 2, 128])
            eidrow = sb("eidrow", [128, 32])
            pc2 = sb("pc2", [128, 6])
            Abc = load_mod_rows(P, nc, st, D["mods"], layer, SC2, tag + "A")
            Bbc = load_mod_rows(P, nc, st, D["mods"], layer, SH2, tag + "B")
            xt = Rot("mxt", [sb("xt%d" % i, [128, 1024]) for i in range(3)])
            junk = sb("junk", [128, 1024])
            t1r = Rot("t1", [sb("t1_%d" % i, [128, 1024]) for i in range(2)])
            h2r = Rot("h2", [sb("h2_%d" % i, [128, 1024]) for i in range(2)])
            h2b = sb("h2b", [128, NTl, 1024], BF16)
            h2Tr = Rot("h2T32", [sb("h2T32_%d" % i, [128, 8, 128]) for i in range(2)])
            small = Rot("msmall", [sb("small%d" % i, [128, 16]) for i in range(2)])
            rt = Rot("mrt", [sb("rt%d" % i, [128, 96]) for i in range(2)])
            selA = sb("selA", [128, NTl, 32]); selB = sb("selB", [128, NTl, 32]); selm = sb("selm", [128, NTl, 32])
            LG = sb("LG", [128, NTl, 36])
            rs_ = sb("rs_", [128, 8, NTl])
            goh = sb("goh", [128, NTl, 4]); gex = sb("gex", [128, NTl, 4])
            etmp = sb("etmp", [128, NTl, 4, 8])
            ein = sb("ein", [128, NTl, 8]); oh1 = sb("oh1", [128, NTl, 8]); e2 = sb("e2", [128, NTl, 8]); oh2 = sb("oh2", [128, NTl, 8])
            zt = sb("zt", [128, 8, 1024], BF16)
            P.v(lambda e: e.memset(zt[:], 0.0), [], ["zt"], eng="pool")
            zkeys = []
            for z0 in range(0, NSLOT, 1024):
                nrow = min(1024, NSLOT - z0)
                P.dma(Xs[z0:z0 + nrow, :].rearrange("(a p) c -> p a c", p=128), zt[:, 0:nrow // 128, :], ["zt"], [("Xsz", z0)])
                zkeys.append(("Xsz", z0))
            P.dma(ident[:], D["ident32"][:, :], [], ["ident"])
            P.dma(gbc[:], D["norm_ffn_g"][layer:layer + 1, :].partition_broadcast(128), [], ["gbc"])
            P.dma(w_r[:, :, 0:4], D["moe_w_rg"][layer].rearrange("(kc p) n -> p kc n", p=128), [], [("w_r", 0)])
            P.dma(w_r[:, :, 4:36], D["moe_w_re"][layer].rearrange("(kc p) n -> p kc n", p=128), [], [("w_r", 1)])
            P.dma(gm[:], D["gmask"][3:5].rearrange("a p n -> p a n"), [], ["gm"])
            P.dma(eidrow[:], D["eidrow"][:, :], [], ["eidrow"])
            P.dma(pc2[:], D["pc2"][:, :], [], ["pc2"])
            for v in range(2):
                a, ak = Abc[v]
                P.v(lambda e, a=a: e.scalar_tensor_tensor(a[:], a[:], 1.0, gbc[:], ALU.add, ALU.mult), [ak, "gbc"], [ak])
            for ti, (r, var) in enumerate(tiles):
                A, Ak = Abc[var]; B, Bk = Bbc[var]
                x_ap, xk = xt.next()
                P.dma(x_ap[:], D[xin][r * 128:(r + 1) * 128, :], [], [xk])
                sm, smk = small.next()
                P.act(junk[:], x_ap[:], AF.Square, [xk], ["junk", (smk, 0)], accum_out=sm[:, 0:1])
                rms_rstd(P, sm[:, 0:1], (smk, 0), sm[:, 2:3], (smk, 2), neghalf, 1024, sm[:, 1:2], (smk, 1))
                t1, t1k = t1r.next(); h2, h2k = h2r.next(); h2T32, hTk = h2Tr.next()
                P.v(lambda e, x_ap=x_ap, sm=sm, A=A, t1=t1: e.scalar_tensor_tensor(
                    t1[:], x_ap[:], sm[:, 2:3], A[:], ALU.mult, ALU.mult), [xk, (smk, 2), Ak], [t1k])
                P.v(lambda e, B=B, t1=t1, h2=h2: e.tensor_tensor(h2[:], t1[:], B[:], ALU.add), [t1k, Bk], [h2k])
                P.act(h2b[:, ti, :], h2[:], AF.Copy, [h2k], [("h2b", ti)])
                for half in range(2):
                    bk, bkey = nA()
                    for j in range(4):
                        kc = half * 4 + j
                        P.tr(bk[:, j * 128:(j + 1) * 128], h2[:, kc * 128:(kc + 1) * 128], ident[:], [h2k, "ident"], [bkey])
                    P.v(lambda e, bk=bk, half=half, h2T32=h2T32: e.tensor_copy(
                        h2T32[:, half * 4:(half + 1) * 4, :], bk[:, :].rearrange("p (a b) -> p a b", a=4)),
                        [bkey], [(hTk, half)])
                lg, lgk = nB()
                for kc in range(8):
                    P.mm(lg[:, 0:36], h2T32[:, kc, :], w_r[:, kc, :], kc == 0, kc == 7,
                         [(hTk, kc // 4), ("w_r", 0), ("w_r", 1)], [lgk])
                P.v(lambda e, lg=lg, ti=ti: e.tensor_copy(LG[:, ti, :], lg[:, 0:36]), [lgk], [("LG", ti)])
            lgk_all = [("LG", ti) for ti in range(NTl)]
            NT = NTl
            G = LG[:, :, 0:4]
            E4 = LG[:, :, 4:36].rearrange("p t (g e) -> p t g e", g=4)
            bc = lambda ap2, n: ap2.unsqueeze(2).to_broadcast([128, NT, n])
            P.v(lambda e: e.tensor_reduce(rs_[:, 0, :], G, AX.X, ALU.max), lgk_all, ["gmax"])
            P.v(lambda e: e.tensor_tensor(goh[:], G, bc(rs_[:, 0, :], 4), ALU.is_equal), lgk_all + ["gmax"], ["goh"])
            P.v(lambda e: e.tensor_tensor(gex[:], G, bc(rs_[:, 0, :], 4), ALU.subtract), lgk_all + ["gmax"], ["gex"])
            P.act(gex[:], gex[:], AF.Exp, ["gex"], ["gex"])
            P.v(lambda e: e.tensor_reduce(rs_[:, 1, :], gex[:], AX.X, ALU.add), ["gex"], ["gsum"])
            P.v(lambda e: e.reciprocal(rs_[:, 2, :], rs_[:, 1, :]), ["gsum"], ["pmax"])
            P.v(lambda e: e.tensor_tensor(etmp[:], E4, goh[:].unsqueeze(3).to_broadcast([128, NT, 4, 8]), ALU.mult),
                lgk_all + ["goh"], ["etmp"])
            P.v(lambda e: e.tensor_tensor(ein[:], etmp[:, :, 0, :], etmp[:, :, 1, :], ALU.add), ["etmp"], ["ein"])
            P.v(lambda e: e.tensor_tensor(ein[:], ein[:], etmp[:, :, 2, :], ALU.add), ["etmp", "ein"], ["ein"])
            P.v(lambda e: e.tensor_tensor(ein[:], ein[:], etmp[:, :, 3, :], ALU.add), ["etmp", "ein"], ["ein"])
            P.v(lambda e: e.tensor_reduce(rs_[:, 3, :], ein[:], AX.X, ALU.max), ["ein"], ["m1"])
            P.v(lambda e: e.tensor_tensor(oh1[:], ein[:], bc(rs_[:, 3, :], 8), ALU.is_equal), ["ein", "m1"], ["oh1"])
            P.v(lambda e: e.scalar_tensor_tensor(e2[:], oh1[:], -1e30, ein[:], ALU.mult, ALU.add), ["oh1", "ein"], ["e2"])
            P.v(lambda e: e.tensor_reduce(rs_[:, 4, :], e2[:], AX.X, ALU.max), ["e2"], ["m2"])
            P.v(lambda e: e.tensor_tensor(oh2[:], e2[:], bc(rs_[:, 4, :], 8), ALU.is_equal), ["e2", "m2"], ["oh2"])
            P.v(lambda e: e.tensor_tensor(rs_[:, 5, :], rs_[:, 4, :], rs_[:, 3, :], ALU.subtract), ["m1", "m2"], ["dd"])
            P.act(rs_[:, 6, :], rs_[:, 5, :], AF.Exp, ["dd"], ["ed"])
            P.v(lambda e: e.tensor_scalar(rs_[:, 7, :], rs_[:, 6, :], 1.0, None, ALU.add), ["ed"], ["w1"])
            P.v(lambda e: e.reciprocal(rs_[:, 7, :], rs_[:, 7, :]), ["w1"], ["w1"])
            P.v(lambda e: e.tensor_tensor(wAB[:, 0, :], rs_[:, 7, :], rs_[:, 2, :], ALU.mult), ["w1", "pmax"], ["wA"])
            P.v(lambda e: e.tensor_tensor(wAB[:, 1, :], wAB[:, 0, :], rs_[:, 6, :], ALU.mult), ["wA", "ed"], ["wB"])
            s4 = lambda ap3: ap3[:].rearrange("p t (g e) -> p t g e", g=4)
            P.v(lambda e: e.tensor_tensor(s4(selA), oh1[:].unsqueeze(2).to_broadcast([128, NT, 4, 8]),
                                          goh[:].unsqueeze(3).to_broadcast([128, NT, 4, 8]), ALU.mult), ["oh1", "goh"], ["selA"])
            P.v(lambda e: e.tensor_tensor(s4(selB), oh2[:].unsqueeze(2).to_broadcast([128, NT, 4, 8]),
                                          goh[:].unsqueeze(3).to_broadcast([128, NT, 4, 8]), ALU.mult), ["oh2", "goh"], ["selB"])
            P.v(lambda e: e.tensor_tensor(selm[:], selA[:], selB[:], ALU.add), ["selA", "selB"], ["selm"])
            for ti in range(NTl):
                P.mm(cntb[:, 0:32], gm[:, 1, :], selm[:, ti, :], ti == 0, ti == NTl - 1, ["gm", "selm"], [cntk])
            seg = sb("seg", [128, 8, 32])
            segT = sb("segT", [32, 128])
            ecol = sb("ecol", [128, NST])
            sti = sb("sti", [128, 2, NST, 32])
            stc = sb("stc", [128, 64])
            P.dma(stc[:], D["stile_c"][:, :], [], ["stc"])
            widxf = sb("widxf", [128, NST, 6])
            slotf = sb("slotf", [128, 2, NTl])
            P.v(lambda e: e.tensor_copy(seg[:, 0, :], cntb[:, 0:32]), [cntk], ["cnt"])
            P.v(lambda e: e.tensor_scalar(seg[:, 1, :], seg[:, 0, :], 0.0, None, ALU.is_gt), ["cnt"], ["nst"])
            for k in range(1, (T + STILE - 1) // STILE + 1):
                P.v(lambda e, k=k: e.scalar_tensor_tensor(seg[:, 1, :], seg[:, 0, :], float(STILE * k), seg[:, 1, :],
                                                          ALU.is_gt, ALU.add), ["cnt", "nst"], ["nst"])
            P.v(lambda e: e.tensor_scalar(seg[:, 2, :], seg[:, 1, :], float(STILE), None, ALU.mult), ["nst"], ["pc"])
            bk, bkey = nA()
            P.tr(bk[0:32, 0:128], seg[:, 2, :], ident[:], ["pc", "ident"], [bkey])
            P.v(lambda e, bk=bk: e.tensor_copy(segT[:], bk[0:32, 0:128]), [bkey], ["segT"])
            bk2, bkey2 = nA()
            P.mm(bk2[:, 0:32], segT[0:32, :], gm[0:32, 0, 0:32], True, True, ["segT", "gm"], [bkey2])
            P.v(lambda e, bk2=bk2: e.tensor_copy(seg[:, 3, :], bk2[:, 0:32]), [bkey2], ["start"])
            P.v(lambda e: e.tensor_tensor(seg[:, 4, :], seg[:, 3, :], seg[:, 2, :], ALU.add), ["start", "pc"], ["end"])
            P.v(lambda e: e.tensor_copy(seg[:, 5, :], seg[:, 3, :]), ["start"], ["base"])
            bci = lambda ap2: ap2.unsqueeze(1).to_broadcast([128, NST, 32])
            cI = stc[:, 0:NST].unsqueeze(2).to_broadcast([128, NST, 32])
            P.v(lambda e: e.tensor_tensor(sti[:, 0, :, :], bci(seg[:, 3, :]), cI, ALU.is_le), ["start", "stc"], ["sti0"])
            P.v(lambda e: e.tensor_tensor(sti[:, 1, :, :], bci(seg[:, 4, :]), cI, ALU.is_gt), ["end", "stc"], ["sti1"])
            P.v(lambda e: e.tensor_tensor(sti[:, 0, :, :], sti[:, 0, :, :], sti[:, 1, :, :], ALU.mult), ["sti0", "sti1"], ["sti0"])
            P.v(lambda e: e.tensor_tensor(sti[:, 0, :, :], sti[:, 0, :, :], bci(eidrow[:]), ALU.mult), ["sti0", "eidrow"], ["sti0"])
            P.v(lambda e: e.tensor_reduce(ecol[:, :], sti[:, 0, :, :], AX.X, ALU.add), ["sti0"], ["ecol"])
            ek = ["ecol"]
            for a_ in range(2):
                P.v(lambda e, a_=a_: e.tensor_scalar(widxf[:, :, a_], ecol[:, :], 256.0, pc2[:, a_:a_ + 1], ALU.mult, ALU.add),
                    ek + ["pc2"], [("widxf", a_)])
            for fc in range(4):
                P.v(lambda e, fc=fc: e.tensor_scalar(widxf[:, :, 2 + fc], ecol[:, :], 512.0, pc2[:, 2 + fc:3 + fc], ALU.mult, ALU.add),
                    ek + ["pc2"], [("widxf", 2 + fc)])
            P.v(lambda e: e.tensor_copy(widx[:].rearrange("p a b -> p (a b)"), widxf[:].rearrange("p a b -> p (a b)")),
                [("widxf", j) for j in range(6)], ["widx"])
            for ti in range(NTl):
                wi, wik = nB()
                P.mm(wi[:, 0:32], gm[:, 0, :], selm[:, ti, :], True, True, ["gm", "selm"], [wik])
                P.mm(wi[:, 32:64], gm[:, 1, :], selm[:, ti, :], True, True, ["gm", "selm"], [wik])
                P.v(lambda e, wi=wi: e.tensor_tensor(seg[:, 6, :], wi[:, 0:32], seg[:, 5, :], ALU.add), [wik, "base"], ["segtmp"])
                P.v(lambda e, ti=ti: e.scalar_tensor_tensor(seg[:, 7, :], seg[:, 6, :], 1.0, selA[:, ti, :], ALU.mult, ALU.mult,
                                                            accum_out=slotf[:, 0, ti:ti + 1]), ["segtmp", "selA"],
                    ["segtmp2", ("slotA", ti)])
                P.v(lambda e, ti=ti: e.scalar_tensor_tensor(seg[:, 7, :], seg[:, 6, :], 1.0, selB[:, ti, :], ALU.mult, ALU.mult,
                                                            accum_out=slotf[:, 1, ti:ti + 1]), ["segtmp", "selB"],
                    ["segtmp2", ("slotB", ti)])
                P.v(lambda e, wi=wi: e.tensor_tensor(seg[:, 5, :], wi[:, 32:64], seg[:, 5, :], ALU.add), [wik, "base"], ["base"])
            P.v(lambda e: e.tensor_copy(idxA[:], slotf[:, 0, :]), [("slotA", ti) for ti in range(NTl)], ["idxA"])
            P.v(lambda e: e.tensor_copy(idxB[:], slotf[:, 1, :]), [("slotB", ti) for ti in range(NTl)], ["idxB"])
            for ti in range(NTl):
                for (ix, ixk) in ((idxA, "idxA"), (idxB, "idxB")):
                    P.op("pool", lambda e, ix=ix, ti=ti: e.indirect_dma_start(
                        out=Xs[0:NSLOT, :], out_offset=bass.IndirectOffsetOnAxis(ap=ix[:, ti:ti + 1], axis=0),
                        in_=h2b[:, ti, :], in_offset=None, bounds_check=None),
                        [("h2b", ti), ixk] + zkeys, [("Xs", ti, ixk)], dma=True, kind="dma")
            P.flush()
        with contextlib.ExitStack() as st:
            sb = lambda name, shape, dt=F32: st.enter_context(nc.sbuf_tensor(tag + name, shape, dt))
            nT, nGU, nY = bank_rot(banks, 0, 2), bank_rot(banks, 2, 6), bank_rot(banks, 6, 8)
            ident = sb("identb", [128, 128], BF16)
            P.dma(ident[:], D["identbf"][:, :], [], ["ident"])
            wg = Rot("wg", [sb("wg%d" % i, [128, 8, 512], BF16) for i in range(2)])
            wu = Rot("wu", [sb("wu%d" % i, [128, 8, 512], BF16) for i in range(2)])
            wd = Rot("wd", [sb("wd%d" % i, [128, 4, 1024], BF16) for i in range(2)])
            xr = Rot("xr", [sb("xr%d" % i, [128, 1024], BF16) for i in range(4)])
            XT = Rot("XT", [sb("XT%d" % i, [128, 8, STILE], BF16) for i in range(2)])
            hg = Rot("hg", [sb("hg%d" % i, [128, 4, STILE], BF16) for i in range(2)])
            sg = Rot("sg", [sb("sg%d" % i, [128, STILE]) for i in range(2)])
            yb = Rot("yb", [sb("yb%d" % i, [128, 1024]) for i in range(3)])

            def gather(dst2d, src, col, i, key):
                P.op("pool", lambda e: e.indirect_dma_start(
                    out=dst2d, out_offset=None, in_=src,
                    in_offset=bass.IndirectOffsetOnAxis(ap=widx[:, i, col:col + 1], axis=0),
                    bounds_check=None), [], [key], dma=True, kind="dma")

            def emit_gu(i):
                g_ap, gk = wg.next(); u_ap, uk = wu.next(); d_ap, dk = wd.next()
                for a_ in range(2):
                    gather(g_ap[:, 4 * a_:4 * a_ + 4, :].rearrange("p a f -> p (a f)"), wgv, a_, i, (gk, a_))
                    gather(u_ap[:, 4 * a_:4 * a_ + 4, :].rearrange("p a f -> p (a f)"), wuv, a_, i, (uk, a_))
                for fc in range(4):
                    gather(d_ap[:, fc, :], wdv, 2 + fc, i, (dk, fc))
                xT, xTk = XT.next()
                for b in range(NB):
                    x_ap, xk = xr.next()
                    P.dma(x_ap[:], Xs[i * STILE + b * 128:i * STILE + (b + 1) * 128, :], [], [xk])
                    bk, bkey = nT()
                    bkb = bk[:].bitcast(BF16)
                    xv = x_ap[:].rearrange("p (m kc) -> p kc m", kc=8)
                    for kc in range(8):
                        P.tr(bkb[:, kc * 128:(kc + 1) * 128], xv[:, kc, :], ident[:], [xk, "ident"], [bkey])
                    P.act(xT[:, :, b * 128:(b + 1) * 128], bkb.rearrange("p (a b) -> p a b", a=8), AF.Copy, [bkey], [(xTk, b)])
                xkeys = [(xTk, b) for b in range(NB)]
                hgt, hgk = hg.next()
                for fc in range(4):
                    Gp, Gk = nGU(); Up, Uk = nGU()
                    for kc in range(8):
                        P.mm(Gp[:, 0:STILE], g_ap[:, kc, fc * 128:(fc + 1) * 128], xT[:, kc, :], kc == 0, kc == 7,
                             [(gk, kc // 4)] + xkeys, [Gk])
                    for kc in range(8):
                        P.mm(Up[:, 0:STILE], u_ap[:, kc, fc * 128:(fc + 1) * 128], xT[:, kc, :], kc == 0, kc == 7,
                             [(uk, kc // 4)] + xkeys, [Uk])
                    s_ap, sk = sg.next()
                    P.act(s_ap[:, :], Gp[:, 0:STILE], AF.Silu, [Gk], [sk])
                    P.v(lambda e, hgt=hgt, Up=Up, s_ap=s_ap, fc=fc: e.tensor_tensor(
                        hgt[:, fc, :], Up[:, 0:STILE], s_ap[:, :], ALU.mult), [Uk, sk], [(hgk, fc)])
                return (i, hgt, hgk, d_ap, dk)

            def emit_down(i, hgt, hgk, d_ap, dk):
                for b in range(NB):
                    y_ap, yk = yb.next()
                    for dh in range(2):
                        Yp, Yk = nY()
                        for fc in range(4):
                            P.mm(Yp[:, :], hgt[:, fc, b * 128:(b + 1) * 128], d_ap[:, fc, dh * 512:(dh + 1) * 512],
                                 fc == 0, fc == 3, [(hgk, f) for f in range(4)] + [(dk, fc)], [Yk])
                        if dh == 0:
                            P.act(y_ap[:, 0:512], Yp[:, :], AF.Copy, [Yk], [(yk, 0)])
                        else:
                            P.v(lambda e, y_ap=y_ap, Yp=Yp: e.tensor_copy(y_ap[:, 512:1024], Yp[:, :]), [Yk], [(yk, 1)])
                    r0 = i * STILE + b * 128
                    P.dma(Ys[r0:r0 + 128, :], y_ap[:], [(yk, 0), (yk, 1)], [("Ys", r0)], eng="act")

            pend = None
            for i in range(NST):
                cur = emit_gu(i)
                if pend is not None:
                    emit_down(*pend)
                pend = cur
            emit_down(*pend)
            P.flush()
        with contextlib.ExitStack() as st:
            sb = lambda name, shape, dt=F32: st.enter_context(nc.sbuf_tensor(tag + name, shape, dt))
            G = load_mod_rows(P, nc, st, D["mods"], layer, GT2, tag + "G")
            xt = Rot("mxt2", [sb("xt2_%d" % i, [128, 1024]) for i in range(4)])
            ya = Rot("ya", [sb("ya%d" % i, [128, 1024]) for i in range(4)])
            ybb = Rot("ybb", [sb("ybb%d" % i, [128, 1024]) for i in range(4)])
            if final_norm:
                fg = sb("fg", [128, 1024])
                P.dma(fg[:], D["final_norm_g"][0:1, :].partition_broadcast(128), [], ["fg"])
                junk = sb("junk3", [128, 1024])
                small = Rot("fsmall", [sb("fsmall%d" % i, [128, 4]) for i in range(2)])
            for ti, (r, var) in enumerate(tiles):
                x_ap, xk = xt.next()
                P.dma(x_ap[:], D[xin][r * 128:(r + 1) * 128, :], [], [xk])
                a_ap, ak = ya.next(); b_ap, bk_ = ybb.next()
                for (dst, dkey, ix) in ((a_ap, ak, idxA), (b_ap, bk_, idxB)):
                    P.op("pool", lambda e, dst=dst, ix=ix, ti=ti: e.indirect_dma_start(
                        out=dst[:, :], out_offset=None, in_=Ys[0:NSLOT, :],
                        in_offset=bass.IndirectOffsetOnAxis(ap=ix[:, ti:ti + 1], axis=0),
                        bounds_check=None), [], [dkey], dma=True, kind="dma")
                P.v(lambda e, a_ap=a_ap, ti=ti: e.tensor_scalar(a_ap[:], a_ap[:], wAB[:, 0, ti:ti + 1], None, ALU.mult), [ak], [ak])
                P.v(lambda e, a_ap=a_ap, b_ap=b_ap, ti=ti: e.scalar_tensor_tensor(
                    a_ap[:], b_ap[:], wAB[:, 1, ti:ti + 1], a_ap[:], ALU.mult, ALU.add), [ak, bk_], [ak])
                P.v(lambda e, a_ap=a_ap, var=var: e.tensor_tensor(a_ap[:], a_ap[:], G[var][0][:], ALU.mult), [ak, G[var][1]], [ak])
                P.v(lambda e, a_ap=a_ap, x_ap=x_ap: e.tensor_tensor(a_ap[:], a_ap[:], x_ap[:], ALU.add), [ak, xk], [ak])
                if final_norm:
                    sm, smk = small.next()
                    P.act(junk[:], a_ap[:], AF.Square, [ak], ["junk3", (smk, 0)], accum_out=sm[:, 0:1])
                    rms_rstd(P, sm[:, 0:1], (smk, 0), sm[:, 2:3], (smk, 2), neghalf, 1024, sm[:, 1:2], (smk, 1))
                    P.v(lambda e, a_ap=a_ap, sm=sm: e.scalar_tensor_tensor(
                        a_ap[:], a_ap[:], sm[:, 2:3], fg[:], ALU.mult, ALU.mult), [ak, (smk, 2), "fg"], [ak])
                P.dma(D[xout][r * 128:(r + 1) * 128, :], a_ap[:], [ak], [(xout, r)], eng="act")
            P.flush()

import numpy as np
import ml_dtypes

BF = ml_dtypes.bfloat16
GRID_W = 64

W_SMALL = {
    "ada_w": [2, 1024, 6144], "ada_b": [2, 6144], "norm_mix_g": [2, 1024], "norm_ffn_g": [2, 1024],
    "even_w_in": [1, 1024, 1184], "mla_q_norm_g": [1, 256], "mla_w_uq": [1, 256, 768], "mla_kv_norm_g": [1, 128],
    "mla_w_ukv": [1, 128, 1024], "win_sink": [1, 8], "even_w_out": [1, 1024, 1024],
    "odd_w_in": [1, 1024, 2592], "gla_w_g2": [1, 2, 16, 256], "gla_b_g": [1, 2, 256], "gla_norm_g": [1, 512],
    "sg_ln_g": [1, 512], "sg_ln_b": [1, 512], "odd_w_out": [1, 1024, 1024],
    "moe_w_rg": [2, 1024, 4], "moe_w_re": [2, 1024, 32], "final_norm_g": [1, 1024],
}
W_MOE = {"moe_w_gate": [32, 1024, 512], "moe_w_up": [32, 1024, 512], "moe_w_down": [32, 512, 1024]}
CONSTS = {"wmask": ([2, 128, 512], BF16), "ident32": ([128, 128], F32), "identbf": ([128, 128], BF16),
          "gmask": ([5, 128, 128], F32), "eidrow": ([128, 32], F32), "pc2": ([128, 6], F32), "stile_c": ([128, 64], F32)}
PERCORE = {"xtok": [NTOK, 1024], "cvec": [2, 1024], "cA": [NTOK, 256], "sA": [NTOK, 256], "cB": [NTOK, 512],
           "sB": [NTOK, 512], "sg_w_sT": [4, 128, 128], "sg_b_sT": [128, 4], "sel": [128, 2]}
SCRATCH = {"mods": ([2, 2, 6144], F32), "QAT": ([96, 8, NTOK], BF16), "KAT": ([96, 8, NTOK], BF16),
           "VA": ([NTOK, 520], BF16), "QBT": ([64, 8, NTOK], BF16), "KBT": ([64, 2, NTOK], BF16),
           "VB": ([NTOK, 130], BF16), "x1": ([NOWN, 1024], F32), "x2": ([NOWN, 1024], F32),
           "g_T": ([128, 18, 8, 128], BF16), "g_kd": ([2, NOWN, 256], BF16), "g_v": ([NOWN, 512], BF16),
           "g_dec": ([128, 18, 4], F32), "rsilu": ([2048, 512], BF16), "dl": ([2048, 512], BF16),
           "OA": ([2048, 512], F32), "cc_in": ([256, 128], F32), "cc_out": ([512, 128], F32),
           "x3": ([2048, 1024], F32), "out": ([2048, 1024], F32),
           "Xs": ([n_stiles(NOWN) * STILE, 1024], BF16), "Ys": ([n_stiles(NOWN) * STILE, 1024], F32)}
HANDOFF = ["mods", "x2", "g_T", "g_kd", "g_v", "g_dec", "rsilu", "dl", "OA"]


def rope_tables(pos, dim, nheads):
    pos = np.asarray(pos)
    half = dim // 2
    inv = np.power(np.float32(10000.0), -np.arange(0, half, 2, dtype=np.float32) / np.float32(half)).astype(np.float32)
    row = (pos // GRID_W).astype(np.float32)
    col = (pos % GRID_W).astype(np.float32)
    ar = row[:, None] * inv[None, :]
    ac = col[:, None] * inv[None, :]
    ang = np.concatenate([ar, ar, ac, ac], axis=-1).astype(np.float32)
    cos = np.cos(ang).astype(np.float32)
    sin = np.sin(ang).astype(np.float32)
    blk = dim // 4
    sign = np.concatenate([-np.ones(blk), np.ones(blk), -np.ones(blk), np.ones(blk)]).astype(np.float32)
    ssin = sin * sign[None, :]
    no = pos < 0
    cos[no] = 1.0
    ssin[no] = 0.0
    return np.tile(cos, (1, nheads)), np.tile(ssin, (1, nheads))


_CONST = {}


def const_inputs():
    if not _CONST:
        j = np.arange(128)[:, None]
        i = np.arange(128)[None, :]
        m0 = np.tile((j >= i).astype(np.float32), (1, 4))
        m1 = np.tile((j <= i).astype(np.float32), (1, 4))
        _CONST["wmask"] = np.stack([m0, m1]).astype(BF)
        _CONST["ident32"] = np.eye(128, dtype=np.float32)
        _CONST["identbf"] = np.eye(128, dtype=np.float32).astype(BF)
        one = np.ones((128, 128), bool)
        _CONST["gmask"] = np.stack([(j <= i), (j >= i), one, (j < i), one]).astype(np.float32)
        _CONST["eidrow"] = np.tile(np.arange(32, dtype=np.float32)[None, :], (128, 1))
        p = np.arange(128, dtype=np.float32)
        _CONST["stile_c"] = np.tile((np.arange(64, dtype=np.float32) * STILE)[None, :], (128, 1))
        _CONST["pc2"] = np.stack([2 * p, 2 * p + 1, p, 128 + p, 256 + p, 384 + p], 1).astype(np.float32)
    return _CONST


def local_order(hf):
    own = np.arange(hf * 2048, (hf + 1) * 2048)
    oth = np.arange((1 - hf) * 2048, (2 - hf) * 2048)
    cidx = np.arange(256)
    if hf == 1:
        own, oth, cidx = own[::-1], oth[::-1], cidx[::-1]
    return own, oth, cidx


def weights_for(core, inp, layers=(0, 1)):
    hf = core % 2
    m = {}
    for k, shp in W_SMALL.items():
        m[k] = np.ascontiguousarray(np.asarray(inp[k]).reshape(shp))
    for l in layers:
        for k in W_MOE:
            m["%s%d" % (k, l)] = np.asarray(inp[k][l])
    ws = np.asarray(inp["sg_w_s"][0])
    bs = np.asarray(inp["sg_b_s"][0])
    if hf == 1:
        m["gla_w_g2"] = np.ascontiguousarray(m["gla_w_g2"][:, ::-1])
        m["gla_b_g"] = np.ascontiguousarray(m["gla_b_g"][:, ::-1])
        w = m["odd_w_in"].copy()
        w[:, :, 1024:1040] = m["odd_w_in"][:, :, 1040:1056]
        w[:, :, 1040:1056] = m["odd_w_in"][:, :, 1024:1040]
        m["odd_w_in"] = w
        ws = ws[:, ::-1, ::-1]
        bs = bs[:, ::-1]
    m["sg_w_sT"] = np.ascontiguousarray(ws.transpose(0, 2, 1))
    m["sg_b_sT"] = np.ascontiguousarray(bs.T)
    return m


def core_inputs(core, inp):
    b, hf = core // 2, core % 2
    own, oth, cidx = local_order(hf)
    pos = np.concatenate([own, oth, -np.ones(256, dtype=np.int64)])
    xtok = np.concatenate([inp["x"][b][own], inp["x"][b][oth], inp["ctx"][b][cidx]], 0)
    cA, sA = rope_tables(pos, 32, 8)
    cB, sB = rope_tables(pos, 64, 8)
    m = {"xtok": np.ascontiguousarray(xtok), "cvec": np.stack([inp["c"][b], inp["c_ctx"]]).astype(np.float32),
         "cA": cA, "sA": sA, "cB": cB, "sB": sB}
    sel = np.zeros((128, 2), np.float32)
    sel[:, 1 - hf] = 1.0
    m["sel"] = sel
    m.update(const_inputs())
    return m


def declare(nc, ext_in, ext_out, moe_layers=(0, 1)):
    D = {}
    dr = lambda n, s, dt=F32, k="ExternalInput": nc.dram_tensor(n, s, dt, kind=k).ap()
    for k, shp in PERCORE.items():
        D[k] = dr(k, shp)
    for k, (shp, dt) in CONSTS.items():
        D[k] = dr(k, shp, dt)
    for k, shp in W_SMALL.items():
        D[k] = dr(k, shp)
    for l in moe_layers:
        for k, shp in W_MOE.items():
            D["%s%d" % (k, l)] = dr("%s%d" % (k, l), shp)
    for k, (shp, dt) in SCRATCH.items():
        kind = "ExternalOutput" if k in ext_out else ("ExternalInput" if k in ext_in else "Internal")
        D[k] = dr(k, shp, dt, kind)
    return D


MOE0_TILES = [(i, 0) for i in range(16)] + [(16, 1), (17, 1)]
MOE1_TILES = [(i, 0) for i in range(16)]


def build_fused(extra_out=()):
    nc = bass.Bass("TRN2", target_bir_lowering=False)
    D = declare(nc, (), ["out"] + list(extra_out), moe_layers=(0, 1))
    banks = [nc.alloc_psum_tensor("bank%d" % i, [128, 512], F32) for i in range(8)]
    P = Prog(nc)
    phase_ada(nc, P, D, banks)
    phase_l0a(nc, P, D, banks)
    phase_l0b(nc, P, D, banks)
    phase_moe_sparse(nc, P, D, banks, 0, "x1", "x2", MOE0_TILES, "m0_")
    phase_l1a(nc, P, D, banks)
    phase_l1b_a(nc, P, D, banks)
    phase_l1b_b(nc, P, D, banks)
    phase_moe_sparse(nc, P, D, banks, 1, "x3", "out", MOE1_TILES, "m1_", final_norm=True)
    P.flush(final=True)
    return nc


def kernel(**inputs):
    inp = {k: np.asarray(v) for k, v in inputs.items()}
    n = 8
    nc = build_fused()
    in_maps = []
    for c in range(n):
        m = core_inputs(c, inp)
        m.update(weights_for(c, inp, layers=(0, 1)))
        in_maps.append(m)
    res = run_bass_kernel_spmd(nc, in_maps, core_ids=list(range(n))).results
    out = np.zeros((4, 4096, 1024), np.float32)
    for c in range(n):
        b, hf = c // 2, c % 2
        own = local_order(hf)[0]
        out[b][own] = np.asarray(res[c]["out"], dtype=np.float32)
    return out
```
